# Optimizing a Trainium2 kernel written in Bass

```python
import math
import jax, jax.numpy as jnp
from jax import lax
import numpy as np

D_MODEL = 1024
BATCH = 16
SEQ = 2048
DEPTH = 2

CTX_LEN = 256
GRID_W = 64
EPS = 1e-6

DIFF_HEADS = 4
DIFF_DK = 64
DIFF_DV = 2 * DIFF_DK
DIFF_WIDTH = DIFF_HEADS * DIFF_DV
DIFF_QK_WIDTH = DIFF_HEADS * 2 * DIFF_DK
ROPE_THETA = 10000.0
Q_BLOCK = 128
FNET_GROUPS = 4
FNET_GROUP_CH = 64
FNET_WIDTH = FNET_GROUPS * FNET_GROUP_CH
S5_CH = 16
S5_GROUPS = 16
S5_STATE = 64
S5_WIDTH = S5_GROUPS * S5_CH
DT_MIN = 1e-3
DT_MAX = 1e-1
D_MIX = DIFF_WIDTH + FNET_WIDTH + S5_WIDTH
IN_WIDTH = 2 * DIFF_QK_WIDTH + DIFF_WIDTH + FNET_WIDTH + S5_WIDTH
F_DENSE = 2816
N_EXPERTS = 8
TOP_K = 2
F_EXPERT = 3584
N_DENSE_LAYERS = (DEPTH + 1) // 2
N_MOE_LAYERS = DEPTH // 2

kernel_name = 'hybrid_diffattn_fnet_s5_moe_prefix_dit'


def rms_norm(x, g):
    xf = x.astype(jnp.float32)
    y = xf * lax.rsqrt(jnp.mean(xf * xf, axis=-1, keepdims=True) + EPS)
    return (y * g.astype(jnp.float32)).astype(x.dtype)


def rope_2d(x, rows, cols):
    half = DIFF_DK // 2
    n_freq = half // 2
    inv = ROPE_THETA ** (-jnp.arange(n_freq, dtype=jnp.float32) / n_freq)
    extra = x.ndim - 3

    def rot(xa, pos):
        ang = pos.astype(jnp.float32)[:, None] * inv
        ang = ang.reshape((ang.shape[0],) + (1,) * extra + (n_freq,))
        cos = jnp.cos(ang).astype(x.dtype)
        sin = jnp.sin(ang).astype(x.dtype)
        x1, x2 = xa[..., :n_freq], xa[..., n_freq:]
        return jnp.concatenate([x1 * cos - x2 * sin, x2 * cos + x1 * sin], axis=-1)

    return jnp.concatenate([rot(x[..., :half], rows), rot(x[..., half:], cols)], axis=-1)


def split_in(p):
    b, l_, _ = p.shape
    q = p[..., :DIFF_QK_WIDTH].reshape(b, l_, DIFF_HEADS, 2, DIFF_DK)
    k = p[..., DIFF_QK_WIDTH:2 * DIFF_QK_WIDTH].reshape(b, l_, DIFF_HEADS, 2, DIFF_DK)
    o = 2 * DIFF_QK_WIDTH
    v = p[..., o:o + DIFF_WIDTH].reshape(b, l_, DIFF_HEADS, DIFF_DV)
    o = o + DIFF_WIDTH
    f = p[..., o:o + FNET_WIDTH]
    u = p[..., o + FNET_WIDTH:]
    return q, k, v, f, u


def diff_attend(q1, q2, k1, k2, v, lam):
    scale = DIFF_DK ** -0.5
    p1 = jax.nn.softmax(jnp.einsum('bhqd,bhkd->bhqk', q1, k1).astype(jnp.float32) * scale, axis=-1)
    p2 = jax.nn.softmax(jnp.einsum('bhqd,bhkd->bhqk', q2, k2).astype(jnp.float32) * scale, axis=-1)
    return jnp.einsum('bhqk,bhkd->bhqd', (p1 - lam * p2).astype(v.dtype), v)


def diff_attention(q, k, v, qc, kc, vc, lam, lam_init, subln, rows, cols, need_ctx):
    q = rope_2d(q, rows, cols)
    k = rope_2d(k, rows, cols)
    heads = lambda t: jnp.moveaxis(t, 2, 1)
    q, k, v, qc, kc, vc = (heads(t) for t in (q, k, v, qc, kc, vc))
    k_all = jnp.concatenate([kc, k], axis=2)
    v_all = jnp.concatenate([vc, v], axis=2)
    k1, k2 = k_all[..., 0, :], k_all[..., 1, :]
    b, h, s = q.shape[:3]
    nblk = s // Q_BLOCK
    qb = jnp.moveaxis(q.reshape(b, h, nblk, Q_BLOCK, 2, DIFF_DK), 2, 0)
    o = lax.map(lambda qq: diff_attend(qq[..., 0, :], qq[..., 1, :], k1, k2, v_all, lam), qb)
    o = jnp.moveaxis(o, 0, 2).reshape(b, h, s, DIFF_DV)

    def finish(t):
        t = rms_norm(t, subln) * (1.0 - lam_init)
        return jnp.moveaxis(t, 1, 2).reshape(t.shape[0], t.shape[2], DIFF_WIDTH)

    out_c = None
    if need_ctx:
        out_c = finish(diff_attend(qc[..., 0, :], qc[..., 1, :], kc[..., 0, :], kc[..., 1, :], vc, lam))
    return finish(o), out_c


def fourier_mix(f, w):
    b, l_, _ = f.shape
    g = f.astype(jnp.float32).reshape(b, l_, FNET_GROUPS, FNET_GROUP_CH)
    mixed = jnp.fft.fft2(g, axes=(1, 3), norm='ortho').real.reshape(b, l_, FNET_WIDTH).astype(f.dtype)
    return mixed @ w


def s5_discretize(a_re, a_im, log_dt, b_re, b_im):
    f32 = jnp.float32
    a_re, a_im = a_re.astype(f32), a_im.astype(f32)
    b_re, b_im = b_re.astype(f32), b_im.astype(f32)
    dt = jnp.exp(log_dt.astype(f32))[:, None]
    mag = jnp.exp(a_re * dt)
    lr, li = mag * jnp.cos(a_im * dt), mag * jnp.sin(a_im * dt)
    nr, ni = lr - 1.0, li
    den = a_re * a_re + a_im * a_im
    cr = (nr * a_re + ni * a_im) / den
    ci = (ni * a_re - nr * a_im) / den
    bbr = cr[..., None] * b_re - ci[..., None] * b_im
    bbi = cr[..., None] * b_im + ci[..., None] * b_re
    return lr, li, bbr, bbi


def complex_scan(lr, li, br, bi, h0r, h0i, reverse):
    l_ = br.shape[1]
    if h0r is not None:
        first = -1 if reverse else 0
        br = br.at[:, first].add(lr * h0r - li * h0i)
        bi = bi.at[:, first].add(lr * h0i + li * h0r)
    ar = jnp.broadcast_to(lr, (1, l_) + lr.shape)
    ai = jnp.broadcast_to(li, (1, l_) + li.shape)

    def combine(e1, e2):
        a1r, a1i, b1r, b1i = e1
        a2r, a2i, b2r, b2i = e2
        return (a2r * a1r - a2i * a1i, a2r * a1i + a2i * a1r,
                a2r * b1r - a2i * b1i + b2r, a2r * b1i + a2i * b1r + b2i)

    _, _, hr, hi = lax.associative_scan(combine, (ar, ai, br, bi), reverse=reverse, axis=1)
    return hr, hi


def s5_readout(hr, hi, c_re, c_im):
    f32 = jnp.float32
    return jnp.einsum('blgp,gcp->blgc', hr, c_re.astype(f32)) - jnp.einsum('blgp,gcp->blgc', hi, c_im.astype(f32))


def s5_mixer(u, uc, a_re, a_im, log_dt, b_re, b_im, c_re, c_im, d, w_glu, need_ctx):
    f32 = jnp.float32

    def drive(t, bbr, bbi):
        tg = t.astype(f32).reshape(t.shape[0], t.shape[1], S5_GROUPS, S5_CH)
        return jnp.einsum('blgc,gpc->blgp', tg, bbr), jnp.einsum('blgc,gpc->blgp', tg, bbi)

    y, yc = 0.0, 0.0
    for direction, reverse in ((0, False), (1, True)):
        lr, li, bbr, bbi = s5_discretize(a_re[direction], a_im[direction], log_dt[direction],
                                         b_re[direction], b_im[direction])
        hcr, hci = complex_scan(lr, li, *drive(uc, bbr, bbi), None, None, reverse)
        end = 0 if reverse else -1
        hr, hi = complex_scan(lr, li, *drive(u, bbr, bbi), hcr[:, end], hci[:, end], reverse)
        y = y + s5_readout(hr, hi, c_re[direction], c_im[direction])
        if need_ctx:
            yc = yc + s5_readout(hcr, hci, c_re[direction], c_im[direction])

    def finish(yy, t):
        yy = yy.reshape(t.shape[0], t.shape[1], S5_WIDTH) + d.astype(f32) * t.astype(f32)
        yy = jax.nn.gelu(yy)
        return (yy * jax.nn.sigmoid(yy @ w_glu.astype(f32))).astype(t.dtype)

    return finish(y, u), (finish(yc, uc) if need_ctx else None)


def swiglu(t, wg, wu, wd):
    return (jax.nn.silu(t @ wg) * (t @ wu)) @ wd


def moe_swiglu(t, router, wg, wu, wd):
    b, l_, d_ = t.shape
    tok = t.reshape(-1, d_)
    probs = jax.nn.softmax((tok @ router).astype(jnp.float32), axis=-1)
    top_p, top_i = lax.top_k(probs, TOP_K)
    top_p = top_p / jnp.sum(top_p, axis=-1, keepdims=True)
    gates = jnp.sum(jax.nn.one_hot(top_i, N_EXPERTS, dtype=jnp.float32) * top_p[..., None], axis=1)
    out = jnp.zeros_like(tok)
    for e in range(N_EXPERTS):
        out = out + gates[:, e:e + 1].astype(t.dtype) * swiglu(tok, wg[e], wu[e], wd[e])
    return out.reshape(b, l_, d_)


def setup_inputs(seed: int = 0) -> dict:
    key = jax.random.key(seed)
    ks = iter(jax.random.split(key, 48))
    f32 = jnp.float32
    nrm = lambda shape, scale: jax.random.normal(next(ks), shape, f32) * scale
    G, P, C, D = S5_GROUPS, S5_STATE, S5_CH, D_MODEL
    a_im_init = jnp.pi * jnp.arange(P, dtype=f32)
    return {
        'x': nrm((BATCH, SEQ, D), 1.0),
        'c': nrm((BATCH, D), 1.0),
        'ctx': nrm((BATCH, CTX_LEN, D), 1.0),
        'c_ctx': nrm((D,), 1.0),
        'ada_w': nrm((DEPTH, D, 6 * D), 0.5 * D ** -0.5),
        'ada_b': nrm((DEPTH, 6 * D), 0.02),
        'norm_mix_pre': 1.0 + nrm((DEPTH, D), 0.05),
        'norm_mix_post': 1.0 + nrm((DEPTH, D), 0.05),
        'norm_ffn_pre': 1.0 + nrm((DEPTH, D), 0.05),
        'norm_ffn_post': 1.0 + nrm((DEPTH, D), 0.05),
        'w_in': nrm((DEPTH, D, IN_WIDTH), D ** -0.5),
        'w_out': nrm((DEPTH, D_MIX, D), D_MIX ** -0.5),
        'diff_lq1': nrm((DEPTH, DIFF_DK), 0.1),
        'diff_lk1': nrm((DEPTH, DIFF_DK), 0.1),
        'diff_lq2': nrm((DEPTH, DIFF_DK), 0.1),
        'diff_lk2': nrm((DEPTH, DIFF_DK), 0.1),
        'diff_subln': 1.0 + nrm((DEPTH, DIFF_DV), 0.05),
        'fnet_w': nrm((DEPTH, FNET_WIDTH, FNET_WIDTH), FNET_WIDTH ** -0.5),
        's5_a_re': -0.5 + nrm((DEPTH, 2, G, P), 0.01),
        's5_a_im': a_im_init + nrm((DEPTH, 2, G, P), 0.01),
        's5_log_dt': jax.random.uniform(next(ks), (DEPTH, 2, G), f32, math.log(DT_MIN), math.log(DT_MAX)),
        's5_b_re': nrm((DEPTH, 2, G, P, C), (2 * C) ** -0.5),
        's5_b_im': nrm((DEPTH, 2, G, P, C), (2 * C) ** -0.5),
        's5_c_re': nrm((DEPTH, 2, G, C, P), (2 * P) ** -0.5),
        's5_c_im': nrm((DEPTH, 2, G, C, P), (2 * P) ** -0.5),
        's5_d': nrm((DEPTH, S5_WIDTH), 1.0),
        's5_w_glu': nrm((DEPTH, S5_WIDTH, S5_WIDTH), S5_WIDTH ** -0.5),
        'ffn_w_gate': nrm((N_DENSE_LAYERS, D, F_DENSE), D ** -0.5),
        'ffn_w_up': nrm((N_DENSE_LAYERS, D, F_DENSE), D ** -0.5),
        'ffn_w_down': nrm((N_DENSE_LAYERS, F_DENSE, D), F_DENSE ** -0.5),
        'moe_router': nrm((N_MOE_LAYERS, D, N_EXPERTS), D ** -0.5),
        'moe_w_gate': nrm((N_MOE_LAYERS, N_EXPERTS, D, F_EXPERT), D ** -0.5),
        'moe_w_up': nrm((N_MOE_LAYERS, N_EXPERTS, D, F_EXPERT), D ** -0.5),
        'moe_w_down': nrm((N_MOE_LAYERS, N_EXPERTS, F_EXPERT, D), F_EXPERT ** -0.5),
    }


def reference(x, c, ctx, c_ctx, ada_w, ada_b, norm_mix_pre, norm_mix_post, norm_ffn_pre, norm_ffn_post,
              w_in, w_out, diff_lq1, diff_lk1, diff_lq2, diff_lk2, diff_subln, fnet_w,
              s5_a_re, s5_a_im, s5_log_dt, s5_b_re, s5_b_im, s5_c_re, s5_c_im, s5_d, s5_w_glu,
              ffn_w_gate, ffn_w_up, ffn_w_down, moe_router, moe_w_gate, moe_w_up, moe_w_down):
    f32 = jnp.float32
    s = x.shape[1]
    ROWS = s // GRID_W
    rows = jnp.repeat(jnp.arange(ROWS, dtype=jnp.int32), GRID_W)
    cols = jnp.tile(jnp.arange(GRID_W, dtype=jnp.int32), ROWS)
    silu_c = jax.nn.silu(c)
    silu_cc = jax.nn.silu(c_ctx)
    xc = ctx
    for l in range(DEPTH):
        need_ctx = l < DEPTH - 1
        mod = silu_c @ ada_w[l] + ada_b[l]
        mod_c = silu_cc @ ada_w[l] + ada_b[l]
        sh_m, sc_m, g_m, sh_f, sc_f, g_f = jnp.split(mod[:, None, :], 6, axis=-1)
        csh_m, csc_m, cg_m, csh_f, csc_f, cg_f = jnp.split(mod_c, 6, axis=-1)

        h = rms_norm(x, norm_mix_pre[l]) * (1.0 + sc_m) + sh_m
        hc = rms_norm(xc, norm_mix_pre[l]) * (1.0 + csc_m) + csh_m
        q, k, v, f, u = split_in(h @ w_in[l])
        qc, kc, vc, fc, uc = split_in(hc @ w_in[l])

        lam_init = 0.8 - 0.6 * math.exp(-0.3 * l)
        lam = (jnp.exp(jnp.sum(diff_lq1[l].astype(f32) * diff_lk1[l].astype(f32)))
               - jnp.exp(jnp.sum(diff_lq2[l].astype(f32) * diff_lk2[l].astype(f32))) + lam_init)
        a_lat, a_ctx = diff_attention(q, k, v, qc, kc, vc, lam, lam_init, diff_subln[l], rows, cols, need_ctx)
        f_lat = fourier_mix(f, fnet_w[l])
        s_lat, s_ctx = s5_mixer(u, uc, s5_a_re[l], s5_a_im[l], s5_log_dt[l], s5_b_re[l], s5_b_im[l],
                                s5_c_re[l], s5_c_im[l], s5_d[l], s5_w_glu[l], need_ctx)
        mix = jnp.concatenate([a_lat, f_lat, s_lat], axis=-1) @ w_out[l]
        x = x + g_m * rms_norm(mix, norm_mix_post[l])
        if need_ctx:
            mix_c = jnp.concatenate([a_ctx, fourier_mix(fc, fnet_w[l]), s_ctx], axis=-1) @ w_out[l]
            xc = xc + cg_m * rms_norm(mix_c, norm_mix_post[l])

        i = l // 2
        if l % 2 == 0:
            ffn = lambda t: swiglu(t, ffn_w_gate[i], ffn_w_up[i], ffn_w_down[i])
        else:
            ffn = lambda t: moe_swiglu(t, moe_router[i], moe_w_gate[i], moe_w_up[i], moe_w_down[i])
        h = rms_norm(x, norm_ffn_pre[l]) * (1.0 + sc_f) + sh_f
        x = x + g_f * rms_norm(ffn(h), norm_ffn_post[l])
        if need_ctx:
            hc = rms_norm(xc, norm_ffn_pre[l]) * (1.0 + csc_f) + csh_f
            xc = xc + cg_f * rms_norm(ffn(hc), norm_ffn_post[l])
    return x
```

```python
import contextlib, math
import numpy as np
import ml_dtypes
import concourse.bass as bass
import concourse.mybir as mybir
from concourse.bass_utils import run_bass_kernel_spmd

F32 = mybir.dt.float32
BF16 = mybir.dt.bfloat16
I32 = mybir.dt.int32
AF = mybir.ActivationFunctionType
ALU = mybir.AluOpType
AX = mybir.AxisListType
NPBF = ml_dtypes.bfloat16

SEM_LIMIT = 30000
EPS = 1e-6
D = 1024
NB = 2
CTX = 256
SEQ = 2048
TPB = CTX + SEQ
NTOK = NB * TPB
NTILE = NTOK // 128
TILES_PB = TPB // 128
DEPTH = 2
F_DENSE = 2816
F_EXPERT = 3584
N_EXP = 8
MOE_TS = 512
MOE_TILES = (2 * NB * SEQ + N_EXP * (MOE_TS - 1)) // MOE_TS + 1
MOE_SLOTS = MOE_TILES * MOE_TS


class Tok:
    __slots__ = ("name", "w", "r")

    def __init__(self, name=""):
        self.name = name
        self.w = None
        self.r = {}


class Eng:
    def __init__(self, kb, name, eng):
        self.kb, self.name, self.eng = kb, name, eng
        self.sem = None
        self.cnt = 0
        self.waited = {}

    def new_sem(self):
        self.sem = self.kb.es.enter_context(self.kb.nc.semaphore(f"s_{self.name}_{self.kb.nsem}"))
        self.kb.nsem += 1
        self.cnt = 0


class KB:
    def __init__(self):
        self.nc = bass.Bass("TRN2", target_bir_lowering=False)
        self.es = contextlib.ExitStack()
        self.nsem = 0
        nc = self.nc
        self.E = {}
        for name, eng in (("pe", nc.tensor), ("act", nc.scalar), ("dve", nc.vector),
                          ("pool", nc.gpsimd), ("sp", nc.sync)):
            e = Eng(self, name, eng)
            e.new_sem()
            self.E[name] = e
        self.dma_sems, self.dma_vals = [], []
        for i in range(64):
            self.dma_sems.append(self.es.enter_context(nc.semaphore(f"s_dma{i}")))
            self.dma_vals.append(0)
        self.dma_cursor = {"sp": 0, "pool": 0}
        self.nalloc = 0
        self.ninstr = 0
        self.stack = [self.es]
        self.dram_t = {}

    def sb(self, shape, dtype, name=None):
        self.nalloc += 1
        return self.stack[-1].enter_context(self.nc.sbuf_tensor(name or f"sb{self.nalloc}", list(shape), dtype))

    def ps(self, shape, dtype, name=None):
        self.nalloc += 1
        esz = 4 if dtype == F32 else 2
        n = int(np.prod(shape[1:]))
        assert n * esz <= 2048, shape
        t = self.stack[-1].enter_context(self.nc.psum_tensor(name or f"ps{self.nalloc}", [128, 2048 // esz], dtype))
        ap = t[0:shape[0], 0:n]
        if len(shape) > 2:
            names = [f"d{i}" for i in range(len(shape) - 1)]
            pat = "p (" + " ".join(names) + ") -> p " + " ".join(names)
            ap = ap.rearrange(pat, **{nm: int(v) for nm, v in zip(names[:-1], shape[1:-1])})
        return ap

    def dram(self, name, shape, dtype, kind="Internal"):
        t = self.nc.dram_tensor(name, list(shape), dtype, kind=kind)
        self.dram_t[name] = t
        return t.ap()

    @contextlib.contextmanager
    def phase(self):
        st = contextlib.ExitStack()
        self.stack.append(st)
        try:
            yield
        finally:
            self.barrier()
            self.stack.pop()
            st.close()

    def barrier(self):
        evs = [(e.sem, e.cnt) for e in self.E.values() if e.cnt > 0]
        evs += [(s, v) for s, v in zip(self.dma_sems, self.dma_vals) if v > 0]
        for e in self.E.values():
            for ev in evs:
                if ev[0] is e.sem:
                    continue
                self._wait(e, ev)

    def _wait(self, e, ev):
        if ev is None:
            return
        if isinstance(ev, list):
            for e_ in ev:
                self._wait(e, e_)
            return
        sem, val = ev
        k = id(sem)
        if e.waited.get(k, 0) >= val:
            return
        if e.name == "pe" and sem is e.sem:
            return
        e.eng.wait_ge(sem, val)
        e.waited[k] = val

    def _deps(self, e, reads, writes):
        for t in reads:
            self._wait(e, t.w)
        for t in writes:
            self._wait(e, t.w)
            for ev in t.r.values():
                self._wait(e, ev)

    def _commit(self, ev, reads, writes):
        for t in reads:
            for e_ in (ev if isinstance(ev, list) else [ev]):
                t.r[id(e_[0])] = e_
        for t in writes:
            t.w = ev
            t.r = {}

    def op(self, en, fn, reads=(), writes=()):
        e = self.E[en]
        if e.cnt >= SEM_LIMIT:
            e.new_sem()
        self._deps(e, reads, writes)
        ins = fn(e.eng)
        e.cnt += 1
        ins.then_inc(e.sem, 1)
        ev = (e.sem, e.cnt)
        self._commit(ev, reads, writes)
        self.ninstr += 1
        return ev

    def _next_dma_sem(self, qn):
        half = len(self.dma_sems) // 2
        c = self.dma_cursor[qn]
        self.dma_cursor[qn] = (c + 1) % half
        return c + (half if qn == "pool" else 0)

    def dma(self, qn, pairs, reads=(), writes=(), **kw):
        e = self.E[qn]
        self._deps(e, reads, writes)
        if qn == "pool" and len(pairs) > 1:
            evs = []
            for (o, i) in pairs:
                j = self._next_dma_sem(qn)
                sem = self.dma_sems[j]
                if self.dma_vals[j] > 0:
                    self._wait(e, (sem, self.dma_vals[j]))
                if self.dma_vals[j] > SEM_LIMIT:
                    raise RuntimeError("dma sem overflow")
                e.eng.dma_start(out=o, in_=i, **kw).then_inc(sem, 16)
                self.dma_vals[j] += 16
                self.ninstr += 1
                evs.append((sem, self.dma_vals[j]))
            self._commit(evs, reads, writes)
            return evs
        j = self._next_dma_sem(qn)
        sem = self.dma_sems[j]
        if self.dma_vals[j] > 0:
            self._wait(e, (sem, self.dma_vals[j]))
        if self.dma_vals[j] > SEM_LIMIT:
            raise RuntimeError("dma sem overflow")
        for (o, i) in pairs:
            e.eng.dma_start(out=o, in_=i, **kw).then_inc(sem, 16)
            self.dma_vals[j] += 16
            self.ninstr += 1
        ev = (sem, self.dma_vals[j])
        self._commit(ev, reads, writes)
        return ev

    def dma_custom(self, qn, fn, reads=(), writes=()):
        e = self.E[qn]
        self._deps(e, reads, writes)
        j = self._next_dma_sem(qn)
        sem = self.dma_sems[j]
        if self.dma_vals[j] > 0:
            self._wait(e, (sem, self.dma_vals[j]))
        if self.dma_vals[j] > SEM_LIMIT:
            raise RuntimeError("dma sem overflow")
        fn(e.eng).then_inc(sem, 16)
        self.dma_vals[j] += 16
        self.ninstr += 1
        ev = (sem, self.dma_vals[j])
        self._commit(ev, reads, writes)
        return ev

    def finish(self, toks):
        e = self.E["sp"]
        for t in toks:
            self._wait(e, t.w)
            for ev in t.r.values():
                self._wait(e, ev)


class Ring:
    def __init__(self, kb, n, shape, dtype, kind="sb", name="ring"):
        self.items = []
        for i in range(n):
            t = kb.sb(shape, dtype) if kind == "sb" else kb.ps(shape, dtype)
            self.items.append((t, Tok(f"{name}{i}")))
        self.i = 0

    def next(self):
        it = self.items[self.i]
        self.i = (self.i + 1) % len(self.items)
        return it

def host_consts():
    c = {}
    c["ident_bf"] = np.eye(128, dtype=np.float32).astype(NPBF)
    c["ident_f"] = np.eye(128, dtype=np.float32)
    n_freq = 16
    inv = 10000.0 ** (-np.arange(n_freq, dtype=np.float64) / n_freq)
    t = np.arange(SEQ)
    rows = (t // 64).astype(np.float64)
    cols = (t % 64).astype(np.float64)
    ang = np.stack([rows[:, None] * inv, cols[:, None] * inv], axis=1)
    ang = (np.stack([rows[:, None].astype(np.float32) * inv.astype(np.float32),
                     cols[:, None].astype(np.float32) * inv.astype(np.float32)], axis=1)).astype(np.float64)
    cos = np.cos(ang); sin = np.sin(ang)
    cos2 = np.stack([cos, cos], axis=2)
    sinS = np.stack([-sin, sin], axis=2)
    c["rope_cos"] = np.ascontiguousarray(cos2.reshape(16, 128, 64).transpose(1, 0, 2)).astype(np.float32)
    c["rope_sin"] = np.ascontiguousarray(sinS.reshape(16, 128, 64).transpose(1, 0, 2)).astype(np.float32)
    def dftm(L):
        t = np.arange(L)
        ph = (np.outer(t, t) % L).astype(np.float64) * (2 * np.pi / L)
        sc = 1.0 / math.sqrt(L * 64)
        return (np.cos(ph) * sc).astype(NPBF), (np.sin(ph) * sc).astype(NPBF)
    c["dft_c"], c["dft_s"] = dftm(SEQ)
    c["dftc_c"], c["dftc_s"] = dftm(CTX)
    t = np.arange(64)
    ph = np.outer(t, t) * (2 * np.pi / 64)
    cb = np.zeros((128, 2, 128), np.float32)
    for g in range(2):
        cb[g * 64:(g + 1) * 64, 0, g * 64:(g + 1) * 64] = np.cos(ph)
        cb[g * 64:(g + 1) * 64, 1, g * 64:(g + 1) * 64] = -np.sin(ph)
    c["cblk"] = cb
    k = np.arange(128)
    ws = np.zeros((128, 8, 240), np.float32)
    for r in range(8):
        for kk in range(128):
            if kk // 16 == r:
                ws[kk, r, 112 + kk % 16] = 1.0
    c["wsel"] = ws.astype(NPBF)
    sblk = (k // 16)[:, None]; tblk = (k // 16)[None, :]
    c["s5_mask"] = np.ascontiguousarray(np.stack([(tblk >= sblk), (tblk <= sblk)], axis=1).astype(np.float32))
    tri = np.zeros((128, 2, 128), np.float32)
    tri[:, 0, :] = (k[:, None] < k[None, :])
    tri[:, 1, :] = 1.0
    c["moe_tri"] = tri.astype(NPBF)
    c["moe_iota"] = np.ascontiguousarray((np.arange(7)[None, :] * 128 + k[:, None]).astype(np.float32))
    c["moe_thr"] = np.ascontiguousarray(np.broadcast_to((np.arange(MOE_TILES) * MOE_TS)[None, :], (128, MOE_TILES)).astype(np.float32))
    return c


class Prog:
    def __init__(self, debug=()):
        self.kb = KB()
        self.debug = set(debug)
        self.I = {}
        self.consts = host_consts()
        self.prep = None
        self.prep_rings_cur = [None]
        self.prep_inflight = []

    def inp(self, name, shape, dtype):
        ap = self.kb.dram(name, shape, dtype, kind="ExternalInput")
        self.I[name] = ap
        return ap

    def scratch(self, name, shape, dtype):
        kind = "ExternalOutput" if name in self.debug else "Internal"
        return self.kb.dram(name, shape, dtype, kind=kind)

    def declare(self):
        inp = self.inp
        inp("xin", [NTOK, D], F32)
        inp("cT", [128, 8, 3], F32)
        inp("ada_w", [DEPTH, D, 6 * D], F32)
        inp("ada_b", [DEPTH, 6 * D], F32)
        for n in ("norm_mix_pre", "norm_mix_post", "norm_ffn_pre", "norm_ffn_post"):
            inp(n, [DEPTH, D], F32)
        inp("w_in", [DEPTH, D, 2048], F32)
        inp("w_out", [DEPTH, D, D], F32)
        for n in ("diff_lq1", "diff_lk1", "diff_lq2", "diff_lk2"):
            inp(n, [DEPTH, 64], F32)
        inp("diff_subln", [DEPTH, 128], F32)
        inp("fnet_w", [DEPTH, 256, 256], F32)
        inp("s5_par", [DEPTH, 128, 3, 32], F32)
        inp("s5_b", [DEPTH, 128, 32, 2, 16], F32)
        inp("s5_c", [DEPTH, 128, 32, 2, 16], F32)
        inp("s5_dd", [DEPTH, 128, 2], F32)
        inp("s5_w_glu", [DEPTH, 256, 256], F32)
        inp("ffn_w_gate", [1, D, F_DENSE], F32); inp("ffn_w_up", [1, D, F_DENSE], F32); inp("ffn_w_down", [1, F_DENSE, D], F32)
        inp("moe_router", [1, D, N_EXP], F32)
        inp("moe_w_gate", [1, N_EXP, D, F_EXPERT], F32); inp("moe_w_up", [1, N_EXP, D, F_EXPERT], F32); inp("moe_w_down", [1, N_EXP, F_EXPERT, D], F32)
        for k, v in self.consts.items():
            inp(k, list(v.shape), BF16 if v.dtype == NPBF else F32)
        sc = self.scratch
        self.modD = [sc(f"modD{l}", [3, 6 * D], F32) for l in range(DEPTH)]
        self.qT = sc("qT", [128, 4, NTOK], BF16)
        self.kT = sc("kT", [128, 4, NTOK], BF16)
        self.vD = sc("vD", [128, 4, NTILE, 130], BF16)
        self.fD = sc("fD", [128, NTILE, 256], BF16)
        self.uT = sc("uT", [128, 2, NTOK], BF16)
        self.catT = sc("catT", [128, 8, NTOK], BF16)
        self.xA = sc("xA", [NTOK, D], F32)
        self.xB = sc("xB", [NTOK, D], F32)
        self.h2T = sc("h2T", [128, 8, NTOK], BF16)
        self.out = self.kb.dram("out", [NB * SEQ, D], F32, kind="ExternalOutput")
        self.moe_declare()

    def phase_adaln(self, l):
        kb, I = self.kb, self.I
        with kb.phase():
            cT = kb.sb([128, 8, 3], F32); sT = kb.sb([128, 8, 3], F32)
            bias = kb.sb([3, 6 * D], F32); mod = kb.sb([3, 6 * D], F32)
            t_c, t_s, t_b, t_m = Tok(), Tok(), Tok(), Tok()
            kb.dma("sp", [(cT[:], I["cT"][:, :, :])], writes=[t_c])
            kb.dma("sp", [(bias[:], I["ada_b"][l].partition_broadcast(3))], writes=[t_b])
            kb.op("act", lambda e: e.activation(out=sT[:], in_=cT[:], func=AF.Silu), reads=[t_c], writes=[t_s])
            wr = Ring(kb, 2, [128, 8, 512], F32, name="adaw")
            pr = Ring(kb, 2, [128, 512], F32, kind="ps", name="adap")
            wv = I["ada_w"][l].rearrange("(k p) n -> p k n", p=128)
            for nt in range(12):
                w, tw = wr.next()
                kb.dma("sp", [(w[:], wv[:, :, nt * 512:(nt + 1) * 512])], writes=[tw])
                p, tp = pr.next()
                for k in range(8):
                    kb.op("pe", lambda e: e.matmul(p[0:3, :], sT[:, k, :], w[:, k, :], start=(k == 0), stop=(k == 7)),
                          reads=[t_s, tw], writes=[tp])
                kb.op("dve", lambda e: e.tensor_tensor(out=mod[0:3, nt * 512:(nt + 1) * 512], in0=p[0:3, :],
                                                       in1=bias[0:3, nt * 512:(nt + 1) * 512], op=ALU.add),
                      reads=[tp, t_b], writes=[t_m])
            kb.dma("sp", [(self.modD[l][:, :], mod[0:3, :])], reads=[t_m])
            if l == 0:
                z = kb.sb([128, 8192], BF16); t_z = Tok()
                kb.op("pool", lambda e: e.memset(z[:], 0.0), writes=[t_z])
                hsv = self.hsorted.rearrange("(a p r) d -> a p (r d)", p=128, r=8)
                for a in range(MOE_SLOTS // 1024):
                    kb.dma("sp", [(hsv[a], z[:])], reads=[t_z])

    def load_mod_tiles(self, l, off_sc, off_sh, gain_name, plus_one=True):
        kb, I = self.kb, self.I
        gsc = kb.sb([128, 3, D], F32); sh = kb.sb([128, 3, D], F32); gn = kb.sb([128, D], F32)
        t_g, t_s, t_n = Tok(), Tok(), Tok()
        kb.dma("sp", [(gn[:], I[gain_name][l].partition_broadcast(128))], writes=[t_n])
        kb.dma("sp", [(gsc[:, j, :], self.modD[l][j, off_sc * D:(off_sc + 1) * D].partition_broadcast(128)) for j in range(3)],
               writes=[t_g])
        if off_sh is not None:
            kb.dma("sp", [(sh[:, j, :], self.modD[l][j, off_sh * D:(off_sh + 1) * D].partition_broadcast(128)) for j in range(3)],
                   writes=[t_s])
        for j in range(3):
            kb.op("dve", lambda e: e.scalar_tensor_tensor(out=gsc[:, j, :], in0=gsc[:, j, :], scalar=(1.0 if plus_one else 0.0), op0=ALU.add,
                                                          in1=gn[:], op1=ALU.mult),
                  reads=[t_g, t_n], writes=[t_g])
        return gsc, sh, t_g, t_s

    def norm_mod_tile(self, xt, t_x, gsc, sh, t_g, t_s, ms, R):
        kb = self.kb
        junk, t_j = R["junk"].next()
        st, t_st = R["stat"].next()
        kb.op("act", lambda e: e.activation(out=junk[:], in_=xt[:], func=AF.Square, accum_out=st[:, 0:1]),
              reads=[t_x], writes=[t_j, t_st])
        kb.op("act", lambda e: e.activation(out=st[:, 1:2], in_=st[:, 0:1], func=AF.Sqrt, scale=1.0 / D, bias=self.eps_t[:, 0:1]),
              reads=[t_st], writes=[t_st])
        kb.op("dve", lambda e: e.reciprocal(out=st[:, 2:3], in_=st[:, 1:2]), reads=[t_st], writes=[t_st])
        tmp, t_t = R["tmp"].next()
        kb.op("dve", lambda e: e.scalar_tensor_tensor(out=tmp[:], in0=xt[:], scalar=st[:, 2:3], op0=ALU.mult,
                                                      in1=gsc[:, ms, :], op1=ALU.mult),
              reads=[t_x, t_st, t_g], writes=[t_t])
        hb, t_h = R["hb"].next()
        kb.op("pool", lambda e: e.tensor_tensor(out=hb[:], in0=tmp[:], in1=sh[:, ms, :], op=ALU.add),
              reads=[t_t, t_s], writes=[t_h])
        return hb, t_h

    def consts_sb(self):
        kb, I = self.kb, self.I
        self.ident_bf = kb.sb([128, 128], BF16); self.t_ident = Tok()
        kb.dma("sp", [(self.ident_bf[:], I["ident_bf"][:, :])], writes=[self.t_ident])
        self.ident_f = kb.sb([128, 128], F32)
        kb.dma("sp", [(self.ident_f[:], I["ident_f"][:, :])], writes=[self.t_ident])
        self.eps_t = kb.sb([128, 1], F32)
        kb.op("pool", lambda e: e.memset(self.eps_t[:], EPS), writes=[self.t_ident])

    def phase_win(self, l, xsrc, prep_win=False):
        kb, I = self.kb, self.I
        with kb.phase():
            gsc, sh, t_g, t_s = self.load_mod_tiles(l, 1, 0, "norm_mix_pre")
            wb = kb.sb([128, 8, 2048], BF16); t_w = Tok()
            wv = I["w_in"][l].rearrange("(k p) n -> p k n", p=128)
            kb.dma("pool", [(wb[:, :, c * 512:(c + 1) * 512], wv[:, :, c * 512:(c + 1) * 512]) for c in range(4)], writes=[t_w])
            rc = kb.sb([128, 16, 64], F32); rs = kb.sb([128, 16, 64], F32); t_r = Tok()
            kb.dma("sp", [(rc[:], I["rope_cos"][:, :, :]), (rs[:], I["rope_sin"][:, :, :])], writes=[t_r])
            R = {"junk": Ring(kb, 1, [128, D], BF16), "stat": Ring(kb, 8, [128, 4], F32), "tmp": Ring(kb, 2, [128, D], F32),
                 "hb": Ring(kb, 3, [128, D], BF16)}
            xr = Ring(kb, 4, [128, D], F32)
            pT = Ring(kb, 1, [128, 8, 128], BF16, kind="ps")
            hTr = Ring(kb, 3, [128, 8, 128], BF16)
            pq = Ring(kb, 2, [128, 512], F32, kind="ps"); pk = Ring(kb, 2, [128, 512], F32, kind="ps")
            pv = Ring(kb, 1, [128, 512], F32, kind="ps"); pfu = Ring(kb, 1, [128, 512], F32, kind="ps")
            ptq = Ring(kb, 1, [128, 2, 4, 128], BF16, kind="ps")
            ropet = Ring(kb, 2, [128, 512], F32); ropem = Ring(kb, 2, [128, 256], F32)
            qbr = Ring(kb, 2, [128, 512], BF16); kbr = Ring(kb, 2, [128, 512], BF16)
            qTg = Ring(kb, 2, [128, 4, 512], BF16); kTg = Ring(kb, 2, [128, 4, 512], BF16)
            uTg = Ring(kb, 2, [128, 2, 512], BF16)
            vtr = Ring(kb, 2, [128, 4, 130], BF16); fbr = Ring(kb, 2, [128, 256], BF16)
            for vt, tv in vtr.items:
                kb.op("pool", lambda e: e.memset(vt[:, :, 128:130], 1.0), writes=[tv])
            C = [dict() for _ in range(NTILE)]
            G = {}

            def s0(ti):
                xt, t_x = xr.next()
                kb.dma("sp", [(xt[:], xsrc[ti * 128:(ti + 1) * 128, :])], writes=[t_x])
                st, t_st = self.nm1(xt, t_x, R)
                C[ti].update(xt=xt, t_x=t_x, st=st, t_st=t_st)

            def s1(ti):
                c = C[ti]
                b, j = divmod(ti, TILES_PB)
                ms = 2 if j < 2 else b
                c["hb"], c["t_h"] = self.nm2(c["xt"], c["t_x"], c["st"], c["t_st"], gsc, sh, t_g, t_s, ms, R)

            def s2(ti):
                c = C[ti]
                p, t_p = pT.next()
                for k in range(8):
                    kb.op("pe", lambda e: e.transpose(out=p[:, k, :], in_=c["hb"][:, k * 128:(k + 1) * 128], identity=self.ident_bf[:]),
                          reads=[c["t_h"], self.t_ident], writes=[t_p])
                hT, t_hT = hTr.next()
                kb.op("act", lambda e: e.copy(out=hT[:], in_=p[:]), reads=[t_p], writes=[t_hT])
                c.update(hT=hT, t_hT=t_hT)

            def s_mm(ti):
                c = C[ti]
                hT, t_hT = c["hT"], c["t_hT"]
                outs = []
                for ring, c0, n in ((pq, 0, 512), (pk, 512, 512), (pv, 1024, 512), (pfu, 1536, 256)):
                    pp, t_pp = ring.next()
                    for k in range(8):
                        kb.op("pe", lambda e: e.matmul(pp[:, 0:n], hT[:, k, :], wb[:, k, c0:c0 + n], start=(k == 0), stop=(k == 7)),
                              reads=[t_hT, t_w], writes=[t_pp])
                    outs.append((pp, t_pp))
                ppf, t_pf = outs[3]
                for ct in range(2):
                    for k in range(8):
                        kb.op("pe", lambda e: e.matmul(ppf[:, 256 + ct * 128:256 + (ct + 1) * 128], wb[:, k, 1792 + ct * 128:1792 + (ct + 1) * 128],
                                                       hT[:, k, :], start=(k == 0), stop=(k == 7)),
                              reads=[t_hT, t_w], writes=[t_pf])
                c["outs"] = outs

            def s_post(ti):
                c = C[ti]
                b, j = divmod(ti, TILES_PB)
                is_ctx = j < 2
                if j == 0 or (j >= 2 and (j - 2) % 4 == 0):
                    G["gq"] = qTg.next(); G["gk"] = kTg.next(); G["gu"] = uTg.next()
                    G["gstart"] = ti; G["gi"] = 0
                    G["gn"] = 2 if j == 0 else 4
                (gq, t_gq), (gk, t_gk), (gu, t_gu) = G["gq"], G["gk"], G["gu"]
                gi = G["gi"]
                (ppq, t_pq), (ppk, t_pk), (ppv, t_pv), (ppf, t_pf) = c["outs"]
                pt, t_pt = ptq.next()
                for which, (pp, t_pp), bring in ((0, (ppq, t_pq), qbr), (1, (ppk, t_pk), kbr)):
                    xb, t_xb = bring.next()
                    if is_ctx:
                        kb.op("dve", lambda e: e.tensor_copy(out=xb[:], in_=pp[:]), reads=[t_pp], writes=[t_xb])
                    else:
                        lt = j - 2
                        t1, t_t1 = ropet.next()
                        kb.op("dve", lambda e: e.tensor_tensor(out=t1[:].rearrange("p (a c) -> p a c", a=8), in0=pp[:].rearrange("p (a c) -> p a c", a=8),
                                                               in1=rc[:, lt:lt + 1, :].broadcast_to([128, 8, 64]), op=ALU.mult),
                              reads=[t_pp, t_r], writes=[t_t1])
                        x5 = pp[:].rearrange("p (a b j f) -> p a b j f", a=8, b=2, j=2)
                        t5 = t1[:].rearrange("p (a b j f) -> p a b j f", a=8, b=2, j=2)
                        o5 = xb[:].rearrange("p (a b j f) -> p a b j f", a=8, b=2, j=2)
                        s4 = rs[:, lt, :].rearrange("p (b j f) -> p b j f", b=2, j=2)
                        for jj in range(2):
                            m, t_m = ropem.next()
                            m4 = m[:].rearrange("p (a b f) -> p a b f", a=8, b=2)
                            kb.op("dve", lambda e: e.tensor_tensor(out=m4, in0=x5[:, :, :, 1 - jj, :],
                                                                   in1=s4[:, :, jj, :].unsqueeze(1).broadcast_to([128, 8, 2, 16]), op=ALU.mult),
                                  reads=[t_pp, t_r], writes=[t_m])
                            kb.op("dve", lambda e: e.tensor_tensor(out=o5[:, :, :, jj, :], in0=t5[:, :, :, jj, :], in1=m4, op=ALU.add),
                                  reads=[t_t1, t_m], writes=[t_xb])
                    for h in range(4):
                        kb.op("pe", lambda e: e.transpose(out=pt[:, which, h, :], in_=xb[:, h * 128:(h + 1) * 128], identity=self.ident_bf[:]),
                              reads=[t_xb, self.t_ident], writes=[t_pt])
                kb.op("act", lambda e: e.copy(out=gq[:, :, gi * 128:(gi + 1) * 128], in_=pt[:, 0, :, :]), reads=[t_pt], writes=[t_gq])
                kb.op("act", lambda e: e.copy(out=gk[:, :, gi * 128:(gi + 1) * 128], in_=pt[:, 1, :, :]), reads=[t_pt], writes=[t_gk])
                vt, t_v = vtr.next()
                kb.op("act", lambda e: e.copy(out=vt[:, :, 0:128], in_=ppv[:].rearrange("p (h d) -> p h d", h=4)), reads=[t_pv], writes=[t_v])
                kb.dma("sp", [(self.vD[:, :, ti, :], vt[:])], reads=[t_v])
                fb, t_f = fbr.next()
                kb.op("dve", lambda e: e.tensor_copy(out=fb[:], in_=ppf[:, 0:256]), reads=[t_pf], writes=[t_f])
                kb.dma("sp", [(self.fD[:, ti, :], fb[:])], reads=[t_f])
                kb.op("act", lambda e: e.copy(out=gu[:, :, gi * 128:(gi + 1) * 128], in_=ppf[:, 256:512].rearrange("p (c t) -> p c t", c=2)),
                      reads=[t_pf], writes=[t_gu])
                G["gi"] = gi + 1
                if G["gi"] == G["gn"]:
                    c0 = G["gstart"] * 128; w = G["gn"] * 128
                    kb.dma("sp", [(self.qT[:, :, c0:c0 + w], gq[:, :, 0:w])], reads=[t_gq])
                    kb.dma("sp", [(self.kT[:, :, c0:c0 + w], gk[:, :, 0:w])], reads=[t_gk])
                    kb.dma("sp", [(self.uT[:, :, c0:c0 + w], gu[:, :, 0:w])], reads=[t_gu])
                C[ti].clear()

            self.prep_begin()
            for it in range(NTILE + 4):
                if prep_win and it < NTILE:
                    self.prep_tick(1)
                for st_, off in ((s0, 0), (s1, 1), (s2, 2), (s_post, 4), (s_mm, 3)):
                    ti = it - off
                    if 0 <= ti < NTILE:
                        st_(ti)
            self.prep_flush()

    def phase_attn(self, l, need_ctx, prep_every=0):
        kb, I = self.kb, self.I
        lam_init = 0.8 - 0.6 * math.exp(-0.3 * l)
        with kb.phase():
            lq = kb.sb([128, 4, 64], F32); t_lq = Tok()
            kb.dma("sp", [(lq[:, i, :], I[n][l].partition_broadcast(128)) for i, n in
                          enumerate(("diff_lq1", "diff_lk1", "diff_lq2", "diff_lk2"))], writes=[t_lq])
            lt = kb.sb([128, 2, 64], F32); ls = kb.sb([128, 8], F32); t_ls = Tok()
            kb.op("dve", lambda e: e.tensor_tensor(out=lt[:, 0, :], in0=lq[:, 0, :], in1=lq[:, 1, :], op=ALU.mult), reads=[t_lq], writes=[t_ls])
            kb.op("dve", lambda e: e.tensor_tensor(out=lt[:, 1, :], in0=lq[:, 2, :], in1=lq[:, 3, :], op=ALU.mult), reads=[t_lq, t_ls], writes=[t_ls])
            kb.op("dve", lambda e: e.tensor_reduce(out=ls[:, 0:2], in_=lt[:], op=ALU.add, axis=AX.X), reads=[t_ls], writes=[t_ls])
            kb.op("act", lambda e: e.activation(out=ls[:, 2:4], in_=ls[:, 0:2], func=AF.Exp), reads=[t_ls], writes=[t_ls])
            kb.op("dve", lambda e: e.tensor_tensor(out=ls[:, 4:5], in0=ls[:, 3:4], in1=ls[:, 2:3], op=ALU.subtract), reads=[t_ls], writes=[t_ls])
            kb.op("dve", lambda e: e.tensor_scalar(out=ls[:, 5:6], in0=ls[:, 4:5], scalar1=-lam_init, scalar2=None, op0=ALU.add), reads=[t_ls], writes=[t_ls])
            nlam = ls[:, 5:6]
            sg = kb.sb([128, 128], F32); t_sg = Tok()
            kb.dma("sp", [(sg[:], I["diff_subln"][l].partition_broadcast(128))], writes=[t_sg])
            kb.op("dve", lambda e: e.tensor_scalar(out=sg[:], in0=sg[:], scalar1=1.0 - lam_init, scalar2=None, op0=ALU.mult), reads=[t_sg], writes=[t_sg])
            kr = Ring(kb, 2, [128, TPB], BF16); qr = Ring(kb, 2, [128, 2, TPB], BF16); vr = Ring(kb, 2, [128, TILES_PB, 130], BF16)
            psr = Ring(kb, 3, [128, 2, 256], F32, kind="ps")
            pacc = [Ring(kb, 2, [128, 2, 130], F32, kind="ps") for _ in range(2)]
            pto = Ring(kb, 1, [128, 2, 128], BF16, kind="ps")
            ptr = Ring(kb, 3, [128, 2, 256], BF16)
            o1r = Ring(kb, 4, [128, 128], F32); o2r = Ring(kb, 4, [128, 128], F32); junkr = Ring(kb, 1, [128, 128], BF16)
            str_ = Ring(kb, 6, [128, 8], F32); abr = Ring(kb, 2, [128, 128], BF16); aTr = Ring(kb, 2, [128, 256], BF16)
            for qz, t_qz in qr.items:
                kb.op("pool", lambda e: e.memset(qz[:], 0.0), writes=[t_qz])
            pending = []
            self.prep_begin()
            qt_count = 0
            for b in range(NB):
                t0 = b * TPB
                for h in range(4):
                    kT, t_k = kr.next(); qT, t_q = qr.next(); vv, t_v = vr.next()
                    kb.dma("sp", [(kT[:], self.kT[:, h, t0:t0 + TPB])], writes=[t_k])
                    kb.dma("sp", [(qT[m * 64:(m + 1) * 64, m, :], self.qT[m * 64:(m + 1) * 64, h, t0:t0 + TPB]) for m in range(2)], writes=[t_q])
                    kb.dma("sp", [(vv[:], self.vD[:, h, b * TILES_PB:(b + 1) * TILES_PB, :])], writes=[t_v])
                    qts = [(CTX + i * 256, list(range(TILES_PB))) for i in range(8)]
                    if need_ctx:
                        qts.append((0, [0, 1]))
                    for (q0, kts) in qts:
                        qt_count += 1
                        if prep_every and qt_count % prep_every == 0:
                            self.prep_tick(1)
                        a1, t_a1 = pacc[0].next(); a2, t_a2 = pacc[1].next()
                        accs = ((a1, t_a1), (a2, t_a2))

                        def emit_st(kt):
                            ps, t_ps = psr.next()
                            for m in range(2):
                                kb.op("pe", lambda e: e.matmul(ps[:, m, :], kT[:, kt * 128:(kt + 1) * 128],
                                                               qT[:, m, q0:q0 + 256], start=(m == 0), stop=True),
                                      reads=[t_k, t_q], writes=[t_ps])
                            return ps, t_ps
                        ahead = [emit_st(kts[0])]
                        if len(kts) > 1:
                            ahead.append(emit_st(kts[1]))
                        for ki, kt in enumerate(kts):
                            ps, t_ps = ahead.pop(0)
                            if ki + 2 < len(kts):
                                ahead.append(emit_st(kts[ki + 2]))
                            pt, t_pt = ptr.next()
                            kb.op("act", lambda e: e.activation(out=pt[:], in_=ps[:], func=AF.Exp, scale=0.125), reads=[t_ps], writes=[t_pt])
                            for m in range(2):
                                for s in range(2):
                                    kb.op("pe", lambda e: e.matmul(accs[m][0][:, s, 0:129], pt[:, m, s * 128:(s + 1) * 128], vv[:, kt, 0:129],
                                                                   start=(ki == 0 and s == 0), stop=(ki == len(kts) - 1)),
                                          reads=[t_pt, t_v], writes=[accs[m][1]])
                            while pending and pending[0][0] <= ki:
                                pending.pop(0)[1]()
                        while pending:
                            pending.pop(0)[1]()
                        sts, o2s = [], []
                        for s in range(2):
                            st, t_st = str_.next()
                            kb.op("dve", lambda e: e.reciprocal(out=st[:, 0:1], in_=a1[:, s, 128:129]), reads=[t_a1], writes=[t_st])
                            kb.op("dve", lambda e: e.reciprocal(out=st[:, 1:2], in_=a2[:, s, 128:129]), reads=[t_a2, t_st], writes=[t_st])
                            kb.op("dve", lambda e: e.tensor_tensor(out=st[:, 2:3], in0=st[:, 1:2], in1=nlam, op=ALU.mult), reads=[t_st, t_ls], writes=[t_st])
                            o1, t_o1 = o1r.next(); o2, t_o2 = o2r.next()
                            kb.op("dve", lambda e: e.tensor_scalar(out=o1[:], in0=a1[:, s, 0:128], scalar1=st[:, 0:1], scalar2=None, op0=ALU.mult),
                                  reads=[t_a1, t_st], writes=[t_o1])
                            kb.op("dve", lambda e: e.scalar_tensor_tensor(out=o2[:], in0=a2[:, s, 0:128], scalar=st[:, 2:3], op0=ALU.mult,
                                                                          in1=o1[:], op1=ALU.add), reads=[t_a2, t_st, t_o1], writes=[t_o2])
                            kb.op("dve", lambda e: e.tensor_tensor(out=o1[:], in0=o2[:], in1=o2[:], op=ALU.mult), reads=[t_o2, t_o1], writes=[t_o1])
                            kb.op("dve", lambda e: e.tensor_reduce(out=st[:, 3:4], in_=o1[:], op=ALU.add, axis=AX.X), reads=[t_o1, t_st], writes=[t_st])
                            sts.append((st, t_st)); o2s.append((o2, t_o2))

                        def n2(sts=sts):
                            for st, t_st in sts:
                                kb.op("act", lambda e: e.activation(out=st[:, 4:5], in_=st[:, 3:4], func=AF.Ln, scale=1.0 / 128, bias=self.eps_t[:, 0:1]),
                                      reads=[t_st], writes=[t_st])
                                kb.op("act", lambda e: e.activation(out=st[:, 5:6], in_=st[:, 4:5], func=AF.Exp, scale=-0.5),
                                      reads=[t_st], writes=[t_st])

                        def n3(sts=sts, o2s=o2s, h=h, c0=t0 + q0):
                            po, t_po = pto.next()
                            for s in range(2):
                                st, t_st = sts[s]; o2, t_o2 = o2s[s]
                                ab, t_ab = abr.next()
                                kb.op("dve", lambda e: e.scalar_tensor_tensor(out=ab[:], in0=o2[:], scalar=st[:, 5:6], op0=ALU.mult, in1=sg[:], op1=ALU.mult),
                                      reads=[t_o2, t_st, t_sg], writes=[t_ab])
                                kb.op("pe", lambda e: e.transpose(out=po[:, s, :], in_=ab[:], identity=self.ident_bf[:]), reads=[t_ab, self.t_ident], writes=[t_po])
                            aT, t_aT = aTr.next()
                            kb.op("dve", lambda e: e.tensor_copy(out=aT[:], in_=po[:].rearrange("p s t -> p (s t)")), reads=[t_po], writes=[t_aT])
                            kb.dma("sp", [(self.catT[:, h, c0:c0 + 256], aT[:])], reads=[t_aT])
                        pending.append((7, n2)); pending.append((10, n3))
            while pending:
                pending.pop(0)[1]()
            self.prep_flush()

    def phase_fnet(self, l, need_ctx):
        kb, I = self.kb, self.I
        with kb.phase():
            fw = kb.sb([128, 2, 256], F32); cb = kb.sb([128, 2, 128], F32); t_fw = Tok()
            kb.dma("sp", [(fw[:], I["fnet_w"][l].rearrange("(c p) m -> p c m", p=128)), (cb[:], I["cblk"][:, :, :])], writes=[t_fw])
            W = kb.sb([128, 2, 2, 256], BF16); t_W = Tok()
            pw = Ring(kb, 2, [128, 512], F32, kind="ps")
            for ab in range(2):
                for ct in range(2):
                    p, t_p = pw.next()
                    kb.op("pe", lambda e: e.matmul(p[:, 0:256], cb[:, ab, :], fw[:, ct, :], start=True, stop=True), reads=[t_fw], writes=[t_p])
                    kb.op("dve", lambda e: e.tensor_copy(out=W[:, ab, ct, :], in_=p[:, 0:256]), reads=[t_p], writes=[t_W])
            fa = kb.sb([128, NTILE, 256], BF16); t_fa = Tok()
            kb.dma("sp", [(fa[:], self.fD[:, :, :])], writes=[t_fa])
            dr = [Ring(kb, 2, [128, 16, 512], BF16) for _ in range(2)]
            absb = Ring(kb, 2, [128, 2, 2, 512], BF16); fo = Ring(kb, 2, [128, 512], BF16)
            pf = Ring(kb, 2, [128, 512], F32, kind="ps")

            def dft(b, tiles, mats, n, tok0):
                ab_t, t_ab = absb.next()
                for ab in range(2):
                    m, t_m = mats[ab]
                    for ct in range(2):
                        p, t_p = pw.next()
                        for i, tt in enumerate(tiles):
                            kb.op("pe", lambda e: e.matmul(p[:, 0:n], fa[:, tt, ct * 128:(ct + 1) * 128], m[:, i, 0:n], start=(i == 0), stop=(i == len(tiles) - 1)),
                                  reads=[t_fa, t_m], writes=[t_p])
                        eng = "act" if ct == 0 else "dve"
                        if eng == "act":
                            kb.op("act", lambda e: e.copy(out=ab_t[:, ab, ct, 0:n], in_=p[:, 0:n]), reads=[t_p], writes=[t_ab])
                        else:
                            kb.op("dve", lambda e: e.tensor_copy(out=ab_t[:, ab, ct, 0:n], in_=p[:, 0:n]), reads=[t_p], writes=[t_ab])
                for mt in range(2):
                    p, t_p = pf.next()
                    i = 0
                    for ab in range(2):
                        for ct in range(2):
                            kb.op("pe", lambda e: e.matmul(p[:, 0:n], W[:, ab, ct, mt * 128:(mt + 1) * 128], ab_t[:, ab, ct, 0:n], start=(i == 0), stop=(i == 3)),
                                  reads=[t_W, t_ab], writes=[t_p])
                            i += 1
                    f, t_f = fo.next()
                    kb.op("act", lambda e: e.copy(out=f[:, 0:n], in_=p[:, 0:n]), reads=[t_p], writes=[t_f])
                    kb.dma("sp", [(self.catT[:, 4 + mt, tok0:tok0 + n], f[:, 0:n])], reads=[t_f])

            mode = getattr(self, "fn_mode", "WMC")
            for pt in range(4 if "M" in mode else 0):
                mats = []
                for ab, nm in enumerate(("dft_c", "dft_s")):
                    m, t_m = dr[ab].next()
                    src = I[nm].rearrange("(t p) n -> p t n", p=128)
                    kb.dma("sp", [(m[:, q * 4:(q + 1) * 4, :], src[:, q * 4:(q + 1) * 4, pt * 512:(pt + 1) * 512]) for q in range(4)], writes=[t_m])
                    mats.append((m, t_m))
                for b in range(NB):
                    dft(b, [b * TILES_PB + 2 + i for i in range(16)], mats, 512, b * TPB + CTX + pt * 512)
            if need_ctx and "C" in mode:
                mats = []
                for ab, nm in enumerate(("dftc_c", "dftc_s")):
                    m, t_m = dr[ab].next()
                    kb.dma("sp", [(m[:, 0:2, 0:256], I[nm].rearrange("(t p) n -> p t n", p=128))], writes=[t_m])
                    mats.append((m, t_m))
                for b in range(NB):
                    dft(b, [b * TILES_PB + i for i in range(2)], mats, 256, b * TPB)

    def cmul(self, o_r, o_i, a_r, a_i, b_r, b_i, t1, t2, T, eng="dve"):
        kb = self.kb
        tt = lambda o, x, y, op: kb.op(eng, lambda e: e.tensor_tensor(out=o, in0=x, in1=y, op=op), reads=T, writes=T)
        tt(t1, a_r, b_r, ALU.mult); tt(t2, a_i, b_i, ALU.mult); tt(o_r, t1, t2, ALU.subtract)
        tt(t1, a_r, b_i, ALU.mult); tt(t2, a_i, b_r, ALU.mult); tt(o_i, t1, t2, ALU.add)

    def phase_s5(self, l):
        kb, I = self.kb, self.I
        TWO_PI = 2.0 * math.pi
        MAGIC = 12582912.0
        with kb.phase():
            A = kb.sb([128, 32, 128], BF16); BsRI = kb.sb([128, 32, 2, 128], BF16); CqRI = kb.sb([128, 32, 2, 128], BF16)
            Wsel = kb.sb([128, 8, 240], BF16); V = None; Hb = None
            PL = kb.sb([128, 2, 10, 16], F32)
            nPLi = kb.sb([128, 10, 16], F32)
            dd = kb.sb([128, 2], F32); wg = kb.sb([128, 2, 256], BF16)
            t_A, t_Bs, t_Cq, t_W, t_V, t_H, t_PL, t_misc = (Tok() for _ in range(8))
            kb.dma("sp", [(Wsel[:], I["wsel"][:, :, :])], writes=[t_W])
            kb.dma("sp", [(dd[:], I["s5_dd"][l])], writes=[t_misc])
            kb.dma("pool", [(wg[:], I["s5_w_glu"][l].rearrange("(c p) m -> p c m", p=128))], writes=[t_misc])
            with kb.phase():
                T = [Tok()]
                par = kb.sb([128, 3, 32], F32)
                bc = kb.sb([128, 2, 32, 2, 16], F32)
                msk = kb.sb([128, 2, 128], F32)
                kb.dma("sp", [(par[:], I["s5_par"][l]), (bc[:, 0], I["s5_b"][l]), (bc[:, 1], I["s5_c"][l]), (msk[:], I["s5_mask"][:, :, :])], writes=T)
                w = kb.sb([128, 24, 32], F32)
                W_ = lambda i: w[:, i, :]
                ts = lambda o, x, s1, o0, s2=None, o1=None: kb.op("dve", lambda e: e.tensor_scalar(out=o, in0=x, scalar1=s1, scalar2=s2, op0=o0, **({"op1": o1} if o1 else {})), reads=T, writes=T)
                tt = lambda o, x, y, op: kb.op("dve", lambda e: e.tensor_tensor(out=o, in0=x, in1=y, op=op), reads=T, writes=T)
                act = lambda o, x, f, **kw: kb.op("act", lambda e: e.activation(out=o, in_=x, func=f, **kw), reads=T, writes=T)
                are, aim, ldt = par[:, 0, :], par[:, 1, :], par[:, 2, :]
                dt, mag, ang, lr, li = W_(0), W_(1), W_(2), W_(3), W_(4)
                act(dt, ldt, AF.Exp)
                tt(mag, are, dt, ALU.mult); act(mag, mag, AF.Exp)
                tt(ang, aim, dt, ALU.mult)

                def sin_of(o, x, shift):
                    a, r = W_(5), W_(6)
                    ts(a, x, shift, ALU.add)
                    ts(r, a, 1.0 / TWO_PI, ALU.mult)
                    ts(r, r, MAGIC, ALU.add)
                    ts(r, r, MAGIC, ALU.subtract)
                    kb.op("dve", lambda e: e.scalar_tensor_tensor(out=a, in0=r, scalar=-TWO_PI, op0=ALU.mult, in1=a, op1=ALU.add), reads=T, writes=T)
                    act(o, a, AF.Sin)
                sin_of(li, ang, 0.0); sin_of(lr, ang, math.pi / 2)
                tt(lr, lr, mag, ALU.mult); tt(li, li, mag, ALU.mult)
                nr, den, cr, ci, t1, t2 = W_(7), W_(8), W_(9), W_(10), W_(11), W_(12)
                ts(nr, lr, -1.0, ALU.add)
                tt(t1, are, are, ALU.mult); tt(t2, aim, aim, ALU.mult); tt(den, t1, t2, ALU.add)
                kb.op("dve", lambda e: e.reciprocal(out=den, in_=den), reads=T, writes=T)
                tt(t1, nr, are, ALU.mult); tt(t2, li, aim, ALU.mult); tt(cr, t1, t2, ALU.add); tt(cr, cr, den, ALU.mult)
                tt(t1, li, are, ALU.mult); tt(t2, nr, aim, ALU.mult); tt(ci, t1, t2, ALU.subtract); tt(ci, ci, den, ALU.mult)
                ilr, ili, m2 = W_(13), W_(14), W_(15)
                tt(t1, lr, lr, ALU.mult); tt(t2, li, li, ALU.mult); tt(m2, t1, t2, ALU.add)
                kb.op("dve", lambda e: e.reciprocal(out=m2, in_=m2), reads=T, writes=T)
                tt(ilr, lr, m2, ALU.mult); tt(ili, li, m2, ALU.mult); ts(ili, ili, -1.0, ALU.mult)
                bb = kb.sb([128, 2, 32, 16], F32); tb = kb.sb([128, 2, 32, 16], F32)
                bcast = lambda v: v.unsqueeze(2).broadcast_to([128, 32, 16])
                self.cmul(bb[:, 0], bb[:, 1], bcast(cr), bcast(ci), bc[:, 0, :, 0, :], bc[:, 0, :, 1, :], tb[:, 0], tb[:, 1], T)
                mu = kb.sb([128, 2, 2, 3, 32], F32)
                cp = lambda o, x: kb.op("dve", lambda e: e.tensor_copy(out=o, in_=x), reads=T, writes=T)
                for ri, (fw_, bw_) in enumerate(((ilr, lr), (ili, li))):
                    cp(mu[:, 0, ri, 0, 0:16], fw_[:, 0:16]); cp(mu[:, 0, ri, 0, 16:32], bw_[:, 16:32])
                    cp(mu[:, 1, ri, 0, 0:16], bw_[:, 0:16]); cp(mu[:, 1, ri, 0, 16:32], fw_[:, 16:32])
                for tb_i in range(2):
                    for pw in range(2):
                        self.cmul(mu[:, tb_i, 0, pw + 1], mu[:, tb_i, 1, pw + 1], mu[:, tb_i, 0, pw], mu[:, tb_i, 1, pw],
                                  mu[:, tb_i, 0, pw], mu[:, tb_i, 1, pw], t1, t2, T)
                ch = kb.sb([128, 2, 2, 32, 8], F32)
                for tb_i in range(2):
                    kb.op("dve", lambda e: e.memset(ch[:, tb_i, 0, :, 0:1], 1.0), reads=T, writes=T)
                    kb.op("dve", lambda e: e.memset(ch[:, tb_i, 1, :, 0:1], 0.0), reads=T, writes=T)
                    n = 1
                    tmpc = kb.sb([128, 2, 32, 4], F32)
                    for pw in range(3):
                        mb = lambda ri: mu[:, tb_i, ri, pw, :].unsqueeze(2).broadcast_to([128, 32, n])
                        self.cmul(ch[:, tb_i, 0, :, n:2 * n], ch[:, tb_i, 1, :, n:2 * n], ch[:, tb_i, 0, :, 0:n], ch[:, tb_i, 1, :, 0:n],
                                  mb(0), mb(1), tmpc[:, 0, :, 0:n], tmpc[:, 1, :, 0:n], T)
                        n *= 2
                l8 = kb.sb([128, 2, 32], F32); l7 = kb.sb([128, 2, 32], F32); l2 = kb.sb([128, 2, 2, 32], F32)
                self.cmul(l2[:, 0, 0], l2[:, 0, 1], lr, li, lr, li, t1, t2, T)
                self.cmul(l2[:, 1, 0], l2[:, 1, 1], l2[:, 0, 0], l2[:, 0, 1], l2[:, 0, 0], l2[:, 0, 1], t1, t2, T)
                self.cmul(l8[:, 0], l8[:, 1], l2[:, 1, 0], l2[:, 1, 1], l2[:, 1, 0], l2[:, 1, 1], t1, t2, T)
                self.cmul(l7[:, 0], l7[:, 1], l8[:, 0], l8[:, 1], ilr, ili, t1, t2, T)
                sf = kb.sb([128, 2, 2, 32], F32)
                cp(sf[:, 0, 0, 0:16], l7[:, 0, 0:16]); cp(sf[:, 0, 1, 0:16], l7[:, 1, 0:16])
                kb.op("dve", lambda e: e.memset(sf[:, 0, 0, 16:32], 1.0), reads=T, writes=T)
                kb.op("dve", lambda e: e.memset(sf[:, 0, 1, 16:32], 0.0), reads=T, writes=T)
                cp(sf[:, 1, 0, 0:16], lr[:, 0:16]); cp(sf[:, 1, 1, 0:16], li[:, 0:16])
                cp(sf[:, 1, 0, 16:32], l8[:, 0, 16:32]); cp(sf[:, 1, 1, 16:32], l8[:, 1, 16:32])
                ch2 = kb.sb([128, 2, 2, 32, 8], F32)
                tmp8 = kb.sb([128, 2, 32, 8], F32)
                for tb_i in range(2):
                    sb_ = lambda ri: sf[:, tb_i, ri, :].unsqueeze(2).broadcast_to([128, 32, 8])
                    self.cmul(ch2[:, tb_i, 0], ch2[:, tb_i, 1], ch[:, tb_i, 0], ch[:, tb_i, 1], sb_(0), sb_(1), tmp8[:, 0], tmp8[:, 1], T)
                full = kb.sb([128, 2, 32, 8, 16], F32); ftmp = kb.sb([128, 2, 32, 8, 16], F32)
                st = kb.sb([128, 32, 128], BF16)
                pA = Ring(kb, 2, [128, 4, 128], F32, kind="ps"); pTt = Ring(kb, 2, [128, 4, 128], BF16, kind="ps")
                KBst = kb.sb([128, 32, 128], BF16); QCst = kb.sb([128, 32, 128], BF16)

                def build(chain, tb_i, src_r, src_i, dst, neg_im):
                    cb = lambda ri: chain[:, tb_i, ri].unsqueeze(3).broadcast_to([128, 32, 8, 16])
                    sbq = lambda v: v.unsqueeze(2).broadcast_to([128, 32, 8, 16])
                    self.cmul(full[:, 0], full[:, 1], cb(0), cb(1), sbq(src_r), sbq(src_i), ftmp[:, 0], ftmp[:, 1], T)
                    cp(dst[0:64], full[0:64, 0].rearrange("p a s c -> p a (s c)"))
                    if neg_im:
                        ts(dst[64:128], full[64:128, 1].rearrange("p a s c -> p a (s c)"), -1.0, ALU.mult)
                    else:
                        cp(dst[64:128], full[64:128, 1].rearrange("p a s c -> p a (s c)"))
                build(ch, 0, bb[:, 0], bb[:, 1], KBst, False)
                build(ch, 1, bc[:, 1, :, 0, :], bc[:, 1, :, 1, :], QCst, True)
                for q4 in range(8):
                    p, t_p = pA.next()
                    for i in range(4):
                        dg = q4 * 4 + i
                        kb.op("pe", lambda e: e.matmul(p[:, i, :], KBst[:, dg, :], QCst[:, dg, :], start=(i == 0), stop=True), reads=T, writes=[t_p])
                    d = 0 if q4 < 4 else 1
                    kb.op("dve", lambda e: e.tensor_tensor(out=A[:, q4 * 4:(q4 + 1) * 4, :], in0=p[:],
                                                           in1=msk[:, d:d + 1, :].broadcast_to([128, 4, 128]), op=ALU.mult),
                          reads=[t_p] + T, writes=[t_A])
                kb.op("pool", lambda e: e.memset(BsRI[:], 0.0), writes=[t_Bs])
                kb.op("pool", lambda e: e.memset(CqRI[:], 0.0), writes=[t_Cq])
                build(ch2, 0, bb[:, 0], bb[:, 1], st, False)
                for q4 in range(8):
                    p, t_p = pTt.next()
                    for i in range(4):
                        dg = q4 * 4 + i
                        kb.op("pe", lambda e: e.transpose(out=p[:, i, :], in_=st[:, dg, :], identity=self.ident_bf[:]), reads=T + [self.t_ident], writes=[t_p])
                    for i in range(4):
                        dg = q4 * 4 + i
                        g2 = dg % 2
                        kb.op("act", lambda e: e.copy(out=BsRI[:, dg, :, g2 * 64:(g2 + 1) * 64], in_=p[:, i, :].rearrange("p (r q) -> p r q", r=2)),
                              reads=[t_p], writes=[t_Bs])
                build(ch2, 1, bc[:, 1, :, 0, :], bc[:, 1, :, 1, :], st, True)
                for g2 in range(2):
                    rows = slice(g2 * 64, (g2 + 1) * 64)
                    sv = st[rows].rearrange("p (a g) x -> p a g x", g=2)
                    dv = CqRI[rows].rearrange("p (a g) r x -> p a g r x", g=2)
                    fr = full[rows, 0].rearrange("p (a g) s c -> p a g (s c)", g=2)
                    fi = full[rows, 1].rearrange("p (a g) s c -> p a g (s c)", g=2)
                    kb.op("dve", lambda e: e.tensor_copy(out=dv[:, :, g2, 0, :], in_=fr[:, :, g2, :]), reads=T, writes=[t_Cq])
                    kb.op("dve", lambda e: e.tensor_scalar(out=dv[:, :, g2, 1, :], in0=fi[:, :, g2, :], scalar1=-1.0, scalar2=None, op0=ALU.mult), reads=T, writes=[t_Cq])
                for ri in range(2):
                    for g2 in range(2):
                        rows = slice(g2 * 64, (g2 + 1) * 64)
                        kb.op("dve", lambda e: e.tensor_copy(out=PL[rows, ri, 0, :], in_=l8[rows, ri, :].rearrange("p (a g) -> p a g", g=2)[:, :, g2]),
                              reads=T, writes=[t_PL])
                pt1 = kb.sb([128, 16], F32); pt2 = kb.sb([128, 16], F32)
                for i in range(9):
                    self.cmul(PL[:, 0, i + 1], PL[:, 1, i + 1], PL[:, 0, i], PL[:, 1, i], PL[:, 0, i], PL[:, 1, i], pt1[:], pt2[:], [t_PL])
                kb.op("dve", lambda e: e.tensor_scalar(out=nPLi[:], in0=PL[:, 1], scalar1=-1.0, scalar2=None, op0=ALU.mult), reads=[t_PL], writes=[t_PL])
            self._s5_main(l, A, BsRI, CqRI, Wsel, V, Hb, PL, nPLi, dd, wg, (t_A, t_Bs, t_Cq, t_W, t_V, t_H, t_PL, t_misc))

    def _s5_main(self, l, A, BsRI, CqRI, Wsel, V, Hb, PL, nPLi, dd, wg, toks):
        kb, I = self.kb, self.I
        t_A, t_Bs, t_Cq, t_W, t_V, t_H, t_PL, t_misc = toks
        NCH = 288
        with kb.phase():
            V = kb.sb([128, 16, NB, 320], BF16)
            Hb = kb.sb([128, 2, 2, 8, NB, 288], BF16)
            with kb.phase():
                with kb.phase():
                    U = kb.sb([128, 2, NTOK], BF16); t_U = Tok()
                    kb.dma("sp", [(U[:, ct, :], self.uT[:, ct, :]) for ct in range(2)], writes=[t_U])
                    pv = Ring(kb, 2, [128, NCH], F32, kind="ps")
                    i = 0
                    for g in range(16):
                        gt, gl = divmod(g, 8)
                        for b in range(NB):
                            p, t_p = pv.next()
                            ub = U[:, gt, b * TPB:(b + 1) * TPB].rearrange("p (k s) -> p k s", s=8)
                            for s in range(8):
                                kb.op("pe", lambda e: e.matmul(p[:, :], Wsel[:, gl, 112 - 16 * s:240 - 16 * s], ub[:, :, s], start=(s == 0), stop=(s == 7)),
                                      reads=[t_U, t_W], writes=[t_p])
                            if i % 2 == 0:
                                kb.op("act", lambda e: e.copy(out=V[:, g, b, 0:NCH], in_=p[:, :]), reads=[t_p], writes=[t_V])
                                kb.op("act", lambda e: e.copy(out=V[:, g, b, NCH:320], in_=p[:, 0:32]), reads=[t_p], writes=[t_V])
                            else:
                                kb.op("dve", lambda e: e.tensor_copy(out=V[:, g, b, 0:NCH], in_=p[:, :]), reads=[t_p], writes=[t_V])
                                kb.op("dve", lambda e: e.tensor_copy(out=V[:, g, b, NCH:320], in_=p[:, 0:32]), reads=[t_p], writes=[t_V])
                            i += 1
                X = kb.sb([128, 2, 2, 8, NB, NCH], F32)
                sctmp = None
                t_X = [Tok(), Tok()]
                t_XG = [[[Tok(), Tok()] for _ in range(8)] for _ in range(2)]
                pS = Ring(kb, 4, [128, NCH], F32, kind="ps")
                for d in range(2):
                    k0 = 0 if d == 0 else 32
                    for gp in range(8):
                        for b in range(NB):
                            for ri in range(2):
                                p, t_p = pS.next()
                                for g2 in range(2):
                                    g = 2 * gp + g2
                                    kb.op("pe", lambda e: e.matmul(p[:, :], BsRI[:, d * 16 + g, ri, :], V[:, g, b, k0:k0 + NCH], start=(g2 == 0), stop=(g2 == 1)),
                                          reads=[t_Bs, t_V], writes=[t_p])
                                if ri == 0:
                                    kb.op("act", lambda e: e.copy(out=X[:, 0, ri, gp, b, :], in_=p[:, :]), reads=[t_p], writes=[t_XG[0][gp][ri]])
                                else:
                                    kb.op("dve", lambda e: e.tensor_copy(out=X[:, 0, ri, gp, b, :], in_=p[:, :]), reads=[t_p], writes=[t_XG[0][gp][ri]])
                    cur = 0
                    for si in range(9):
                        sh = 1 << si
                        nxt = 1 - cur
                        n = NCH - sh
                        if d == 0:
                            dst, src, keep = slice(sh, NCH), slice(0, n), slice(0, sh)
                        else:
                            dst, src, keep = slice(0, n), slice(sh, NCH), slice(n, NCH)
                        for ri in range(2):
                            kb.op("pool", lambda e: e.tensor_copy(out=X[:, nxt, ri, :, :, keep], in_=X[:, cur, ri, :, :, keep]), reads=[t_XG[cur][g_][ri] for g_ in range(8)], writes=[t_XG[nxt][g_][ri] for g_ in range(8)])
                        for opi in range(4):
                            for gp in range(8):
                                c = d * 8 + gp
                                Pr, Pi, nPi = PL[:, 0, si, c:c + 1], PL[:, 1, si, c:c + 1], nPLi[:, si, c:c + 1]
                                xr, xi = X[:, cur, 0, gp], X[:, cur, 1, gp]
                                yr, yi = X[:, nxt, 0, gp], X[:, nxt, 1, gp]
                                o_, a_, sc_, b_ = ((yr, xr, Pr, xr), (yi, xi, Pr, xi), (yr, xi, nPi, yr), (yi, xr, Pi, yi))[opi]
                                kb.op("dve", lambda e: e.scalar_tensor_tensor(out=o_[:, :, dst], in0=a_[:, :, src], scalar=sc_, op0=ALU.mult, in1=b_[:, :, dst], op1=ALU.add),
                                      reads=[t_XG[cur][gp][0], t_XG[cur][gp][1], t_PL], writes=[t_XG[nxt][gp][opi % 2]])
                        cur = nxt
                    for ri in range(2):
                        kb.op("act", lambda e: e.copy(out=Hb[:, d, ri], in_=X[:, cur, ri]), reads=[t_XG[cur][g_][ri] for g_ in range(8)], writes=[t_H])
            with kb.phase():
                U = kb.sb([128, 2, NTOK], BF16); t_U = Tok()
                kb.dma("sp", [(U[:, ct, :], self.uT[:, ct, :]) for ct in range(2)], writes=[t_U])
                Yc = kb.sb([128, 16, NB, NCH], BF16); t_Y = Tok()
                G = kb.sb([128, 2, NTOK], BF16); t_G = Tok()
                py = Ring(kb, 2, [128, NCH], F32, kind="ps")
                i = 0
                for g in range(16):
                    gp, g2 = divmod(g, 2)
                    for b in range(NB):
                        p, t_p = py.next()
                        mm = lambda o, lh, rh, first=False: kb.op("pe", lambda e: e.matmul(o, lh, rh, start=first, stop=True),
                                                                  reads=[t_A, t_Cq, t_V, t_H], writes=[t_p])
                        mm(p[:, 0:NCH], A[:, g, :], V[:, g, b, 0:NCH], True)
                        for ri in range(2):
                            mm(p[:, 1:NCH], CqRI[:, g, ri, :], Hb[:, 0, ri, gp, b, 0:NCH - 1])
                        mm(p[:, 32:NCH], A[:, 16 + g, :], V[:, g, b, 32:NCH])
                        mm(p[:, 0:32], A[:, 16 + g, :], V[:, g, b, NCH:320])
                        for ri in range(2):
                            mm(p[:, 32:NCH], CqRI[:, 16 + g, ri, :], Hb[:, 1, ri, gp, b, 1:257])
                            mm(p[:, 0:31], CqRI[:, 16 + g, ri, :], Hb[:, 1, ri, gp, b, 257:NCH])
                        if i % 2 == 0:
                            kb.op("act", lambda e: e.copy(out=Yc[:, g, b, :], in_=p[:, :]), reads=[t_p], writes=[t_Y])
                        else:
                            kb.op("dve", lambda e: e.tensor_copy(out=Yc[:, g, b, :], in_=p[:, :]), reads=[t_p], writes=[t_Y])
                        i += 1
                pu = Ring(kb, 2, [128, 64, 8], F32, kind="ps")
                yyr = Ring(kb, 2, [128, 512], F32)
                for gt in range(2):
                    for b in range(NB):
                        for seg in range(5):
                            nk = 64 if seg < 4 else 32
                            p, t_p = pu.next()
                            first = True
                            for t in range(8):
                                for gl in range(8):
                                    kb.op("pe", lambda e: e.matmul(p[:, 0:nk, t], Wsel[:, t, 112 - 16 * gl:240 - 16 * gl], Yc[:, gt * 8 + gl, b, seg * 64:seg * 64 + nk],
                                                                   start=first, stop=True), reads=[t_W, t_Y], writes=[t_p])
                                    first = False
                            tok0 = b * TPB + seg * 512
                            nt = nk * 8
                            yy, t_yy = yyr.next()
                            kb.op("dve", lambda e: e.scalar_tensor_tensor(out=yy[:, 0:nt], in0=U[:, gt, tok0:tok0 + nt], scalar=dd[:, gt:gt + 1], op0=ALU.mult,
                                                                          in1=p[:, 0:nk, :].rearrange("p k t -> p (k t)"), op1=ALU.add),
                                  reads=[t_U, t_misc, t_p], writes=[t_yy])
                            kb.op("act", lambda e: e.activation(out=G[:, gt, tok0:tok0 + nt], in_=yy[:, 0:nt], func=AF.Gelu_apprx_tanh), reads=[t_yy], writes=[t_G])
                pz = Ring(kb, 2, [128, 512], F32, kind="ps")
                sgr = Ring(kb, 2, [128, 512], BF16); sor = Ring(kb, 2, [128, 512], BF16)
                for tt_ in range(NTOK // 512):
                    for mt in range(2):
                        p, t_p = pz.next()
                        for gt in range(2):
                            kb.op("pe", lambda e: e.matmul(p[:, :], wg[:, gt, mt * 128:(mt + 1) * 128], G[:, gt, tt_ * 512:(tt_ + 1) * 512], start=(gt == 0), stop=(gt == 1)),
                                  reads=[t_misc, t_G], writes=[t_p])
                        sg_, t_sg = sgr.next()
                        kb.op("act", lambda e: e.activation(out=sg_[:], in_=p[:, :], func=AF.Sigmoid), reads=[t_p], writes=[t_sg])
                        so, t_so = sor.next()
                        kb.op("dve", lambda e: e.tensor_tensor(out=so[:], in0=G[:, mt, tt_ * 512:(tt_ + 1) * 512], in1=sg_[:], op=ALU.mult), reads=[t_G, t_sg], writes=[t_so])
                        kb.dma("sp", [(self.catT[:, 6 + mt, tt_ * 512:(tt_ + 1) * 512], so[:])], reads=[t_so])

    def groups(self, with_ctx):
        gs = []
        for b in range(NB):
            if with_ctx:
                gs.append((b * TPB, CTX, 2))
            for i in range(4):
                gs.append((b * TPB + CTX + i * 512, 512, b))
        return gs

    def post_norm_tile(self, halves, t_halves, xt, t_x, gg, t_gg, ms, R):
        kb = self.kb
        st, t_st = R["stat2"].next()
        for nh in range(2):
            jk, t_jk = R["junk2"].next()
            kb.op("act", lambda e: e.activation(out=jk[:], in_=halves[nh], func=AF.Square, accum_out=st[:, nh:nh + 1]),
                  reads=[t_halves[nh], t_st], writes=[t_jk, t_st])
        kb.op("dve", lambda e: e.tensor_tensor(out=st[:, 2:3], in0=st[:, 0:1], in1=st[:, 1:2], op=ALU.add), reads=[t_st], writes=[t_st])
        kb.op("act", lambda e: e.activation(out=st[:, 3:4], in_=st[:, 2:3], func=AF.Sqrt, scale=1.0 / D, bias=self.eps_t[:, 0:1]), reads=[t_st], writes=[t_st])
        kb.op("dve", lambda e: e.reciprocal(out=st[:, 4:5], in_=st[:, 3:4]), reads=[t_st], writes=[t_st])
        tmp, t_t = R["tmp"].next()
        for nh in range(2):
            kb.op("dve", lambda e: e.scalar_tensor_tensor(out=tmp[:, nh * 512:(nh + 1) * 512], in0=halves[nh], scalar=st[:, 4:5], op0=ALU.mult,
                                                          in1=gg[:, ms, nh * 512:(nh + 1) * 512], op1=ALU.mult),
                  reads=[t_halves[nh], t_st, t_gg], writes=[t_t])
        xn, t_xn = R["xn"].next()
        kb.op("pool", lambda e: e.tensor_tensor(out=xn[:], in0=tmp[:], in1=xt[:], op=ALU.add), reads=[t_t, t_x], writes=[t_xn])
        return xn, t_xn

    def pn1(self, halves, t_halves, R):
        kb = self.kb
        st, t_st = R["stat2"].next()
        for nh in range(2):
            jk, t_jk = R["junk2"].next()
            kb.op("act", lambda e: e.activation(out=jk[:], in_=halves[nh], func=AF.Square, accum_out=st[:, nh:nh + 1]),
                  reads=[t_halves[nh], t_st], writes=[t_jk, t_st])
        kb.op("dve", lambda e: e.tensor_tensor(out=st[:, 2:3], in0=st[:, 0:1], in1=st[:, 1:2], op=ALU.add), reads=[t_st], writes=[t_st])
        kb.op("act", lambda e: e.activation(out=st[:, 3:4], in_=st[:, 2:3], func=AF.Sqrt, scale=1.0 / D, bias=self.eps_t[:, 0:1]), reads=[t_st], writes=[t_st])
        kb.op("dve", lambda e: e.reciprocal(out=st[:, 4:5], in_=st[:, 3:4]), reads=[t_st], writes=[t_st])
        return st, t_st

    def pn2(self, halves, t_halves, st, t_st, xt, t_x, gg, t_gg, ms, R):
        kb = self.kb
        tmp, t_t = R["tmp"].next()
        for nh in range(2):
            kb.op("dve", lambda e: e.scalar_tensor_tensor(out=tmp[:, nh * 512:(nh + 1) * 512], in0=halves[nh], scalar=st[:, 4:5], op0=ALU.mult,
                                                          in1=gg[:, ms, nh * 512:(nh + 1) * 512], op1=ALU.mult),
                  reads=[t_halves[nh], t_st, t_gg], writes=[t_t])
        xn, t_xn = R["xn"].next()
        kb.op("pool", lambda e: e.tensor_tensor(out=xn[:], in0=tmp[:], in1=xt[:], op=ALU.add), reads=[t_t, t_x], writes=[t_xn])
        return xn, t_xn

    def nm1(self, xt, t_x, R):
        kb = self.kb
        junk, t_j = R["junk"].next()
        st, t_st = R["stat"].next()
        kb.op("act", lambda e: e.activation(out=junk[:], in_=xt[:], func=AF.Square, accum_out=st[:, 0:1]), reads=[t_x], writes=[t_j, t_st])
        kb.op("act", lambda e: e.activation(out=st[:, 1:2], in_=st[:, 0:1], func=AF.Sqrt, scale=1.0 / D, bias=self.eps_t[:, 0:1]), reads=[t_st], writes=[t_st])
        kb.op("dve", lambda e: e.reciprocal(out=st[:, 2:3], in_=st[:, 1:2]), reads=[t_st], writes=[t_st])
        return st, t_st

    def nm2(self, xt, t_x, st, t_st, gsc, sh, t_g, t_s, ms, R):
        kb = self.kb
        tmp, t_t = R["tmp"].next()
        kb.op("dve", lambda e: e.scalar_tensor_tensor(out=tmp[:], in0=xt[:], scalar=st[:, 2:3], op0=ALU.mult, in1=gsc[:, ms, :], op1=ALU.mult),
              reads=[t_x, t_st, t_g], writes=[t_t])
        hb, t_h = R["hb"].next()
        kb.op("pool", lambda e: e.tensor_tensor(out=hb[:], in0=tmp[:], in1=sh[:, ms, :], op=ALU.add), reads=[t_t, t_s], writes=[t_h])
        return hb, t_h

    @staticmethod
    def run_pipeline(n, stages):
        for it in range(n + len(stages) - 1):
            for s_, f in enumerate(stages):
                j = it - s_
                if 0 <= j < n:
                    f(j)

    def phase_wout(self, l, xsrc, xdst, need_ctx, prep_wout=False):
        kb, I = self.kb, self.I
        with kb.phase():
            gg, _, t_gg, _ = self.load_mod_tiles(l, 2, None, "norm_mix_post", plus_one=False)
            gsc2, sh2, t_g2, t_s2 = self.load_mod_tiles(l, 4, 3, "norm_ffn_pre")
            wo = kb.sb([128, 8, D], BF16); t_wo = Tok()
            wv = I["w_out"][l].rearrange("(k p) n -> p k n", p=128)
            kb.dma("pool", [(wo[:, :, c * 512:(c + 1) * 512], wv[:, :, c * 512:(c + 1) * 512]) for c in range(2)], writes=[t_wo])
            R = {"junk": Ring(kb, 1, [128, D], BF16), "stat": Ring(kb, 8, [128, 4], F32), "tmp": Ring(kb, 3, [128, D], F32),
                 "hb": Ring(kb, 3, [128, D], BF16), "stat2": Ring(kb, 8, [128, 8], F32), "junk2": Ring(kb, 1, [128, 512], BF16),
                 "xn": Ring(kb, 4, [128, D], F32)}
            xr = Ring(kb, 4, [128, D], F32)
            cgr = Ring(kb, 2, [128, 8, 512], BF16); hgr = Ring(kb, 2, [128, 8, 512], BF16)
            pm = [Ring(kb, 3, [128, 512], F32, kind="ps") for _ in range(2)]
            pT = Ring(kb, 2, [128, 8, 128], BF16, kind="ps")
            items = []
            for (tok0, w, ms) in self.groups(need_ctx):
                for j in range(w // 128):
                    items.append((tok0, w, ms, j))
            C = [dict() for _ in items]

            self.prep_begin()

            def s_mm(i):
                tok0, w, ms, j = items[i]
                if prep_wout:
                    self.prep_tick(1)
                if j == 0:
                    cg, t_cg = cgr.next()
                    kb.dma("sp", [(cg[:, :, 0:w], self.catT[:, :, tok0:tok0 + w])], writes=[t_cg])
                    self._cg = (cg, t_cg)
                    self._hg = hgr.next()
                cg, t_cg = self._cg
                C[i]["hg"] = self._hg
                r0 = tok0 + j * 128
                xt, t_x = xr.next()
                kb.dma("sp", [(xt[:], xsrc[r0:r0 + 128, :])], writes=[t_x])
                hs, ths = [], []
                for nh in range(2):
                    p, t_p = pm[nh].next()
                    for k in range(8):
                        kb.op("pe", lambda e: e.matmul(p[:, :], cg[:, k, j * 128:(j + 1) * 128], wo[:, k, nh * 512:(nh + 1) * 512], start=(k == 0), stop=(k == 7)),
                              reads=[t_cg, t_wo], writes=[t_p])
                    hs.append(p[:, :]); ths.append(t_p)
                C[i].update(hs=hs, ths=ths, xt=xt, t_x=t_x)

            def s_p1(i):
                c = C[i]
                c["st"], c["t_st"] = self.pn1(c["hs"], c["ths"], R)

            def s_p2(i):
                c = C[i]
                tok0, w, ms, j = items[i]
                r0 = tok0 + j * 128
                c["xn"], c["t_xn"] = self.pn2(c["hs"], c["ths"], c["st"], c["t_st"], c["xt"], c["t_x"], gg, t_gg, ms, R)
                kb.dma("sp", [(xdst[r0:r0 + 128, :], c["xn"][:])], reads=[c["t_xn"]])

            def s_n1(i):
                c = C[i]
                c["st2"], c["t_st2"] = self.nm1(c["xn"], c["t_xn"], R)

            def s_n2(i):
                c = C[i]
                tok0, w, ms, j = items[i]
                r0 = tok0 + j * 128
                c["hb"], c["t_h"] = self.nm2(c["xn"], c["t_xn"], c["st2"], c["t_st2"], gsc2, sh2, t_g2, t_s2, ms, R)
                if l == 1:
                    kb.dma("sp", [(self.h2tm[r0:r0 + 128, :], c["hb"][:])], reads=[c["t_h"]])

            def s_t(i):
                c = C[i]
                tok0, w, ms, j = items[i]
                hg, t_hg = c["hg"]
                p, t_p = pT.next()
                for k in range(8):
                    kb.op("pe", lambda e: e.transpose(out=p[:, k, :], in_=c["hb"][:, k * 128:(k + 1) * 128], identity=self.ident_bf[:]),
                          reads=[c["t_h"], self.t_ident], writes=[t_p])
                kb.op("act", lambda e: e.copy(out=hg[:, :, j * 128:(j + 1) * 128], in_=p[:]), reads=[t_p], writes=[t_hg])
                if j == w // 128 - 1:
                    kb.dma("sp", [(self.h2T[:, :, tok0:tok0 + w], hg[:, :, 0:w])], reads=[t_hg])
                C[i].clear()
            self.run_pipeline(len(items), [s_mm, s_p1, s_p2, s_n1, s_n2, s_t])
            self.prep_flush()

    def phase_ffn(self, l, xsrc, xdst, moe, final):
        kb, I = self.kb, self.I
        FG = 512
        NFC = FG // 128
        for b in range(NB):
            tok0 = b * TPB + (CTX if moe else 0)
            TG = SEQ if moe else TPB
            ntile = TG // 128
            with kb.phase():
                hT = kb.sb([128, 8, TG], BF16); t_hT = Tok()
                kb.dma("sp", [(hT[:, k, :], self.h2T[:, k, tok0:tok0 + TG]) for k in range(8)], writes=[t_hT])
                acc = kb.sb([128, ntile, D], F32); t_acc = [Tok() for _ in range(ntile)]
                gates = None
                if moe:
                    gates = kb.sb([128, ntile, 8], F32); t_gt = Tok()
                    with kb.phase():
                        rb = kb.sb([128, 8, 8], BF16); t_rb = Tok()
                        kb.dma("pool", [(rb[:], I["moe_router"][0].rearrange("(k p) e -> p k e", p=128))], writes=[t_rb])
                        pl = Ring(kb, 2, [128, 8], F32, kind="ps")
                        wk = Ring(kb, 2, [128, 6, 8], F32); sm = Ring(kb, 2, [128, 8], F32)
                        for j in range(ntile):
                            p, t_p = pl.next()
                            for k in range(8):
                                kb.op("pe", lambda e: e.matmul(p[:, :], hT[:, k, j * 128:(j + 1) * 128], rb[:, k, :], start=(k == 0), stop=(k == 7)),
                                      reads=[t_hT, t_rb], writes=[t_p])
                            w_, t_w = wk.next(); s_, t_s = sm.next()
                            T = [t_w, t_s]
                            kb.op("dve", lambda e: e.tensor_reduce(out=s_[:, 0:1], in_=p[:, :], op=ALU.max, axis=AX.X), reads=[t_p], writes=T)
                            kb.op("dve", lambda e: e.tensor_scalar(out=s_[:, 1:2], in0=s_[:, 0:1], scalar1=-1.0, scalar2=None, op0=ALU.mult), reads=T, writes=T)
                            kb.op("act", lambda e: e.activation(out=w_[:, 0, :], in_=p[:, :], func=AF.Exp, bias=s_[:, 1:2]), reads=[t_p] + T, writes=T)
                            kb.op("dve", lambda e: e.tensor_scalar(out=w_[:, 1, :], in0=w_[:, 0, :], scalar1=1.0, scalar2=None, op0=ALU.is_lt), reads=T, writes=T)
                            kb.op("dve", lambda e: e.tensor_tensor(out=w_[:, 2, :], in0=w_[:, 0, :], in1=w_[:, 1, :], op=ALU.mult), reads=T, writes=T)
                            kb.op("dve", lambda e: e.tensor_reduce(out=s_[:, 2:3], in_=w_[:, 2, :], op=ALU.max, axis=AX.X), reads=T, writes=T)
                            kb.op("dve", lambda e: e.tensor_scalar(out=s_[:, 3:4], in0=s_[:, 2:3], scalar1=1.0, scalar2=None, op0=ALU.add), reads=T, writes=T)
                            kb.op("dve", lambda e: e.reciprocal(out=s_[:, 4:5], in_=s_[:, 3:4]), reads=T, writes=T)
                            kb.op("dve", lambda e: e.tensor_scalar(out=w_[:, 3, :], in0=w_[:, 0, :], scalar1=s_[:, 2:3], scalar2=None, op0=ALU.is_ge), reads=T, writes=T)
                            kb.op("dve", lambda e: e.tensor_tensor(out=w_[:, 4, :], in0=w_[:, 0, :], in1=w_[:, 3, :], op=ALU.mult), reads=T, writes=T)
                            kb.op("dve", lambda e: e.tensor_scalar(out=gates[:, j, :], in0=w_[:, 4, :], scalar1=s_[:, 4:5], scalar2=None, op0=ALU.mult), reads=T, writes=[t_gt])
                with kb.phase():
                    wgr = Ring(kb, 2, [128, 8, FG], BF16); wur = Ring(kb, 2, [128, 8, FG], BF16); wdr = Ring(kb, 2, [128, NFC, D], BF16)
                    actr = Ring(kb, 2, [128, NFC, TG], BF16)
                    sgr = Ring(kb, 3, [128, 512], BF16); gtm = Ring(kb, 2, [128, 512], F32) if moe else None
                    evr = Ring(kb, 2, [128, 512], F32)
                    pG = Ring(kb, 2, [128, 512], F32, kind="ps"); pU = Ring(kb, 2, [128, 512], F32, kind="ps")
                    pD = Ring(kb, 3, [128, 512], F32, kind="ps"); pB = Ring(kb, 1, [128, 4, 128], F32, kind="ps")
                    gbr = Ring(kb, 2, [128, TG], BF16) if moe else None
                    gxr = Ring(kb, 2, [128, 128], BF16) if moe else None
                    experts = range(N_EXP) if moe else [0]
                    F = F_EXPERT if moe else F_DENSE
                    state = {"first": True, "nbank": 0}
                    pending = []

                    def emit_down(at, t_at, wd_, t_wd, nfc, tiles, first):
                        for j in tiles:
                            for nh in range(2):
                                p, t_p = pD.next()
                                for fc in range(nfc):
                                    kb.op("pe", lambda e: e.matmul(p[:, :], at[:, fc, j * 128:(j + 1) * 128], wd_[:, fc, nh * 512:(nh + 1) * 512],
                                                                   start=(fc == 0), stop=(fc == nfc - 1)), reads=[t_at, t_wd], writes=[t_p])
                                dst = acc[:, j, nh * 512:(nh + 1) * 512]
                                if first:
                                    kb.op("act", lambda e: e.copy(out=dst, in_=p[:, :]), reads=[t_p], writes=[t_acc[j]])
                                else:
                                    kb.op("dve", lambda e: e.tensor_tensor(out=dst, in0=p[:, :], in1=dst, op=ALU.add), reads=[t_p, t_acc[j]], writes=[t_acc[j]])
                                state["nbank"] += 1

                    for ex in experts:
                        if moe:
                            Wg, Wu, Wd = I["moe_w_gate"][0, ex], I["moe_w_up"][0, ex], I["moe_w_down"][0, ex]
                            gb, t_gb = gbr.next()
                            for q4 in range(ntile // 4):
                                p, t_p = pB.next()
                                for i in range(4):
                                    j = q4 * 4 + i
                                    gx, t_gx = gxr.next()
                                    kb.op("dve", lambda e: e.tensor_copy(out=gx[:], in_=gates[:, j, ex:ex + 1].broadcast_to([128, 128])), reads=[t_gt], writes=[t_gx])
                                    kb.op("pe", lambda e: e.matmul(p[:, i, :], gx[:], self.ident_bf[:], start=(i == 0), stop=True), reads=[t_gx, self.t_ident], writes=[t_p])
                                kb.op("act", lambda e: e.copy(out=gb[:, q4 * 512:(q4 + 1) * 512], in_=p[:].rearrange("p a b -> p (a b)")), reads=[t_p], writes=[t_gb])
                        else:
                            Wg, Wu, Wd = I["ffn_w_gate"][0], I["ffn_w_up"][0], I["ffn_w_down"][0]
                        wgv = Wg.rearrange("(k p) n -> p k n", p=128); wuv = Wu.rearrange("(k p) n -> p k n", p=128)
                        wdv = Wd.rearrange("(c p) n -> p c n", p=128)
                        f0 = 0
                        while f0 < F:
                            fw = min(FG, F - f0)
                            nfc = fw // 128
                            wg_, t_wg = wgr.next(); wu_, t_wu = wur.next(); wd_, t_wd = wdr.next()
                            kb.dma("pool", [(wg_[:, 0:4, 0:fw], wgv[:, 0:4, f0:f0 + fw]), (wg_[:, 4:8, 0:fw], wgv[:, 4:8, f0:f0 + fw])], writes=[t_wg])
                            kb.dma("pool", [(wu_[:, 0:4, 0:fw], wuv[:, 0:4, f0:f0 + fw]), (wu_[:, 4:8, 0:fw], wuv[:, 4:8, f0:f0 + fw])], writes=[t_wu])
                            kb.dma("pool", [(wd_[:, 0:nfc, :], wdv[:, f0 // 128:f0 // 128 + nfc, :])], writes=[t_wd])
                            at, t_at = actr.next()
                            c0 = 0
                            while c0 < TG:
                                n = min(512, TG - c0)
                                for fc in range(nfc):
                                    g_, t_g = pG.next(); u_, t_u = pU.next()
                                    for k in range(8):
                                        kb.op("pe", lambda e: e.matmul(g_[:, 0:n], wg_[:, k, fc * 128:(fc + 1) * 128], hT[:, k, c0:c0 + n], start=(k == 0), stop=(k == 7)),
                                              reads=[t_wg, t_hT], writes=[t_g])
                                    for k in range(8):
                                        kb.op("pe", lambda e: e.matmul(u_[:, 0:n], wu_[:, k, fc * 128:(fc + 1) * 128], hT[:, k, c0:c0 + n], start=(k == 0), stop=(k == 7)),
                                              reads=[t_wu, t_hT], writes=[t_u])
                                    sg_, t_sg = sgr.next()
                                    kb.op("act", lambda e: e.activation(out=sg_[:, 0:n], in_=g_[:, 0:n], func=AF.Silu), reads=[t_g], writes=[t_sg])
                                    if moe:
                                        tm, t_tm = gtm.next()
                                        kb.op("dve", lambda e: e.tensor_tensor(out=tm[:, 0:n], in0=u_[:, 0:n], in1=gb[:, c0:c0 + n], op=ALU.mult), reads=[t_u, t_gb], writes=[t_tm])
                                        kb.op("dve", lambda e: e.tensor_tensor(out=at[:, fc, c0:c0 + n], in0=tm[:, 0:n], in1=sg_[:, 0:n], op=ALU.mult), reads=[t_tm, t_sg], writes=[t_at])
                                    else:
                                        kb.op("dve", lambda e: e.tensor_tensor(out=at[:, fc, c0:c0 + n], in0=u_[:, 0:n], in1=sg_[:, 0:n], op=ALU.mult), reads=[t_u, t_sg], writes=[t_at])
                                while pending:
                                    pending.pop(0)()
                                tiles = list(range(c0 // 128, (c0 + n) // 128))
                                pending.append(lambda at=at, t_at=t_at, wd_=wd_, t_wd=t_wd, nfc=nfc, tiles=tiles, first=state["first"]:
                                               emit_down(at, t_at, wd_, t_wd, nfc, tiles, first))
                                c0 += n
                            state["first"] = False
                            f0 += fw
                    while pending:
                        pending.pop(0)()
                with kb.phase():
                    gg, _, t_gg, _ = self.load_mod_tiles(l, 5, None, "norm_ffn_post", plus_one=False)
                    R = {"tmp": Ring(kb, 3, [128, D], F32), "stat2": Ring(kb, 8, [128, 8], F32), "junk2": Ring(kb, 1, [128, 512], BF16),
                         "xn": Ring(kb, 3, [128, D], F32)}
                    xr = Ring(kb, 4, [128, D], F32)
                    C = [dict() for _ in range(ntile)]

                    def f_load(j):
                        xt, t_x = xr.next()
                        kb.dma("sp", [(xt[:], xsrc[tok0 + j * 128:tok0 + (j + 1) * 128, :])], writes=[t_x])
                        C[j].update(xt=xt, t_x=t_x, hs=[acc[:, j, 0:512], acc[:, j, 512:1024]], ths=[t_acc[j], t_acc[j]])

                    def f_p1(j):
                        C[j]["st"], C[j]["t_st"] = self.pn1(C[j]["hs"], C[j]["ths"], R)

                    def f_p2(j):
                        c = C[j]
                        is_ctx = (not moe) and j < 2
                        ms = 2 if is_ctx else b
                        xn, t_xn = self.pn2(c["hs"], c["ths"], c["st"], c["t_st"], c["xt"], c["t_x"], gg, t_gg, ms, R)
                        if final:
                            o0 = b * SEQ + j * 128
                            kb.dma("sp", [(xdst[o0:o0 + 128, :], xn[:])], reads=[t_xn])
                        else:
                            r0 = tok0 + j * 128
                            kb.dma("sp", [(xdst[r0:r0 + 128, :], xn[:])], reads=[t_xn])
                    self.run_pipeline(ntile, [f_load, f_p1, f_p2])

    def moe_declare(self):
        sc = self.scratch
        self.h2tm = sc("h2tm", [NTOK, D], BF16)
        nrow = N_EXP * 7 * 128
        self.Wg_s = sc("Wg_s", [nrow, 8 * 512], BF16); self.Wu_s = sc("Wu_s", [nrow, 8 * 512], BF16)
        self.Wd_s = sc("Wd_s", [nrow, 4 * D], BF16)
        self.hsorted = sc("hsorted", [MOE_SLOTS, D], BF16)
        self.ysorted = sc("ysorted", [MOE_SLOTS, D], F32)

    def moe_prep_gen(self, rings):
        kb, I = self.kb, self.I
        inflight = self.prep_inflight
        for ex in range(N_EXP):
            wgv = I["moe_w_gate"][0, ex].rearrange("(k p) n -> p k n", p=128)
            wuv = I["moe_w_up"][0, ex].rearrange("(k p) n -> p k n", p=128)
            wdv = I["moe_w_down"][0, ex].rearrange("(c p) n -> p c n", p=128)
            for fg in range(7):
                r0 = (ex * 7 + fg) * 128
                for kind, src, dst in ((0, wgv, self.Wg_s), (0, wuv, self.Wu_s), (1, wdv, self.Wd_s)):
                    t, tk = self.prep_rings_cur[0][kind].next()
                    if kind == 0:
                        kb.dma("pool", [(t[:, 0:4, :], src[:, 0:4, fg * 512:(fg + 1) * 512]), (t[:, 4:8, :], src[:, 4:8, fg * 512:(fg + 1) * 512])], writes=[tk])
                        flat = t[:].rearrange("p k n -> p (k n)")
                    else:
                        kb.dma("pool", [(t[:], src[:, fg * 4:(fg + 1) * 4, :])], writes=[tk])
                        flat = t[:].rearrange("p c n -> p (c n)")
                    inflight.append((dst[r0:r0 + 128, :], flat, tk))
                    if len(inflight) > 2:
                        d_, f_, k_ = inflight.pop(0)
                        kb.dma("pool", [(d_, f_)], reads=[k_])
                    yield
        self.prep_flush()
        yield

    def prep_begin(self):
        if self.prep is not None:
            self.prep_rings_cur[0] = self.prep_rings()

    def prep_tick(self, k=1):
        for _ in range(k):
            if self.prep is not None and next(self.prep, "done") == "done":
                self.prep = None

    def prep_flush(self):
        while self.prep_inflight:
            d_, f_, k_ = self.prep_inflight.pop(0)
            self.kb.dma("pool", [(d_, f_)], reads=[k_])

    def prep_rings(self):
        kb = self.kb
        return [Ring(kb, 3, [128, 8, 512], BF16), Ring(kb, 2, [128, 4, D], BF16)]

    def phase_moe_prep(self):
        kb = self.kb
        if self.prep is None:
            return
        with kb.phase():
            rings = self.prep_rings()
            self.prep_rings_cur[0] = rings
            for _ in self.prep:
                pass
            self.prep_flush()
            self.prep = None

    def phase_moe_sparse(self, l, xsrc, xdst):
        kb, I = self.kb, self.I
        NT = NB * SEQ // 128
        BIG = 1.0e6
        MAGIC = 12582912.0
        lat_tok0 = lambda j: (j // 16) * TPB + CTX + (j % 16) * 128
        with kb.phase():
            glo = kb.sb([128, NT], F32); ghi = kb.sb([128, NT], F32)
            ilo = kb.sb([128, NT], I32); ihi = kb.sb([128, NT], I32)
            widx = kb.sb([128, MOE_TILES, 7], I32)
            t_rt = Tok()
            with kb.phase():
                rb = kb.sb([128, 8, 8], BF16); t_rb = Tok()
                kb.dma("pool", [(rb[:], I["moe_router"][0].rearrange("(k p) e -> p k e", p=128))], writes=[t_rb])
                tri = kb.sb([128, 2, 128], BF16); io7 = kb.sb([128, 7], F32); thr = kb.sb([128, MOE_TILES], F32)
                kb.dma("sp", [(tri[:], I["moe_tri"][:, :, :]), (io7[:], I["moe_iota"][:, :]), (thr[:], I["moe_thr"][:, :])], writes=[t_rb])
                hT = kb.sb([128, 8, NB * SEQ], BF16); t_hT = Tok()
                kb.dma("sp", [(hT[:, k, b * SEQ:(b + 1) * SEQ], self.h2T[:, k, b * TPB + CTX:(b + 1) * TPB]) for k in range(8) for b in range(NB)], writes=[t_hT])
                gates = kb.sb([128, NT, 8], F32); maskf = kb.sb([128, NT, 8], F32); maskb = kb.sb([128, NT, 8], BF16)
                rank = kb.sb([128, NT, 8], F32)
                t_g, t_m, t_rk = Tok(), Tok(), Tok()
                pl = Ring(kb, 2, [128, 8], F32, kind="ps")
                wk = Ring(kb, 2, [128, 6, 8], F32); sm = Ring(kb, 2, [128, 8], F32)
                for j in range(NT):
                    p, t_p = pl.next()
                    for k in range(8):
                        kb.op("pe", lambda e: e.matmul(p[:, :], hT[:, k, j * 128:(j + 1) * 128], rb[:, k, :], start=(k == 0), stop=(k == 7)),
                              reads=[t_hT, t_rb], writes=[t_p])
                    w_, t_w = wk.next(); s_, t_s = sm.next()
                    T = [t_w, t_s]
                    kb.op("dve", lambda e: e.tensor_reduce(out=s_[:, 0:1], in_=p[:, :], op=ALU.max, axis=AX.X), reads=[t_p], writes=T)
                    kb.op("dve", lambda e: e.tensor_scalar(out=s_[:, 1:2], in0=s_[:, 0:1], scalar1=-1.0, scalar2=None, op0=ALU.mult), reads=T, writes=T)
                    kb.op("act", lambda e: e.activation(out=w_[:, 0, :], in_=p[:, :], func=AF.Exp, bias=s_[:, 1:2]), reads=[t_p] + T, writes=T)
                    kb.op("dve", lambda e: e.tensor_scalar(out=w_[:, 1, :], in0=w_[:, 0, :], scalar1=1.0, scalar2=None, op0=ALU.is_lt), reads=T, writes=T)
                    kb.op("dve", lambda e: e.tensor_tensor(out=w_[:, 2, :], in0=w_[:, 0, :], in1=w_[:, 1, :], op=ALU.mult), reads=T, writes=T)
                    kb.op("dve", lambda e: e.tensor_reduce(out=s_[:, 2:3], in_=w_[:, 2, :], op=ALU.max, axis=AX.X), reads=T, writes=T)
                    kb.op("dve", lambda e: e.tensor_scalar(out=s_[:, 3:4], in0=s_[:, 2:3], scalar1=1.0, scalar2=None, op0=ALU.add), reads=T, writes=T)
                    kb.op("dve", lambda e: e.reciprocal(out=s_[:, 4:5], in_=s_[:, 3:4]), reads=T, writes=T)
                    kb.op("dve", lambda e: e.tensor_scalar(out=maskf[:, j, :], in0=w_[:, 0, :], scalar1=s_[:, 2:3], scalar2=None, op0=ALU.is_ge), reads=T, writes=[t_m])
                    kb.op("dve", lambda e: e.tensor_copy(out=maskb[:, j, :], in_=maskf[:, j, :]), reads=[t_m], writes=[t_m])
                    kb.op("dve", lambda e: e.tensor_tensor(out=w_[:, 4, :], in0=w_[:, 0, :], in1=maskf[:, j, :], op=ALU.mult), reads=T + [t_m], writes=T)
                    kb.op("dve", lambda e: e.tensor_scalar(out=gates[:, j, :], in0=w_[:, 4, :], scalar1=s_[:, 4:5], scalar2=None, op0=ALU.mult), reads=T, writes=[t_g])
                prk = Ring(kb, 2, [128, 8], F32, kind="ps")
                for j in range(NT + 1):
                    p, t_p = prk.next()
                    n = 0
                    for i in range(min(j, NT)):
                        kb.op("pe", lambda e: e.matmul(p[:, :], tri[:, 1, :], maskb[:, i, :], start=(n == 0), stop=(j == NT and i == NT - 1)),
                              reads=[t_m, t_rb], writes=[t_p])
                        n += 1
                    if j < NT:
                        kb.op("pe", lambda e: e.matmul(p[:, :], tri[:, 0, :], maskb[:, j, :], start=(n == 0), stop=True), reads=[t_m, t_rb], writes=[t_p])
                        kb.op("act", lambda e: e.copy(out=rank[:, j, :], in_=p[:, :]), reads=[t_p], writes=[t_rk])
                    else:
                        tot = kb.sb([128, 8], F32)
                        kb.op("act", lambda e: e.copy(out=tot[:], in_=p[:, :]), reads=[t_p], writes=[t_rk])
                T = [t_rk]
                ts = lambda o, x, s1, o0: kb.op("dve", lambda e: e.tensor_scalar(out=o, in0=x, scalar1=s1, scalar2=None, op0=o0), reads=T + [t_m, t_g, t_rb], writes=T)
                tt = lambda o, x, y, op: kb.op("dve", lambda e: e.tensor_tensor(out=o, in0=x, in1=y, op=op), reads=T + [t_m, t_g, t_rb], writes=T)
                red = lambda o, x, op: kb.op("dve", lambda e: e.tensor_reduce(out=o, in_=x, op=op, axis=AX.X), reads=T, writes=T)
                pad = kb.sb([128, 8], F32); incl = kb.sb([128, 8], F32); base = kb.sb([128, 8], F32)
                ts(pad[:], tot[:], 511.0, ALU.add); ts(pad[:], pad[:], 1.0 / 512, ALU.mult)
                ts(pad[:], pad[:], -0.5 + 1.0 / 1024, ALU.add); ts(pad[:], pad[:], MAGIC, ALU.add); ts(pad[:], pad[:], MAGIC, ALU.subtract)
                ts(pad[:], pad[:], 512.0, ALU.mult)
                kb.op("dve", lambda e: e.tensor_copy(out=incl[:, 0:1], in_=pad[:, 0:1]), reads=T, writes=T)
                for e_ in range(1, 8):
                    tt(incl[:, e_:e_ + 1], incl[:, e_ - 1:e_], pad[:, e_:e_ + 1], ALU.add)
                tt(base[:], incl[:], pad[:], ALU.subtract)
                slot = kb.sb([128, NT, 8], F32); v1 = kb.sb([128, NT, 8], F32); v2 = kb.sb([128, NT, 8], F32)
                slo = kb.sb([128, NT], F32); shi = kb.sb([128, NT], F32)
                tt(slot[:], rank[:], base[:].unsqueeze(1).broadcast_to([128, NT, 8]), ALU.add)
                tt(v2[:], slot[:], maskf[:], ALU.mult)
                ts(v1[:], maskf[:], -BIG, ALU.mult); ts(v1[:], v1[:], BIG, ALU.add); tt(v1[:], v1[:], v2[:], ALU.add)
                red(slo[:], v1[:], ALU.min)
                tt(v1[:], v2[:], maskf[:], ALU.add); ts(v1[:], v1[:], -1.0, ALU.add)
                red(shi[:], v1[:], ALU.max)
                for sl_, g_ in ((slo, glo), (shi, ghi)):
                    tt(v1[:], slot[:], sl_[:].unsqueeze(2).broadcast_to([128, NT, 8]), ALU.is_equal)
                    tt(v1[:], v1[:], gates[:], ALU.mult)
                    kb.op("dve", lambda e: e.tensor_reduce(out=g_[:], in_=v1[:], op=ALU.add, axis=AX.X), reads=T, writes=T + [t_rt])
                kb.op("dve", lambda e: e.tensor_copy(out=ilo[:], in_=slo[:]), reads=T, writes=[t_rt])
                kb.op("dve", lambda e: e.tensor_copy(out=ihi[:], in_=shi[:]), reads=T, writes=[t_rt])
                cmp_ = kb.sb([128, MOE_TILES, 8], F32); cnt = kb.sb([128, MOE_TILES], F32); wf = kb.sb([128, MOE_TILES, 7], F32)
                tt(cmp_[:], incl[:].unsqueeze(1).broadcast_to([128, MOE_TILES, 8]), thr[:].unsqueeze(2).broadcast_to([128, MOE_TILES, 8]), ALU.is_le)
                red(cnt[:], cmp_[:], ALU.add)
                ts(cnt[:], cnt[:], 7.0, ALU.min); ts(cnt[:], cnt[:], 896.0, ALU.mult)
                tt(wf[:], cnt[:].unsqueeze(2).broadcast_to([128, MOE_TILES, 7]), io7[:].unsqueeze(1).broadcast_to([128, MOE_TILES, 7]), ALU.add)
                kb.op("dve", lambda e: e.tensor_copy(out=widx[:], in_=wf[:]), reads=T, writes=[t_rt])
            with kb.phase():
                t_fill = Tok()
                hr = Ring(kb, 3, [128, D], BF16); icr = Ring(kb, 4, [128, 1], I32)
                for j in range(NT):
                    hb, t_h = hr.next()
                    r0 = lat_tok0(j)
                    kb.dma("sp", [(hb[:], self.h2tm[r0:r0 + 128, :])], writes=[t_h])
                    for ix in (ilo, ihi):
                        kb.dma_custom("pool", lambda g: g.indirect_dma_start(out=self.hsorted[:, :], out_offset=bass.IndirectOffsetOnAxis(ap=ix[:, j:j + 1], axis=0),
                                                                             in_=hb[:, :], in_offset=None, bounds_check=None),
                                      reads=[t_h, t_rt, t_fill])
            with kb.phase():
                hsr = Ring(kb, 4, [128, D], BF16); hTr = Ring(kb, 2, [128, 8, MOE_TS], BF16)
                wgr = Ring(kb, 3, [128, 8, 512], BF16); wur = Ring(kb, 3, [128, 8, 512], BF16); wdr = Ring(kb, 3, [128, 4, D], BF16)
                atr = Ring(kb, 2, [128, 4, MOE_TS], BF16); sgr = Ring(kb, 3, [128, 512], BF16)
                accr = Ring(kb, 2, [128, 4, D], F32); icr = Ring(kb, 8, [128, 1], I32)
                pT = Ring(kb, 1, [128, 8, 128], BF16, kind="ps")
                pG = Ring(kb, 2, [128, 512], F32, kind="ps"); pU = Ring(kb, 2, [128, 512], F32, kind="ps"); pD = Ring(kb, 3, [128, 512], F32, kind="ps")
                pending = []

                def emit_down(at, t_at, wd_, t_wd, acc, t_acc, first, last, i):
                    for sub in range(4):
                        for nh in range(2):
                            p, t_p = pD.next()
                            for fc in range(4):
                                kb.op("pe", lambda e: e.matmul(p[:, :], at[:, fc, sub * 128:(sub + 1) * 128], wd_[:, fc, nh * 512:(nh + 1) * 512], start=(fc == 0), stop=(fc == 3)),
                                      reads=[t_at, t_wd], writes=[t_p])
                            dst = acc[:, sub, nh * 512:(nh + 1) * 512]
                            if first:
                                kb.op("act", lambda e: e.copy(out=dst, in_=p[:, :]), reads=[t_p], writes=[t_acc])
                            else:
                                kb.op("dve", lambda e: e.tensor_tensor(out=dst, in0=p[:, :], in1=dst, op=ALU.add), reads=[t_p, t_acc], writes=[t_acc])
                    if last:
                        kb.dma("sp", [(self.ysorted[i * MOE_TS + sub * 128:i * MOE_TS + (sub + 1) * 128, :], acc[:, sub, :]) for sub in range(4)], reads=[t_acc])

                for i in range(MOE_TILES):
                    hT, t_hT = hTr.next()
                    for sub in range(4):
                        hs, t_hs = hsr.next()
                        r0 = i * MOE_TS + sub * 128
                        kb.dma("sp", [(hs[:], self.hsorted[r0:r0 + 128, :])], writes=[t_hs])
                        p, t_p = pT.next()
                        for k in range(8):
                            kb.op("pe", lambda e: e.transpose(out=p[:, k, :], in_=hs[:, k * 128:(k + 1) * 128], identity=self.ident_bf[:]),
                                  reads=[t_hs, self.t_ident], writes=[t_p])
                        kb.op("act", lambda e: e.copy(out=hT[:, :, sub * 128:(sub + 1) * 128], in_=p[:]), reads=[t_p], writes=[t_hT])
                    acc, t_acc = accr.next()
                    for fg in range(7):
                        wg_, t_wg = wgr.next(); wu_, t_wu = wur.next(); wd_, t_wd = wdr.next()
                        for (wt, tw, src) in ((wg_, t_wg, self.Wg_s), (wu_, t_wu, self.Wu_s), (wd_, t_wd, self.Wd_s)):
                            flat = wt[:].rearrange("p a n -> p (a n)")
                            kb.dma_custom("pool", lambda g: g.indirect_dma_start(out=flat, out_offset=None, in_=src[:, :],
                                                                                 in_offset=bass.IndirectOffsetOnAxis(ap=widx[:, i, fg:fg + 1], axis=0),
                                                                                 bounds_check=None),
                                          reads=[t_rt], writes=[tw])
                        at, t_at = atr.next()
                        for fc in range(4):
                            g_, t_g = pG.next(); u_, t_u = pU.next()
                            for k in range(8):
                                kb.op("pe", lambda e: e.matmul(g_[:, :], wg_[:, k, fc * 128:(fc + 1) * 128], hT[:, k, :], start=(k == 0), stop=(k == 7)),
                                      reads=[t_wg, t_hT], writes=[t_g])
                            for k in range(8):
                                kb.op("pe", lambda e: e.matmul(u_[:, :], wu_[:, k, fc * 128:(fc + 1) * 128], hT[:, k, :], start=(k == 0), stop=(k == 7)),
                                      reads=[t_wu, t_hT], writes=[t_u])
                            sg_, t_sg = sgr.next()
                            kb.op("act", lambda e: e.activation(out=sg_[:], in_=g_[:, :], func=AF.Silu), reads=[t_g], writes=[t_sg])
                            kb.op("dve", lambda e: e.tensor_tensor(out=at[:, fc, :], in0=u_[:, :], in1=sg_[:], op=ALU.mult), reads=[t_u, t_sg], writes=[t_at])
                        while pending:
                            pending.pop(0)()
                        pending.append(lambda at=at, t_at=t_at, wd_=wd_, t_wd=t_wd, acc=acc, t_acc=t_acc, first=(fg == 0), last=(fg == 6), i=i:
                                       emit_down(at, t_at, wd_, t_wd, acc, t_acc, first, last, i))
                while pending:
                    pending.pop(0)()
            with kb.phase():
                gg, _, t_gg, _ = self.load_mod_tiles(l, 5, None, "norm_ffn_post", plus_one=False)
                R = {"tmp": Ring(kb, 2, [128, D], F32), "stat2": Ring(kb, 4, [128, 8], F32), "junk2": Ring(kb, 1, [128, 512], BF16),
                     "xn": Ring(kb, 2, [128, D], F32)}
                icr = Ring(kb, 4, [128, 1], I32)
                xr = Ring(kb, 4, [128, D], F32); ylr = Ring(kb, 3, [128, D], F32); yhr = Ring(kb, 3, [128, D], F32); mxr = Ring(kb, 4, [128, D], F32)
                R["stat2"] = Ring(kb, 8, [128, 8], F32); R["tmp"] = Ring(kb, 3, [128, D], F32); R["xn"] = Ring(kb, 3, [128, D], F32)
                C = [dict() for _ in range(NT)]

                def c_load(j):
                    r0 = lat_tok0(j)
                    xt, t_x = xr.next()
                    kb.dma("sp", [(xt[:], xsrc[r0:r0 + 128, :])], writes=[t_x])
                    yl, t_yl = ylr.next(); yh, t_yh = yhr.next()
                    for (yt, ty, ix) in ((yl, t_yl, ilo), (yh, t_yh, ihi)):
                        kb.dma_custom("pool", lambda g: g.indirect_dma_start(out=yt[:, :], out_offset=None, in_=self.ysorted[:, :],
                                                                             in_offset=bass.IndirectOffsetOnAxis(ap=ix[:, j:j + 1], axis=0),
                                                                             bounds_check=None),
                                      reads=[t_rt], writes=[ty])
                    C[j].update(xt=xt, t_x=t_x, yl=yl, t_yl=t_yl, yh=yh, t_yh=t_yh)

                def c_mix(j):
                    c = C[j]
                    mx, t_mx = mxr.next()
                    kb.op("dve", lambda e: e.tensor_scalar(out=mx[:], in0=c["yl"][:], scalar1=glo[:, j:j + 1], scalar2=None, op0=ALU.mult), reads=[c["t_yl"], t_rt], writes=[t_mx])
                    kb.op("dve", lambda e: e.scalar_tensor_tensor(out=mx[:], in0=c["yh"][:], scalar=ghi[:, j:j + 1], op0=ALU.mult, in1=mx[:], op1=ALU.add),
                          reads=[c["t_yh"], t_rt, t_mx], writes=[t_mx])
                    c.update(hs=[mx[:, 0:512], mx[:, 512:1024]], ths=[t_mx, t_mx])

                def c_p1(j):
                    C[j]["st"], C[j]["t_st"] = self.pn1(C[j]["hs"], C[j]["ths"], R)

                def c_p2(j):
                    c = C[j]
                    xn, t_xn = self.pn2(c["hs"], c["ths"], c["st"], c["t_st"], c["xt"], c["t_x"], gg, t_gg, j // 16, R)
                    kb.dma("sp", [(xdst[j * 128:(j + 1) * 128, :], xn[:])], reads=[t_xn])
                self.run_pipeline(NT, [c_load, c_mix, c_p1, c_p2])
def core_inputs(inputs, core, consts):
    b0 = core * NB
    m = {}
    xs = []
    for b in range(b0, b0 + NB):
        xs.append(inputs["ctx"][b]); xs.append(inputs["x"][b])
    m["xin"] = np.ascontiguousarray(np.concatenate(xs, axis=0), dtype=np.float32)
    cv = np.stack([inputs["c"][b0], inputs["c"][b0 + 1], inputs["c_ctx"]], axis=0)
    m["cT"] = np.ascontiguousarray(cv.reshape(3, 8, 128).transpose(2, 1, 0), dtype=np.float32)
    dup = lambda a: np.concatenate([a, a], axis=0)
    L = inputs["s5_a_re"].shape[0]
    par = np.zeros((L, 128, 3, 32), np.float32); sb = np.zeros((L, 128, 32, 2, 16), np.float32); sc = np.zeros((L, 128, 32, 2, 16), np.float32)
    for l in range(L):
        par[l, :, 0, :] = dup(inputs["s5_a_re"][l].reshape(32, 64).T)
        par[l, :, 1, :] = dup(inputs["s5_a_im"][l].reshape(32, 64).T)
        par[l, :, 2, :] = inputs["s5_log_dt"][l].reshape(1, 32)
        sb[l, :, :, 0, :] = dup(inputs["s5_b_re"][l].reshape(32, 64, 16).transpose(1, 0, 2))
        sb[l, :, :, 1, :] = dup(inputs["s5_b_im"][l].reshape(32, 64, 16).transpose(1, 0, 2))
        sc[l, :, :, 0, :] = dup(inputs["s5_c_re"][l].reshape(32, 16, 64).transpose(2, 0, 1))
        sc[l, :, :, 1, :] = dup(inputs["s5_c_im"][l].reshape(32, 16, 64).transpose(2, 0, 1))
    m["s5_par"], m["s5_b"], m["s5_c"] = par, sb, sc
    m["s5_dd"] = np.ascontiguousarray(inputs["s5_d"].reshape(L, 2, 128).transpose(0, 2, 1))
    return m


_CACHE = {}


def build_program():
    P = Prog()
    P.declare(); P.consts_sb()
    xin = P.I["xin"]
    P.phase_adaln(0)
    P.prep = P.moe_prep_gen(None)
    P.phase_win(0, xin, prep_win=True)
    P.phase_attn(0, True, prep_every=2); P.phase_fnet(0, True); P.phase_s5(0)
    P.phase_wout(0, xin, P.xA, True, prep_wout=True)
    P.phase_ffn(0, P.xA, P.xB, False, False)
    P.phase_adaln(1)
    P.phase_win(1, P.xB, prep_win=True)
    P.phase_attn(1, False, prep_every=2); P.phase_fnet(1, False); P.phase_s5(1)
    P.phase_wout(1, P.xB, P.xA, False)
    P.phase_moe_prep()
    P.phase_moe_sparse(1, P.xA, P.out)
    P.kb.barrier()
    return P


def kernel(**inputs):
    inputs = {k: np.asarray(v) for k, v in inputs.items()}
    if "P" not in _CACHE:
        _CACHE["P"] = build_program()
    P = _CACHE["P"]
    n_cores = 8
    shared = {}
    for k in P.I:
        if k in P.consts:
            shared[k] = P.consts[k]
        elif k in inputs:
            shared[k] = np.ascontiguousarray(inputs[k], dtype=np.float32)
    in_maps = []
    for core in range(n_cores):
        m = core_inputs(inputs, core, P.consts)
        for k, v in shared.items():
            if k not in m:
                m[k] = v
        in_maps.append(m)
    res = run_bass_kernel_spmd(P.kb.nc, in_maps, core_ids=list(range(n_cores)))
    outs = [np.asarray(r["out"], dtype=np.float32).reshape(NB, SEQ, D) for r in res.results]
    return np.concatenate(outs, axis=0)
```

```python
import contextlib, math
import numpy as np
import ml_dtypes
import concourse.bass as bass
import concourse.mybir as mybir
from concourse.bass_utils import run_bass_kernel_spmd

F32 = mybir.dt.float32
BF16 = mybir.dt.bfloat16
I32 = mybir.dt.int32
AF = mybir.ActivationFunctionType
ALU = mybir.AluOpType
AX = mybir.AxisListType
NPBF = ml_dtypes.bfloat16

SEM_LIMIT = 30000
EPS = 1e-6
D = 1024
NB = 2
CTX = 256
SEQ = 2048
TPB = CTX + SEQ
NTOK = NB * TPB
NTILE = NTOK // 128
TILES_PB = TPB // 128
DEPTH = 2
F_DENSE = 2816
F_EXPERT = 3584
N_EXP = 8
MOE_TS = 512
MOE_TILES = (2 * NB * SEQ + N_EXP * (MOE_TS - 1)) // MOE_TS + 1
MOE_SLOTS = MOE_TILES * MOE_TS


class Tok:
    __slots__ = ("name", "w", "r")

    def __init__(self, name=""):
        self.name = name
        self.w = None
        self.r = {}


class Eng:
    def __init__(self, kb, name, eng):
        self.kb, self.name, self.eng = kb, name, eng
        self.sem = None
        self.cnt = 0
        self.waited = {}

    def new_sem(self):
        self.sem = self.kb.es.enter_context(self.kb.nc.semaphore(f"s_{self.name}_{self.kb.nsem}"))
        self.kb.nsem += 1
        self.cnt = 0


class KB:
    def __init__(self):
        self.nc = bass.Bass("TRN2", target_bir_lowering=False)
        self.es = contextlib.ExitStack()
        self.nsem = 0
        nc = self.nc
        self.E = {}
        for name, eng in (("pe", nc.tensor), ("act", nc.scalar), ("dve", nc.vector),
                          ("pool", nc.gpsimd), ("sp", nc.sync)):
            e = Eng(self, name, eng)
            e.new_sem()
            self.E[name] = e
        self.dma_sems, self.dma_vals = [], []
        for i in range(64):
            self.dma_sems.append(self.es.enter_context(nc.semaphore(f"s_dma{i}")))
            self.dma_vals.append(0)
        self.dma_cursor = {"sp": 0, "pool": 0}
        self.nalloc = 0
        self.ninstr = 0
        self.stack = [self.es]
        self.dram_t = {}

    def sb(self, shape, dtype, name=None):
        self.nalloc += 1
        return self.stack[-1].enter_context(self.nc.sbuf_tensor(name or f"sb{self.nalloc}", list(shape), dtype))

    def ps(self, shape, dtype, name=None):
        self.nalloc += 1
        esz = 4 if dtype == F32 else 2
        n = int(np.prod(shape[1:]))
        assert n * esz <= 2048, shape
        t = self.stack[-1].enter_context(self.nc.psum_tensor(name or f"ps{self.nalloc}", [128, 2048 // esz], dtype))
        ap = t[0:shape[0], 0:n]
        if len(shape) > 2:
            names = [f"d{i}" for i in range(len(shape) - 1)]
            pat = "p (" + " ".join(names) + ") -> p " + " ".join(names)
            ap = ap.rearrange(pat, **{nm: int(v) for nm, v in zip(names[:-1], shape[1:-1])})
        return ap

    def dram(self, name, shape, dtype, kind="Internal"):
        t = self.nc.dram_tensor(name, list(shape), dtype, kind=kind)
        self.dram_t[name] = t
        return t.ap()

    @contextlib.contextmanager
    def phase(self):
        st = contextlib.ExitStack()
        self.stack.append(st)
        try:
            yield
        finally:
            self.barrier()
            self.stack.pop()
            st.close()

    def barrier(self):
        evs = [(e.sem, e.cnt) for e in self.E.values() if e.cnt > 0]
        evs += [(s, v) for s, v in zip(self.dma_sems, self.dma_vals) if v > 0]
        for e in self.E.values():
            for ev in evs:
                if ev[0] is e.sem:
                    continue
                self._wait(e, ev)

    def _wait(self, e, ev):
        if ev is None:
            return
        if isinstance(ev, list):
            for e_ in ev:
                self._wait(e, e_)
            return
        sem, val = ev
        k = id(sem)
        if e.waited.get(k, 0) >= val:
            return
        if e.name == "pe" and sem is e.sem:
            return
        e.eng.wait_ge(sem, val)
        e.waited[k] = val

    def _deps(self, e, reads, writes):
        for t in reads:
            self._wait(e, t.w)
        for t in writes:
            self._wait(e, t.w)
            for ev in t.r.values():
                self._wait(e, ev)

    def _commit(self, ev, reads, writes):
        for t in reads:
            for e_ in (ev if isinstance(ev, list) else [ev]):
                t.r[id(e_[0])] = e_
        for t in writes:
            t.w = ev
            t.r = {}

    def op(self, en, fn, reads=(), writes=()):
        e = self.E[en]
        if e.cnt >= SEM_LIMIT:
            e.new_sem()
        self._deps(e, reads, writes)
        ins = fn(e.eng)
        e.cnt += 1
        ins.then_inc(e.sem, 1)
        ev = (e.sem, e.cnt)
        self._commit(ev, reads, writes)
        self.ninstr += 1
        return ev

    def _next_dma_sem(self, qn):
        half = len(self.dma_sems) // 2
        c = self.dma_cursor[qn]
        self.dma_cursor[qn] = (c + 1) % half
        return c + (half if qn == "pool" else 0)

    def dma(self, qn, pairs, reads=(), writes=(), **kw):
        e = self.E[qn]
        self._deps(e, reads, writes)
        if qn == "pool" and len(pairs) > 1:
            evs = []
            for (o, i) in pairs:
                j = self._next_dma_sem(qn)
                sem = self.dma_sems[j]
                if self.dma_vals[j] > 0:
                    self._wait(e, (sem, self.dma_vals[j]))
                if self.dma_vals[j] > SEM_LIMIT:
                    raise RuntimeError("dma sem overflow")
                e.eng.dma_start(out=o, in_=i, **kw).then_inc(sem, 16)
                self.dma_vals[j] += 16
                self.ninstr += 1
                evs.append((sem, self.dma_vals[j]))
            self._commit(evs, reads, writes)
            return evs
        j = self._next_dma_sem(qn)
        sem = self.dma_sems[j]
        if self.dma_vals[j] > 0:
            self._wait(e, (sem, self.dma_vals[j]))
        if self.dma_vals[j] > SEM_LIMIT:
            raise RuntimeError("dma sem overflow")
        for (o, i) in pairs:
            e.eng.dma_start(out=o, in_=i, **kw).then_inc(sem, 16)
            self.dma_vals[j] += 16
            self.ninstr += 1
        ev = (sem, self.dma_vals[j])
        self._commit(ev, reads, writes)
        return ev

    def dma_custom(self, qn, fn, reads=(), writes=()):
        e = self.E[qn]
        self._deps(e, reads, writes)
        j = self._next_dma_sem(qn)
        sem = self.dma_sems[j]
        if self.dma_vals[j] > 0:
            self._wait(e, (sem, self.dma_vals[j]))
        if self.dma_vals[j] > SEM_LIMIT:
            raise RuntimeError("dma sem overflow")
        fn(e.eng).then_inc(sem, 16)
        self.dma_vals[j] += 16
        self.ninstr += 1
        ev = (sem, self.dma_vals[j])
        self._commit(ev, reads, writes)
        return ev

    def finish(self, toks):
        e = self.E["sp"]
        for t in toks:
            self._wait(e, t.w)
            for ev in t.r.values():
                self._wait(e, ev)


class Ring:
    def __init__(self, kb, n, shape, dtype, kind="sb", name="ring"):
        self.items = []
        for i in range(n):
            t = kb.sb(shape, dtype) if kind == "sb" else kb.ps(shape, dtype)
            self.items.append((t, Tok(f"{name}{i}")))
        self.i = 0

    def next(self):
        it = self.items[self.i]
        self.i = (self.i + 1) % len(self.items)
        return it

def host_consts():
    c = {}
    c["ident_bf"] = np.eye(128, dtype=np.float32).astype(NPBF)
    c["ident_f"] = np.eye(128, dtype=np.float32)
    n_freq = 16
    inv = 10000.0 ** (-np.arange(n_freq, dtype=np.float64) / n_freq)
    t = np.arange(SEQ)
    rows = (t // 64).astype(np.float64)
    cols = (t % 64).astype(np.float64)
    ang = np.stack([rows[:, None] * inv, cols[:, None] * inv], axis=1)
    ang = (np.stack([rows[:, None].astype(np.float32) * inv.astype(np.float32),
                     cols[:, None].astype(np.float32) * inv.astype(np.float32)], axis=1)).astype(np.float64)
    cos = np.cos(ang); sin = np.sin(ang)
    cos2 = np.stack([cos, cos], axis=2)
    sinS = np.stack([-sin, sin], axis=2)
    c["rope_cos"] = np.ascontiguousarray(cos2.reshape(16, 128, 64).transpose(1, 0, 2)).astype(np.float32)
    c["rope_sin"] = np.ascontiguousarray(sinS.reshape(16, 128, 64).transpose(1, 0, 2)).astype(np.float32)
    def dftm(L):
        t = np.arange(L)
        ph = (np.outer(t, t) % L).astype(np.float64) * (2 * np.pi / L)
        sc = 1.0 / math.sqrt(L * 64)
        return (np.cos(ph) * sc).astype(NPBF), (np.sin(ph) * sc).astype(NPBF)
    c["dft_c"], c["dft_s"] = dftm(SEQ)
    c["dftc_c"], c["dftc_s"] = dftm(CTX)
    t = np.arange(64)
    ph = np.outer(t, t) * (2 * np.pi / 64)
    cb = np.zeros((128, 2, 128), np.float32)
    for g in range(2):
        cb[g * 64:(g + 1) * 64, 0, g * 64:(g + 1) * 64] = np.cos(ph)
        cb[g * 64:(g + 1) * 64, 1, g * 64:(g + 1) * 64] = -np.sin(ph)
    c["cblk"] = cb
    k = np.arange(128)
    ws = np.zeros((128, 8, 240), np.float32)
    for r in range(8):
        for kk in range(128):
            if kk // 16 == r:
                ws[kk, r, 112 + kk % 16] = 1.0
    c["wsel"] = ws.astype(NPBF)
    sblk = (k // 16)[:, None]; tblk = (k // 16)[None, :]
    c["s5_mask"] = np.ascontiguousarray(np.stack([(tblk >= sblk), (tblk <= sblk)], axis=1).astype(np.float32))
    tri = np.zeros((128, 2, 128), np.float32)
    tri[:, 0, :] = (k[:, None] < k[None, :])
    tri[:, 1, :] = 1.0
    c["moe_tri"] = tri.astype(NPBF)
    c["moe_iota"] = np.ascontiguousarray((np.arange(7)[None, :] * 128 + k[:, None]).astype(np.float32))
    c["moe_thr"] = np.ascontiguousarray(np.broadcast_to((np.arange(MOE_TILES) * MOE_TS)[None, :], (128, MOE_TILES)).astype(np.float32))
    return c


class Prog:
    def __init__(self, debug=()):
        self.kb = KB()
        self.debug = set(debug)
        self.I = {}
        self.consts = host_consts()
        self.prep = None
        self.prep_rings_cur = [None]
        self.prep_inflight = []

    def inp(self, name, shape, dtype):
        ap = self.kb.dram(name, shape, dtype, kind="ExternalInput")
        self.I[name] = ap
        return ap

    def scratch(self, name, shape, dtype):
        kind = "ExternalOutput" if name in self.debug else "Internal"
        return self.kb.dram(name, shape, dtype, kind=kind)

    def declare(self):
        inp = self.inp
        inp("xin", [NTOK, D], F32)
        inp("cT", [128, 8, 3], F32)
        inp("ada_w", [DEPTH, D, 6 * D], F32)
        inp("ada_b", [DEPTH, 6 * D], F32)
        for n in ("norm_mix_pre", "norm_mix_post", "norm_ffn_pre", "norm_ffn_post"):
            inp(n, [DEPTH, D], F32)
        inp("w_in", [DEPTH, D, 2048], F32)
        inp("w_out", [DEPTH, D, D], F32)
        for n in ("diff_lq1", "diff_lk1", "diff_lq2", "diff_lk2"):
            inp(n, [DEPTH, 64], F32)
        inp("diff_subln", [DEPTH, 128], F32)
        inp("fnet_w", [DEPTH, 256, 256], F32)
        inp("s5_par", [DEPTH, 128, 3, 32], F32)
        inp("s5_b", [DEPTH, 128, 32, 2, 16], F32)
        inp("s5_c", [DEPTH, 128, 32, 2, 16], F32)
        inp("s5_dd", [DEPTH, 128, 2], F32)
        inp("s5_w_glu", [DEPTH, 256, 256], F32)
        inp("ffn_w_gate", [1, D, F_DENSE], F32); inp("ffn_w_up", [1, D, F_DENSE], F32); inp("ffn_w_down", [1, F_DENSE, D], F32)
        inp("moe_router", [1, D, N_EXP], F32)
        inp("moe_w_gate", [1, N_EXP, D, F_EXPERT], F32); inp("moe_w_up", [1, N_EXP, D, F_EXPERT], F32); inp("moe_w_down", [1, N_EXP, F_EXPERT, D], F32)
        for k, v in self.consts.items():
            inp(k, list(v.shape), BF16 if v.dtype == NPBF else F32)
        sc = self.scratch
        self.modD = [sc(f"modD{l}", [3, 6 * D], F32) for l in range(DEPTH)]
        self.qT = sc("qT", [128, 4, NTOK], BF16)
        self.kT = sc("kT", [128, 4, NTOK], BF16)
        self.vD = sc("vD", [128, 4, NTILE, 130], BF16)
        self.fD = sc("fD", [128, NTILE, 256], BF16)
        self.uT = sc("uT", [128, 2, NTOK], BF16)
        self.catT = sc("catT", [128, 8, NTOK], BF16)
        self.xA = sc("xA", [NTOK, D], F32)
        self.xB = sc("xB", [NTOK, D], F32)
        self.h2T = sc("h2T", [128, 8, NTOK], BF16)
        self.out = self.kb.dram("out", [NB * SEQ, D], F32, kind="ExternalOutput")
        self.moe_declare()

    def phase_adaln(self, l):
        kb, I = self.kb, self.I
        with kb.phase():
            cT = kb.sb([128, 8, 3], F32); sT = kb.sb([128, 8, 3], F32)
            bias = kb.sb([3, 6 * D], F32); mod = kb.sb([3, 6 * D], F32)
            t_c, t_s, t_b, t_m = Tok(), Tok(), Tok(), Tok()
            kb.dma("sp", [(cT[:], I["cT"][:, :, :])], writes=[t_c])
            kb.dma("sp", [(bias[:], I["ada_b"][l].partition_broadcast(3))], writes=[t_b])
            kb.op("act", lambda e: e.activation(out=sT[:], in_=cT[:], func=AF.Silu), reads=[t_c], writes=[t_s])
            wr = Ring(kb, 2, [128, 8, 512], F32, name="adaw")
            pr = Ring(kb, 2, [128, 512], F32, kind="ps", name="adap")
            wv = I["ada_w"][l].rearrange("(k p) n -> p k n", p=128)
            for nt in range(12):
                w, tw = wr.next()
                kb.dma("sp", [(w[:], wv[:, :, nt * 512:(nt + 1) * 512])], writes=[tw])
                p, tp = pr.next()
                for k in range(8):
                    kb.op("pe", lambda e: e.matmul(p[0:3, :], sT[:, k, :], w[:, k, :], start=(k == 0), stop=(k == 7)),
                          reads=[t_s, tw], writes=[tp])
                kb.op("dve", lambda e: e.tensor_tensor(out=mod[0:3, nt * 512:(nt + 1) * 512], in0=p[0:3, :],
                                                       in1=bias[0:3, nt * 512:(nt + 1) * 512], op=ALU.add),
                      reads=[tp, t_b], writes=[t_m])
            kb.dma("sp", [(self.modD[l][:, :], mod[0:3, :])], reads=[t_m])
            if l == 0:
                z = kb.sb([128, 8192], BF16); t_z = Tok()
                kb.op("pool", lambda e: e.memset(z[:], 0.0), writes=[t_z])
                hsv = self.hsorted.rearrange("(a p r) d -> a p (r d)", p=128, r=8)
                for a in range(MOE_SLOTS // 1024):
                    kb.dma("sp", [(hsv[a], z[:])], reads=[t_z])

    def load_mod_tiles(self, l, off_sc, off_sh, gain_name, plus_one=True):
        kb, I = self.kb, self.I
        gsc = kb.sb([128, 3, D], F32); sh = kb.sb([128, 3, D], F32); gn = kb.sb([128, D], F32)
        t_g, t_s, t_n = Tok(), Tok(), Tok()
        kb.dma("sp", [(gn[:], I[gain_name][l].partition_broadcast(128))], writes=[t_n])
        kb.dma("sp", [(gsc[:, j, :], self.modD[l][j, off_sc * D:(off_sc + 1) * D].partition_broadcast(128)) for j in range(3)],
               writes=[t_g])
        if off_sh is not None:
            kb.dma("sp", [(sh[:, j, :], self.modD[l][j, off_sh * D:(off_sh + 1) * D].partition_broadcast(128)) for j in range(3)],
                   writes=[t_s])
        for j in range(3):
            kb.op("dve", lambda e: e.scalar_tensor_tensor(out=gsc[:, j, :], in0=gsc[:, j, :], scalar=(1.0 if plus_one else 0.0), op0=ALU.add,
                                                          in1=gn[:], op1=ALU.mult),
                  reads=[t_g, t_n], writes=[t_g])
        return gsc, sh, t_g, t_s

    def norm_mod_tile(self, xt, t_x, gsc, sh, t_g, t_s, ms, R):
        kb = self.kb
        junk, t_j = R["junk"].next()
        st, t_st = R["stat"].next()
        kb.op("act", lambda e: e.activation(out=junk[:], in_=xt[:], func=AF.Square, accum_out=st[:, 0:1]),
              reads=[t_x], writes=[t_j, t_st])
        kb.op("act", lambda e: e.activation(out=st[:, 1:2], in_=st[:, 0:1], func=AF.Sqrt, scale=1.0 / D, bias=self.eps_t[:, 0:1]),
              reads=[t_st], writes=[t_st])
        kb.op("dve", lambda e: e.reciprocal(out=st[:, 2:3], in_=st[:, 1:2]), reads=[t_st], writes=[t_st])
        tmp, t_t = R["tmp"].next()
        kb.op("dve", lambda e: e.scalar_tensor_tensor(out=tmp[:], in0=xt[:], scalar=st[:, 2:3], op0=ALU.mult,
                                                      in1=gsc[:, ms, :], op1=ALU.mult),
              reads=[t_x, t_st, t_g], writes=[t_t])
        hb, t_h = R["hb"].next()
        kb.op("pool", lambda e: e.tensor_tensor(out=hb[:], in0=tmp[:], in1=sh[:, ms, :], op=ALU.add),
              reads=[t_t, t_s], writes=[t_h])
        return hb, t_h

    def consts_sb(self):
        kb, I = self.kb, self.I
        self.ident_bf = kb.sb([128, 128], BF16); self.t_ident = Tok()
        kb.dma("sp", [(self.ident_bf[:], I["ident_bf"][:, :])], writes=[self.t_ident])
        self.ident_f = kb.sb([128, 128], F32)
        kb.dma("sp", [(self.ident_f[:], I["ident_f"][:, :])], writes=[self.t_ident])
        self.eps_t = kb.sb([128, 1], F32)
        kb.op("pool", lambda e: e.memset(self.eps_t[:], EPS), writes=[self.t_ident])

    def phase_win(self, l, xsrc, prep_win=False):
        kb, I = self.kb, self.I
        with kb.phase():
            gsc, sh, t_g, t_s = self.load_mod_tiles(l, 1, 0, "norm_mix_pre")
            wb = kb.sb([128, 8, 2048], BF16); t_w = Tok()
            wv = I["w_in"][l].rearrange("(k p) n -> p k n", p=128)
            kb.dma("pool", [(wb[:, :, c * 512:(c + 1) * 512], wv[:, :, c * 512:(c + 1) * 512]) for c in range(4)], writes=[t_w])
            rc = kb.sb([128, 16, 64], F32); rs = kb.sb([128, 16, 64], F32); t_r = Tok()
            kb.dma("sp", [(rc[:], I["rope_cos"][:, :, :]), (rs[:], I["rope_sin"][:, :, :])], writes=[t_r])
            R = {"junk": Ring(kb, 1, [128, D], BF16), "stat": Ring(kb, 8, [128, 4], F32), "tmp": Ring(kb, 2, [128, D], F32),
                 "hb": Ring(kb, 3, [128, D], BF16)}
            xr = Ring(kb, 4, [128, D], F32)
            pT = Ring(kb, 1, [128, 8, 128], BF16, kind="ps")
            hTr = Ring(kb, 3, [128, 8, 128], BF16)
            pq = Ring(kb, 2, [128, 512], F32, kind="ps"); pk = Ring(kb, 2, [128, 512], F32, kind="ps")
            pv = Ring(kb, 1, [128, 512], F32, kind="ps"); pfu = Ring(kb, 1, [128, 512], F32, kind="ps")
            ptq = Ring(kb, 1, [128, 2, 4, 128], BF16, kind="ps")
            ropet = Ring(kb, 2, [128, 512], F32); ropem = Ring(kb, 2, [128, 256], F32)
            qbr = Ring(kb, 2, [128, 512], BF16); kbr = Ring(kb, 2, [128, 512], BF16)
            qTg = Ring(kb, 2, [128, 4, 512], BF16); kTg = Ring(kb, 2, [128, 4, 512], BF16)
            uTg = Ring(kb, 2, [128, 2, 512], BF16)
            vtr = Ring(kb, 2, [128, 4, 130], BF16); fbr = Ring(kb, 2, [128, 256], BF16)
            for vt, tv in vtr.items:
                kb.op("pool", lambda e: e.memset(vt[:, :, 128:130], 1.0), writes=[tv])
            C = [dict() for _ in range(NTILE)]
            G = {}

            def s0(ti):
                xt, t_x = xr.next()
                kb.dma("sp", [(xt[:], xsrc[ti * 128:(ti + 1) * 128, :])], writes=[t_x])
                st, t_st = self.nm1(xt, t_x, R)
                C[ti].update(xt=xt, t_x=t_x, st=st, t_st=t_st)

            def s1(ti):
                c = C[ti]
                b, j = divmod(ti, TILES_PB)
                ms = 2 if j < 2 else b
                c["hb"], c["t_h"] = self.nm2(c["xt"], c["t_x"], c["st"], c["t_st"], gsc, sh, t_g, t_s, ms, R)

            def s2(ti):
                c = C[ti]
                p, t_p = pT.next()
                for k in range(8):
                    kb.op("pe", lambda e: e.transpose(out=p[:, k, :], in_=c["hb"][:, k * 128:(k + 1) * 128], identity=self.ident_bf[:]),
                          reads=[c["t_h"], self.t_ident], writes=[t_p])
                hT, t_hT = hTr.next()
                kb.op("act", lambda e: e.copy(out=hT[:], in_=p[:]), reads=[t_p], writes=[t_hT])
                c.update(hT=hT, t_hT=t_hT)

            def s_mm(ti):
                c = C[ti]
                hT, t_hT = c["hT"], c["t_hT"]
                outs = []
                for ring, c0, n in ((pq, 0, 512), (pk, 512, 512), (pv, 1024, 512), (pfu, 1536, 256)):
                    pp, t_pp = ring.next()
                    for k in range(8):
                        kb.op("pe", lambda e: e.matmul(pp[:, 0:n], hT[:, k, :], wb[:, k, c0:c0 + n], start=(k == 0), stop=(k == 7)),
                              reads=[t_hT, t_w], writes=[t_pp])
                    outs.append((pp, t_pp))
                ppf, t_pf = outs[3]
                for ct in range(2):
                    for k in range(8):
                        kb.op("pe", lambda e: e.matmul(ppf[:, 256 + ct * 128:256 + (ct + 1) * 128], wb[:, k, 1792 + ct * 128:1792 + (ct + 1) * 128],
                                                       hT[:, k, :], start=(k == 0), stop=(k == 7)),
                              reads=[t_hT, t_w], writes=[t_pf])
                c["outs"] = outs

            def s_post(ti):
                c = C[ti]
                b, j = divmod(ti, TILES_PB)
                is_ctx = j < 2
                if j == 0 or (j >= 2 and (j - 2) % 4 == 0):
                    G["gq"] = qTg.next(); G["gk"] = kTg.next(); G["gu"] = uTg.next()
                    G["gstart"] = ti; G["gi"] = 0
                    G["gn"] = 2 if j == 0 else 4
                (gq, t_gq), (gk, t_gk), (gu, t_gu) = G["gq"], G["gk"], G["gu"]
                gi = G["gi"]
                (ppq, t_pq), (ppk, t_pk), (ppv, t_pv), (ppf, t_pf) = c["outs"]
                pt, t_pt = ptq.next()
                for which, (pp, t_pp), bring in ((0, (ppq, t_pq), qbr), (1, (ppk, t_pk), kbr)):
                    xb, t_xb = bring.next()
                    if is_ctx:
                        kb.op("dve", lambda e: e.tensor_copy(out=xb[:], in_=pp[:]), reads=[t_pp], writes=[t_xb])
                    else:
                        lt = j - 2
                        t1, t_t1 = ropet.next()
                        kb.op("dve", lambda e: e.tensor_tensor(out=t1[:].rearrange("p (a c) -> p a c", a=8), in0=pp[:].rearrange("p (a c) -> p a c", a=8),
                                                               in1=rc[:, lt:lt + 1, :].broadcast_to([128, 8, 64]), op=ALU.mult),
                              reads=[t_pp, t_r], writes=[t_t1])
                        x5 = pp[:].rearrange("p (a b j f) -> p a b j f", a=8, b=2, j=2)
                        t5 = t1[:].rearrange("p (a b j f) -> p a b j f", a=8, b=2, j=2)
                        o5 = xb[:].rearrange("p (a b j f) -> p a b j f", a=8, b=2, j=2)
                        s4 = rs[:, lt, :].rearrange("p (b j f) -> p b j f", b=2, j=2)
                        for jj in range(2):
                            m, t_m = ropem.next()
                            m4 = m[:].rearrange("p (a b f) -> p a b f", a=8, b=2)
                            kb.op("dve", lambda e: e.tensor_tensor(out=m4, in0=x5[:, :, :, 1 - jj, :],
                                                                   in1=s4[:, :, jj, :].unsqueeze(1).broadcast_to([128, 8, 2, 16]), op=ALU.mult),
                                  reads=[t_pp, t_r], writes=[t_m])
                            kb.op("dve", lambda e: e.tensor_tensor(out=o5[:, :, :, jj, :], in0=t5[:, :, :, jj, :], in1=m4, op=ALU.add),
                                  reads=[t_t1, t_m], writes=[t_xb])
                    for h in range(4):
                        kb.op("pe", lambda e: e.transpose(out=pt[:, which, h, :], in_=xb[:, h * 128:(h + 1) * 128], identity=self.ident_bf[:]),
                              reads=[t_xb, self.t_ident], writes=[t_pt])
                kb.op("act", lambda e: e.copy(out=gq[:, :, gi * 128:(gi + 1) * 128], in_=pt[:, 0, :, :]), reads=[t_pt], writes=[t_gq])
                kb.op("act", lambda e: e.copy(out=gk[:, :, gi * 128:(gi + 1) * 128], in_=pt[:, 1, :, :]), reads=[t_pt], writes=[t_gk])
                vt, t_v = vtr.next()
                kb.op("act", lambda e: e.copy(out=vt[:, :, 0:128], in_=ppv[:].rearrange("p (h d) -> p h d", h=4)), reads=[t_pv], writes=[t_v])
                kb.dma("sp", [(self.vD[:, :, ti, :], vt[:])], reads=[t_v])
                fb, t_f = fbr.next()
                kb.op("dve", lambda e: e.tensor_copy(out=fb[:], in_=ppf[:, 0:256]), reads=[t_pf], writes=[t_f])
                kb.dma("sp", [(self.fD[:, ti, :], fb[:])], reads=[t_f])
                kb.op("act", lambda e: e.copy(out=gu[:, :, gi * 128:(gi + 1) * 128], in_=ppf[:, 256:512].rearrange("p (c t) -> p c t", c=2)),
                      reads=[t_pf], writes=[t_gu])
                G["gi"] = gi + 1
                if G["gi"] == G["gn"]:
                    c0 = G["gstart"] * 128; w = G["gn"] * 128
                    kb.dma("sp", [(self.qT[:, :, c0:c0 + w], gq[:, :, 0:w])], reads=[t_gq])
                    kb.dma("sp", [(self.kT[:, :, c0:c0 + w], gk[:, :, 0:w])], reads=[t_gk])
                    kb.dma("sp", [(self.uT[:, :, c0:c0 + w], gu[:, :, 0:w])], reads=[t_gu])
                C[ti].clear()

            self.prep_begin()
            for it in range(NTILE + 4):
                if prep_win and it < NTILE:
                    self.prep_tick(1)
                for st_, off in ((s0, 0), (s1, 1), (s2, 2), (s_post, 4), (s_mm, 3)):
                    ti = it - off
                    if 0 <= ti < NTILE:
                        st_(ti)
            self.prep_flush()

    def phase_attn(self, l, need_ctx, prep_every=0):
        kb, I = self.kb, self.I
        lam_init = 0.8 - 0.6 * math.exp(-0.3 * l)
        with kb.phase():
            lq = kb.sb([128, 4, 64], F32); t_lq = Tok()
            kb.dma("sp", [(lq[:, i, :], I[n][l].partition_broadcast(128)) for i, n in
                          enumerate(("diff_lq1", "diff_lk1", "diff_lq2", "diff_lk2"))], writes=[t_lq])
            lt = kb.sb([128, 2, 64], F32); ls = kb.sb([128, 8], F32); t_ls = Tok()
            kb.op("dve", lambda e: e.tensor_tensor(out=lt[:, 0, :], in0=lq[:, 0, :], in1=lq[:, 1, :], op=ALU.mult), reads=[t_lq], writes=[t_ls])
            kb.op("dve", lambda e: e.tensor_tensor(out=lt[:, 1, :], in0=lq[:, 2, :], in1=lq[:, 3, :], op=ALU.mult), reads=[t_lq, t_ls], writes=[t_ls])
            kb.op("dve", lambda e: e.tensor_reduce(out=ls[:, 0:2], in_=lt[:], op=ALU.add, axis=AX.X), reads=[t_ls], writes=[t_ls])
            kb.op("act", lambda e: e.activation(out=ls[:, 2:4], in_=ls[:, 0:2], func=AF.Exp), reads=[t_ls], writes=[t_ls])
            kb.op("dve", lambda e: e.tensor_tensor(out=ls[:, 4:5], in0=ls[:, 3:4], in1=ls[:, 2:3], op=ALU.subtract), reads=[t_ls], writes=[t_ls])
            kb.op("dve", lambda e: e.tensor_scalar(out=ls[:, 5:6], in0=ls[:, 4:5], scalar1=-lam_init, scalar2=None, op0=ALU.add), reads=[t_ls], writes=[t_ls])
            nlam = ls[:, 5:6]
            sg = kb.sb([128, 128], F32); t_sg = Tok()
            kb.dma("sp", [(sg[:], I["diff_subln"][l].partition_broadcast(128))], writes=[t_sg])
            kb.op("dve", lambda e: e.tensor_scalar(out=sg[:], in0=sg[:], scalar1=1.0 - lam_init, scalar2=None, op0=ALU.mult), reads=[t_sg], writes=[t_sg])
            kr = Ring(kb, 2, [128, TPB], BF16); qr = Ring(kb, 2, [128, 2, TPB], BF16); vr = Ring(kb, 2, [128, TILES_PB, 130], BF16)
            psr = Ring(kb, 3, [128, 2, 256], F32, kind="ps")
            pacc = [Ring(kb, 2, [128, 2, 130], F32, kind="ps") for _ in range(2)]
            pto = Ring(kb, 1, [128, 2, 128], BF16, kind="ps")
            ptr = Ring(kb, 3, [128, 2, 256], BF16)
            o1r = Ring(kb, 4, [128, 128], F32); o2r = Ring(kb, 4, [128, 128], F32); junkr = Ring(kb, 1, [128, 128], BF16)
            str_ = Ring(kb, 6, [128, 8], F32); abr = Ring(kb, 2, [128, 128], BF16); aTr = Ring(kb, 2, [128, 256], BF16)
            for qz, t_qz in qr.items:
                kb.op("pool", lambda e: e.memset(qz[:], 0.0), writes=[t_qz])
            pending = []
            self.prep_begin()
            qt_count = 0
            for b in range(NB):
                t0 = b * TPB
                for h in range(4):
                    kT, t_k = kr.next(); qT, t_q = qr.next(); vv, t_v = vr.next()
                    kb.dma("sp", [(kT[:], self.kT[:, h, t0:t0 + TPB])], writes=[t_k])
                    kb.dma("sp", [(qT[m * 64:(m + 1) * 64, m, :], self.qT[m * 64:(m + 1) * 64, h, t0:t0 + TPB]) for m in range(2)], writes=[t_q])
                    kb.dma("sp", [(vv[:], self.vD[:, h, b * TILES_PB:(b + 1) * TILES_PB, :])], writes=[t_v])
                    qts = [(CTX + i * 256, list(range(TILES_PB))) for i in range(8)]
                    if need_ctx:
                        qts.append((0, [0, 1]))
                    for (q0, kts) in qts:
                        qt_count += 1
                        if prep_every and qt_count % prep_every == 0:
                            self.prep_tick(1)
                        a1, t_a1 = pacc[0].next(); a2, t_a2 = pacc[1].next()
                        accs = ((a1, t_a1), (a2, t_a2))

                        def emit_st(kt):
                            ps, t_ps = psr.next()
                            for m in range(2):
                                kb.op("pe", lambda e: e.matmul(ps[:, m, :], kT[:, kt * 128:(kt + 1) * 128],
                                                               qT[:, m, q0:q0 + 256], start=(m == 0), stop=True, skip_group_check=True),
                                      reads=[t_k, t_q], writes=[t_ps])
                            return ps, t_ps
                        ahead = [emit_st(kts[0])]
                        if len(kts) > 1:
                            ahead.append(emit_st(kts[1]))
                        for ki, kt in enumerate(kts):
                            ps, t_ps = ahead.pop(0)
                            if ki + 2 < len(kts):
                                ahead.append(emit_st(kts[ki + 2]))
                            pt, t_pt = ptr.next()
                            kb.op("act", lambda e: e.activation(out=pt[:], in_=ps[:], func=AF.Exp, scale=0.125), reads=[t_ps], writes=[t_pt])
                            for m in range(2):
                                for s in range(2):
                                    kb.op("pe", lambda e: e.matmul(accs[m][0][:, s, 0:129], pt[:, m, s * 128:(s + 1) * 128], vv[:, kt, 0:129],
                                                                   start=(ki == 0 and s == 0), stop=(ki == len(kts) - 1), skip_group_check=True),
                                          reads=[t_pt, t_v], writes=[accs[m][1]])
                            while pending and pending[0][0] <= ki:
                                pending.pop(0)[1]()
                        while pending:
                            pending.pop(0)[1]()
                        sts, o2s = [], []
                        for s in range(2):
                            st, t_st = str_.next()
                            kb.op("dve", lambda e: e.reciprocal(out=st[:, 0:1], in_=a1[:, s, 128:129]), reads=[t_a1], writes=[t_st])
                            kb.op("dve", lambda e: e.reciprocal(out=st[:, 1:2], in_=a2[:, s, 128:129]), reads=[t_a2, t_st], writes=[t_st])
                            kb.op("dve", lambda e: e.tensor_tensor(out=st[:, 2:3], in0=st[:, 1:2], in1=nlam, op=ALU.mult), reads=[t_st, t_ls], writes=[t_st])
                            o1, t_o1 = o1r.next(); o2, t_o2 = o2r.next()
                            kb.op("dve", lambda e: e.tensor_scalar(out=o1[:], in0=a1[:, s, 0:128], scalar1=st[:, 0:1], scalar2=None, op0=ALU.mult),
                                  reads=[t_a1, t_st], writes=[t_o1])
                            kb.op("dve", lambda e: e.scalar_tensor_tensor(out=o2[:], in0=a2[:, s, 0:128], scalar=st[:, 2:3], op0=ALU.mult,
                                                                          in1=o1[:], op1=ALU.add), reads=[t_a2, t_st, t_o1], writes=[t_o2])
                            kb.op("dve", lambda e: e.tensor_tensor(out=o1[:], in0=o2[:], in1=o2[:], op=ALU.mult), reads=[t_o2, t_o1], writes=[t_o1])
                            kb.op("dve", lambda e: e.tensor_reduce(out=st[:, 3:4], in_=o1[:], op=ALU.add, axis=AX.X), reads=[t_o1, t_st], writes=[t_st])
                            sts.append((st, t_st)); o2s.append((o2, t_o2))

                        def n2(sts=sts):
                            for st, t_st in sts:
                                kb.op("act", lambda e: e.activation(out=st[:, 4:5], in_=st[:, 3:4], func=AF.Ln, scale=1.0 / 128, bias=self.eps_t[:, 0:1]),
                                      reads=[t_st], writes=[t_st])
                                kb.op("act", lambda e: e.activation(out=st[:, 5:6], in_=st[:, 4:5], func=AF.Exp, scale=-0.5),
                                      reads=[t_st], writes=[t_st])

                        def n3(sts=sts, o2s=o2s, h=h, c0=t0 + q0):
                            po, t_po = pto.next()
                            for s in range(2):
                                st, t_st = sts[s]; o2, t_o2 = o2s[s]
                                ab, t_ab = abr.next()
                                kb.op("dve", lambda e: e.scalar_tensor_tensor(out=ab[:], in0=o2[:], scalar=st[:, 5:6], op0=ALU.mult, in1=sg[:], op1=ALU.mult),
                                      reads=[t_o2, t_st, t_sg], writes=[t_ab])
                                kb.op("pe", lambda e: e.transpose(out=po[:, s, :], in_=ab[:], identity=self.ident_bf[:]), reads=[t_ab, self.t_ident], writes=[t_po])
                            aT, t_aT = aTr.next()
                            kb.op("dve", lambda e: e.tensor_copy(out=aT[:], in_=po[:].rearrange("p s t -> p (s t)")), reads=[t_po], writes=[t_aT])
                            kb.dma("sp", [(self.catT[:, h, c0:c0 + 256], aT[:])], reads=[t_aT])
                        pending.append((7, n2)); pending.append((10, n3))
            while pending:
                pending.pop(0)[1]()
            self.prep_flush()

    def phase_fnet(self, l, need_ctx):
        kb, I = self.kb, self.I
        with kb.phase():
            fw = kb.sb([128, 2, 256], F32); cb = kb.sb([128, 2, 128], F32); t_fw = Tok()
            kb.dma("sp", [(fw[:], I["fnet_w"][l].rearrange("(c p) m -> p c m", p=128)), (cb[:], I["cblk"][:, :, :])], writes=[t_fw])
            W = kb.sb([128, 2, 2, 256], BF16); t_W = Tok()
            pw = Ring(kb, 2, [128, 512], F32, kind="ps")
            for ab in range(2):
                for ct in range(2):
                    p, t_p = pw.next()
                    kb.op("pe", lambda e: e.matmul(p[:, 0:256], cb[:, ab, :], fw[:, ct, :], start=True, stop=True), reads=[t_fw], writes=[t_p])
                    kb.op("dve", lambda e: e.tensor_copy(out=W[:, ab, ct, :], in_=p[:, 0:256]), reads=[t_p], writes=[t_W])
            fa = kb.sb([128, NTILE, 256], BF16); t_fa = Tok()
            kb.dma("sp", [(fa[:], self.fD[:, :, :])], writes=[t_fa])
            dr = [Ring(kb, 2, [128, 16, 512], BF16) for _ in range(2)]
            absb = Ring(kb, 2, [128, 2, 2, 512], BF16); fo = Ring(kb, 2, [128, 512], BF16)
            pf = Ring(kb, 2, [128, 512], F32, kind="ps")

            def dft(b, tiles, mats, n, tok0):
                ab_t, t_ab = absb.next()
                for ab in range(2):
                    m, t_m = mats[ab]
                    for ct in range(2):
                        p, t_p = pw.next()
                        for i, tt in enumerate(tiles):
                            kb.op("pe", lambda e: e.matmul(p[:, 0:n], fa[:, tt, ct * 128:(ct + 1) * 128], m[:, i, 0:n], start=(i == 0), stop=(i == len(tiles) - 1)),
                                  reads=[t_fa, t_m], writes=[t_p])
                        eng = "act" if ct == 0 else "dve"
                        if eng == "act":
                            kb.op("act", lambda e: e.copy(out=ab_t[:, ab, ct, 0:n], in_=p[:, 0:n]), reads=[t_p], writes=[t_ab])
                        else:
                            kb.op("dve", lambda e: e.tensor_copy(out=ab_t[:, ab, ct, 0:n], in_=p[:, 0:n]), reads=[t_p], writes=[t_ab])
                for mt in range(2):
                    p, t_p = pf.next()
                    i = 0
                    for ab in range(2):
                        for ct in range(2):
                            kb.op("pe", lambda e: e.matmul(p[:, 0:n], W[:, ab, ct, mt * 128:(mt + 1) * 128], ab_t[:, ab, ct, 0:n], start=(i == 0), stop=(i == 3)),
                                  reads=[t_W, t_ab], writes=[t_p])
                            i += 1
                    f, t_f = fo.next()
                    kb.op("act", lambda e: e.copy(out=f[:, 0:n], in_=p[:, 0:n]), reads=[t_p], writes=[t_f])
                    kb.dma("sp", [(self.catT[:, 4 + mt, tok0:tok0 + n], f[:, 0:n])], reads=[t_f])

            mode = getattr(self, "fn_mode", "WMC")
            for pt in range(4 if "M" in mode else 0):
                mats = []
                for ab, nm in enumerate(("dft_c", "dft_s")):
                    m, t_m = dr[ab].next()
                    src = I[nm].rearrange("(t p) n -> p t n", p=128)
                    kb.dma("sp", [(m[:, q * 4:(q + 1) * 4, :], src[:, q * 4:(q + 1) * 4, pt * 512:(pt + 1) * 512]) for q in range(4)], writes=[t_m])
                    mats.append((m, t_m))
                for b in range(NB):
                    dft(b, [b * TILES_PB + 2 + i for i in range(16)], mats, 512, b * TPB + CTX + pt * 512)
            if need_ctx and "C" in mode:
                mats = []
                for ab, nm in enumerate(("dftc_c", "dftc_s")):
                    m, t_m = dr[ab].next()
                    kb.dma("sp", [(m[:, 0:2, 0:256], I[nm].rearrange("(t p) n -> p t n", p=128))], writes=[t_m])
                    mats.append((m, t_m))
                for b in range(NB):
                    dft(b, [b * TILES_PB + i for i in range(2)], mats, 256, b * TPB)

    def cmul(self, o_r, o_i, a_r, a_i, b_r, b_i, t1, t2, T, eng="dve"):
        kb = self.kb
        tt = lambda o, x, y, op: kb.op(eng, lambda e: e.tensor_tensor(out=o, in0=x, in1=y, op=op), reads=T, writes=T)
        tt(t1, a_r, b_r, ALU.mult); tt(t2, a_i, b_i, ALU.mult); tt(o_r, t1, t2, ALU.subtract)
        tt(t1, a_r, b_i, ALU.mult); tt(t2, a_i, b_r, ALU.mult); tt(o_i, t1, t2, ALU.add)

    def phase_s5(self, l):
        kb, I = self.kb, self.I
        TWO_PI = 2.0 * math.pi
        MAGIC = 12582912.0
        with kb.phase():
            A = kb.sb([128, 32, 128], BF16); BsRI = kb.sb([128, 32, 2, 128], BF16); CqRI = kb.sb([128, 32, 2, 128], BF16)
            Wsel = kb.sb([128, 8, 240], BF16); V = None; Hb = None
            PL = kb.sb([128, 2, 10, 16], F32)
            nPLi = kb.sb([128, 10, 16], F32)
            dd = kb.sb([128, 2], F32); wg = kb.sb([128, 2, 256], BF16)
            t_A, t_Bs, t_Cq, t_W, t_V, t_H, t_PL, t_misc = (Tok() for _ in range(8))
            kb.dma("sp", [(Wsel[:], I["wsel"][:, :, :])], writes=[t_W])
            kb.dma("sp", [(dd[:], I["s5_dd"][l])], writes=[t_misc])
            kb.dma("pool", [(wg[:], I["s5_w_glu"][l].rearrange("(c p) m -> p c m", p=128))], writes=[t_misc])
            with kb.phase():
                T = [Tok()]
                par = kb.sb([128, 3, 32], F32)
                bc = kb.sb([128, 2, 32, 2, 16], F32)
                msk = kb.sb([128, 2, 128], F32)
                kb.dma("sp", [(par[:], I["s5_par"][l]), (bc[:, 0], I["s5_b"][l]), (bc[:, 1], I["s5_c"][l]), (msk[:], I["s5_mask"][:, :, :])], writes=T)
                w = kb.sb([128, 24, 32], F32)
                W_ = lambda i: w[:, i, :]
                ts = lambda o, x, s1, o0, s2=None, o1=None: kb.op("dve", lambda e: e.tensor_scalar(out=o, in0=x, scalar1=s1, scalar2=s2, op0=o0, **({"op1": o1} if o1 else {})), reads=T, writes=T)
                tt = lambda o, x, y, op: kb.op("dve", lambda e: e.tensor_tensor(out=o, in0=x, in1=y, op=op), reads=T, writes=T)
                act = lambda o, x, f, **kw: kb.op("act", lambda e: e.activation(out=o, in_=x, func=f, **kw), reads=T, writes=T)
                are, aim, ldt = par[:, 0, :], par[:, 1, :], par[:, 2, :]
                dt, mag, ang, lr, li = W_(0), W_(1), W_(2), W_(3), W_(4)
                act(dt, ldt, AF.Exp)
                tt(mag, are, dt, ALU.mult); act(mag, mag, AF.Exp)
                tt(ang, aim, dt, ALU.mult)

                def sin_of(o, x, shift):
                    a, r = W_(5), W_(6)
                    ts(a, x, shift, ALU.add)
                    ts(r, a, 1.0 / TWO_PI, ALU.mult)
                    ts(r, r, MAGIC, ALU.add)
                    ts(r, r, MAGIC, ALU.subtract)
                    kb.op("dve", lambda e: e.scalar_tensor_tensor(out=a, in0=r, scalar=-TWO_PI, op0=ALU.mult, in1=a, op1=ALU.add), reads=T, writes=T)
                    act(o, a, AF.Sin)
                sin_of(li, ang, 0.0); sin_of(lr, ang, math.pi / 2)
                tt(lr, lr, mag, ALU.mult); tt(li, li, mag, ALU.mult)
                nr, den, cr, ci, t1, t2 = W_(7), W_(8), W_(9), W_(10), W_(11), W_(12)
                ts(nr, lr, -1.0, ALU.add)
                tt(t1, are, are, ALU.mult); tt(t2, aim, aim, ALU.mult); tt(den, t1, t2, ALU.add)
                kb.op("dve", lambda e: e.reciprocal(out=den, in_=den), reads=T, writes=T)
                tt(t1, nr, are, ALU.mult); tt(t2, li, aim, ALU.mult); tt(cr, t1, t2, ALU.add); tt(cr, cr, den, ALU.mult)
                tt(t1, li, are, ALU.mult); tt(t2, nr, aim, ALU.mult); tt(ci, t1, t2, ALU.subtract); tt(ci, ci, den, ALU.mult)
                ilr, ili, m2 = W_(13), W_(14), W_(15)
                tt(t1, lr, lr, ALU.mult); tt(t2, li, li, ALU.mult); tt(m2, t1, t2, ALU.add)
                kb.op("dve", lambda e: e.reciprocal(out=m2, in_=m2), reads=T, writes=T)
                tt(ilr, lr, m2, ALU.mult); tt(ili, li, m2, ALU.mult); ts(ili, ili, -1.0, ALU.mult)
                bb = kb.sb([128, 2, 32, 16], F32); tb = kb.sb([128, 2, 32, 16], F32)
                bcast = lambda v: v.unsqueeze(2).broadcast_to([128, 32, 16])
                self.cmul(bb[:, 0], bb[:, 1], bcast(cr), bcast(ci), bc[:, 0, :, 0, :], bc[:, 0, :, 1, :], tb[:, 0], tb[:, 1], T)
                mu = kb.sb([128, 2, 2, 3, 32], F32)
                cp = lambda o, x: kb.op("dve", lambda e: e.tensor_copy(out=o, in_=x), reads=T, writes=T)
                for ri, (fw_, bw_) in enumerate(((ilr, lr), (ili, li))):
                    cp(mu[:, 0, ri, 0, 0:16], fw_[:, 0:16]); cp(mu[:, 0, ri, 0, 16:32], bw_[:, 16:32])
                    cp(mu[:, 1, ri, 0, 0:16], bw_[:, 0:16]); cp(mu[:, 1, ri, 0, 16:32], fw_[:, 16:32])
                for tb_i in range(2):
                    for pw in range(2):
                        self.cmul(mu[:, tb_i, 0, pw + 1], mu[:, tb_i, 1, pw + 1], mu[:, tb_i, 0, pw], mu[:, tb_i, 1, pw],
                                  mu[:, tb_i, 0, pw], mu[:, tb_i, 1, pw], t1, t2, T)
                ch = kb.sb([128, 2, 2, 32, 8], F32)
                for tb_i in range(2):
                    kb.op("dve", lambda e: e.memset(ch[:, tb_i, 0, :, 0:1], 1.0), reads=T, writes=T)
                    kb.op("dve", lambda e: e.memset(ch[:, tb_i, 1, :, 0:1], 0.0), reads=T, writes=T)
                    n = 1
                    tmpc = kb.sb([128, 2, 32, 4], F32)
                    for pw in range(3):
                        mb = lambda ri: mu[:, tb_i, ri, pw, :].unsqueeze(2).broadcast_to([128, 32, n])
                        self.cmul(ch[:, tb_i, 0, :, n:2 * n], ch[:, tb_i, 1, :, n:2 * n], ch[:, tb_i, 0, :, 0:n], ch[:, tb_i, 1, :, 0:n],
                                  mb(0), mb(1), tmpc[:, 0, :, 0:n], tmpc[:, 1, :, 0:n], T)
                        n *= 2
                l8 = kb.sb([128, 2, 32], F32); l7 = kb.sb([128, 2, 32], F32); l2 = kb.sb([128, 2, 2, 32], F32)
                self.cmul(l2[:, 0, 0], l2[:, 0, 1], lr, li, lr, li, t1, t2, T)
                self.cmul(l2[:, 1, 0], l2[:, 1, 1], l2[:, 0, 0], l2[:, 0, 1], l2[:, 0, 0], l2[:, 0, 1], t1, t2, T)
                self.cmul(l8[:, 0], l8[:, 1], l2[:, 1, 0], l2[:, 1, 1], l2[:, 1, 0], l2[:, 1, 1], t1, t2, T)
                self.cmul(l7[:, 0], l7[:, 1], l8[:, 0], l8[:, 1], ilr, ili, t1, t2, T)
                sf = kb.sb([128, 2, 2, 32], F32)
                cp(sf[:, 0, 0, 0:16], l7[:, 0, 0:16]); cp(sf[:, 0, 1, 0:16], l7[:, 1, 0:16])
                kb.op("dve", lambda e: e.memset(sf[:, 0, 0, 16:32], 1.0), reads=T, writes=T)
                kb.op("dve", lambda e: e.memset(sf[:, 0, 1, 16:32], 0.0), reads=T, writes=T)
                cp(sf[:, 1, 0, 0:16], lr[:, 0:16]); cp(sf[:, 1, 1, 0:16], li[:, 0:16])
                cp(sf[:, 1, 0, 16:32], l8[:, 0, 16:32]); cp(sf[:, 1, 1, 16:32], l8[:, 1, 16:32])
                ch2 = kb.sb([128, 2, 2, 32, 8], F32)
                tmp8 = kb.sb([128, 2, 32, 8], F32)
                for tb_i in range(2):
                    sb_ = lambda ri: sf[:, tb_i, ri, :].unsqueeze(2).broadcast_to([128, 32, 8])
                    self.cmul(ch2[:, tb_i, 0], ch2[:, tb_i, 1], ch[:, tb_i, 0], ch[:, tb_i, 1], sb_(0), sb_(1), tmp8[:, 0], tmp8[:, 1], T)
                full = kb.sb([128, 2, 32, 8, 16], F32); ftmp = kb.sb([128, 2, 32, 8, 16], F32)
                st = kb.sb([128, 32, 128], BF16)
                pA = Ring(kb, 2, [128, 4, 128], F32, kind="ps"); pTt = Ring(kb, 2, [128, 4, 128], BF16, kind="ps")
                KBst = kb.sb([128, 32, 128], BF16); QCst = kb.sb([128, 32, 128], BF16)

                def build(chain, tb_i, src_r, src_i, dst, neg_im):
                    cb = lambda ri: chain[:, tb_i, ri].unsqueeze(3).broadcast_to([128, 32, 8, 16])
                    sbq = lambda v: v.unsqueeze(2).broadcast_to([128, 32, 8, 16])
                    self.cmul(full[:, 0], full[:, 1], cb(0), cb(1), sbq(src_r), sbq(src_i), ftmp[:, 0], ftmp[:, 1], T)
                    cp(dst[0:64], full[0:64, 0].rearrange("p a s c -> p a (s c)"))
                    if neg_im:
                        ts(dst[64:128], full[64:128, 1].rearrange("p a s c -> p a (s c)"), -1.0, ALU.mult)
                    else:
                        cp(dst[64:128], full[64:128, 1].rearrange("p a s c -> p a (s c)"))
                build(ch, 0, bb[:, 0], bb[:, 1], KBst, False)
                build(ch, 1, bc[:, 1, :, 0, :], bc[:, 1, :, 1, :], QCst, True)
                for q4 in range(8):
                    p, t_p = pA.next()
                    for i in range(4):
                        dg = q4 * 4 + i
                        kb.op("pe", lambda e: e.matmul(p[:, i, :], KBst[:, dg, :], QCst[:, dg, :], start=(i == 0), stop=True, skip_group_check=True), reads=T, writes=[t_p])
                    d = 0 if q4 < 4 else 1
                    kb.op("dve", lambda e: e.tensor_tensor(out=A[:, q4 * 4:(q4 + 1) * 4, :], in0=p[:],
                                                           in1=msk[:, d:d + 1, :].broadcast_to([128, 4, 128]), op=ALU.mult),
                          reads=[t_p] + T, writes=[t_A])
                kb.op("pool", lambda e: e.memset(BsRI[:], 0.0), writes=[t_Bs])
                kb.op("pool", lambda e: e.memset(CqRI[:], 0.0), writes=[t_Cq])
                build(ch2, 0, bb[:, 0], bb[:, 1], st, False)
                for q4 in range(8):
                    p, t_p = pTt.next()
                    for i in range(4):
                        dg = q4 * 4 + i
                        kb.op("pe", lambda e: e.transpose(out=p[:, i, :], in_=st[:, dg, :], identity=self.ident_bf[:]), reads=T + [self.t_ident], writes=[t_p])
                    for i in range(4):
                        dg = q4 * 4 + i
                        g2 = dg % 2
                        kb.op("act", lambda e: e.copy(out=BsRI[:, dg, :, g2 * 64:(g2 + 1) * 64], in_=p[:, i, :].rearrange("p (r q) -> p r q", r=2)),
                              reads=[t_p], writes=[t_Bs])
                build(ch2, 1, bc[:, 1, :, 0, :], bc[:, 1, :, 1, :], st, True)
                for g2 in range(2):
                    rows = slice(g2 * 64, (g2 + 1) * 64)
                    sv = st[rows].rearrange("p (a g) x -> p a g x", g=2)
                    dv = CqRI[rows].rearrange("p (a g) r x -> p a g r x", g=2)
                    fr = full[rows, 0].rearrange("p (a g) s c -> p a g (s c)", g=2)
                    fi = full[rows, 1].rearrange("p (a g) s c -> p a g (s c)", g=2)
                    kb.op("dve", lambda e: e.tensor_copy(out=dv[:, :, g2, 0, :], in_=fr[:, :, g2, :]), reads=T, writes=[t_Cq])
                    kb.op("dve", lambda e: e.tensor_scalar(out=dv[:, :, g2, 1, :], in0=fi[:, :, g2, :], scalar1=-1.0, scalar2=None, op0=ALU.mult), reads=T, writes=[t_Cq])
                for ri in range(2):
                    for g2 in range(2):
                        rows = slice(g2 * 64, (g2 + 1) * 64)
                        kb.op("dve", lambda e: e.tensor_copy(out=PL[rows, ri, 0, :], in_=l8[rows, ri, :].rearrange("p (a g) -> p a g", g=2)[:, :, g2]),
                              reads=T, writes=[t_PL])
                pt1 = kb.sb([128, 16], F32); pt2 = kb.sb([128, 16], F32)
                for i in range(9):
                    self.cmul(PL[:, 0, i + 1], PL[:, 1, i + 1], PL[:, 0, i], PL[:, 1, i], PL[:, 0, i], PL[:, 1, i], pt1[:], pt2[:], [t_PL])
                kb.op("dve", lambda e: e.tensor_scalar(out=nPLi[:], in0=PL[:, 1], scalar1=-1.0, scalar2=None, op0=ALU.mult), reads=[t_PL], writes=[t_PL])
            self._s5_main(l, A, BsRI, CqRI, Wsel, V, Hb, PL, nPLi, dd, wg, (t_A, t_Bs, t_Cq, t_W, t_V, t_H, t_PL, t_misc))

    def _s5_main(self, l, A, BsRI, CqRI, Wsel, V, Hb, PL, nPLi, dd, wg, toks):
        kb, I = self.kb, self.I
        t_A, t_Bs, t_Cq, t_W, t_V, t_H, t_PL, t_misc = toks
        NCH = 288
        with kb.phase():
            V = kb.sb([128, 16, NB, 320], BF16)
            Hb = kb.sb([128, 2, 2, 8, NB, 288], BF16)
            with kb.phase():
                with kb.phase():
                    U = kb.sb([128, 2, NTOK], BF16); t_U = Tok()
                    kb.dma("sp", [(U[:, ct, :], self.uT[:, ct, :]) for ct in range(2)], writes=[t_U])
                    pv = Ring(kb, 2, [128, NCH], F32, kind="ps")
                    i = 0
                    for g in range(16):
                        gt, gl = divmod(g, 8)
                        for b in range(NB):
                            p, t_p = pv.next()
                            ub = U[:, gt, b * TPB:(b + 1) * TPB].rearrange("p (k s) -> p k s", s=8)
                            for s in range(8):
                                kb.op("pe", lambda e: e.matmul(p[:, :], Wsel[:, gl, 112 - 16 * s:240 - 16 * s], ub[:, :, s], start=(s == 0), stop=(s == 7)),
                                      reads=[t_U, t_W], writes=[t_p])
                            if i % 2 == 0:
                                kb.op("act", lambda e: e.copy(out=V[:, g, b, 0:NCH], in_=p[:, :]), reads=[t_p], writes=[t_V])
                                kb.op("act", lambda e: e.copy(out=V[:, g, b, NCH:320], in_=p[:, 0:32]), reads=[t_p], writes=[t_V])
                            else:
                                kb.op("dve", lambda e: e.tensor_copy(out=V[:, g, b, 0:NCH], in_=p[:, :]), reads=[t_p], writes=[t_V])
                                kb.op("dve", lambda e: e.tensor_copy(out=V[:, g, b, NCH:320], in_=p[:, 0:32]), reads=[t_p], writes=[t_V])
                            i += 1
                X = kb.sb([128, 2, 2, 8, NB, NCH], F32)
                sctmp = None
                t_X = [Tok(), Tok()]
                t_XG = [[[Tok(), Tok()] for _ in range(8)] for _ in range(2)]
                pS = Ring(kb, 4, [128, NCH], F32, kind="ps")
                for d in range(2):
                    k0 = 0 if d == 0 else 32
                    for gp in range(8):
                        for b in range(NB):
                            for ri in range(2):
                                p, t_p = pS.next()
                                for g2 in range(2):
                                    g = 2 * gp + g2
                                    kb.op("pe", lambda e: e.matmul(p[:, :], BsRI[:, d * 16 + g, ri, :], V[:, g, b, k0:k0 + NCH], start=(g2 == 0), stop=(g2 == 1)),
                                          reads=[t_Bs, t_V], writes=[t_p])
                                if ri == 0:
                                    kb.op("act", lambda e: e.copy(out=X[:, 0, ri, gp, b, :], in_=p[:, :]), reads=[t_p], writes=[t_XG[0][gp][ri]])
                                else:
                                    kb.op("dve", lambda e: e.tensor_copy(out=X[:, 0, ri, gp, b, :], in_=p[:, :]), reads=[t_p], writes=[t_XG[0][gp][ri]])
                    cur = 0
                    for si in range(9):
                        sh = 1 << si
                        nxt = 1 - cur
                        n = NCH - sh
                        if d == 0:
                            dst, src, keep = slice(sh, NCH), slice(0, n), slice(0, sh)
                        else:
                            dst, src, keep = slice(0, n), slice(sh, NCH), slice(n, NCH)
                        for ri in range(2):
                            kb.op("pool", lambda e: e.tensor_copy(out=X[:, nxt, ri, :, :, keep], in_=X[:, cur, ri, :, :, keep]), reads=[t_XG[cur][g_][ri] for g_ in range(8)], writes=[t_XG[nxt][g_][ri] for g_ in range(8)])
                        for opi in range(4):
                            for gp in range(8):
                                c = d * 8 + gp
                                Pr, Pi, nPi = PL[:, 0, si, c:c + 1], PL[:, 1, si, c:c + 1], nPLi[:, si, c:c + 1]
                                xr, xi = X[:, cur, 0, gp], X[:, cur, 1, gp]
                                yr, yi = X[:, nxt, 0, gp], X[:, nxt, 1, gp]
                                o_, a_, sc_, b_ = ((yr, xr, Pr, xr), (yi, xi, Pr, xi), (yr, xi, nPi, yr), (yi, xr, Pi, yi))[opi]
                                kb.op("dve", lambda e: e.scalar_tensor_tensor(out=o_[:, :, dst], in0=a_[:, :, src], scalar=sc_, op0=ALU.mult, in1=b_[:, :, dst], op1=ALU.add),
                                      reads=[t_XG[cur][gp][0], t_XG[cur][gp][1], t_PL], writes=[t_XG[nxt][gp][opi % 2]])
                        cur = nxt
                    for ri in range(2):
                        kb.op("act", lambda e: e.copy(out=Hb[:, d, ri], in_=X[:, cur, ri]), reads=[t_XG[cur][g_][ri] for g_ in range(8)], writes=[t_H])
            with kb.phase():
                U = kb.sb([128, 2, NTOK], BF16); t_U = Tok()
                kb.dma("sp", [(U[:, ct, :], self.uT[:, ct, :]) for ct in range(2)], writes=[t_U])
                Yc = kb.sb([128, 16, NB, NCH], BF16); t_Y = Tok()
                G = kb.sb([128, 2, NTOK], BF16); t_G = Tok()
                py = Ring(kb, 2, [128, NCH], F32, kind="ps")
                i = 0
                for g in range(16):
                    gp, g2 = divmod(g, 2)
                    for b in range(NB):
                        p, t_p = py.next()
                        mm = lambda o, lh, rh, first=False: kb.op("pe", lambda e: e.matmul(o, lh, rh, start=first, stop=True, skip_group_check=True),
                                                                  reads=[t_A, t_Cq, t_V, t_H], writes=[t_p])
                        mm(p[:, 0:NCH], A[:, g, :], V[:, g, b, 0:NCH], True)
                        for ri in range(2):
                            mm(p[:, 1:NCH], CqRI[:, g, ri, :], Hb[:, 0, ri, gp, b, 0:NCH - 1])
                        mm(p[:, 32:NCH], A[:, 16 + g, :], V[:, g, b, 32:NCH])
                        mm(p[:, 0:32], A[:, 16 + g, :], V[:, g, b, NCH:320])
                        for ri in range(2):
                            mm(p[:, 32:NCH], CqRI[:, 16 + g, ri, :], Hb[:, 1, ri, gp, b, 1:257])
                            mm(p[:, 0:31], CqRI[:, 16 + g, ri, :], Hb[:, 1, ri, gp, b, 257:NCH])
                        if i % 2 == 0:
                            kb.op("act", lambda e: e.copy(out=Yc[:, g, b, :], in_=p[:, :]), reads=[t_p], writes=[t_Y])
                        else:
                            kb.op("dve", lambda e: e.tensor_copy(out=Yc[:, g, b, :], in_=p[:, :]), reads=[t_p], writes=[t_Y])
                        i += 1
                pu = Ring(kb, 2, [128, 64, 8], F32, kind="ps")
                yyr = Ring(kb, 2, [128, 512], F32)
                for gt in range(2):
                    for b in range(NB):
                        for seg in range(5):
                            nk = 64 if seg < 4 else 32
                            p, t_p = pu.next()
                            first = True
                            for t in range(8):
                                for gl in range(8):
                                    kb.op("pe", lambda e: e.matmul(p[:, 0:nk, t], Wsel[:, t, 112 - 16 * gl:240 - 16 * gl], Yc[:, gt * 8 + gl, b, seg * 64:seg * 64 + nk],
                                                                   start=first, stop=True, skip_group_check=True), reads=[t_W, t_Y], writes=[t_p])
                                    first = False
                            tok0 = b * TPB + seg * 512
                            nt = nk * 8
                            yy, t_yy = yyr.next()
                            kb.op("dve", lambda e: e.scalar_tensor_tensor(out=yy[:, 0:nt], in0=U[:, gt, tok0:tok0 + nt], scalar=dd[:, gt:gt + 1], op0=ALU.mult,
                                                                          in1=p[:, 0:nk, :].rearrange("p k t -> p (k t)"), op1=ALU.add),
                                  reads=[t_U, t_misc, t_p], writes=[t_yy])
                            kb.op("act", lambda e: e.activation(out=G[:, gt, tok0:tok0 + nt], in_=yy[:, 0:nt], func=AF.Gelu_apprx_tanh), reads=[t_yy], writes=[t_G])
                pz = Ring(kb, 2, [128, 512], F32, kind="ps")
                sgr = Ring(kb, 2, [128, 512], BF16); sor = Ring(kb, 2, [128, 512], BF16)
                for tt_ in range(NTOK // 512):
                    for mt in range(2):
                        p, t_p = pz.next()
                        for gt in range(2):
                            kb.op("pe", lambda e: e.matmul(p[:, :], wg[:, gt, mt * 128:(mt + 1) * 128], G[:, gt, tt_ * 512:(tt_ + 1) * 512], start=(gt == 0), stop=(gt == 1)),
                                  reads=[t_misc, t_G], writes=[t_p])
                        sg_, t_sg = sgr.next()
                        kb.op("act", lambda e: e.activation(out=sg_[:], in_=p[:, :], func=AF.Sigmoid), reads=[t_p], writes=[t_sg])
                        so, t_so = sor.next()
                        kb.op("dve", lambda e: e.tensor_tensor(out=so[:], in0=G[:, mt, tt_ * 512:(tt_ + 1) * 512], in1=sg_[:], op=ALU.mult), reads=[t_G, t_sg], writes=[t_so])
                        kb.dma("sp", [(self.catT[:, 6 + mt, tt_ * 512:(tt_ + 1) * 512], so[:])], reads=[t_so])

    def groups(self, with_ctx):
        gs = []
        for b in range(NB):
            if with_ctx:
                gs.append((b * TPB, CTX, 2))
            for i in range(4):
                gs.append((b * TPB + CTX + i * 512, 512, b))
        return gs

    def post_norm_tile(self, halves, t_halves, xt, t_x, gg, t_gg, ms, R):
        kb = self.kb
        st, t_st = R["stat2"].next()
        for nh in range(2):
            jk, t_jk = R["junk2"].next()
            kb.op("act", lambda e: e.activation(out=jk[:], in_=halves[nh], func=AF.Square, accum_out=st[:, nh:nh + 1]),
                  reads=[t_halves[nh], t_st], writes=[t_jk, t_st])
        kb.op("dve", lambda e: e.tensor_tensor(out=st[:, 2:3], in0=st[:, 0:1], in1=st[:, 1:2], op=ALU.add), reads=[t_st], writes=[t_st])
        kb.op("act", lambda e: e.activation(out=st[:, 3:4], in_=st[:, 2:3], func=AF.Sqrt, scale=1.0 / D, bias=self.eps_t[:, 0:1]), reads=[t_st], writes=[t_st])
        kb.op("dve", lambda e: e.reciprocal(out=st[:, 4:5], in_=st[:, 3:4]), reads=[t_st], writes=[t_st])
        tmp, t_t = R["tmp"].next()
        for nh in range(2):
            kb.op("dve", lambda e: e.scalar_tensor_tensor(out=tmp[:, nh * 512:(nh + 1) * 512], in0=halves[nh], scalar=st[:, 4:5], op0=ALU.mult,
                                                          in1=gg[:, ms, nh * 512:(nh + 1) * 512], op1=ALU.mult),
                  reads=[t_halves[nh], t_st, t_gg], writes=[t_t])
        xn, t_xn = R["xn"].next()
        kb.op("pool", lambda e: e.tensor_tensor(out=xn[:], in0=tmp[:], in1=xt[:], op=ALU.add), reads=[t_t, t_x], writes=[t_xn])
        return xn, t_xn

    def pn1(self, halves, t_halves, R):
        kb = self.kb
        st, t_st = R["stat2"].next()
        for nh in range(2):
            jk, t_jk = R["junk2"].next()
            kb.op("act", lambda e: e.activation(out=jk[:], in_=halves[nh], func=AF.Square, accum_out=st[:, nh:nh + 1]),
                  reads=[t_halves[nh], t_st], writes=[t_jk, t_st])
        kb.op("dve", lambda e: e.tensor_tensor(out=st[:, 2:3], in0=st[:, 0:1], in1=st[:, 1:2], op=ALU.add), reads=[t_st], writes=[t_st])
        kb.op("act", lambda e: e.activation(out=st[:, 3:4], in_=st[:, 2:3], func=AF.Sqrt, scale=1.0 / D, bias=self.eps_t[:, 0:1]), reads=[t_st], writes=[t_st])
        kb.op("dve", lambda e: e.reciprocal(out=st[:, 4:5], in_=st[:, 3:4]), reads=[t_st], writes=[t_st])
        return st, t_st

    def pn2(self, halves, t_halves, st, t_st, xt, t_x, gg, t_gg, ms, R):
        kb = self.kb
        tmp, t_t = R["tmp"].next()
        for nh in range(2):
            kb.op("dve", lambda e: e.scalar_tensor_tensor(out=tmp[:, nh * 512:(nh + 1) * 512], in0=halves[nh], scalar=st[:, 4:5], op0=ALU.mult,
                                                          in1=gg[:, ms, nh * 512:(nh + 1) * 512], op1=ALU.mult),
                  reads=[t_halves[nh], t_st, t_gg], writes=[t_t])
        xn, t_xn = R["xn"].next()
        kb.op("pool", lambda e: e.tensor_tensor(out=xn[:], in0=tmp[:], in1=xt[:], op=ALU.add), reads=[t_t, t_x], writes=[t_xn])
        return xn, t_xn

    def nm1(self, xt, t_x, R):
        kb = self.kb
        junk, t_j = R["junk"].next()
        st, t_st = R["stat"].next()
        kb.op("act", lambda e: e.activation(out=junk[:], in_=xt[:], func=AF.Square, accum_out=st[:, 0:1]), reads=[t_x], writes=[t_j, t_st])
        kb.op("act", lambda e: e.activation(out=st[:, 1:2], in_=st[:, 0:1], func=AF.Sqrt, scale=1.0 / D, bias=self.eps_t[:, 0:1]), reads=[t_st], writes=[t_st])
        kb.op("dve", lambda e: e.reciprocal(out=st[:, 2:3], in_=st[:, 1:2]), reads=[t_st], writes=[t_st])
        return st, t_st

    def nm2(self, xt, t_x, st, t_st, gsc, sh, t_g, t_s, ms, R):
        kb = self.kb
        tmp, t_t = R["tmp"].next()
        kb.op("dve", lambda e: e.scalar_tensor_tensor(out=tmp[:], in0=xt[:], scalar=st[:, 2:3], op0=ALU.mult, in1=gsc[:, ms, :], op1=ALU.mult),
              reads=[t_x, t_st, t_g], writes=[t_t])
        hb, t_h = R["hb"].next()
        kb.op("pool", lambda e: e.tensor_tensor(out=hb[:], in0=tmp[:], in1=sh[:, ms, :], op=ALU.add), reads=[t_t, t_s], writes=[t_h])
        return hb, t_h

    @staticmethod
    def run_pipeline(n, stages):
        for it in range(n + len(stages) - 1):
            for s_, f in enumerate(stages):
                j = it - s_
                if 0 <= j < n:
                    f(j)

    def phase_wout(self, l, xsrc, xdst, need_ctx, prep_wout=False):
        kb, I = self.kb, self.I
        with kb.phase():
            gg, _, t_gg, _ = self.load_mod_tiles(l, 2, None, "norm_mix_post", plus_one=False)
            gsc2, sh2, t_g2, t_s2 = self.load_mod_tiles(l, 4, 3, "norm_ffn_pre")
            wo = kb.sb([128, 8, D], BF16); t_wo = Tok()
            wv = I["w_out"][l].rearrange("(k p) n -> p k n", p=128)
            kb.dma("pool", [(wo[:, :, c * 512:(c + 1) * 512], wv[:, :, c * 512:(c + 1) * 512]) for c in range(2)], writes=[t_wo])
            R = {"junk": Ring(kb, 1, [128, D], BF16), "stat": Ring(kb, 8, [128, 4], F32), "tmp": Ring(kb, 3, [128, D], F32),
                 "hb": Ring(kb, 3, [128, D], BF16), "stat2": Ring(kb, 8, [128, 8], F32), "junk2": Ring(kb, 1, [128, 512], BF16),
                 "xn": Ring(kb, 4, [128, D], F32)}
            xr = Ring(kb, 4, [128, D], F32)
            cgr = Ring(kb, 2, [128, 8, 512], BF16); hgr = Ring(kb, 2, [128, 8, 512], BF16)
            pm = [Ring(kb, 3, [128, 512], F32, kind="ps") for _ in range(2)]
            pT = Ring(kb, 2, [128, 8, 128], BF16, kind="ps")
            items = []
            for (tok0, w, ms) in self.groups(need_ctx):
                for j in range(w // 128):
                    items.append((tok0, w, ms, j))
            C = [dict() for _ in items]

            self.prep_begin()

            def s_mm(i):
                tok0, w, ms, j = items[i]
                if prep_wout:
                    self.prep_tick(1)
                if j == 0:
                    cg, t_cg = cgr.next()
                    kb.dma("sp", [(cg[:, :, 0:w], self.catT[:, :, tok0:tok0 + w])], writes=[t_cg])
                    self._cg = (cg, t_cg)
                    self._hg = hgr.next()
                cg, t_cg = self._cg
                C[i]["hg"] = self._hg
                r0 = tok0 + j * 128
                xt, t_x = xr.next()
                kb.dma("sp", [(xt[:], xsrc[r0:r0 + 128, :])], writes=[t_x])
                hs, ths = [], []
                for nh in range(2):
                    p, t_p = pm[nh].next()
                    for k in range(8):
                        kb.op("pe", lambda e: e.matmul(p[:, :], cg[:, k, j * 128:(j + 1) * 128], wo[:, k, nh * 512:(nh + 1) * 512], start=(k == 0), stop=(k == 7)),
                              reads=[t_cg, t_wo], writes=[t_p])
                    hs.append(p[:, :]); ths.append(t_p)
                C[i].update(hs=hs, ths=ths, xt=xt, t_x=t_x)

            def s_p1(i):
                c = C[i]
                c["st"], c["t_st"] = self.pn1(c["hs"], c["ths"], R)

            def s_p2(i):
                c = C[i]
                tok0, w, ms, j = items[i]
                r0 = tok0 + j * 128
                c["xn"], c["t_xn"] = self.pn2(c["hs"], c["ths"], c["st"], c["t_st"], c["xt"], c["t_x"], gg, t_gg, ms, R)
                kb.dma("sp", [(xdst[r0:r0 + 128, :], c["xn"][:])], reads=[c["t_xn"]])

            def s_n1(i):
                c = C[i]
                c["st2"], c["t_st2"] = self.nm1(c["xn"], c["t_xn"], R)

            def s_n2(i):
                c = C[i]
                tok0, w, ms, j = items[i]
                r0 = tok0 + j * 128
                c["hb"], c["t_h"] = self.nm2(c["xn"], c["t_xn"], c["st2"], c["t_st2"], gsc2, sh2, t_g2, t_s2, ms, R)
                if l == 1:
                    kb.dma("sp", [(self.h2tm[r0:r0 + 128, :], c["hb"][:])], reads=[c["t_h"]])

            def s_t(i):
                c = C[i]
                tok0, w, ms, j = items[i]
                hg, t_hg = c["hg"]
                p, t_p = pT.next()
                for k in range(8):
                    kb.op("pe", lambda e: e.transpose(out=p[:, k, :], in_=c["hb"][:, k * 128:(k + 1) * 128], identity=self.ident_bf[:]),
                          reads=[c["t_h"], self.t_ident], writes=[t_p])
                kb.op("act", lambda e: e.copy(out=hg[:, :, j * 128:(j + 1) * 128], in_=p[:]), reads=[t_p], writes=[t_hg])
                if j == w // 128 - 1:
                    kb.dma("sp", [(self.h2T[:, :, tok0:tok0 + w], hg[:, :, 0:w])], reads=[t_hg])
                C[i].clear()
            self.run_pipeline(len(items), [s_mm, s_p1, s_p2, s_n1, s_n2, s_t])
            self.prep_flush()

    def phase_ffn(self, l, xsrc, xdst, moe, final):
        kb, I = self.kb, self.I
        FG = 512
        NFC = FG // 128
        for b in range(NB):
            tok0 = b * TPB + (CTX if moe else 0)
            TG = SEQ if moe else TPB
            ntile = TG // 128
            with kb.phase():
                hT = kb.sb([128, 8, TG], BF16); t_hT = Tok()
                kb.dma("sp", [(hT[:, k, :], self.h2T[:, k, tok0:tok0 + TG]) for k in range(8)], writes=[t_hT])
                acc = kb.sb([128, ntile, D], F32); t_acc = [Tok() for _ in range(ntile)]
                gates = None
                if moe:
                    gates = kb.sb([128, ntile, 8], F32); t_gt = Tok()
                    with kb.phase():
                        rb = kb.sb([128, 8, 8], BF16); t_rb = Tok()
                        kb.dma("pool", [(rb[:], I["moe_router"][0].rearrange("(k p) e -> p k e", p=128))], writes=[t_rb])
                        pl = Ring(kb, 2, [128, 8], F32, kind="ps")
                        wk = Ring(kb, 2, [128, 6, 8], F32); sm = Ring(kb, 2, [128, 8], F32)
                        for j in range(ntile):
                            p, t_p = pl.next()
                            for k in range(8):
                                kb.op("pe", lambda e: e.matmul(p[:, :], hT[:, k, j * 128:(j + 1) * 128], rb[:, k, :], start=(k == 0), stop=(k == 7)),
                                      reads=[t_hT, t_rb], writes=[t_p])
                            w_, t_w = wk.next(); s_, t_s = sm.next()
                            T = [t_w, t_s]
                            kb.op("dve", lambda e: e.tensor_reduce(out=s_[:, 0:1], in_=p[:, :], op=ALU.max, axis=AX.X), reads=[t_p], writes=T)
                            kb.op("dve", lambda e: e.tensor_scalar(out=s_[:, 1:2], in0=s_[:, 0:1], scalar1=-1.0, scalar2=None, op0=ALU.mult), reads=T, writes=T)
                            kb.op("act", lambda e: e.activation(out=w_[:, 0, :], in_=p[:, :], func=AF.Exp, bias=s_[:, 1:2]), reads=[t_p] + T, writes=T)
                            kb.op("dve", lambda e: e.tensor_scalar(out=w_[:, 1, :], in0=w_[:, 0, :], scalar1=1.0, scalar2=None, op0=ALU.is_lt), reads=T, writes=T)
                            kb.op("dve", lambda e: e.tensor_tensor(out=w_[:, 2, :], in0=w_[:, 0, :], in1=w_[:, 1, :], op=ALU.mult), reads=T, writes=T)
                            kb.op("dve", lambda e: e.tensor_reduce(out=s_[:, 2:3], in_=w_[:, 2, :], op=ALU.max, axis=AX.X), reads=T, writes=T)
                            kb.op("dve", lambda e: e.tensor_scalar(out=s_[:, 3:4], in0=s_[:, 2:3], scalar1=1.0, scalar2=None, op0=ALU.add), reads=T, writes=T)
                            kb.op("dve", lambda e: e.reciprocal(out=s_[:, 4:5], in_=s_[:, 3:4]), reads=T, writes=T)
                            kb.op("dve", lambda e: e.tensor_scalar(out=w_[:, 3, :], in0=w_[:, 0, :], scalar1=s_[:, 2:3], scalar2=None, op0=ALU.is_ge), reads=T, writes=T)
                            kb.op("dve", lambda e: e.tensor_tensor(out=w_[:, 4, :], in0=w_[:, 0, :], in1=w_[:, 3, :], op=ALU.mult), reads=T, writes=T)
                            kb.op("dve", lambda e: e.tensor_scalar(out=gates[:, j, :], in0=w_[:, 4, :], scalar1=s_[:, 4:5], scalar2=None, op0=ALU.mult), reads=T, writes=[t_gt])
                with kb.phase():
                    wgr = Ring(kb, 2, [128, 8, FG], BF16); wur = Ring(kb, 2, [128, 8, FG], BF16); wdr = Ring(kb, 2, [128, NFC, D], BF16)
                    actr = Ring(kb, 2, [128, NFC, TG], BF16)
                    sgr = Ring(kb, 3, [128, 512], BF16); gtm = Ring(kb, 2, [128, 512], F32) if moe else None
                    evr = Ring(kb, 2, [128, 512], F32)
                    pG = Ring(kb, 2, [128, 512], F32, kind="ps"); pU = Ring(kb, 2, [128, 512], F32, kind="ps")
                    pD = Ring(kb, 3, [128, 512], F32, kind="ps"); pB = Ring(kb, 1, [128, 4, 128], F32, kind="ps")
                    gbr = Ring(kb, 2, [128, TG], BF16) if moe else None
                    gxr = Ring(kb, 2, [128, 128], BF16) if moe else None
                    experts = range(N_EXP) if moe else [0]
                    F = F_EXPERT if moe else F_DENSE
                    state = {"first": True, "nbank": 0}
                    pending = []

                    def emit_down(at, t_at, wd_, t_wd, nfc, tiles, first):
                        for j in tiles:
                            for nh in range(2):
                                p, t_p = pD.next()
                                for fc in range(nfc):
                                    kb.op("pe", lambda e: e.matmul(p[:, :], at[:, fc, j * 128:(j + 1) * 128], wd_[:, fc, nh * 512:(nh + 1) * 512],
                                                                   start=(fc == 0), stop=(fc == nfc - 1)), reads=[t_at, t_wd], writes=[t_p])
                                dst = acc[:, j, nh * 512:(nh + 1) * 512]
                                if first:
                                    kb.op("act", lambda e: e.copy(out=dst, in_=p[:, :]), reads=[t_p], writes=[t_acc[j]])
                                else:
                                    kb.op("dve", lambda e: e.tensor_tensor(out=dst, in0=p[:, :], in1=dst, op=ALU.add), reads=[t_p, t_acc[j]], writes=[t_acc[j]])
                                state["nbank"] += 1

                    for ex in experts:
                        if moe:
                            Wg, Wu, Wd = I["moe_w_gate"][0, ex], I["moe_w_up"][0, ex], I["moe_w_down"][0, ex]
                            gb, t_gb = gbr.next()
                            for q4 in range(ntile // 4):
                                p, t_p = pB.next()
                                for i in range(4):
                                    j = q4 * 4 + i
                                    gx, t_gx = gxr.next()
                                    kb.op("dve", lambda e: e.tensor_copy(out=gx[:], in_=gates[:, j, ex:ex + 1].broadcast_to([128, 128])), reads=[t_gt], writes=[t_gx])
                                    kb.op("pe", lambda e: e.matmul(p[:, i, :], gx[:], self.ident_bf[:], start=(i == 0), stop=True, skip_group_check=True), reads=[t_gx, self.t_ident], writes=[t_p])
                                kb.op("act", lambda e: e.copy(out=gb[:, q4 * 512:(q4 + 1) * 512], in_=p[:].rearrange("p a b -> p (a b)")), reads=[t_p], writes=[t_gb])
                        else:
                            Wg, Wu, Wd = I["ffn_w_gate"][0], I["ffn_w_up"][0], I["ffn_w_down"][0]
                        wgv = Wg.rearrange("(k p) n -> p k n", p=128); wuv = Wu.rearrange("(k p) n -> p k n", p=128)
                        wdv = Wd.rearrange("(c p) n -> p c n", p=128)
                        f0 = 0
                        while f0 < F:
                            fw = min(FG, F - f0)
                            nfc = fw // 128
                            wg_, t_wg = wgr.next(); wu_, t_wu = wur.next(); wd_, t_wd = wdr.next()
                            kb.dma("pool", [(wg_[:, 0:4, 0:fw], wgv[:, 0:4, f0:f0 + fw]), (wg_[:, 4:8, 0:fw], wgv[:, 4:8, f0:f0 + fw])], writes=[t_wg])
                            kb.dma("pool", [(wu_[:, 0:4, 0:fw], wuv[:, 0:4, f0:f0 + fw]), (wu_[:, 4:8, 0:fw], wuv[:, 4:8, f0:f0 + fw])], writes=[t_wu])
                            kb.dma("pool", [(wd_[:, 0:nfc, :], wdv[:, f0 // 128:f0 // 128 + nfc, :])], writes=[t_wd])
                            at, t_at = actr.next()
                            c0 = 0
                            while c0 < TG:
                                n = min(512, TG - c0)
                                for fc in range(nfc):
                                    g_, t_g = pG.next(); u_, t_u = pU.next()
                                    for k in range(8):
                                        kb.op("pe", lambda e: e.matmul(g_[:, 0:n], wg_[:, k, fc * 128:(fc + 1) * 128], hT[:, k, c0:c0 + n], start=(k == 0), stop=(k == 7)),
                                              reads=[t_wg, t_hT], writes=[t_g])
                                    for k in range(8):
                                        kb.op("pe", lambda e: e.matmul(u_[:, 0:n], wu_[:, k, fc * 128:(fc + 1) * 128], hT[:, k, c0:c0 + n], start=(k == 0), stop=(k == 7)),
                                              reads=[t_wu, t_hT], writes=[t_u])
                                    sg_, t_sg = sgr.next()
                                    kb.op("act", lambda e: e.activation(out=sg_[:, 0:n], in_=g_[:, 0:n], func=AF.Silu), reads=[t_g], writes=[t_sg])
                                    if moe:
                                        tm, t_tm = gtm.next()
                                        kb.op("dve", lambda e: e.tensor_tensor(out=tm[:, 0:n], in0=u_[:, 0:n], in1=gb[:, c0:c0 + n], op=ALU.mult), reads=[t_u, t_gb], writes=[t_tm])
                                        kb.op("dve", lambda e: e.tensor_tensor(out=at[:, fc, c0:c0 + n], in0=tm[:, 0:n], in1=sg_[:, 0:n], op=ALU.mult), reads=[t_tm, t_sg], writes=[t_at])
                                    else:
                                        kb.op("dve", lambda e: e.tensor_tensor(out=at[:, fc, c0:c0 + n], in0=u_[:, 0:n], in1=sg_[:, 0:n], op=ALU.mult), reads=[t_u, t_sg], writes=[t_at])
                                while pending:
                                    pending.pop(0)()
                                tiles = list(range(c0 // 128, (c0 + n) // 128))
                                pending.append(lambda at=at, t_at=t_at, wd_=wd_, t_wd=t_wd, nfc=nfc, tiles=tiles, first=state["first"]:
                                               emit_down(at, t_at, wd_, t_wd, nfc, tiles, first))
                                c0 += n
                            state["first"] = False
                            f0 += fw
                    while pending:
                        pending.pop(0)()
                with kb.phase():
                    gg, _, t_gg, _ = self.load_mod_tiles(l, 5, None, "norm_ffn_post", plus_one=False)
                    R = {"tmp": Ring(kb, 3, [128, D], F32), "stat2": Ring(kb, 8, [128, 8], F32), "junk2": Ring(kb, 1, [128, 512], BF16),
                         "xn": Ring(kb, 3, [128, D], F32)}
                    xr = Ring(kb, 4, [128, D], F32)
                    C = [dict() for _ in range(ntile)]

                    def f_load(j):
                        xt, t_x = xr.next()
                        kb.dma("sp", [(xt[:], xsrc[tok0 + j * 128:tok0 + (j + 1) * 128, :])], writes=[t_x])
                        C[j].update(xt=xt, t_x=t_x, hs=[acc[:, j, 0:512], acc[:, j, 512:1024]], ths=[t_acc[j], t_acc[j]])

                    def f_p1(j):
                        C[j]["st"], C[j]["t_st"] = self.pn1(C[j]["hs"], C[j]["ths"], R)

                    def f_p2(j):
                        c = C[j]
                        is_ctx = (not moe) and j < 2
                        ms = 2 if is_ctx else b
                        xn, t_xn = self.pn2(c["hs"], c["ths"], c["st"], c["t_st"], c["xt"], c["t_x"], gg, t_gg, ms, R)
                        if final:
                            o0 = b * SEQ + j * 128
                            kb.dma("sp", [(xdst[o0:o0 + 128, :], xn[:])], reads=[t_xn])
                        else:
                            r0 = tok0 + j * 128
                            kb.dma("sp", [(xdst[r0:r0 + 128, :], xn[:])], reads=[t_xn])
                    self.run_pipeline(ntile, [f_load, f_p1, f_p2])

    def moe_declare(self):
        sc = self.scratch
        self.h2tm = sc("h2tm", [NTOK, D], BF16)
        nrow = N_EXP * 7 * 128
        self.Wg_s = sc("Wg_s", [nrow, 8 * 512], BF16); self.Wu_s = sc("Wu_s", [nrow, 8 * 512], BF16)
        self.Wd_s = sc("Wd_s", [nrow, 4 * D], BF16)
        self.hsorted = sc("hsorted", [MOE_SLOTS, D], BF16)
        self.ysorted = sc("ysorted", [MOE_SLOTS, D], F32)

    def moe_prep_gen(self, rings):
        kb, I = self.kb, self.I
        inflight = self.prep_inflight
        for ex in range(N_EXP):
            wgv = I["moe_w_gate"][0, ex].rearrange("(k p) n -> p k n", p=128)
            wuv = I["moe_w_up"][0, ex].rearrange("(k p) n -> p k n", p=128)
            wdv = I["moe_w_down"][0, ex].rearrange("(c p) n -> p c n", p=128)
            for fg in range(7):
                r0 = (ex * 7 + fg) * 128
                for kind, src, dst in ((0, wgv, self.Wg_s), (0, wuv, self.Wu_s), (1, wdv, self.Wd_s)):
                    t, tk = self.prep_rings_cur[0][kind].next()
                    if kind == 0:
                        kb.dma("pool", [(t[:, 0:4, :], src[:, 0:4, fg * 512:(fg + 1) * 512]), (t[:, 4:8, :], src[:, 4:8, fg * 512:(fg + 1) * 512])], writes=[tk])
                        flat = t[:].rearrange("p k n -> p (k n)")
                    else:
                        kb.dma("pool", [(t[:], src[:, fg * 4:(fg + 1) * 4, :])], writes=[tk])
                        flat = t[:].rearrange("p c n -> p (c n)")
                    inflight.append((dst[r0:r0 + 128, :], flat, tk))
                    if len(inflight) > 2:
                        d_, f_, k_ = inflight.pop(0)
                        kb.dma("pool", [(d_, f_)], reads=[k_])
                    yield
        self.prep_flush()
        yield

    def prep_begin(self):
        if self.prep is not None:
            self.prep_rings_cur[0] = self.prep_rings()

    def prep_tick(self, k=1):
        for _ in range(k):
            if self.prep is not None and next(self.prep, "done") == "done":
                self.prep = None

    def prep_flush(self):
        while self.prep_inflight:
            d_, f_, k_ = self.prep_inflight.pop(0)
            self.kb.dma("pool", [(d_, f_)], reads=[k_])

    def prep_rings(self):
        kb = self.kb
        return [Ring(kb, 3, [128, 8, 512], BF16), Ring(kb, 2, [128, 4, D], BF16)]

    def phase_moe_prep(self):
        kb = self.kb
        if self.prep is None:
            return
        with kb.phase():
            rings = self.prep_rings()
            self.prep_rings_cur[0] = rings
            for _ in self.prep:
                pass
            self.prep_flush()
            self.prep = None

    def phase_moe_sparse(self, l, xsrc, xdst):
        kb, I = self.kb, self.I
        NT = NB * SEQ // 128
        BIG = 1.0e6
        MAGIC = 12582912.0
        lat_tok0 = lambda j: (j // 16) * TPB + CTX + (j % 16) * 128
        with kb.phase():
            glo = kb.sb([128, NT], F32); ghi = kb.sb([128, NT], F32)
            ilo = kb.sb([128, NT], I32); ihi = kb.sb([128, NT], I32)
            widx = kb.sb([128, MOE_TILES, 7], I32)
            t_rt = Tok()
            with kb.phase():
                rb = kb.sb([128, 8, 8], BF16); t_rb = Tok()
                kb.dma("pool", [(rb[:], I["moe_router"][0].rearrange("(k p) e -> p k e", p=128))], writes=[t_rb])
                tri = kb.sb([128, 2, 128], BF16); io7 = kb.sb([128, 7], F32); thr = kb.sb([128, MOE_TILES], F32)
                kb.dma("sp", [(tri[:], I["moe_tri"][:, :, :]), (io7[:], I["moe_iota"][:, :]), (thr[:], I["moe_thr"][:, :])], writes=[t_rb])
                hT = kb.sb([128, 8, NB * SEQ], BF16); t_hT = Tok()
                kb.dma("sp", [(hT[:, k, b * SEQ:(b + 1) * SEQ], self.h2T[:, k, b * TPB + CTX:(b + 1) * TPB]) for k in range(8) for b in range(NB)], writes=[t_hT])
                gates = kb.sb([128, NT, 8], F32); maskf = kb.sb([128, NT, 8], F32); maskb = kb.sb([128, NT, 8], BF16)
                rank = kb.sb([128, NT, 8], F32)
                t_g, t_m, t_rk = Tok(), Tok(), Tok()
                pl = Ring(kb, 2, [128, 8], F32, kind="ps")
                wk = Ring(kb, 2, [128, 6, 8], F32); sm = Ring(kb, 2, [128, 8], F32)
                for j in range(NT):
                    p, t_p = pl.next()
                    for k in range(8):
                        kb.op("pe", lambda e: e.matmul(p[:, :], hT[:, k, j * 128:(j + 1) * 128], rb[:, k, :], start=(k == 0), stop=(k == 7)),
                              reads=[t_hT, t_rb], writes=[t_p])
                    w_, t_w = wk.next(); s_, t_s = sm.next()
                    T = [t_w, t_s]
                    kb.op("dve", lambda e: e.tensor_reduce(out=s_[:, 0:1], in_=p[:, :], op=ALU.max, axis=AX.X), reads=[t_p], writes=T)
                    kb.op("dve", lambda e: e.tensor_scalar(out=s_[:, 1:2], in0=s_[:, 0:1], scalar1=-1.0, scalar2=None, op0=ALU.mult), reads=T, writes=T)
                    kb.op("act", lambda e: e.activation(out=w_[:, 0, :], in_=p[:, :], func=AF.Exp, bias=s_[:, 1:2]), reads=[t_p] + T, writes=T)
                    kb.op("dve", lambda e: e.tensor_scalar(out=w_[:, 1, :], in0=w_[:, 0, :], scalar1=1.0, scalar2=None, op0=ALU.is_lt), reads=T, writes=T)
                    kb.op("dve", lambda e: e.tensor_tensor(out=w_[:, 2, :], in0=w_[:, 0, :], in1=w_[:, 1, :], op=ALU.mult), reads=T, writes=T)
                    kb.op("dve", lambda e: e.tensor_reduce(out=s_[:, 2:3], in_=w_[:, 2, :], op=ALU.max, axis=AX.X), reads=T, writes=T)
                    kb.op("dve", lambda e: e.tensor_scalar(out=s_[:, 3:4], in0=s_[:, 2:3], scalar1=1.0, scalar2=None, op0=ALU.add), reads=T, writes=T)
                    kb.op("dve", lambda e: e.reciprocal(out=s_[:, 4:5], in_=s_[:, 3:4]), reads=T, writes=T)
                    kb.op("dve", lambda e: e.tensor_scalar(out=maskf[:, j, :], in0=w_[:, 0, :], scalar1=s_[:, 2:3], scalar2=None, op0=ALU.is_ge), reads=T, writes=[t_m])
                    kb.op("dve", lambda e: e.tensor_copy(out=maskb[:, j, :], in_=maskf[:, j, :]), reads=[t_m], writes=[t_m])
                    kb.op("dve", lambda e: e.tensor_tensor(out=w_[:, 4, :], in0=w_[:, 0, :], in1=maskf[:, j, :], op=ALU.mult), reads=T + [t_m], writes=T)
                    kb.op("dve", lambda e: e.tensor_scalar(out=gates[:, j, :], in0=w_[:, 4, :], scalar1=s_[:, 4:5], scalar2=None, op0=ALU.mult), reads=T, writes=[t_g])
                prk = Ring(kb, 2, [128, 8], F32, kind="ps")
                for j in range(NT + 1):
                    p, t_p = prk.next()
                    n = 0
                    for i in range(min(j, NT)):
                        kb.op("pe", lambda e: e.matmul(p[:, :], tri[:, 1, :], maskb[:, i, :], start=(n == 0), stop=(j == NT and i == NT - 1)),
                              reads=[t_m, t_rb], writes=[t_p])
                        n += 1
                    if j < NT:
                        kb.op("pe", lambda e: e.matmul(p[:, :], tri[:, 0, :], maskb[:, j, :], start=(n == 0), stop=True), reads=[t_m, t_rb], writes=[t_p])
                        kb.op("act", lambda e: e.copy(out=rank[:, j, :], in_=p[:, :]), reads=[t_p], writes=[t_rk])
                    else:
                        tot = kb.sb([128, 8], F32)
                        kb.op("act", lambda e: e.copy(out=tot[:], in_=p[:, :]), reads=[t_p], writes=[t_rk])
                T = [t_rk]
                ts = lambda o, x, s1, o0: kb.op("dve", lambda e: e.tensor_scalar(out=o, in0=x, scalar1=s1, scalar2=None, op0=o0), reads=T + [t_m, t_g, t_rb], writes=T)
                tt = lambda o, x, y, op: kb.op("dve", lambda e: e.tensor_tensor(out=o, in0=x, in1=y, op=op), reads=T + [t_m, t_g, t_rb], writes=T)
                red = lambda o, x, op: kb.op("dve", lambda e: e.tensor_reduce(out=o, in_=x, op=op, axis=AX.X), reads=T, writes=T)
                pad = kb.sb([128, 8], F32); incl = kb.sb([128, 8], F32); base = kb.sb([128, 8], F32)
                ts(pad[:], tot[:], 511.0, ALU.add); ts(pad[:], pad[:], 1.0 / 512, ALU.mult)
                ts(pad[:], pad[:], -0.5 + 1.0 / 1024, ALU.add); ts(pad[:], pad[:], MAGIC, ALU.add); ts(pad[:], pad[:], MAGIC, ALU.subtract)
                ts(pad[:], pad[:], 512.0, ALU.mult)
                kb.op("dve", lambda e: e.tensor_copy(out=incl[:, 0:1], in_=pad[:, 0:1]), reads=T, writes=T)
                for e_ in range(1, 8):
                    tt(incl[:, e_:e_ + 1], incl[:, e_ - 1:e_], pad[:, e_:e_ + 1], ALU.add)
                tt(base[:], incl[:], pad[:], ALU.subtract)
                slot = kb.sb([128, NT, 8], F32); v1 = kb.sb([128, NT, 8], F32); v2 = kb.sb([128, NT, 8], F32)
                slo = kb.sb([128, NT], F32); shi = kb.sb([128, NT], F32)
                tt(slot[:], rank[:], base[:].unsqueeze(1).broadcast_to([128, NT, 8]), ALU.add)
                tt(v2[:], slot[:], maskf[:], ALU.mult)
                ts(v1[:], maskf[:], -BIG, ALU.mult); ts(v1[:], v1[:], BIG, ALU.add); tt(v1[:], v1[:], v2[:], ALU.add)
                red(slo[:], v1[:], ALU.min)
                tt(v1[:], v2[:], maskf[:], ALU.add); ts(v1[:], v1[:], -1.0, ALU.add)
                red(shi[:], v1[:], ALU.max)
                for sl_, g_ in ((slo, glo), (shi, ghi)):
                    tt(v1[:], slot[:], sl_[:].unsqueeze(2).broadcast_to([128, NT, 8]), ALU.is_equal)
                    tt(v1[:], v1[:], gates[:], ALU.mult)
                    kb.op("dve", lambda e: e.tensor_reduce(out=g_[:], in_=v1[:], op=ALU.add, axis=AX.X), reads=T, writes=T + [t_rt])
                kb.op("dve", lambda e: e.tensor_copy(out=ilo[:], in_=slo[:]), reads=T, writes=[t_rt])
                kb.op("dve", lambda e: e.tensor_copy(out=ihi[:], in_=shi[:]), reads=T, writes=[t_rt])
                cmp_ = kb.sb([128, MOE_TILES, 8], F32); cnt = kb.sb([128, MOE_TILES], F32); wf = kb.sb([128, MOE_TILES, 7], F32)
                tt(cmp_[:], incl[:].unsqueeze(1).broadcast_to([128, MOE_TILES, 8]), thr[:].unsqueeze(2).broadcast_to([128, MOE_TILES, 8]), ALU.is_le)
                red(cnt[:], cmp_[:], ALU.add)
                ts(cnt[:], cnt[:], 7.0, ALU.min); ts(cnt[:], cnt[:], 896.0, ALU.mult)
                tt(wf[:], cnt[:].unsqueeze(2).broadcast_to([128, MOE_TILES, 7]), io7[:].unsqueeze(1).broadcast_to([128, MOE_TILES, 7]), ALU.add)
                kb.op("dve", lambda e: e.tensor_copy(out=widx[:], in_=wf[:]), reads=T, writes=[t_rt])
            with kb.phase():
                t_fill = Tok()
                hr = Ring(kb, 3, [128, D], BF16); icr = Ring(kb, 4, [128, 1], I32)
                for j in range(NT):
                    hb, t_h = hr.next()
                    r0 = lat_tok0(j)
                    kb.dma("sp", [(hb[:], self.h2tm[r0:r0 + 128, :])], writes=[t_h])
                    for ix in (ilo, ihi):
                        kb.dma_custom("pool", lambda g: g.indirect_dma_start(out=self.hsorted[:, :], out_offset=bass.IndirectOffsetOnAxis(ap=ix[:, j:j + 1], axis=0),
                                                                             in_=hb[:, :], in_offset=None, bounds_check=None),
                                      reads=[t_h, t_rt, t_fill])
            with kb.phase():
                hsr = Ring(kb, 4, [128, D], BF16); hTr = Ring(kb, 2, [128, 8, MOE_TS], BF16)
                wgr = Ring(kb, 3, [128, 8, 512], BF16); wur = Ring(kb, 3, [128, 8, 512], BF16); wdr = Ring(kb, 3, [128, 4, D], BF16)
                atr = Ring(kb, 2, [128, 4, MOE_TS], BF16); sgr = Ring(kb, 3, [128, 512], BF16)
                accr = Ring(kb, 2, [128, 4, D], F32); icr = Ring(kb, 8, [128, 1], I32)
                pT = Ring(kb, 1, [128, 8, 128], BF16, kind="ps")
                pG = Ring(kb, 2, [128, 512], F32, kind="ps"); pU = Ring(kb, 2, [128, 512], F32, kind="ps"); pD = Ring(kb, 3, [128, 512], F32, kind="ps")
                pending = []

                def emit_down(at, t_at, wd_, t_wd, acc, t_acc, first, last, i):
                    for sub in range(4):
                        for nh in range(2):
                            p, t_p = pD.next()
                            for fc in range(4):
                                kb.op("pe", lambda e: e.matmul(p[:, :], at[:, fc, sub * 128:(sub + 1) * 128], wd_[:, fc, nh * 512:(nh + 1) * 512], start=(fc == 0), stop=(fc == 3)),
                                      reads=[t_at, t_wd], writes=[t_p])
                            dst = acc[:, sub, nh * 512:(nh + 1) * 512]
                            if first:
                                kb.op("act", lambda e: e.copy(out=dst, in_=p[:, :]), reads=[t_p], writes=[t_acc])
                            else:
                                kb.op("dve", lambda e: e.tensor_tensor(out=dst, in0=p[:, :], in1=dst, op=ALU.add), reads=[t_p, t_acc], writes=[t_acc])
                    if last:
                        kb.dma("sp", [(self.ysorted[i * MOE_TS + sub * 128:i * MOE_TS + (sub + 1) * 128, :], acc[:, sub, :]) for sub in range(4)], reads=[t_acc])

                for i in range(MOE_TILES):
                    hT, t_hT = hTr.next()
                    for sub in range(4):
                        hs, t_hs = hsr.next()
                        r0 = i * MOE_TS + sub * 128
                        kb.dma("sp", [(hs[:], self.hsorted[r0:r0 + 128, :])], writes=[t_hs])
                        p, t_p = pT.next()
                        for k in range(8):
                            kb.op("pe", lambda e: e.transpose(out=p[:, k, :], in_=hs[:, k * 128:(k + 1) * 128], identity=self.ident_bf[:]),
                                  reads=[t_hs, self.t_ident], writes=[t_p])
                        kb.op("act", lambda e: e.copy(out=hT[:, :, sub * 128:(sub + 1) * 128], in_=p[:]), reads=[t_p], writes=[t_hT])
                    acc, t_acc = accr.next()
                    for fg in range(7):
                        wg_, t_wg = wgr.next(); wu_, t_wu = wur.next(); wd_, t_wd = wdr.next()
                        for (wt, tw, src) in ((wg_, t_wg, self.Wg_s), (wu_, t_wu, self.Wu_s), (wd_, t_wd, self.Wd_s)):
                            flat = wt[:].rearrange("p a n -> p (a n)")
                            kb.dma_custom("pool", lambda g: g.indirect_dma_start(out=flat, out_offset=None, in_=src[:, :],
                                                                                 in_offset=bass.IndirectOffsetOnAxis(ap=widx[:, i, fg:fg + 1], axis=0),
                                                                                 bounds_check=None),
                                          reads=[t_rt], writes=[tw])
                        at, t_at = atr.next()
                        for fc in range(4):
                            g_, t_g = pG.next(); u_, t_u = pU.next()
                            for k in range(8):
                                kb.op("pe", lambda e: e.matmul(g_[:, :], wg_[:, k, fc * 128:(fc + 1) * 128], hT[:, k, :], start=(k == 0), stop=(k == 7)),
                                      reads=[t_wg, t_hT], writes=[t_g])
                            for k in range(8):
                                kb.op("pe", lambda e: e.matmul(u_[:, :], wu_[:, k, fc * 128:(fc + 1) * 128], hT[:, k, :], start=(k == 0), stop=(k == 7)),
                                      reads=[t_wu, t_hT], writes=[t_u])
                            sg_, t_sg = sgr.next()
                            kb.op("act", lambda e: e.activation(out=sg_[:], in_=g_[:, :], func=AF.Silu), reads=[t_g], writes=[t_sg])
                            kb.op("dve", lambda e: e.tensor_tensor(out=at[:, fc, :], in0=u_[:, :], in1=sg_[:], op=ALU.mult), reads=[t_u, t_sg], writes=[t_at])
                        while pending:
                            pending.pop(0)()
                        pending.append(lambda at=at, t_at=t_at, wd_=wd_, t_wd=t_wd, acc=acc, t_acc=t_acc, first=(fg == 0), last=(fg == 6), i=i:
                                       emit_down(at, t_at, wd_, t_wd, acc, t_acc, first, last, i))
                while pending:
                    pending.pop(0)()
            with kb.phase():
                gg, _, t_gg, _ = self.load_mod_tiles(l, 5, None, "norm_ffn_post", plus_one=False)
                R = {"tmp": Ring(kb, 2, [128, D], F32), "stat2": Ring(kb, 4, [128, 8], F32), "junk2": Ring(kb, 1, [128, 512], BF16),
                     "xn": Ring(kb, 2, [128, D], F32)}
                icr = Ring(kb, 4, [128, 1], I32)
                xr = Ring(kb, 4, [128, D], F32); ylr = Ring(kb, 3, [128, D], F32); yhr = Ring(kb, 3, [128, D], F32); mxr = Ring(kb, 4, [128, D], F32)
                R["stat2"] = Ring(kb, 8, [128, 8], F32); R["tmp"] = Ring(kb, 3, [128, D], F32); R["xn"] = Ring(kb, 3, [128, D], F32)
                C = [dict() for _ in range(NT)]

                def c_load(j):
                    r0 = lat_tok0(j)
                    xt, t_x = xr.next()
                    kb.dma("sp", [(xt[:], xsrc[r0:r0 + 128, :])], writes=[t_x])
                    yl, t_yl = ylr.next(); yh, t_yh = yhr.next()
                    for (yt, ty, ix) in ((yl, t_yl, ilo), (yh, t_yh, ihi)):
                        kb.dma_custom("pool", lambda g: g.indirect_dma_start(out=yt[:, :], out_offset=None, in_=self.ysorted[:, :],
                                                                             in_offset=bass.IndirectOffsetOnAxis(ap=ix[:, j:j + 1], axis=0),
                                                                             bounds_check=None),
                                      reads=[t_rt], writes=[ty])
                    C[j].update(xt=xt, t_x=t_x, yl=yl, t_yl=t_yl, yh=yh, t_yh=t_yh)

                def c_mix(j):
                    c = C[j]
                    mx, t_mx = mxr.next()
                    kb.op("dve", lambda e: e.tensor_scalar(out=mx[:], in0=c["yl"][:], scalar1=glo[:, j:j + 1], scalar2=None, op0=ALU.mult), reads=[c["t_yl"], t_rt], writes=[t_mx])
                    kb.op("dve", lambda e: e.scalar_tensor_tensor(out=mx[:], in0=c["yh"][:], scalar=ghi[:, j:j + 1], op0=ALU.mult, in1=mx[:], op1=ALU.add),
                          reads=[c["t_yh"], t_rt, t_mx], writes=[t_mx])
                    c.update(hs=[mx[:, 0:512], mx[:, 512:1024]], ths=[t_mx, t_mx])

                def c_p1(j):
                    C[j]["st"], C[j]["t_st"] = self.pn1(C[j]["hs"], C[j]["ths"], R)

                def c_p2(j):
                    c = C[j]
                    xn, t_xn = self.pn2(c["hs"], c["ths"], c["st"], c["t_st"], c["xt"], c["t_x"], gg, t_gg, j // 16, R)
                    kb.dma("sp", [(xdst[j * 128:(j + 1) * 128, :], xn[:])], reads=[t_xn])
                self.run_pipeline(NT, [c_load, c_mix, c_p1, c_p2])
def core_inputs(inputs, core, consts):
    b0 = core * NB
    m = {}
    xs = []
    for b in range(b0, b0 + NB):
        xs.append(inputs["ctx"][b]); xs.append(inputs["x"][b])
    m["xin"] = np.ascontiguousarray(np.concatenate(xs, axis=0), dtype=np.float32)
    cv = np.stack([inputs["c"][b0], inputs["c"][b0 + 1], inputs["c_ctx"]], axis=0)
    m["cT"] = np.ascontiguousarray(cv.reshape(3, 8, 128).transpose(2, 1, 0), dtype=np.float32)
    dup = lambda a: np.concatenate([a, a], axis=0)
    L = inputs["s5_a_re"].shape[0]
    par = np.zeros((L, 128, 3, 32), np.float32); sb = np.zeros((L, 128, 32, 2, 16), np.float32); sc = np.zeros((L, 128, 32, 2, 16), np.float32)
    for l in range(L):
        par[l, :, 0, :] = dup(inputs["s5_a_re"][l].reshape(32, 64).T)
        par[l, :, 1, :] = dup(inputs["s5_a_im"][l].reshape(32, 64).T)
        par[l, :, 2, :] = inputs["s5_log_dt"][l].reshape(1, 32)
        sb[l, :, :, 0, :] = dup(inputs["s5_b_re"][l].reshape(32, 64, 16).transpose(1, 0, 2))
        sb[l, :, :, 1, :] = dup(inputs["s5_b_im"][l].reshape(32, 64, 16).transpose(1, 0, 2))
        sc[l, :, :, 0, :] = dup(inputs["s5_c_re"][l].reshape(32, 16, 64).transpose(2, 0, 1))
        sc[l, :, :, 1, :] = dup(inputs["s5_c_im"][l].reshape(32, 16, 64).transpose(2, 0, 1))
    m["s5_par"], m["s5_b"], m["s5_c"] = par, sb, sc
    m["s5_dd"] = np.ascontiguousarray(inputs["s5_d"].reshape(L, 2, 128).transpose(0, 2, 1))
    return m


_CACHE = {}


def build_program():
    P = Prog()
    P.declare(); P.consts_sb()
    xin = P.I["xin"]
    P.phase_adaln(0)
    P.prep = P.moe_prep_gen(None)
    P.phase_win(0, xin, prep_win=True)
    P.phase_attn(0, True, prep_every=2); P.phase_fnet(0, True); P.phase_s5(0)
    P.phase_wout(0, xin, P.xA, True, prep_wout=True)
    P.phase_ffn(0, P.xA, P.xB, False, False)
    P.phase_adaln(1)
    P.phase_win(1, P.xB, prep_win=True)
    P.phase_attn(1, False, prep_every=2); P.phase_fnet(1, False); P.phase_s5(1)
    P.phase_wout(1, P.xB, P.xA, False)
    P.phase_moe_prep()
    P.phase_moe_sparse(1, P.xA, P.out)
    P.kb.barrier()
    return P


def kernel(**inputs):
    inputs = {k: np.asarray(v) for k, v in inputs.items()}
    if "P" not in _CACHE:
        _CACHE["P"] = build_program()
    P = _CACHE["P"]
    n_cores = 8
    shared = {}
    for k in P.I:
        if k in P.consts:
            shared[k] = P.consts[k]
        elif k in inputs:
            shared[k] = np.ascontiguousarray(inputs[k], dtype=np.float32)
    in_maps = []
    for core in range(n_cores):
        m = core_inputs(inputs, core, P.consts)
        for k, v in shared.items():
            if k not in m:
                m[k] = v
        in_maps.append(m)
    res = run_bass_kernel_spmd(P.kb.nc, in_maps, core_ids=list(range(n_cores)))
    outs = [np.asarray(r["out"], dtype=np.float32).reshape(NB, SEQ, D) for r in res.results]
    return np.concatenate(outs, axis=0)
```

```python
import contextlib, math
import numpy as np
import ml_dtypes
import concourse.bass as bass
import concourse.mybir as mybir
from concourse.bass_utils import run_bass_kernel_spmd

F32 = mybir.dt.float32
BF16 = mybir.dt.bfloat16
I32 = mybir.dt.int32
AF = mybir.ActivationFunctionType
ALU = mybir.AluOpType
AX = mybir.AxisListType
NPBF = ml_dtypes.bfloat16

SEM_LIMIT = 30000
EPS = 1e-6
D = 1024
NB = 2
CTX = 256
SEQ = 2048
TPB = CTX + SEQ
NTOK = NB * TPB
NTILE = NTOK // 128
TILES_PB = TPB // 128
DEPTH = 2
F_DENSE = 2816
F_EXPERT = 3584
N_EXP = 8
MOE_TS = 512
MOE_TILES = (2 * NB * SEQ + N_EXP * (MOE_TS - 1)) // MOE_TS + 1
MOE_SLOTS = MOE_TILES * MOE_TS


class Tok:
    __slots__ = ("name", "w", "r")

    def __init__(self, name=""):
        self.name = name
        self.w = None
        self.r = {}


class Eng:
    def __init__(self, kb, name, eng):
        self.kb, self.name, self.eng = kb, name, eng
        self.sem = None
        self.cnt = 0
        self.waited = {}

    def new_sem(self):
        self.sem = self.kb.es.enter_context(self.kb.nc.semaphore(f"s_{self.name}_{self.kb.nsem}"))
        self.kb.nsem += 1
        self.cnt = 0


class KB:
    def __init__(self):
        self.nc = bass.Bass("TRN2", target_bir_lowering=False)
        self.es = contextlib.ExitStack()
        self.nsem = 0
        nc = self.nc
        self.E = {}
        for name, eng in (("pe", nc.tensor), ("act", nc.scalar), ("dve", nc.vector),
                          ("pool", nc.gpsimd), ("sp", nc.sync)):
            e = Eng(self, name, eng)
            e.new_sem()
            self.E[name] = e
        self.dma_sems, self.dma_vals = [], []
        for i in range(64):
            self.dma_sems.append(self.es.enter_context(nc.semaphore(f"s_dma{i}")))
            self.dma_vals.append(0)
        self.dma_cursor = {"sp": 0, "pool": 0}
        self.nalloc = 0
        self.ninstr = 0
        self.stack = [self.es]
        self.dram_t = {}

    def sb(self, shape, dtype, name=None):
        self.nalloc += 1
        return self.stack[-1].enter_context(self.nc.sbuf_tensor(name or f"sb{self.nalloc}", list(shape), dtype))

    def ps(self, shape, dtype, name=None):
        self.nalloc += 1
        esz = 4 if dtype == F32 else 2
        n = int(np.prod(shape[1:]))
        assert n * esz <= 2048, shape
        t = self.stack[-1].enter_context(self.nc.psum_tensor(name or f"ps{self.nalloc}", [128, 2048 // esz], dtype))
        ap = t[0:shape[0], 0:n]
        if len(shape) > 2:
            names = [f"d{i}" for i in range(len(shape) - 1)]
            pat = "p (" + " ".join(names) + ") -> p " + " ".join(names)
            ap = ap.rearrange(pat, **{nm: int(v) for nm, v in zip(names[:-1], shape[1:-1])})
        return ap

    def dram(self, name, shape, dtype, kind="Internal"):
        t = self.nc.dram_tensor(name, list(shape), dtype, kind=kind)
        self.dram_t[name] = t
        return t.ap()

    @contextlib.contextmanager
    def phase(self):
        st = contextlib.ExitStack()
        self.stack.append(st)
        try:
            yield
        finally:
            self.barrier()
            self.stack.pop()
            st.close()

    def barrier(self):
        evs = [(e.sem, e.cnt) for e in self.E.values() if e.cnt > 0]
        evs += [(s, v) for s, v in zip(self.dma_sems, self.dma_vals) if v > 0]
        for e in self.E.values():
            for ev in evs:
                if ev[0] is e.sem:
                    continue
                self._wait(e, ev)

    def _wait(self, e, ev):
        if ev is None:
            return
        if isinstance(ev, list):
            for e_ in ev:
                self._wait(e, e_)
            return
        sem, val = ev
        k = id(sem)
        if e.waited.get(k, 0) >= val:
            return
        if e.name == "pe" and sem is e.sem:
            return
        e.eng.wait_ge(sem, val)
        e.waited[k] = val

    def _deps(self, e, reads, writes):
        for t in reads:
            self._wait(e, t.w)
        for t in writes:
            self._wait(e, t.w)
            for ev in t.r.values():
                self._wait(e, ev)

    def _commit(self, ev, reads, writes):
        for t in reads:
            for e_ in (ev if isinstance(ev, list) else [ev]):
                t.r[id(e_[0])] = e_
        for t in writes:
            t.w = ev
            t.r = {}

    def op(self, en, fn, reads=(), writes=()):
        e = self.E[en]
        if e.cnt >= SEM_LIMIT:
            e.new_sem()
        self._deps(e, reads, writes)
        ins = fn(e.eng)
        e.cnt += 1
        ins.then_inc(e.sem, 1)
        ev = (e.sem, e.cnt)
        self._commit(ev, reads, writes)
        self.ninstr += 1
        return ev

    def _next_dma_sem(self, qn):
        half = len(self.dma_sems) // 2
        c = self.dma_cursor[qn]
        self.dma_cursor[qn] = (c + 1) % half
        return c + (half if qn == "pool" else 0)

    def dma(self, qn, pairs, reads=(), writes=(), **kw):
        e = self.E[qn]
        self._deps(e, reads, writes)
        if qn == "pool" and len(pairs) > 1:
            evs = []
            for (o, i) in pairs:
                j = self._next_dma_sem(qn)
                sem = self.dma_sems[j]
                if self.dma_vals[j] > 0:
                    self._wait(e, (sem, self.dma_vals[j]))
                if self.dma_vals[j] > SEM_LIMIT:
                    raise RuntimeError("dma sem overflow")
                e.eng.dma_start(out=o, in_=i, **kw).then_inc(sem, 16)
                self.dma_vals[j] += 16
                self.ninstr += 1
                evs.append((sem, self.dma_vals[j]))
            self._commit(evs, reads, writes)
            return evs
        j = self._next_dma_sem(qn)
        sem = self.dma_sems[j]
        if self.dma_vals[j] > 0:
            self._wait(e, (sem, self.dma_vals[j]))
        if self.dma_vals[j] > SEM_LIMIT:
            raise RuntimeError("dma sem overflow")
        for (o, i) in pairs:
            e.eng.dma_start(out=o, in_=i, **kw).then_inc(sem, 16)
            self.dma_vals[j] += 16
            self.ninstr += 1
        ev = (sem, self.dma_vals[j])
        self._commit(ev, reads, writes)
        return ev

    def dma_custom(self, qn, fn, reads=(), writes=()):
        e = self.E[qn]
        self._deps(e, reads, writes)
        j = self._next_dma_sem(qn)
        sem = self.dma_sems[j]
        if self.dma_vals[j] > 0:
            self._wait(e, (sem, self.dma_vals[j]))
        if self.dma_vals[j] > SEM_LIMIT:
            raise RuntimeError("dma sem overflow")
        fn(e.eng).then_inc(sem, 16)
        self.dma_vals[j] += 16
        self.ninstr += 1
        ev = (sem, self.dma_vals[j])
        self._commit(ev, reads, writes)
        return ev

    def finish(self, toks):
        e = self.E["sp"]
        for t in toks:
            self._wait(e, t.w)
            for ev in t.r.values():
                self._wait(e, ev)


class Ring:
    def __init__(self, kb, n, shape, dtype, kind="sb", name="ring"):
        self.items = []
        for i in range(n):
            t = kb.sb(shape, dtype) if kind == "sb" else kb.ps(shape, dtype)
            self.items.append((t, Tok(f"{name}{i}")))
        self.i = 0

    def next(self):
        it = self.items[self.i]
        self.i = (self.i + 1) % len(self.items)
        return it

def host_consts():
    c = {}
    c["ident_bf"] = np.eye(128, dtype=np.float32).astype(NPBF)
    c["ident_f"] = np.eye(128, dtype=np.float32)
    n_freq = 16
    inv = 10000.0 ** (-np.arange(n_freq, dtype=np.float64) / n_freq)
    t = np.arange(SEQ)
    rows = (t // 64).astype(np.float64)
    cols = (t % 64).astype(np.float64)
    ang = np.stack([rows[:, None] * inv, cols[:, None] * inv], axis=1)
    ang = (np.stack([rows[:, None].astype(np.float32) * inv.astype(np.float32),
                     cols[:, None].astype(np.float32) * inv.astype(np.float32)], axis=1)).astype(np.float64)
    cos = np.cos(ang); sin = np.sin(ang)
    cos2 = np.stack([cos, cos], axis=2)
    sinS = np.stack([-sin, sin], axis=2)
    c["rope_cos"] = np.ascontiguousarray(cos2.reshape(16, 128, 64).transpose(1, 0, 2)).astype(np.float32)
    c["rope_sin"] = np.ascontiguousarray(sinS.reshape(16, 128, 64).transpose(1, 0, 2)).astype(np.float32)
    def dftm(L):
        t = np.arange(L)
        ph = (np.outer(t, t) % L).astype(np.float64) * (2 * np.pi / L)
        sc = 1.0 / math.sqrt(L * 64)
        return (np.cos(ph) * sc).astype(NPBF), (np.sin(ph) * sc).astype(NPBF)
    c["dft_c"], c["dft_s"] = dftm(SEQ)
    c["dftc_c"], c["dftc_s"] = dftm(CTX)
    t = np.arange(64)
    ph = np.outer(t, t) * (2 * np.pi / 64)
    cb = np.zeros((128, 2, 128), np.float32)
    for g in range(2):
        cb[g * 64:(g + 1) * 64, 0, g * 64:(g + 1) * 64] = np.cos(ph)
        cb[g * 64:(g + 1) * 64, 1, g * 64:(g + 1) * 64] = -np.sin(ph)
    c["cblk"] = cb
    k = np.arange(128)
    ws = np.zeros((128, 8, 240), np.float32)
    for r in range(8):
        for kk in range(128):
            if kk // 16 == r:
                ws[kk, r, 112 + kk % 16] = 1.0
    c["wsel"] = ws.astype(NPBF)
    sblk = (k // 16)[:, None]; tblk = (k // 16)[None, :]
    c["s5_mask"] = np.ascontiguousarray(np.stack([(tblk >= sblk), (tblk <= sblk)], axis=1).astype(np.float32))
    tri = np.zeros((128, 2, 128), np.float32)
    tri[:, 0, :] = (k[:, None] < k[None, :])
    tri[:, 1, :] = 1.0
    c["moe_tri"] = tri.astype(NPBF)
    c["moe_iota"] = np.ascontiguousarray((np.arange(7)[None, :] * 128 + k[:, None]).astype(np.float32))
    c["moe_thr"] = np.ascontiguousarray(np.broadcast_to((np.arange(MOE_TILES) * MOE_TS)[None, :], (128, MOE_TILES)).astype(np.float32))
    return c


class Prog:
    def __init__(self, debug=()):
        self.kb = KB()
        self.debug = set(debug)
        self.I = {}
        self.consts = host_consts()
        self.prep = None
        self.prep_rings_cur = [None]
        self.prep_inflight = []

    def inp(self, name, shape, dtype):
        ap = self.kb.dram(name, shape, dtype, kind="ExternalInput")
        self.I[name] = ap
        return ap

    def scratch(self, name, shape, dtype):
        kind = "ExternalOutput" if name in self.debug else "Internal"
        return self.kb.dram(name, shape, dtype, kind=kind)

    def declare(self):
        inp = self.inp
        inp("xin", [NTOK, D], F32)
        inp("cT", [128, 8, 3], F32)
        inp("ada_w", [DEPTH, D, 6 * D], F32)
        inp("ada_b", [DEPTH, 6 * D], F32)
        for n in ("norm_mix_pre", "norm_mix_post", "norm_ffn_pre", "norm_ffn_post"):
            inp(n, [DEPTH, D], F32)
        inp("w_in", [DEPTH, D, 2048], F32)
        inp("w_out", [DEPTH, D, D], F32)
        for n in ("diff_lq1", "diff_lk1", "diff_lq2", "diff_lk2"):
            inp(n, [DEPTH, 64], F32)
        inp("diff_subln", [DEPTH, 128], F32)
        inp("fnet_w", [DEPTH, 256, 256], F32)
        inp("s5_par", [DEPTH, 128, 3, 32], F32)
        inp("s5_b", [DEPTH, 128, 32, 2, 16], F32)
        inp("s5_c", [DEPTH, 128, 32, 2, 16], F32)
        inp("s5_dd", [DEPTH, 128, 2], F32)
        inp("s5_w_glu", [DEPTH, 256, 256], F32)
        inp("ffn_w_gate", [1, D, F_DENSE], F32); inp("ffn_w_up", [1, D, F_DENSE], F32); inp("ffn_w_down", [1, F_DENSE, D], F32)
        inp("moe_router", [1, D, N_EXP], F32)
        inp("moe_w_gate", [1, N_EXP, D, F_EXPERT], F32); inp("moe_w_up", [1, N_EXP, D, F_EXPERT], F32); inp("moe_w_down", [1, N_EXP, F_EXPERT, D], F32)
        for k, v in self.consts.items():
            inp(k, list(v.shape), BF16 if v.dtype == NPBF else F32)
        sc = self.scratch
        self.modD = [sc(f"modD{l}", [3, 6 * D], F32) for l in range(DEPTH)]
        self.qT = sc("qT", [128, 4, NTOK], BF16)
        self.kT = sc("kT", [128, 4, NTOK], BF16)
        self.vD = sc("vD", [128, 4, NTILE, 130], BF16)
        self.fD = sc("fD", [128, NTILE, 256], BF16)
        self.uT = sc("uT", [128, 2, NTOK], BF16)
        self.catT = sc("catT", [128, 8, NTOK], BF16)
        self.xA = sc("xA", [NTOK, D], F32)
        self.xB = sc("xB", [NTOK, D], F32)
        self.h2T = sc("h2T", [128, 8, NTOK], BF16)
        self.out = self.kb.dram("out", [NB * SEQ, D], F32, kind="ExternalOutput")
        self.moe_declare()

    def phase_adaln(self, l):
        kb, I = self.kb, self.I
        with kb.phase():
            cT = kb.sb([128, 8, 3], F32); sT = kb.sb([128, 8, 3], F32)
            bias = kb.sb([3, 6 * D], F32); mod = kb.sb([3, 6 * D], F32)
            t_c, t_s, t_b, t_m = Tok(), Tok(), Tok(), Tok()
            kb.dma("sp", [(cT[:], I["cT"][:, :, :])], writes=[t_c])
            kb.dma("sp", [(bias[:], I["ada_b"][l].partition_broadcast(3))], writes=[t_b])
            kb.op("act", lambda e: e.activation(out=sT[:], in_=cT[:], func=AF.Silu), reads=[t_c], writes=[t_s])
            wr = Ring(kb, 2, [128, 8, 512], F32, name="adaw")
            pr = Ring(kb, 2, [128, 512], F32, kind="ps", name="adap")
            wv = I["ada_w"][l].rearrange("(k p) n -> p k n", p=128)
            for nt in range(12):
                w, tw = wr.next()
                kb.dma("sp", [(w[:], wv[:, :, nt * 512:(nt + 1) * 512])], writes=[tw])
                p, tp = pr.next()
                for k in range(8):
                    kb.op("pe", lambda e: e.matmul(p[0:3, :], sT[:, k, :], w[:, k, :], start=(k == 0), stop=(k == 7)),
                          reads=[t_s, tw], writes=[tp])
                kb.op("dve", lambda e: e.tensor_tensor(out=mod[0:3, nt * 512:(nt + 1) * 512], in0=p[0:3, :],
                                                       in1=bias[0:3, nt * 512:(nt + 1) * 512], op=ALU.add),
                      reads=[tp, t_b], writes=[t_m])
            kb.dma("sp", [(self.modD[l][:, :], mod[0:3, :])], reads=[t_m])
            if l == 0:
                z = kb.sb([128, 8192], BF16); t_z = Tok()
                kb.op("pool", lambda e: e.memset(z[:], 0.0), writes=[t_z])
                hsv = self.hsorted.rearrange("(a p r) d -> a p (r d)", p=128, r=8)
                for a in range(MOE_SLOTS // 1024):
                    kb.dma("sp", [(hsv[a], z[:])], reads=[t_z])

    def load_mod_tiles(self, l, off_sc, off_sh, gain_name, plus_one=True):
        kb, I = self.kb, self.I
        gsc = kb.sb([128, 3, D], F32); sh = kb.sb([128, 3, D], F32); gn = kb.sb([128, D], F32)
        t_g, t_s, t_n = Tok(), Tok(), Tok()
        kb.dma("sp", [(gn[:], I[gain_name][l].partition_broadcast(128))], writes=[t_n])
        kb.dma("sp", [(gsc[:, j, :], self.modD[l][j, off_sc * D:(off_sc + 1) * D].partition_broadcast(128)) for j in range(3)],
               writes=[t_g])
        if off_sh is not None:
            kb.dma("sp", [(sh[:, j, :], self.modD[l][j, off_sh * D:(off_sh + 1) * D].partition_broadcast(128)) for j in range(3)],
                   writes=[t_s])
        for j in range(3):
            kb.op("dve", lambda e: e.scalar_tensor_tensor(out=gsc[:, j, :], in0=gsc[:, j, :], scalar=(1.0 if plus_one else 0.0), op0=ALU.add,
                                                          in1=gn[:], op1=ALU.mult),
                  reads=[t_g, t_n], writes=[t_g])
        return gsc, sh, t_g, t_s

    def norm_mod_tile(self, xt, t_x, gsc, sh, t_g, t_s, ms, R):
        kb = self.kb
        junk, t_j = R["junk"].next()
        st, t_st = R["stat"].next()
        kb.op("act", lambda e: e.activation(out=junk[:], in_=xt[:], func=AF.Square, accum_out=st[:, 0:1]),
              reads=[t_x], writes=[t_j, t_st])
        kb.op("act", lambda e: e.activation(out=st[:, 1:2], in_=st[:, 0:1], func=AF.Sqrt, scale=1.0 / D, bias=self.eps_t[:, 0:1]),
              reads=[t_st], writes=[t_st])
        kb.op("dve", lambda e: e.reciprocal(out=st[:, 2:3], in_=st[:, 1:2]), reads=[t_st], writes=[t_st])
        tmp, t_t = R["tmp"].next()
        kb.op("dve", lambda e: e.scalar_tensor_tensor(out=tmp[:], in0=xt[:], scalar=st[:, 2:3], op0=ALU.mult,
                                                      in1=gsc[:, ms, :], op1=ALU.mult),
              reads=[t_x, t_st, t_g], writes=[t_t])
        hb, t_h = R["hb"].next()
        kb.op("pool", lambda e: e.tensor_tensor(out=hb[:], in0=tmp[:], in1=sh[:, ms, :], op=ALU.add),
              reads=[t_t, t_s], writes=[t_h])
        return hb, t_h

    def consts_sb(self):
        kb, I = self.kb, self.I
        self.ident_bf = kb.sb([128, 128], BF16); self.t_ident = Tok()
        kb.dma("sp", [(self.ident_bf[:], I["ident_bf"][:, :])], writes=[self.t_ident])
        self.ident_f = kb.sb([128, 128], F32)
        kb.dma("sp", [(self.ident_f[:], I["ident_f"][:, :])], writes=[self.t_ident])
        self.eps_t = kb.sb([128, 1], F32)
        kb.op("pool", lambda e: e.memset(self.eps_t[:], EPS), writes=[self.t_ident])

    def phase_win(self, l, xsrc, prep_win=False):
        kb, I = self.kb, self.I
        with kb.phase():
            gsc, sh, t_g, t_s = self.load_mod_tiles(l, 1, 0, "norm_mix_pre")
            wb = kb.sb([128, 8, 2048], BF16); t_w = Tok()
            wv = I["w_in"][l].rearrange("(k p) n -> p k n", p=128)
            kb.dma("pool", [(wb[:, :, c * 512:(c + 1) * 512], wv[:, :, c * 512:(c + 1) * 512]) for c in range(4)], writes=[t_w])
            rc = kb.sb([128, 16, 64], F32); rs = kb.sb([128, 16, 64], F32); t_r = Tok()
            kb.dma("sp", [(rc[:], I["rope_cos"][:, :, :]), (rs[:], I["rope_sin"][:, :, :])], writes=[t_r])
            R = {"junk": Ring(kb, 1, [128, D], BF16), "stat": Ring(kb, 8, [128, 4], F32), "tmp": Ring(kb, 2, [128, D], F32),
                 "hb": Ring(kb, 3, [128, D], BF16)}
            xr = Ring(kb, 4, [128, D], F32)
            pT = Ring(kb, 1, [128, 8, 128], BF16, kind="ps")
            hTr = Ring(kb, 3, [128, 8, 128], BF16)
            pq = Ring(kb, 2, [128, 512], F32, kind="ps"); pk = Ring(kb, 2, [128, 512], F32, kind="ps")
            pv = Ring(kb, 1, [128, 512], F32, kind="ps"); pfu = Ring(kb, 1, [128, 512], F32, kind="ps")
            ptq = Ring(kb, 1, [128, 2, 4, 128], BF16, kind="ps")
            ropet = Ring(kb, 2, [128, 512], F32); ropem = Ring(kb, 2, [128, 256], F32)
            qbr = Ring(kb, 2, [128, 512], BF16); kbr = Ring(kb, 2, [128, 512], BF16)
            qTg = Ring(kb, 2, [128, 4, 512], BF16); kTg = Ring(kb, 2, [128, 4, 512], BF16)
            uTg = Ring(kb, 2, [128, 2, 512], BF16)
            vtr = Ring(kb, 2, [128, 4, 130], BF16); fbr = Ring(kb, 2, [128, 256], BF16)
            for vt, tv in vtr.items:
                kb.op("pool", lambda e: e.memset(vt[:, :, 128:130], 1.0), writes=[tv])
            C = [dict() for _ in range(NTILE)]
            G = {}

            def s0(ti):
                xt, t_x = xr.next()
                kb.dma("sp", [(xt[:], xsrc[ti * 128:(ti + 1) * 128, :])], writes=[t_x])
                st, t_st = self.nm1(xt, t_x, R)
                C[ti].update(xt=xt, t_x=t_x, st=st, t_st=t_st)

            def s1(ti):
                c = C[ti]
                b, j = divmod(ti, TILES_PB)
                ms = 2 if j < 2 else b
                c["hb"], c["t_h"] = self.nm2(c["xt"], c["t_x"], c["st"], c["t_st"], gsc, sh, t_g, t_s, ms, R)

            def s2(ti):
                c = C[ti]
                p, t_p = pT.next()
                for k in range(8):
                    kb.op("pe", lambda e: e.transpose(out=p[:, k, :], in_=c["hb"][:, k * 128:(k + 1) * 128], identity=self.ident_bf[:]),
                          reads=[c["t_h"], self.t_ident], writes=[t_p])
                hT, t_hT = hTr.next()
                kb.op("act", lambda e: e.copy(out=hT[:], in_=p[:]), reads=[t_p], writes=[t_hT])
                c.update(hT=hT, t_hT=t_hT)

            def s_mm(ti):
                c = C[ti]
                hT, t_hT = c["hT"], c["t_hT"]
                outs = []
                for ring, c0, n in ((pq, 0, 512), (pk, 512, 512), (pv, 1024, 512), (pfu, 1536, 256)):
                    pp, t_pp = ring.next()
                    for k in range(8):
                        kb.op("pe", lambda e: e.matmul(pp[:, 0:n], hT[:, k, :], wb[:, k, c0:c0 + n], start=(k == 0), stop=(k == 7)),
                              reads=[t_hT, t_w], writes=[t_pp])
                    outs.append((pp, t_pp))
                ppf, t_pf = outs[3]
                for ct in range(2):
                    for k in range(8):
                        kb.op("pe", lambda e: e.matmul(ppf[:, 256 + ct * 128:256 + (ct + 1) * 128], wb[:, k, 1792 + ct * 128:1792 + (ct + 1) * 128],
                                                       hT[:, k, :], start=(k == 0), stop=(k == 7)),
                              reads=[t_hT, t_w], writes=[t_pf])
                c["outs"] = outs

            def s_post(ti):
                c = C[ti]
                b, j = divmod(ti, TILES_PB)
                is_ctx = j < 2
                if j == 0 or (j >= 2 and (j - 2) % 4 == 0):
                    G["gq"] = qTg.next(); G["gk"] = kTg.next(); G["gu"] = uTg.next()
                    G["gstart"] = ti; G["gi"] = 0
                    G["gn"] = 2 if j == 0 else 4
                (gq, t_gq), (gk, t_gk), (gu, t_gu) = G["gq"], G["gk"], G["gu"]
                gi = G["gi"]
                (ppq, t_pq), (ppk, t_pk), (ppv, t_pv), (ppf, t_pf) = c["outs"]
                pt, t_pt = ptq.next()
                for which, (pp, t_pp), bring in ((0, (ppq, t_pq), qbr), (1, (ppk, t_pk), kbr)):
                    xb, t_xb = bring.next()
                    if is_ctx:
                        kb.op("dve", lambda e: e.tensor_copy(out=xb[:], in_=pp[:]), reads=[t_pp], writes=[t_xb])
                    else:
                        lt = j - 2
                        t1, t_t1 = ropet.next()
                        kb.op("dve", lambda e: e.tensor_tensor(out=t1[:].rearrange("p (a c) -> p a c", a=8), in0=pp[:].rearrange("p (a c) -> p a c", a=8),
                                                               in1=rc[:, lt:lt + 1, :].broadcast_to([128, 8, 64]), op=ALU.mult),
                              reads=[t_pp, t_r], writes=[t_t1])
                        x5 = pp[:].rearrange("p (a b j f) -> p a b j f", a=8, b=2, j=2)
                        t5 = t1[:].rearrange("p (a b j f) -> p a b j f", a=8, b=2, j=2)
                        o5 = xb[:].rearrange("p (a b j f) -> p a b j f", a=8, b=2, j=2)
                        s4 = rs[:, lt, :].rearrange("p (b j f) -> p b j f", b=2, j=2)
                        for jj in range(2):
                            m, t_m = ropem.next()
                            m4 = m[:].rearrange("p (a b f) -> p a b f", a=8, b=2)
                            kb.op("dve", lambda e: e.tensor_tensor(out=m4, in0=x5[:, :, :, 1 - jj, :],
                                                                   in1=s4[:, :, jj, :].unsqueeze(1).broadcast_to([128, 8, 2, 16]), op=ALU.mult),
                                  reads=[t_pp, t_r], writes=[t_m])
                            kb.op("dve", lambda e: e.tensor_tensor(out=o5[:, :, :, jj, :], in0=t5[:, :, :, jj, :], in1=m4, op=ALU.add),
                                  reads=[t_t1, t_m], writes=[t_xb])
                    for h in range(4):
                        kb.op("pe", lambda e: e.transpose(out=pt[:, which, h, :], in_=xb[:, h * 128:(h + 1) * 128], identity=self.ident_bf[:]),
                              reads=[t_xb, self.t_ident], writes=[t_pt])
                kb.op("act", lambda e: e.copy(out=gq[:, :, gi * 128:(gi + 1) * 128], in_=pt[:, 0, :, :]), reads=[t_pt], writes=[t_gq])
                kb.op("act", lambda e: e.copy(out=gk[:, :, gi * 128:(gi + 1) * 128], in_=pt[:, 1, :, :]), reads=[t_pt], writes=[t_gk])
                vt, t_v = vtr.next()
                kb.op("act", lambda e: e.copy(out=vt[:, :, 0:128], in_=ppv[:].rearrange("p (h d) -> p h d", h=4)), reads=[t_pv], writes=[t_v])
                kb.dma("sp", [(self.vD[:, :, ti, :], vt[:])], reads=[t_v])
                fb, t_f = fbr.next()
                kb.op("dve", lambda e: e.tensor_copy(out=fb[:], in_=ppf[:, 0:256]), reads=[t_pf], writes=[t_f])
                kb.dma("sp", [(self.fD[:, ti, :], fb[:])], reads=[t_f])
                kb.op("act", lambda e: e.copy(out=gu[:, :, gi * 128:(gi + 1) * 128], in_=ppf[:, 256:512].rearrange("p (c t) -> p c t", c=2)),
                      reads=[t_pf], writes=[t_gu])
                G["gi"] = gi + 1
                if G["gi"] == G["gn"]:
                    c0 = G["gstart"] * 128; w = G["gn"] * 128
                    kb.dma("sp", [(self.qT[:, :, c0:c0 + w], gq[:, :, 0:w])], reads=[t_gq])
                    kb.dma("sp", [(self.kT[:, :, c0:c0 + w], gk[:, :, 0:w])], reads=[t_gk])
                    kb.dma("sp", [(self.uT[:, :, c0:c0 + w], gu[:, :, 0:w])], reads=[t_gu])
                C[ti].clear()

            self.prep_begin()
            for it in range(NTILE + 4):
                if prep_win and it < NTILE:
                    self.prep_tick(1)
                for st_, off in ((s0, 0), (s1, 1), (s2, 2), (s_post, 4), (s_mm, 3)):
                    ti = it - off
                    if 0 <= ti < NTILE:
                        st_(ti)
            self.prep_flush()

    def phase_attn(self, l, need_ctx, prep_every=0):
        kb, I = self.kb, self.I
        lam_init = 0.8 - 0.6 * math.exp(-0.3 * l)
        with kb.phase():
            lq = kb.sb([128, 4, 64], F32); t_lq = Tok()
            kb.dma("sp", [(lq[:, i, :], I[n][l].partition_broadcast(128)) for i, n in
                          enumerate(("diff_lq1", "diff_lk1", "diff_lq2", "diff_lk2"))], writes=[t_lq])
            lt = kb.sb([128, 2, 64], F32); ls = kb.sb([128, 8], F32); t_ls = Tok()
            kb.op("dve", lambda e: e.tensor_tensor(out=lt[:, 0, :], in0=lq[:, 0, :], in1=lq[:, 1, :], op=ALU.mult), reads=[t_lq], writes=[t_ls])
            kb.op("dve", lambda e: e.tensor_tensor(out=lt[:, 1, :], in0=lq[:, 2, :], in1=lq[:, 3, :], op=ALU.mult), reads=[t_lq, t_ls], writes=[t_ls])
            kb.op("dve", lambda e: e.tensor_reduce(out=ls[:, 0:2], in_=lt[:], op=ALU.add, axis=AX.X), reads=[t_ls], writes=[t_ls])
            kb.op("act", lambda e: e.activation(out=ls[:, 2:4], in_=ls[:, 0:2], func=AF.Exp), reads=[t_ls], writes=[t_ls])
            kb.op("dve", lambda e: e.tensor_tensor(out=ls[:, 4:5], in0=ls[:, 3:4], in1=ls[:, 2:3], op=ALU.subtract), reads=[t_ls], writes=[t_ls])
            kb.op("dve", lambda e: e.tensor_scalar(out=ls[:, 5:6], in0=ls[:, 4:5], scalar1=-lam_init, scalar2=None, op0=ALU.add), reads=[t_ls], writes=[t_ls])
            nlam = ls[:, 5:6]
            sg = kb.sb([128, 128], F32); t_sg = Tok()
            kb.dma("sp", [(sg[:], I["diff_subln"][l].partition_broadcast(128))], writes=[t_sg])
            kb.op("dve", lambda e: e.tensor_scalar(out=sg[:], in0=sg[:], scalar1=1.0 - lam_init, scalar2=None, op0=ALU.mult), reads=[t_sg], writes=[t_sg])
            kr = Ring(kb, 2, [128, TPB], BF16); qr = Ring(kb, 2, [128, 2, TPB], BF16); vr = Ring(kb, 2, [128, TILES_PB, 130], BF16)
            psr = Ring(kb, 3, [128, 2, 256], F32, kind="ps")
            pacc = [Ring(kb, 2, [128, 2, 130], F32, kind="ps") for _ in range(2)]
            pto = Ring(kb, 1, [128, 2, 128], BF16, kind="ps")
            ptr = Ring(kb, 3, [128, 2, 256], BF16)
            o1r = Ring(kb, 4, [128, 128], F32); o2r = Ring(kb, 4, [128, 128], F32); junkr = Ring(kb, 1, [128, 128], BF16)
            str_ = Ring(kb, 6, [128, 8], F32); abr = Ring(kb, 2, [128, 128], BF16); aTr = Ring(kb, 2, [128, 256], BF16)
            for qz, t_qz in qr.items:
                kb.op("pool", lambda e: e.memset(qz[:], 0.0), writes=[t_qz])
            pending = []
            self.prep_begin()
            qt_count = 0
            for b in range(NB):
                t0 = b * TPB
                for h in range(4):
                    kT, t_k = kr.next(); qT, t_q = qr.next(); vv, t_v = vr.next()
                    kb.dma("sp", [(kT[:], self.kT[:, h, t0:t0 + TPB])], writes=[t_k])
                    kb.dma("sp", [(qT[m * 64:(m + 1) * 64, m, :], self.qT[m * 64:(m + 1) * 64, h, t0:t0 + TPB]) for m in range(2)], writes=[t_q])
                    kb.dma("sp", [(vv[:], self.vD[:, h, b * TILES_PB:(b + 1) * TILES_PB, :])], writes=[t_v])
                    qts = [(CTX + i * 256, list(range(TILES_PB))) for i in range(8)]
                    if need_ctx:
                        qts.append((0, [0, 1]))
                    for (q0, kts) in qts:
                        qt_count += 1
                        if prep_every and qt_count % prep_every == 0:
                            self.prep_tick(1)
                        a1, t_a1 = pacc[0].next(); a2, t_a2 = pacc[1].next()
                        accs = ((a1, t_a1), (a2, t_a2))

                        def emit_st(kt):
                            ps, t_ps = psr.next()
                            for m in range(2):
                                kb.op("pe", lambda e: e.matmul(ps[:, m, :], kT[:, kt * 128:(kt + 1) * 128],
                                                               qT[:, m, q0:q0 + 256], start=(m == 0), stop=True, skip_group_check=True),
                                      reads=[t_k, t_q], writes=[t_ps])
                            return ps, t_ps
                        ahead = [emit_st(kts[0])]
                        if len(kts) > 1:
                            ahead.append(emit_st(kts[1]))
                        for ki, kt in enumerate(kts):
                            ps, t_ps = ahead.pop(0)
                            if ki + 2 < len(kts):
                                ahead.append(emit_st(kts[ki + 2]))
                            pt, t_pt = ptr.next()
                            kb.op("act", lambda e: e.activation(out=pt[:], in_=ps[:], func=AF.Exp, scale=0.125), reads=[t_ps], writes=[t_pt])
                            for m in range(2):
                                for s in range(2):
                                    kb.op("pe", lambda e: e.matmul(accs[m][0][:, s, 0:129], pt[:, m, s * 128:(s + 1) * 128], vv[:, kt, 0:129],
                                                                   start=(ki == 0 and s == 0), stop=(ki == len(kts) - 1), skip_group_check=True),
                                          reads=[t_pt, t_v], writes=[accs[m][1]])
                            while pending and pending[0][0] <= ki:
                                pending.pop(0)[1]()
                        while pending:
                            pending.pop(0)[1]()
                        sts, o2s = [], []
                        for s in range(2):
                            st, t_st = str_.next()
                            kb.op("dve", lambda e: e.reciprocal(out=st[:, 0:1], in_=a1[:, s, 128:129]), reads=[t_a1], writes=[t_st])
                            kb.op("dve", lambda e: e.reciprocal(out=st[:, 1:2], in_=a2[:, s, 128:129]), reads=[t_a2, t_st], writes=[t_st])
                            kb.op("dve", lambda e: e.tensor_tensor(out=st[:, 2:3], in0=st[:, 1:2], in1=nlam, op=ALU.mult), reads=[t_st, t_ls], writes=[t_st])
                            o1, t_o1 = o1r.next(); o2, t_o2 = o2r.next()
                            kb.op("dve", lambda e: e.tensor_scalar(out=o1[:], in0=a1[:, s, 0:128], scalar1=st[:, 0:1], scalar2=None, op0=ALU.mult),
                                  reads=[t_a1, t_st], writes=[t_o1])
                            kb.op("dve", lambda e: e.scalar_tensor_tensor(out=o2[:], in0=a2[:, s, 0:128], scalar=st[:, 2:3], op0=ALU.mult,
                                                                          in1=o1[:], op1=ALU.add), reads=[t_a2, t_st, t_o1], writes=[t_o2])
                            kb.op("dve", lambda e: e.tensor_tensor(out=o1[:], in0=o2[:], in1=o2[:], op=ALU.mult), reads=[t_o2, t_o1], writes=[t_o1])
                            kb.op("dve", lambda e: e.tensor_reduce(out=st[:, 3:4], in_=o1[:], op=ALU.add, axis=AX.X), reads=[t_o1, t_st], writes=[t_st])
                            sts.append((st, t_st)); o2s.append((o2, t_o2))

                        def n2(sts=sts):
                            for st, t_st in sts:
                                kb.op("act", lambda e: e.activation(out=st[:, 4:5], in_=st[:, 3:4], func=AF.Ln, scale=1.0 / 128, bias=self.eps_t[:, 0:1]),
                                      reads=[t_st], writes=[t_st])
                                kb.op("act", lambda e: e.activation(out=st[:, 5:6], in_=st[:, 4:5], func=AF.Exp, scale=-0.5),
                                      reads=[t_st], writes=[t_st])

                        def n3(sts=sts, o2s=o2s, h=h, c0=t0 + q0):
                            po, t_po = pto.next()
                            for s in range(2):
                                st, t_st = sts[s]; o2, t_o2 = o2s[s]
                                ab, t_ab = abr.next()
                                kb.op("dve", lambda e: e.scalar_tensor_tensor(out=ab[:], in0=o2[:], scalar=st[:, 5:6], op0=ALU.mult, in1=sg[:], op1=ALU.mult),
                                      reads=[t_o2, t_st, t_sg], writes=[t_ab])
                                kb.op("pe", lambda e: e.transpose(out=po[:, s, :], in_=ab[:], identity=self.ident_bf[:]), reads=[t_ab, self.t_ident], writes=[t_po])
                            aT, t_aT = aTr.next()
                            kb.op("dve", lambda e: e.tensor_copy(out=aT[:], in_=po[:].rearrange("p s t -> p (s t)")), reads=[t_po], writes=[t_aT])
                            kb.dma("sp", [(self.catT[:, h, c0:c0 + 256], aT[:])], reads=[t_aT])
                        pending.append((7, n2)); pending.append((10, n3))
            while pending:
                pending.pop(0)[1]()
            self.prep_flush()

    def phase_fnet(self, l, need_ctx):
        kb, I = self.kb, self.I
        with kb.phase():
            fw = kb.sb([128, 2, 256], F32); cb = kb.sb([128, 2, 128], F32); t_fw = Tok()
            kb.dma("sp", [(fw[:], I["fnet_w"][l].rearrange("(c p) m -> p c m", p=128)), (cb[:], I["cblk"][:, :, :])], writes=[t_fw])
            W = kb.sb([128, 2, 2, 256], BF16); t_W = Tok()
            pw = Ring(kb, 2, [128, 512], F32, kind="ps")
            for ab in range(2):
                for ct in range(2):
                    p, t_p = pw.next()
                    kb.op("pe", lambda e: e.matmul(p[:, 0:256], cb[:, ab, :], fw[:, ct, :], start=True, stop=True), reads=[t_fw], writes=[t_p])
                    kb.op("dve", lambda e: e.tensor_copy(out=W[:, ab, ct, :], in_=p[:, 0:256]), reads=[t_p], writes=[t_W])
            fa = kb.sb([128, NTILE, 256], BF16); t_fa = Tok()
            kb.dma("sp", [(fa[:], self.fD[:, :, :])], writes=[t_fa])
            dr = [Ring(kb, 2, [128, 16, 512], BF16) for _ in range(2)]
            absb = Ring(kb, 2, [128, 2, 2, 512], BF16); fo = Ring(kb, 2, [128, 512], BF16)
            pf = Ring(kb, 2, [128, 512], F32, kind="ps")

            def dft(b, tiles, mats, n, tok0):
                ab_t, t_ab = absb.next()
                for ab in range(2):
                    m, t_m = mats[ab]
                    for ct in range(2):
                        p, t_p = pw.next()
                        for i, tt in enumerate(tiles):
                            kb.op("pe", lambda e: e.matmul(p[:, 0:n], fa[:, tt, ct * 128:(ct + 1) * 128], m[:, i, 0:n], start=(i == 0), stop=(i == len(tiles) - 1)),
                                  reads=[t_fa, t_m], writes=[t_p])
                        eng = "act" if ct == 0 else "dve"
                        if eng == "act":
                            kb.op("act", lambda e: e.copy(out=ab_t[:, ab, ct, 0:n], in_=p[:, 0:n]), reads=[t_p], writes=[t_ab])
                        else:
                            kb.op("dve", lambda e: e.tensor_copy(out=ab_t[:, ab, ct, 0:n], in_=p[:, 0:n]), reads=[t_p], writes=[t_ab])
                for mt in range(2):
                    p, t_p = pf.next()
                    i = 0
                    for ab in range(2):
                        for ct in range(2):
                            kb.op("pe", lambda e: e.matmul(p[:, 0:n], W[:, ab, ct, mt * 128:(mt + 1) * 128], ab_t[:, ab, ct, 0:n], start=(i == 0), stop=(i == 3)),
                                  reads=[t_W, t_ab], writes=[t_p])
                            i += 1
                    f, t_f = fo.next()
                    kb.op("act", lambda e: e.copy(out=f[:, 0:n], in_=p[:, 0:n]), reads=[t_p], writes=[t_f])
                    kb.dma("sp", [(self.catT[:, 4 + mt, tok0:tok0 + n], f[:, 0:n])], reads=[t_f])

            mode = getattr(self, "fn_mode", "WMC")
            for pt in range(4 if "M" in mode else 0):
                mats = []
                for ab, nm in enumerate(("dft_c", "dft_s")):
                    m, t_m = dr[ab].next()
                    src = I[nm].rearrange("(t p) n -> p t n", p=128)
                    kb.dma("sp", [(m[:, q * 4:(q + 1) * 4, :], src[:, q * 4:(q + 1) * 4, pt * 512:(pt + 1) * 512]) for q in range(4)], writes=[t_m])
                    mats.append((m, t_m))
                for b in range(NB):
                    dft(b, [b * TILES_PB + 2 + i for i in range(16)], mats, 512, b * TPB + CTX + pt * 512)
            if need_ctx and "C" in mode:
                mats = []
                for ab, nm in enumerate(("dftc_c", "dftc_s")):
                    m, t_m = dr[ab].next()
                    kb.dma("sp", [(m[:, 0:2, 0:256], I[nm].rearrange("(t p) n -> p t n", p=128))], writes=[t_m])
                    mats.append((m, t_m))
                for b in range(NB):
                    dft(b, [b * TILES_PB + i for i in range(2)], mats, 256, b * TPB)

    def cmul(self, o_r, o_i, a_r, a_i, b_r, b_i, t1, t2, T, eng="dve"):
        kb = self.kb
        tt = lambda o, x, y, op: kb.op(eng, lambda e: e.tensor_tensor(out=o, in0=x, in1=y, op=op), reads=T, writes=T)
        tt(t1, a_r, b_r, ALU.mult); tt(t2, a_i, b_i, ALU.mult); tt(o_r, t1, t2, ALU.subtract)
        tt(t1, a_r, b_i, ALU.mult); tt(t2, a_i, b_r, ALU.mult); tt(o_i, t1, t2, ALU.add)

    def phase_s5(self, l):
        kb, I = self.kb, self.I
        TWO_PI = 2.0 * math.pi
        MAGIC = 12582912.0
        with kb.phase():
            A = kb.sb([128, 32, 128], BF16); BsRI = kb.sb([128, 32, 2, 128], BF16); CqRI = kb.sb([128, 32, 2, 128], BF16)
            Wsel = kb.sb([128, 8, 240], BF16); V = None; Hb = None
            PL = kb.sb([128, 2, 10, 16], F32)
            nPLi = kb.sb([128, 10, 16], F32)
            dd = kb.sb([128, 2], F32); wg = kb.sb([128, 2, 256], BF16)
            t_A, t_Bs, t_Cq, t_W, t_V, t_H, t_PL, t_misc = (Tok() for _ in range(8))
            kb.dma("sp", [(Wsel[:], I["wsel"][:, :, :])], writes=[t_W])
            kb.dma("sp", [(dd[:], I["s5_dd"][l])], writes=[t_misc])
            kb.dma("pool", [(wg[:], I["s5_w_glu"][l].rearrange("(c p) m -> p c m", p=128))], writes=[t_misc])
            with kb.phase():
                T = [Tok()]
                par = kb.sb([128, 3, 32], F32)
                bc = kb.sb([128, 2, 32, 2, 16], F32)
                msk = kb.sb([128, 2, 128], F32)
                kb.dma("sp", [(par[:], I["s5_par"][l]), (bc[:, 0], I["s5_b"][l]), (bc[:, 1], I["s5_c"][l]), (msk[:], I["s5_mask"][:, :, :])], writes=T)
                w = kb.sb([128, 24, 32], F32)
                W_ = lambda i: w[:, i, :]
                ts = lambda o, x, s1, o0, s2=None, o1=None: kb.op("dve", lambda e: e.tensor_scalar(out=o, in0=x, scalar1=s1, scalar2=s2, op0=o0, **({"op1": o1} if o1 else {})), reads=T, writes=T)
                tt = lambda o, x, y, op: kb.op("dve", lambda e: e.tensor_tensor(out=o, in0=x, in1=y, op=op), reads=T, writes=T)
                act = lambda o, x, f, **kw: kb.op("act", lambda e: e.activation(out=o, in_=x, func=f, **kw), reads=T, writes=T)
                are, aim, ldt = par[:, 0, :], par[:, 1, :], par[:, 2, :]
                dt, mag, ang, lr, li = W_(0), W_(1), W_(2), W_(3), W_(4)
                act(dt, ldt, AF.Exp)
                tt(mag, are, dt, ALU.mult); act(mag, mag, AF.Exp)
                tt(ang, aim, dt, ALU.mult)

                def sin_of(o, x, shift):
                    a, r = W_(5), W_(6)
                    ts(a, x, shift, ALU.add)
                    ts(r, a, 1.0 / TWO_PI, ALU.mult)
                    ts(r, r, MAGIC, ALU.add)
                    ts(r, r, MAGIC, ALU.subtract)
                    kb.op("dve", lambda e: e.scalar_tensor_tensor(out=a, in0=r, scalar=-TWO_PI, op0=ALU.mult, in1=a, op1=ALU.add), reads=T, writes=T)
                    act(o, a, AF.Sin)
                sin_of(li, ang, 0.0); sin_of(lr, ang, math.pi / 2)
                tt(lr, lr, mag, ALU.mult); tt(li, li, mag, ALU.mult)
                nr, den, cr, ci, t1, t2 = W_(7), W_(8), W_(9), W_(10), W_(11), W_(12)
                ts(nr, lr, -1.0, ALU.add)
                tt(t1, are, are, ALU.mult); tt(t2, aim, aim, ALU.mult); tt(den, t1, t2, ALU.add)
                kb.op("dve", lambda e: e.reciprocal(out=den, in_=den), reads=T, writes=T)
                tt(t1, nr, are, ALU.mult); tt(t2, li, aim, ALU.mult); tt(cr, t1, t2, ALU.add); tt(cr, cr, den, ALU.mult)
                tt(t1, li, are, ALU.mult); tt(t2, nr, aim, ALU.mult); tt(ci, t1, t2, ALU.subtract); tt(ci, ci, den, ALU.mult)
                ilr, ili, m2 = W_(13), W_(14), W_(15)
                tt(t1, lr, lr, ALU.mult); tt(t2, li, li, ALU.mult); tt(m2, t1, t2, ALU.add)
                kb.op("dve", lambda e: e.reciprocal(out=m2, in_=m2), reads=T, writes=T)
                tt(ilr, lr, m2, ALU.mult); tt(ili, li, m2, ALU.mult); ts(ili, ili, -1.0, ALU.mult)
                bb = kb.sb([128, 2, 32, 16], F32); tb = kb.sb([128, 2, 32, 16], F32)
                bcast = lambda v: v.unsqueeze(2).broadcast_to([128, 32, 16])
                self.cmul(bb[:, 0], bb[:, 1], bcast(cr), bcast(ci), bc[:, 0, :, 0, :], bc[:, 0, :, 1, :], tb[:, 0], tb[:, 1], T)
                mu = kb.sb([128, 2, 2, 3, 32], F32)
                cp = lambda o, x: kb.op("dve", lambda e: e.tensor_copy(out=o, in_=x), reads=T, writes=T)
                for ri, (fw_, bw_) in enumerate(((ilr, lr), (ili, li))):
                    cp(mu[:, 0, ri, 0, 0:16], fw_[:, 0:16]); cp(mu[:, 0, ri, 0, 16:32], bw_[:, 16:32])
                    cp(mu[:, 1, ri, 0, 0:16], bw_[:, 0:16]); cp(mu[:, 1, ri, 0, 16:32], fw_[:, 16:32])
                for tb_i in range(2):
                    for pw in range(2):
                        self.cmul(mu[:, tb_i, 0, pw + 1], mu[:, tb_i, 1, pw + 1], mu[:, tb_i, 0, pw], mu[:, tb_i, 1, pw],
                                  mu[:, tb_i, 0, pw], mu[:, tb_i, 1, pw], t1, t2, T)
                ch = kb.sb([128, 2, 2, 32, 8], F32)
                for tb_i in range(2):
                    kb.op("dve", lambda e: e.memset(ch[:, tb_i, 0, :, 0:1], 1.0), reads=T, writes=T)
                    kb.op("dve", lambda e: e.memset(ch[:, tb_i, 1, :, 0:1], 0.0), reads=T, writes=T)
                    n = 1
                    tmpc = kb.sb([128, 2, 32, 4], F32)
                    for pw in range(3):
                        mb = lambda ri: mu[:, tb_i, ri, pw, :].unsqueeze(2).broadcast_to([128, 32, n])
                        self.cmul(ch[:, tb_i, 0, :, n:2 * n], ch[:, tb_i, 1, :, n:2 * n], ch[:, tb_i, 0, :, 0:n], ch[:, tb_i, 1, :, 0:n],
                                  mb(0), mb(1), tmpc[:, 0, :, 0:n], tmpc[:, 1, :, 0:n], T)
                        n *= 2
                l8 = kb.sb([128, 2, 32], F32); l7 = kb.sb([128, 2, 32], F32); l2 = kb.sb([128, 2, 2, 32], F32)
                self.cmul(l2[:, 0, 0], l2[:, 0, 1], lr, li, lr, li, t1, t2, T)
                self.cmul(l2[:, 1, 0], l2[:, 1, 1], l2[:, 0, 0], l2[:, 0, 1], l2[:, 0, 0], l2[:, 0, 1], t1, t2, T)
                self.cmul(l8[:, 0], l8[:, 1], l2[:, 1, 0], l2[:, 1, 1], l2[:, 1, 0], l2[:, 1, 1], t1, t2, T)
                self.cmul(l7[:, 0], l7[:, 1], l8[:, 0], l8[:, 1], ilr, ili, t1, t2, T)
                sf = kb.sb([128, 2, 2, 32], F32)
                cp(sf[:, 0, 0, 0:16], l7[:, 0, 0:16]); cp(sf[:, 0, 1, 0:16], l7[:, 1, 0:16])
                kb.op("dve", lambda e: e.memset(sf[:, 0, 0, 16:32], 1.0), reads=T, writes=T)
                kb.op("dve", lambda e: e.memset(sf[:, 0, 1, 16:32], 0.0), reads=T, writes=T)
                cp(sf[:, 1, 0, 0:16], lr[:, 0:16]); cp(sf[:, 1, 1, 0:16], li[:, 0:16])
                cp(sf[:, 1, 0, 16:32], l8[:, 0, 16:32]); cp(sf[:, 1, 1, 16:32], l8[:, 1, 16:32])
                ch2 = kb.sb([128, 2, 2, 32, 8], F32)
                tmp8 = kb.sb([128, 2, 32, 8], F32)
                for tb_i in range(2):
                    sb_ = lambda ri: sf[:, tb_i, ri, :].unsqueeze(2).broadcast_to([128, 32, 8])
                    self.cmul(ch2[:, tb_i, 0], ch2[:, tb_i, 1], ch[:, tb_i, 0], ch[:, tb_i, 1], sb_(0), sb_(1), tmp8[:, 0], tmp8[:, 1], T)
                full = kb.sb([128, 2, 32, 8, 16], F32); ftmp = kb.sb([128, 2, 32, 8, 16], F32)
                st = kb.sb([128, 32, 128], BF16)
                pA = Ring(kb, 2, [128, 4, 128], F32, kind="ps"); pTt = Ring(kb, 2, [128, 4, 128], BF16, kind="ps")
                KBst = kb.sb([128, 32, 128], BF16); QCst = kb.sb([128, 32, 128], BF16)

                def build(chain, tb_i, src_r, src_i, dst, neg_im):
                    cb = lambda ri: chain[:, tb_i, ri].unsqueeze(3).broadcast_to([128, 32, 8, 16])
                    sbq = lambda v: v.unsqueeze(2).broadcast_to([128, 32, 8, 16])
                    self.cmul(full[:, 0], full[:, 1], cb(0), cb(1), sbq(src_r), sbq(src_i), ftmp[:, 0], ftmp[:, 1], T)
                    cp(dst[0:64], full[0:64, 0].rearrange("p a s c -> p a (s c)"))
                    if neg_im:
                        ts(dst[64:128], full[64:128, 1].rearrange("p a s c -> p a (s c)"), -1.0, ALU.mult)
                    else:
                        cp(dst[64:128], full[64:128, 1].rearrange("p a s c -> p a (s c)"))
                build(ch, 0, bb[:, 0], bb[:, 1], KBst, False)
                build(ch, 1, bc[:, 1, :, 0, :], bc[:, 1, :, 1, :], QCst, True)
                for q4 in range(8):
                    p, t_p = pA.next()
                    for i in range(4):
                        dg = q4 * 4 + i
                        kb.op("pe", lambda e: e.matmul(p[:, i, :], KBst[:, dg, :], QCst[:, dg, :], start=(i == 0), stop=True, skip_group_check=True), reads=T, writes=[t_p])
                    d = 0 if q4 < 4 else 1
                    kb.op("dve", lambda e: e.tensor_tensor(out=A[:, q4 * 4:(q4 + 1) * 4, :], in0=p[:],
                                                           in1=msk[:, d:d + 1, :].broadcast_to([128, 4, 128]), op=ALU.mult),
                          reads=[t_p] + T, writes=[t_A])
                kb.op("pool", lambda e: e.memset(BsRI[:], 0.0), writes=[t_Bs])
                kb.op("pool", lambda e: e.memset(CqRI[:], 0.0), writes=[t_Cq])
                build(ch2, 0, bb[:, 0], bb[:, 1], st, False)
                for q4 in range(8):
                    p, t_p = pTt.next()
                    for i in range(4):
                        dg = q4 * 4 + i
                        kb.op("pe", lambda e: e.transpose(out=p[:, i, :], in_=st[:, dg, :], identity=self.ident_bf[:]), reads=T + [self.t_ident], writes=[t_p])
                    for i in range(4):
                        dg = q4 * 4 + i
                        g2 = dg % 2
                        kb.op("act", lambda e: e.copy(out=BsRI[:, dg, :, g2 * 64:(g2 + 1) * 64], in_=p[:, i, :].rearrange("p (r q) -> p r q", r=2)),
                              reads=[t_p], writes=[t_Bs])
                build(ch2, 1, bc[:, 1, :, 0, :], bc[:, 1, :, 1, :], st, True)
                for g2 in range(2):
                    rows = slice(g2 * 64, (g2 + 1) * 64)
                    sv = st[rows].rearrange("p (a g) x -> p a g x", g=2)
                    dv = CqRI[rows].rearrange("p (a g) r x -> p a g r x", g=2)
                    fr = full[rows, 0].rearrange("p (a g) s c -> p a g (s c)", g=2)
                    fi = full[rows, 1].rearrange("p (a g) s c -> p a g (s c)", g=2)
                    kb.op("dve", lambda e: e.tensor_copy(out=dv[:, :, g2, 0, :], in_=fr[:, :, g2, :]), reads=T, writes=[t_Cq])
                    kb.op("dve", lambda e: e.tensor_scalar(out=dv[:, :, g2, 1, :], in0=fi[:, :, g2, :], scalar1=-1.0, scalar2=None, op0=ALU.mult), reads=T, writes=[t_Cq])
                for ri in range(2):
                    for g2 in range(2):
                        rows = slice(g2 * 64, (g2 + 1) * 64)
                        kb.op("dve", lambda e: e.tensor_copy(out=PL[rows, ri, 0, :], in_=l8[rows, ri, :].rearrange("p (a g) -> p a g", g=2)[:, :, g2]),
                              reads=T, writes=[t_PL])
                pt1 = kb.sb([128, 16], F32); pt2 = kb.sb([128, 16], F32)
                for i in range(9):
                    self.cmul(PL[:, 0, i + 1], PL[:, 1, i + 1], PL[:, 0, i], PL[:, 1, i], PL[:, 0, i], PL[:, 1, i], pt1[:], pt2[:], [t_PL])
                kb.op("dve", lambda e: e.tensor_scalar(out=nPLi[:], in0=PL[:, 1], scalar1=-1.0, scalar2=None, op0=ALU.mult), reads=[t_PL], writes=[t_PL])
            self._s5_main(l, A, BsRI, CqRI, Wsel, V, Hb, PL, nPLi, dd, wg, (t_A, t_Bs, t_Cq, t_W, t_V, t_H, t_PL, t_misc))

    def _s5_main(self, l, A, BsRI, CqRI, Wsel, V, Hb, PL, nPLi, dd, wg, toks):
        kb, I = self.kb, self.I
        t_A, t_Bs, t_Cq, t_W, t_V, t_H, t_PL, t_misc = toks
        NCH = 288
        with kb.phase():
            V = kb.sb([128, 16, NB, 320], BF16)
            Hb = kb.sb([128, 2, 2, 8, NB, 288], BF16)
            with kb.phase():
                with kb.phase():
                    U = kb.sb([128, 2, NTOK], BF16); t_U = Tok()
                    kb.dma("sp", [(U[:, ct, :], self.uT[:, ct, :]) for ct in range(2)], writes=[t_U])
                    pv = Ring(kb, 2, [128, NCH], F32, kind="ps")
                    i = 0
                    for g in range(16):
                        gt, gl = divmod(g, 8)
                        for b in range(NB):
                            p, t_p = pv.next()
                            ub = U[:, gt, b * TPB:(b + 1) * TPB].rearrange("p (k s) -> p k s", s=8)
                            for s in range(8):
                                kb.op("pe", lambda e: e.matmul(p[:, :], Wsel[:, gl, 112 - 16 * s:240 - 16 * s], ub[:, :, s], start=(s == 0), stop=(s == 7)),
                                      reads=[t_U, t_W], writes=[t_p])
                            if i % 2 == 0:
                                kb.op("act", lambda e: e.copy(out=V[:, g, b, 0:NCH], in_=p[:, :]), reads=[t_p], writes=[t_V])
                                kb.op("act", lambda e: e.copy(out=V[:, g, b, NCH:320], in_=p[:, 0:32]), reads=[t_p], writes=[t_V])
                            else:
                                kb.op("dve", lambda e: e.tensor_copy(out=V[:, g, b, 0:NCH], in_=p[:, :]), reads=[t_p], writes=[t_V])
                                kb.op("dve", lambda e: e.tensor_copy(out=V[:, g, b, NCH:320], in_=p[:, 0:32]), reads=[t_p], writes=[t_V])
                            i += 1
                X = kb.sb([128, 2, 2, 8, NB, NCH], F32)
                sctmp = None
                t_X = [Tok(), Tok()]
                t_XG = [[[Tok(), Tok()] for _ in range(8)] for _ in range(2)]
                pS = Ring(kb, 4, [128, NCH], F32, kind="ps")
                for d in range(2):
                    k0 = 0 if d == 0 else 32
                    for gp in range(8):
                        for b in range(NB):
                            for ri in range(2):
                                p, t_p = pS.next()
                                for g2 in range(2):
                                    g = 2 * gp + g2
                                    kb.op("pe", lambda e: e.matmul(p[:, :], BsRI[:, d * 16 + g, ri, :], V[:, g, b, k0:k0 + NCH], start=(g2 == 0), stop=(g2 == 1)),
                                          reads=[t_Bs, t_V], writes=[t_p])
                                if ri == 0:
                                    kb.op("act", lambda e: e.copy(out=X[:, 0, ri, gp, b, :], in_=p[:, :]), reads=[t_p], writes=[t_XG[0][gp][ri]])
                                else:
                                    kb.op("dve", lambda e: e.tensor_copy(out=X[:, 0, ri, gp, b, :], in_=p[:, :]), reads=[t_p], writes=[t_XG[0][gp][ri]])
                    cur = 0
                    for si in range(9):
                        sh = 1 << si
                        nxt = 1 - cur
                        n = NCH - sh
                        if d == 0:
                            dst, src, keep = slice(sh, NCH), slice(0, n), slice(0, sh)
                        else:
                            dst, src, keep = slice(0, n), slice(sh, NCH), slice(n, NCH)
                        for ri in range(2):
                            kb.op("pool", lambda e: e.tensor_copy(out=X[:, nxt, ri, :, :, keep], in_=X[:, cur, ri, :, :, keep]), reads=[t_XG[cur][g_][ri] for g_ in range(8)], writes=[t_XG[nxt][g_][ri] for g_ in range(8)])
                        for opi in range(4):
                            for gp in range(8):
                                c = d * 8 + gp
                                Pr, Pi, nPi = PL[:, 0, si, c:c + 1], PL[:, 1, si, c:c + 1], nPLi[:, si, c:c + 1]
                                xr, xi = X[:, cur, 0, gp], X[:, cur, 1, gp]
                                yr, yi = X[:, nxt, 0, gp], X[:, nxt, 1, gp]
                                o_, a_, sc_, b_ = ((yr, xr, Pr, xr), (yi, xi, Pr, xi), (yr, xi, nPi, yr), (yi, xr, Pi, yi))[opi]
                                kb.op("dve", lambda e: e.scalar_tensor_tensor(out=o_[:, :, dst], in0=a_[:, :, src], scalar=sc_, op0=ALU.mult, in1=b_[:, :, dst], op1=ALU.add),
                                      reads=[t_XG[cur][gp][0], t_XG[cur][gp][1], t_PL], writes=[t_XG[nxt][gp][opi % 2]])
                        cur = nxt
                    for ri in range(2):
                        kb.op("act", lambda e: e.copy(out=Hb[:, d, ri], in_=X[:, cur, ri]), reads=[t_XG[cur][g_][ri] for g_ in range(8)], writes=[t_H])
            with kb.phase():
                U = kb.sb([128, 2, NTOK], BF16); t_U = Tok()
                kb.dma("sp", [(U[:, ct, :], self.uT[:, ct, :]) for ct in range(2)], writes=[t_U])
                Yc = kb.sb([128, 16, NB, NCH], BF16); t_Y = Tok()
                G = kb.sb([128, 2, NTOK], BF16); t_G = Tok()
                py = Ring(kb, 2, [128, NCH], F32, kind="ps")
                i = 0
                for g in range(16):
                    gp, g2 = divmod(g, 2)
                    for b in range(NB):
                        p, t_p = py.next()
                        mm = lambda o, lh, rh, first=False: kb.op("pe", lambda e: e.matmul(o, lh, rh, start=first, stop=True, skip_group_check=True),
                                                                  reads=[t_A, t_Cq, t_V, t_H], writes=[t_p])
                        mm(p[:, 0:NCH], A[:, g, :], V[:, g, b, 0:NCH], True)
                        for ri in range(2):
                            mm(p[:, 1:NCH], CqRI[:, g, ri, :], Hb[:, 0, ri, gp, b, 0:NCH - 1])
                        mm(p[:, 32:NCH], A[:, 16 + g, :], V[:, g, b, 32:NCH])
                        mm(p[:, 0:32], A[:, 16 + g, :], V[:, g, b, NCH:320])
                        for ri in range(2):
                            mm(p[:, 32:NCH], CqRI[:, 16 + g, ri, :], Hb[:, 1, ri, gp, b, 1:257])
                            mm(p[:, 0:31], CqRI[:, 16 + g, ri, :], Hb[:, 1, ri, gp, b, 257:NCH])
                        if i % 2 == 0:
                            kb.op("act", lambda e: e.copy(out=Yc[:, g, b, :], in_=p[:, :]), reads=[t_p], writes=[t_Y])
                        else:
                            kb.op("dve", lambda e: e.tensor_copy(out=Yc[:, g, b, :], in_=p[:, :]), reads=[t_p], writes=[t_Y])
                        i += 1
                pu = Ring(kb, 2, [128, 64, 8], F32, kind="ps")
                yyr = Ring(kb, 2, [128, 512], F32)
                for gt in range(2):
                    for b in range(NB):
                        for seg in range(5):
                            nk = 64 if seg < 4 else 32
                            p, t_p = pu.next()
                            first = True
                            for t in range(8):
                                for gl in range(8):
                                    kb.op("pe", lambda e: e.matmul(p[:, 0:nk, t], Wsel[:, t, 112 - 16 * gl:240 - 16 * gl], Yc[:, gt * 8 + gl, b, seg * 64:seg * 64 + nk],
                                                                   start=first, stop=True, skip_group_check=True), reads=[t_W, t_Y], writes=[t_p])
                                    first = False
                            tok0 = b * TPB + seg * 512
                            nt = nk * 8
                            yy, t_yy = yyr.next()
                            kb.op("dve", lambda e: e.scalar_tensor_tensor(out=yy[:, 0:nt], in0=U[:, gt, tok0:tok0 + nt], scalar=dd[:, gt:gt + 1], op0=ALU.mult,
                                                                          in1=p[:, 0:nk, :].rearrange("p k t -> p (k t)"), op1=ALU.add),
                                  reads=[t_U, t_misc, t_p], writes=[t_yy])
                            kb.op("act", lambda e: e.activation(out=G[:, gt, tok0:tok0 + nt], in_=yy[:, 0:nt], func=AF.Gelu_apprx_tanh), reads=[t_yy], writes=[t_G])
                pz = Ring(kb, 2, [128, 512], F32, kind="ps")
                sgr = Ring(kb, 2, [128, 512], BF16); sor = Ring(kb, 2, [128, 512], BF16)
                for tt_ in range(NTOK // 512):
                    for mt in range(2):
                        p, t_p = pz.next()
                        for gt in range(2):
                            kb.op("pe", lambda e: e.matmul(p[:, :], wg[:, gt, mt * 128:(mt + 1) * 128], G[:, gt, tt_ * 512:(tt_ + 1) * 512], start=(gt == 0), stop=(gt == 1)),
                                  reads=[t_misc, t_G], writes=[t_p])
                        sg_, t_sg = sgr.next()
                        kb.op("act", lambda e: e.activation(out=sg_[:], in_=p[:, :], func=AF.Sigmoid), reads=[t_p], writes=[t_sg])
                        so, t_so = sor.next()
                        kb.op("dve", lambda e: e.tensor_tensor(out=so[:], in0=G[:, mt, tt_ * 512:(tt_ + 1) * 512], in1=sg_[:], op=ALU.mult), reads=[t_G, t_sg], writes=[t_so])
                        kb.dma("sp", [(self.catT[:, 6 + mt, tt_ * 512:(tt_ + 1) * 512], so[:])], reads=[t_so])

    def groups(self, with_ctx):
        gs = []
        for b in range(NB):
            if with_ctx:
                gs.append((b * TPB, CTX, 2))
            for i in range(4):
                gs.append((b * TPB + CTX + i * 512, 512, b))
        return gs

    def post_norm_tile(self, halves, t_halves, xt, t_x, gg, t_gg, ms, R):
        kb = self.kb
        st, t_st = R["stat2"].next()
        for nh in range(2):
            jk, t_jk = R["junk2"].next()
            kb.op("act", lambda e: e.activation(out=jk[:], in_=halves[nh], func=AF.Square, accum_out=st[:, nh:nh + 1]),
                  reads=[t_halves[nh], t_st], writes=[t_jk, t_st])
        kb.op("dve", lambda e: e.tensor_tensor(out=st[:, 2:3], in0=st[:, 0:1], in1=st[:, 1:2], op=ALU.add), reads=[t_st], writes=[t_st])
        kb.op("act", lambda e: e.activation(out=st[:, 3:4], in_=st[:, 2:3], func=AF.Sqrt, scale=1.0 / D, bias=self.eps_t[:, 0:1]), reads=[t_st], writes=[t_st])
        kb.op("dve", lambda e: e.reciprocal(out=st[:, 4:5], in_=st[:, 3:4]), reads=[t_st], writes=[t_st])
        tmp, t_t = R["tmp"].next()
        for nh in range(2):
            kb.op("dve", lambda e: e.scalar_tensor_tensor(out=tmp[:, nh * 512:(nh + 1) * 512], in0=halves[nh], scalar=st[:, 4:5], op0=ALU.mult,
                                                          in1=gg[:, ms, nh * 512:(nh + 1) * 512], op1=ALU.mult),
                  reads=[t_halves[nh], t_st, t_gg], writes=[t_t])
        xn, t_xn = R["xn"].next()
        kb.op("pool", lambda e: e.tensor_tensor(out=xn[:], in0=tmp[:], in1=xt[:], op=ALU.add), reads=[t_t, t_x], writes=[t_xn])
        return xn, t_xn

    def pn1(self, halves, t_halves, R):
        kb = self.kb
        st, t_st = R["stat2"].next()
        for nh in range(2):
            jk, t_jk = R["junk2"].next()
            kb.op("act", lambda e: e.activation(out=jk[:], in_=halves[nh], func=AF.Square, accum_out=st[:, nh:nh + 1]),
                  reads=[t_halves[nh], t_st], writes=[t_jk, t_st])
        kb.op("dve", lambda e: e.tensor_tensor(out=st[:, 2:3], in0=st[:, 0:1], in1=st[:, 1:2], op=ALU.add), reads=[t_st], writes=[t_st])
        kb.op("act", lambda e: e.activation(out=st[:, 3:4], in_=st[:, 2:3], func=AF.Sqrt, scale=1.0 / D, bias=self.eps_t[:, 0:1]), reads=[t_st], writes=[t_st])
        kb.op("dve", lambda e: e.reciprocal(out=st[:, 4:5], in_=st[:, 3:4]), reads=[t_st], writes=[t_st])
        return st, t_st

    def pn2(self, halves, t_halves, st, t_st, xt, t_x, gg, t_gg, ms, R):
        kb = self.kb
        tmp, t_t = R["tmp"].next()
        for nh in range(2):
            kb.op("dve", lambda e: e.scalar_tensor_tensor(out=tmp[:, nh * 512:(nh + 1) * 512], in0=halves[nh], scalar=st[:, 4:5], op0=ALU.mult,
                                                          in1=gg[:, ms, nh * 512:(nh + 1) * 512], op1=ALU.mult),
                  reads=[t_halves[nh], t_st, t_gg], writes=[t_t])
        xn, t_xn = R["xn"].next()
        kb.op("pool", lambda e: e.tensor_tensor(out=xn[:], in0=tmp[:], in1=xt[:], op=ALU.add), reads=[t_t, t_x], writes=[t_xn])
        return xn, t_xn

    def nm1(self, xt, t_x, R):
        kb = self.kb
        junk, t_j = R["junk"].next()
        st, t_st = R["stat"].next()
        kb.op("act", lambda e: e.activation(out=junk[:], in_=xt[:], func=AF.Square, accum_out=st[:, 0:1]), reads=[t_x], writes=[t_j, t_st])
        kb.op("act", lambda e: e.activation(out=st[:, 1:2], in_=st[:, 0:1], func=AF.Sqrt, scale=1.0 / D, bias=self.eps_t[:, 0:1]), reads=[t_st], writes=[t_st])
        kb.op("dve", lambda e: e.reciprocal(out=st[:, 2:3], in_=st[:, 1:2]), reads=[t_st], writes=[t_st])
        return st, t_st

    def nm2(self, xt, t_x, st, t_st, gsc, sh, t_g, t_s, ms, R):
        kb = self.kb
        tmp, t_t = R["tmp"].next()
        kb.op("dve", lambda e: e.scalar_tensor_tensor(out=tmp[:], in0=xt[:], scalar=st[:, 2:3], op0=ALU.mult, in1=gsc[:, ms, :], op1=ALU.mult),
              reads=[t_x, t_st, t_g], writes=[t_t])
        hb, t_h = R["hb"].next()
        kb.op("pool", lambda e: e.tensor_tensor(out=hb[:], in0=tmp[:], in1=sh[:, ms, :], op=ALU.add), reads=[t_t, t_s], writes=[t_h])
        return hb, t_h

    @staticmethod
    def run_pipeline(n, stages):
        for it in range(n + len(stages) - 1):
            for s_, f in enumerate(stages):
                j = it - s_
                if 0 <= j < n:
                    f(j)

    def phase_wout(self, l, xsrc, xdst, need_ctx, prep_wout=False):
        kb, I = self.kb, self.I
        with kb.phase():
            gg, _, t_gg, _ = self.load_mod_tiles(l, 2, None, "norm_mix_post", plus_one=False)
            gsc2, sh2, t_g2, t_s2 = self.load_mod_tiles(l, 4, 3, "norm_ffn_pre")
            wo = kb.sb([128, 8, D], BF16); t_wo = Tok()
            wv = I["w_out"][l].rearrange("(k p) n -> p k n", p=128)
            kb.dma("pool", [(wo[:, :, c * 512:(c + 1) * 512], wv[:, :, c * 512:(c + 1) * 512]) for c in range(2)], writes=[t_wo])
            R = {"junk": Ring(kb, 1, [128, D], BF16), "stat": Ring(kb, 8, [128, 4], F32), "tmp": Ring(kb, 3, [128, D], F32),
                 "hb": Ring(kb, 3, [128, D], BF16), "stat2": Ring(kb, 8, [128, 8], F32), "junk2": Ring(kb, 1, [128, 512], BF16),
                 "xn": Ring(kb, 4, [128, D], F32)}
            xr = Ring(kb, 4, [128, D], F32)
            cgr = Ring(kb, 2, [128, 8, 512], BF16); hgr = Ring(kb, 2, [128, 8, 512], BF16)
            pm = [Ring(kb, 3, [128, 512], F32, kind="ps") for _ in range(2)]
            pT = Ring(kb, 2, [128, 8, 128], BF16, kind="ps")
            items = []
            for (tok0, w, ms) in self.groups(need_ctx):
                for j in range(w // 128):
                    items.append((tok0, w, ms, j))
            C = [dict() for _ in items]

            self.prep_begin()

            def s_mm(i):
                tok0, w, ms, j = items[i]
                if prep_wout:
                    self.prep_tick(1)
                if j == 0:
                    cg, t_cg = cgr.next()
                    kb.dma("sp", [(cg[:, :, 0:w], self.catT[:, :, tok0:tok0 + w])], writes=[t_cg])
                    self._cg = (cg, t_cg)
                    self._hg = hgr.next()
                cg, t_cg = self._cg
                C[i]["hg"] = self._hg
                r0 = tok0 + j * 128
                xt, t_x = xr.next()
                kb.dma("sp", [(xt[:], xsrc[r0:r0 + 128, :])], writes=[t_x])
                hs, ths = [], []
                for nh in range(2):
                    p, t_p = pm[nh].next()
                    for k in range(8):
                        kb.op("pe", lambda e: e.matmul(p[:, :], cg[:, k, j * 128:(j + 1) * 128], wo[:, k, nh * 512:(nh + 1) * 512], start=(k == 0), stop=(k == 7)),
                              reads=[t_cg, t_wo], writes=[t_p])
                    hs.append(p[:, :]); ths.append(t_p)
                C[i].update(hs=hs, ths=ths, xt=xt, t_x=t_x)

            def s_p1(i):
                c = C[i]
                c["st"], c["t_st"] = self.pn1(c["hs"], c["ths"], R)

            def s_p2(i):
                c = C[i]
                tok0, w, ms, j = items[i]
                r0 = tok0 + j * 128
                c["xn"], c["t_xn"] = self.pn2(c["hs"], c["ths"], c["st"], c["t_st"], c["xt"], c["t_x"], gg, t_gg, ms, R)
                kb.dma("sp", [(xdst[r0:r0 + 128, :], c["xn"][:])], reads=[c["t_xn"]])

            def s_n1(i):
                c = C[i]
                c["st2"], c["t_st2"] = self.nm1(c["xn"], c["t_xn"], R)

            def s_n2(i):
                c = C[i]
                tok0, w, ms, j = items[i]
                r0 = tok0 + j * 128
                c["hb"], c["t_h"] = self.nm2(c["xn"], c["t_xn"], c["st2"], c["t_st2"], gsc2, sh2, t_g2, t_s2, ms, R)
                if l == 1:
                    kb.dma("sp", [(self.h2tm[r0:r0 + 128, :], c["hb"][:])], reads=[c["t_h"]])

            def s_t(i):
                c = C[i]
                tok0, w, ms, j = items[i]
                hg, t_hg = c["hg"]
                p, t_p = pT.next()
                for k in range(8):
                    kb.op("pe", lambda e: e.transpose(out=p[:, k, :], in_=c["hb"][:, k * 128:(k + 1) * 128], identity=self.ident_bf[:]),
                          reads=[c["t_h"], self.t_ident], writes=[t_p])
                kb.op("act", lambda e: e.copy(out=hg[:, :, j * 128:(j + 1) * 128], in_=p[:]), reads=[t_p], writes=[t_hg])
                if j == w // 128 - 1:
                    kb.dma("sp", [(self.h2T[:, :, tok0:tok0 + w], hg[:, :, 0:w])], reads=[t_hg])
                C[i].clear()
            self.run_pipeline(len(items), [s_mm, s_p1, s_p2, s_n1, s_n2, s_t])
            self.prep_flush()

    def phase_ffn(self, l, xsrc, xdst, moe, final):
        kb, I = self.kb, self.I
        FG = 512
        NFC = FG // 128
        for b in range(NB):
            tok0 = b * TPB + (CTX if moe else 0)
            TG = SEQ if moe else TPB
            ntile = TG // 128
            with kb.phase():
                hT = kb.sb([128, 8, TG], BF16); t_hT = Tok()
                kb.dma("sp", [(hT[:, k, :], self.h2T[:, k, tok0:tok0 + TG]) for k in range(8)], writes=[t_hT])
                acc = kb.sb([128, ntile, D], F32); t_acc = [Tok() for _ in range(ntile)]
                gates = None
                if moe:
                    gates = kb.sb([128, ntile, 8], F32); t_gt = Tok()
                    with kb.phase():
                        rb = kb.sb([128, 8, 8], BF16); t_rb = Tok()
                        kb.dma("pool", [(rb[:], I["moe_router"][0].rearrange("(k p) e -> p k e", p=128))], writes=[t_rb])
                        pl = Ring(kb, 2, [128, 8], F32, kind="ps")
                        wk = Ring(kb, 2, [128, 6, 8], F32); sm = Ring(kb, 2, [128, 8], F32)
                        for j in range(ntile):
                            p, t_p = pl.next()
                            for k in range(8):
                                kb.op("pe", lambda e: e.matmul(p[:, :], hT[:, k, j * 128:(j + 1) * 128], rb[:, k, :], start=(k == 0), stop=(k == 7)),
                                      reads=[t_hT, t_rb], writes=[t_p])
                            w_, t_w = wk.next(); s_, t_s = sm.next()
                            T = [t_w, t_s]
                            kb.op("dve", lambda e: e.tensor_reduce(out=s_[:, 0:1], in_=p[:, :], op=ALU.max, axis=AX.X), reads=[t_p], writes=T)
                            kb.op("dve", lambda e: e.tensor_scalar(out=s_[:, 1:2], in0=s_[:, 0:1], scalar1=-1.0, scalar2=None, op0=ALU.mult), reads=T, writes=T)
                            kb.op("act", lambda e: e.activation(out=w_[:, 0, :], in_=p[:, :], func=AF.Exp, bias=s_[:, 1:2]), reads=[t_p] + T, writes=T)
                            kb.op("dve", lambda e: e.tensor_scalar(out=w_[:, 1, :], in0=w_[:, 0, :], scalar1=1.0, scalar2=None, op0=ALU.is_lt), reads=T, writes=T)
                            kb.op("dve", lambda e: e.tensor_tensor(out=w_[:, 2, :], in0=w_[:, 0, :], in1=w_[:, 1, :], op=ALU.mult), reads=T, writes=T)
                            kb.op("dve", lambda e: e.tensor_reduce(out=s_[:, 2:3], in_=w_[:, 2, :], op=ALU.max, axis=AX.X), reads=T, writes=T)
                            kb.op("dve", lambda e: e.tensor_scalar(out=s_[:, 3:4], in0=s_[:, 2:3], scalar1=1.0, scalar2=None, op0=ALU.add), reads=T, writes=T)
                            kb.op("dve", lambda e: e.reciprocal(out=s_[:, 4:5], in_=s_[:, 3:4]), reads=T, writes=T)
                            kb.op("dve", lambda e: e.tensor_scalar(out=w_[:, 3, :], in0=w_[:, 0, :], scalar1=s_[:, 2:3], scalar2=None, op0=ALU.is_ge), reads=T, writes=T)
                            kb.op("dve", lambda e: e.tensor_tensor(out=w_[:, 4, :], in0=w_[:, 0, :], in1=w_[:, 3, :], op=ALU.mult), reads=T, writes=T)
                            kb.op("dve", lambda e: e.tensor_scalar(out=gates[:, j, :], in0=w_[:, 4, :], scalar1=s_[:, 4:5], scalar2=None, op0=ALU.mult), reads=T, writes=[t_gt])
                with kb.phase():
                    wgr = Ring(kb, 2, [128, 8, FG], BF16); wur = Ring(kb, 2, [128, 8, FG], BF16); wdr = Ring(kb, 2, [128, NFC, D], BF16)
                    actr = Ring(kb, 2, [128, NFC, TG], BF16)
                    sgr = Ring(kb, 3, [128, 512], BF16); gtm = Ring(kb, 2, [128, 512], F32) if moe else None
                    evr = Ring(kb, 2, [128, 512], F32)
                    pG = Ring(kb, 2, [128, 512], F32, kind="ps"); pU = Ring(kb, 2, [128, 512], F32, kind="ps")
                    pD = Ring(kb, 3, [128, 512], F32, kind="ps"); pB = Ring(kb, 1, [128, 4, 128], F32, kind="ps")
                    gbr = Ring(kb, 2, [128, TG], BF16) if moe else None
                    gxr = Ring(kb, 2, [128, 128], BF16) if moe else None
                    experts = range(N_EXP) if moe else [0]
                    F = F_EXPERT if moe else F_DENSE
                    state = {"first": True, "nbank": 0}
                    pending = []

                    def emit_down(at, t_at, wd_, t_wd, nfc, tiles, first):
                        for j in tiles:
                            for nh in range(2):
                                p, t_p = pD.next()
                                for fc in range(nfc):
                                    kb.op("pe", lambda e: e.matmul(p[:, :], at[:, fc, j * 128:(j + 1) * 128], wd_[:, fc, nh * 512:(nh + 1) * 512],
                                                                   start=(fc == 0), stop=(fc == nfc - 1)), reads=[t_at, t_wd], writes=[t_p])
                                dst = acc[:, j, nh * 512:(nh + 1) * 512]
                                if first:
                                    kb.op("act", lambda e: e.copy(out=dst, in_=p[:, :]), reads=[t_p], writes=[t_acc[j]])
                                else:
                                    kb.op("dve", lambda e: e.tensor_tensor(out=dst, in0=p[:, :], in1=dst, op=ALU.add), reads=[t_p, t_acc[j]], writes=[t_acc[j]])
                                state["nbank"] += 1

                    for ex in experts:
                        if moe:
                            Wg, Wu, Wd = I["moe_w_gate"][0, ex], I["moe_w_up"][0, ex], I["moe_w_down"][0, ex]
                            gb, t_gb = gbr.next()
                            for q4 in range(ntile // 4):
                                p, t_p = pB.next()
                                for i in range(4):
                                    j = q4 * 4 + i
                                    gx, t_gx = gxr.next()
                                    kb.op("dve", lambda e: e.tensor_copy(out=gx[:], in_=gates[:, j, ex:ex + 1].broadcast_to([128, 128])), reads=[t_gt], writes=[t_gx])
                                    kb.op("pe", lambda e: e.matmul(p[:, i, :], gx[:], self.ident_bf[:], start=(i == 0), stop=True, skip_group_check=True), reads=[t_gx, self.t_ident], writes=[t_p])
                                kb.op("act", lambda e: e.copy(out=gb[:, q4 * 512:(q4 + 1) * 512], in_=p[:].rearrange("p a b -> p (a b)")), reads=[t_p], writes=[t_gb])
                        else:
                            Wg, Wu, Wd = I["ffn_w_gate"][0], I["ffn_w_up"][0], I["ffn_w_down"][0]
                        wgv = Wg.rearrange("(k p) n -> p k n", p=128); wuv = Wu.rearrange("(k p) n -> p k n", p=128)
                        wdv = Wd.rearrange("(c p) n -> p c n", p=128)
                        f0 = 0
                        while f0 < F:
                            fw = min(FG, F - f0)
                            nfc = fw // 128
                            wg_, t_wg = wgr.next(); wu_, t_wu = wur.next(); wd_, t_wd = wdr.next()
                            kb.dma("pool", [(wg_[:, 0:4, 0:fw], wgv[:, 0:4, f0:f0 + fw]), (wg_[:, 4:8, 0:fw], wgv[:, 4:8, f0:f0 + fw])], writes=[t_wg])
                            kb.dma("pool", [(wu_[:, 0:4, 0:fw], wuv[:, 0:4, f0:f0 + fw]), (wu_[:, 4:8, 0:fw], wuv[:, 4:8, f0:f0 + fw])], writes=[t_wu])
                            kb.dma("pool", [(wd_[:, 0:nfc, :], wdv[:, f0 // 128:f0 // 128 + nfc, :])], writes=[t_wd])
                            at, t_at = actr.next()
                            c0 = 0
                            while c0 < TG:
                                n = min(512, TG - c0)
                                for fc in range(nfc):
                                    g_, t_g = pG.next(); u_, t_u = pU.next()
                                    for k in range(8):
                                        kb.op("pe", lambda e: e.matmul(g_[:, 0:n], wg_[:, k, fc * 128:(fc + 1) * 128], hT[:, k, c0:c0 + n], start=(k == 0), stop=(k == 7)),
                                              reads=[t_wg, t_hT], writes=[t_g])
                                    for k in range(8):
                                        kb.op("pe", lambda e: e.matmul(u_[:, 0:n], wu_[:, k, fc * 128:(fc + 1) * 128], hT[:, k, c0:c0 + n], start=(k == 0), stop=(k == 7)),
                                              reads=[t_wu, t_hT], writes=[t_u])
                                    sg_, t_sg = sgr.next()
                                    kb.op("act", lambda e: e.activation(out=sg_[:, 0:n], in_=g_[:, 0:n], func=AF.Silu), reads=[t_g], writes=[t_sg])
                                    if moe:
                                        tm, t_tm = gtm.next()
                                        kb.op("dve", lambda e: e.tensor_tensor(out=tm[:, 0:n], in0=u_[:, 0:n], in1=gb[:, c0:c0 + n], op=ALU.mult), reads=[t_u, t_gb], writes=[t_tm])
                                        kb.op("dve", lambda e: e.tensor_tensor(out=at[:, fc, c0:c0 + n], in0=tm[:, 0:n], in1=sg_[:, 0:n], op=ALU.mult), reads=[t_tm, t_sg], writes=[t_at])
                                    else:
                                        kb.op("dve", lambda e: e.tensor_tensor(out=at[:, fc, c0:c0 + n], in0=u_[:, 0:n], in1=sg_[:, 0:n], op=ALU.mult), reads=[t_u, t_sg], writes=[t_at])
                                while pending:
                                    pending.pop(0)()
                                tiles = list(range(c0 // 128, (c0 + n) // 128))
                                pending.append(lambda at=at, t_at=t_at, wd_=wd_, t_wd=t_wd, nfc=nfc, tiles=tiles, first=state["first"]:
                                               emit_down(at, t_at, wd_, t_wd, nfc, tiles, first))
                                c0 += n
                            state["first"] = False
                            f0 += fw
                    while pending:
                        pending.pop(0)()
                with kb.phase():
                    gg, _, t_gg, _ = self.load_mod_tiles(l, 5, None, "norm_ffn_post", plus_one=False)
                    R = {"tmp": Ring(kb, 3, [128, D], F32), "stat2": Ring(kb, 8, [128, 8], F32), "junk2": Ring(kb, 1, [128, 512], BF16),
                         "xn": Ring(kb, 3, [128, D], F32)}
                    xr = Ring(kb, 4, [128, D], F32)
                    C = [dict() for _ in range(ntile)]

                    def f_load(j):
                        xt, t_x = xr.next()
                        kb.dma("sp", [(xt[:], xsrc[tok0 + j * 128:tok0 + (j + 1) * 128, :])], writes=[t_x])
                        C[j].update(xt=xt, t_x=t_x, hs=[acc[:, j, 0:512], acc[:, j, 512:1024]], ths=[t_acc[j], t_acc[j]])

                    def f_p1(j):
                        C[j]["st"], C[j]["t_st"] = self.pn1(C[j]["hs"], C[j]["ths"], R)

                    def f_p2(j):
                        c = C[j]
                        is_ctx = (not moe) and j < 2
                        ms = 2 if is_ctx else b
                        xn, t_xn = self.pn2(c["hs"], c["ths"], c["st"], c["t_st"], c["xt"], c["t_x"], gg, t_gg, ms, R)
                        if final:
                            o0 = b * SEQ + j * 128
                            kb.dma("sp", [(xdst[o0:o0 + 128, :], xn[:])], reads=[t_xn])
                        else:
                            r0 = tok0 + j * 128
                            kb.dma("sp", [(xdst[r0:r0 + 128, :], xn[:])], reads=[t_xn])
                    self.run_pipeline(ntile, [f_load, f_p1, f_p2])

    def moe_declare(self):
        sc = self.scratch
        self.h2tm = sc("h2tm", [NTOK, D], BF16)
        nrow = N_EXP * 7 * 128
        self.Wg_s = sc("Wg_s", [nrow, 8 * 512], BF16); self.Wu_s = sc("Wu_s", [nrow, 8 * 512], BF16)
        self.Wd_s = sc("Wd_s", [nrow, 4 * D], BF16)
        self.hsorted = sc("hsorted", [MOE_SLOTS, D], BF16)
        self.ysorted = sc("ysorted", [MOE_SLOTS, D], F32)

    def moe_prep_gen(self, rings):
        kb, I = self.kb, self.I
        inflight = self.prep_inflight
        for ex in range(N_EXP):
            wgv = I["moe_w_gate"][0, ex].rearrange("(k p) n -> p k n", p=128)
            wuv = I["moe_w_up"][0, ex].rearrange("(k p) n -> p k n", p=128)
            wdv = I["moe_w_down"][0, ex].rearrange("(c p) n -> p c n", p=128)
            for fg in range(7):
                r0 = (ex * 7 + fg) * 128
                for kind, src, dst in ((0, wgv, self.Wg_s), (0, wuv, self.Wu_s), (1, wdv, self.Wd_s)):
                    t, tk = self.prep_rings_cur[0][kind].next()
                    if kind == 0:
                        kb.dma("pool", [(t[:, 0:4, :], src[:, 0:4, fg * 512:(fg + 1) * 512]), (t[:, 4:8, :], src[:, 4:8, fg * 512:(fg + 1) * 512])], writes=[tk])
                        flat = t[:].rearrange("p k n -> p (k n)")
                    else:
                        kb.dma("pool", [(t[:], src[:, fg * 4:(fg + 1) * 4, :])], writes=[tk])
                        flat = t[:].rearrange("p c n -> p (c n)")
                    inflight.append((dst[r0:r0 + 128, :], flat, tk))
                    if len(inflight) > 2:
                        d_, f_, k_ = inflight.pop(0)
                        kb.dma("pool", [(d_, f_)], reads=[k_])
                    yield
        self.prep_flush()
        yield

    def prep_begin(self):
        if self.prep is not None:
            self.prep_rings_cur[0] = self.prep_rings()

    def prep_tick(self, k=1):
        for _ in range(k):
            if self.prep is not None and next(self.prep, "done") == "done":
                self.prep = None

    def prep_flush(self):
        while self.prep_inflight:
            d_, f_, k_ = self.prep_inflight.pop(0)
            self.kb.dma("pool", [(d_, f_)], reads=[k_])

    def prep_rings(self):
        kb = self.kb
        return [Ring(kb, 3, [128, 8, 512], BF16), Ring(kb, 2, [128, 4, D], BF16)]

    def phase_moe_prep(self):
        kb = self.kb
        if self.prep is None:
            return
        with kb.phase():
            rings = self.prep_rings()
            self.prep_rings_cur[0] = rings
            for _ in self.prep:
                pass
            self.prep_flush()
            self.prep = None

    def phase_moe_sparse(self, l, xsrc, xdst):
        kb, I = self.kb, self.I
        NT = NB * SEQ // 128
        BIG = 1.0e6
        MAGIC = 12582912.0
        lat_tok0 = lambda j: (j // 16) * TPB + CTX + (j % 16) * 128
        with kb.phase():
            glo = kb.sb([128, NT], F32); ghi = kb.sb([128, NT], F32)
            ilo = kb.sb([128, NT], I32); ihi = kb.sb([128, NT], I32)
            widx = kb.sb([128, MOE_TILES, 7], I32)
            t_rt = Tok()
            with kb.phase():
                rb = kb.sb([128, 8, 8], BF16); t_rb = Tok()
                kb.dma("pool", [(rb[:], I["moe_router"][0].rearrange("(k p) e -> p k e", p=128))], writes=[t_rb])
                tri = kb.sb([128, 2, 128], BF16); io7 = kb.sb([128, 7], F32); thr = kb.sb([128, MOE_TILES], F32)
                kb.dma("sp", [(tri[:], I["moe_tri"][:, :, :]), (io7[:], I["moe_iota"][:, :]), (thr[:], I["moe_thr"][:, :])], writes=[t_rb])
                hT = kb.sb([128, 8, NB * SEQ], BF16); t_hT = Tok()
                kb.dma("sp", [(hT[:, k, b * SEQ:(b + 1) * SEQ], self.h2T[:, k, b * TPB + CTX:(b + 1) * TPB]) for k in range(8) for b in range(NB)], writes=[t_hT])
                gates = kb.sb([128, NT, 8], F32); maskf = kb.sb([128, NT, 8], F32); maskb = kb.sb([128, NT, 8], BF16)
                rank = kb.sb([128, NT, 8], F32)
                t_g, t_m, t_rk = Tok(), Tok(), Tok()
                pl = Ring(kb, 2, [128, 8], F32, kind="ps")
                wk = Ring(kb, 2, [128, 6, 8], F32); sm = Ring(kb, 2, [128, 8], F32)
                for j in range(NT):
                    p, t_p = pl.next()
                    for k in range(8):
                        kb.op("pe", lambda e: e.matmul(p[:, :], hT[:, k, j * 128:(j + 1) * 128], rb[:, k, :], start=(k == 0), stop=(k == 7)),
                              reads=[t_hT, t_rb], writes=[t_p])
                    w_, t_w = wk.next(); s_, t_s = sm.next()
                    T = [t_w, t_s]
                    kb.op("dve", lambda e: e.tensor_reduce(out=s_[:, 0:1], in_=p[:, :], op=ALU.max, axis=AX.X), reads=[t_p], writes=T)
                    kb.op("dve", lambda e: e.tensor_scalar(out=s_[:, 1:2], in0=s_[:, 0:1], scalar1=-1.0, scalar2=None, op0=ALU.mult), reads=T, writes=T)
                    kb.op("act", lambda e: e.activation(out=w_[:, 0, :], in_=p[:, :], func=AF.Exp, bias=s_[:, 1:2]), reads=[t_p] + T, writes=T)
                    kb.op("dve", lambda e: e.tensor_scalar(out=w_[:, 1, :], in0=w_[:, 0, :], scalar1=1.0, scalar2=None, op0=ALU.is_lt), reads=T, writes=T)
                    kb.op("dve", lambda e: e.tensor_tensor(out=w_[:, 2, :], in0=w_[:, 0, :], in1=w_[:, 1, :], op=ALU.mult), reads=T, writes=T)
                    kb.op("dve", lambda e: e.tensor_reduce(out=s_[:, 2:3], in_=w_[:, 2, :], op=ALU.max, axis=AX.X), reads=T, writes=T)
                    kb.op("dve", lambda e: e.tensor_scalar(out=s_[:, 3:4], in0=s_[:, 2:3], scalar1=1.0, scalar2=None, op0=ALU.add), reads=T, writes=T)
                    kb.op("dve", lambda e: e.reciprocal(out=s_[:, 4:5], in_=s_[:, 3:4]), reads=T, writes=T)
                    kb.op("dve", lambda e: e.tensor_scalar(out=maskf[:, j, :], in0=w_[:, 0, :], scalar1=s_[:, 2:3], scalar2=None, op0=ALU.is_ge), reads=T, writes=[t_m])
                    kb.op("dve", lambda e: e.tensor_copy(out=maskb[:, j, :], in_=maskf[:, j, :]), reads=[t_m], writes=[t_m])
                    kb.op("dve", lambda e: e.tensor_tensor(out=w_[:, 4, :], in0=w_[:, 0, :], in1=maskf[:, j, :], op=ALU.mult), reads=T + [t_m], writes=T)
                    kb.op("dve", lambda e: e.tensor_scalar(out=gates[:, j, :], in0=w_[:, 4, :], scalar1=s_[:, 4:5], scalar2=None, op0=ALU.mult), reads=T, writes=[t_g])
                prk = Ring(kb, 2, [128, 8], F32, kind="ps")
                for j in range(NT + 1):
                    p, t_p = prk.next()
                    n = 0
                    for i in range(min(j, NT)):
                        kb.op("pe", lambda e: e.matmul(p[:, :], tri[:, 1, :], maskb[:, i, :], start=(n == 0), stop=(j == NT and i == NT - 1)),
                              reads=[t_m, t_rb], writes=[t_p])
                        n += 1
                    if j < NT:
                        kb.op("pe", lambda e: e.matmul(p[:, :], tri[:, 0, :], maskb[:, j, :], start=(n == 0), stop=True), reads=[t_m, t_rb], writes=[t_p])
                        kb.op("act", lambda e: e.copy(out=rank[:, j, :], in_=p[:, :]), reads=[t_p], writes=[t_rk])
                    else:
                        tot = kb.sb([128, 8], F32)
                        kb.op("act", lambda e: e.copy(out=tot[:], in_=p[:, :]), reads=[t_p], writes=[t_rk])
                T = [t_rk]
                ts = lambda o, x, s1, o0: kb.op("dve", lambda e: e.tensor_scalar(out=o, in0=x, scalar1=s1, scalar2=None, op0=o0), reads=T + [t_m, t_g, t_rb], writes=T)
                tt = lambda o, x, y, op: kb.op("dve", lambda e: e.tensor_tensor(out=o, in0=x, in1=y, op=op), reads=T + [t_m, t_g, t_rb], writes=T)
                red = lambda o, x, op: kb.op("dve", lambda e: e.tensor_reduce(out=o, in_=x, op=op, axis=AX.X), reads=T, writes=T)
                pad = kb.sb([128, 8], F32); incl = kb.sb([128, 8], F32); base = kb.sb([128, 8], F32)
                ts(pad[:], tot[:], 511.0, ALU.add); ts(pad[:], pad[:], 1.0 / 512, ALU.mult)
                ts(pad[:], pad[:], -0.5 + 1.0 / 1024, ALU.add); ts(pad[:], pad[:], MAGIC, ALU.add); ts(pad[:], pad[:], MAGIC, ALU.subtract)
                ts(pad[:], pad[:], 512.0, ALU.mult)
                kb.op("dve", lambda e: e.tensor_copy(out=incl[:, 0:1], in_=pad[:, 0:1]), reads=T, writes=T)
                for e_ in range(1, 8):
                    tt(incl[:, e_:e_ + 1], incl[:, e_ - 1:e_], pad[:, e_:e_ + 1], ALU.add)
                tt(base[:], incl[:], pad[:], ALU.subtract)
                slot = kb.sb([128, NT, 8], F32); v1 = kb.sb([128, NT, 8], F32); v2 = kb.sb([128, NT, 8], F32)
                slo = kb.sb([128, NT], F32); shi = kb.sb([128, NT], F32)
                tt(slot[:], rank[:], base[:].unsqueeze(1).broadcast_to([128, NT, 8]), ALU.add)
                tt(v2[:], slot[:], maskf[:], ALU.mult)
                ts(v1[:], maskf[:], -BIG, ALU.mult); ts(v1[:], v1[:], BIG, ALU.add); tt(v1[:], v1[:], v2[:], ALU.add)
                red(slo[:], v1[:], ALU.min)
                tt(v1[:], v2[:], maskf[:], ALU.add); ts(v1[:], v1[:], -1.0, ALU.add)
                red(shi[:], v1[:], ALU.max)
                for sl_, g_ in ((slo, glo), (shi, ghi)):
                    tt(v1[:], slot[:], sl_[:].unsqueeze(2).broadcast_to([128, NT, 8]), ALU.is_equal)
                    tt(v1[:], v1[:], gates[:], ALU.mult)
                    kb.op("dve", lambda e: e.tensor_reduce(out=g_[:], in_=v1[:], op=ALU.add, axis=AX.X), reads=T, writes=T + [t_rt])
                kb.op("dve", lambda e: e.tensor_copy(out=ilo[:], in_=slo[:]), reads=T, writes=[t_rt])
                kb.op("dve", lambda e: e.tensor_copy(out=ihi[:], in_=shi[:]), reads=T, writes=[t_rt])
                cmp_ = kb.sb([128, MOE_TILES, 8], F32); cnt = kb.sb([128, MOE_TILES], F32); wf = kb.sb([128, MOE_TILES, 7], F32)
                tt(cmp_[:], incl[:].unsqueeze(1).broadcast_to([128, MOE_TILES, 8]), thr[:].unsqueeze(2).broadcast_to([128, MOE_TILES, 8]), ALU.is_le)
                red(cnt[:], cmp_[:], ALU.add)
                ts(cnt[:], cnt[:], 7.0, ALU.min); ts(cnt[:], cnt[:], 896.0, ALU.mult)
                tt(wf[:], cnt[:].unsqueeze(2).broadcast_to([128, MOE_TILES, 7]), io7[:].unsqueeze(1).broadcast_to([128, MOE_TILES, 7]), ALU.add)
                kb.op("dve", lambda e: e.tensor_copy(out=widx[:], in_=wf[:]), reads=T, writes=[t_rt])
            with kb.phase():
                t_fill = Tok()
                hr = Ring(kb, 3, [128, D], BF16); icr = Ring(kb, 4, [128, 1], I32)
                for j in range(NT):
                    hb, t_h = hr.next()
                    r0 = lat_tok0(j)
                    kb.dma("sp", [(hb[:], self.h2tm[r0:r0 + 128, :])], writes=[t_h])
                    for ix in (ilo, ihi):
                        kb.dma_custom("pool", lambda g: g.indirect_dma_start(out=self.hsorted[:, :], out_offset=bass.IndirectOffsetOnAxis(ap=ix[:, j:j + 1], axis=0),
                                                                             in_=hb[:, :], in_offset=None, bounds_check=None),
                                      reads=[t_h, t_rt, t_fill])
            with kb.phase():
                hsr = Ring(kb, 4, [128, D], BF16); hTr = Ring(kb, 2, [128, 8, MOE_TS], BF16)
                wgr = Ring(kb, 3, [128, 8, 512], BF16); wur = Ring(kb, 3, [128, 8, 512], BF16); wdr = Ring(kb, 3, [128, 4, D], BF16)
                atr = Ring(kb, 2, [128, 4, MOE_TS], BF16); sgr = Ring(kb, 3, [128, 512], BF16)
                accr = Ring(kb, 2, [128, 4, D], F32); icr = Ring(kb, 8, [128, 1], I32)
                pT = Ring(kb, 1, [128, 8, 128], BF16, kind="ps")
                pG = Ring(kb, 2, [128, 512], F32, kind="ps"); pU = Ring(kb, 2, [128, 512], F32, kind="ps"); pD = Ring(kb, 3, [128, 512], F32, kind="ps")
                pending = []

                def emit_down(at, t_at, wd_, t_wd, acc, t_acc, first, last, i):
                    for sub in range(4):
                        for nh in range(2):
                            p, t_p = pD.next()
                            for fc in range(4):
                                kb.op("pe", lambda e: e.matmul(p[:, :], at[:, fc, sub * 128:(sub + 1) * 128], wd_[:, fc, nh * 512:(nh + 1) * 512], start=(fc == 0), stop=(fc == 3)),
                                      reads=[t_at, t_wd], writes=[t_p])
                            dst = acc[:, sub, nh * 512:(nh + 1) * 512]
                            if first:
                                kb.op("act", lambda e: e.copy(out=dst, in_=p[:, :]), reads=[t_p], writes=[t_acc])
                            else:
                                kb.op("dve", lambda e: e.tensor_tensor(out=dst, in0=p[:, :], in1=dst, op=ALU.add), reads=[t_p, t_acc], writes=[t_acc])
                    if last:
                        kb.dma("sp", [(self.ysorted[i * MOE_TS + sub * 128:i * MOE_TS + (sub + 1) * 128, :], acc[:, sub, :]) for sub in range(4)], reads=[t_acc])

                def build_hT(i):
                    hT, t_hT = hTr.next()
                    for sub in range(4):
                        hs, t_hs = hsr.next()
                        r0 = i * MOE_TS + sub * 128
                        kb.dma("sp", [(hs[:], self.hsorted[r0:r0 + 128, :])], writes=[t_hs])
                        p, t_p = pT.next()
                        for k in range(8):
                            kb.op("pe", lambda e: e.transpose(out=p[:, k, :], in_=hs[:, k * 128:(k + 1) * 128], identity=self.ident_bf[:]),
                                  reads=[t_hs, self.t_ident], writes=[t_p])
                        kb.op("act", lambda e: e.copy(out=hT[:, :, sub * 128:(sub + 1) * 128], in_=p[:]), reads=[t_p], writes=[t_hT])
                    return hT, t_hT
                nxt_hT = build_hT(0)
                for i in range(MOE_TILES):
                    hT, t_hT = nxt_hT
                    acc, t_acc = accr.next()
                    for fg in range(7):
                        if fg == 5 and i + 1 < MOE_TILES:
                            nxt_hT = build_hT(i + 1)
                        wg_, t_wg = wgr.next(); wu_, t_wu = wur.next(); wd_, t_wd = wdr.next()
                        for (wt, tw, src) in ((wg_, t_wg, self.Wg_s), (wu_, t_wu, self.Wu_s), (wd_, t_wd, self.Wd_s)):
                            flat = wt[:].rearrange("p a n -> p (a n)")
                            kb.dma_custom("pool", lambda g: g.indirect_dma_start(out=flat, out_offset=None, in_=src[:, :],
                                                                                 in_offset=bass.IndirectOffsetOnAxis(ap=widx[:, i, fg:fg + 1], axis=0),
                                                                                 bounds_check=None),
                                          reads=[t_rt], writes=[tw])
                        at, t_at = atr.next()
                        for fc in range(4):
                            g_, t_g = pG.next(); u_, t_u = pU.next()
                            for k in range(8):
                                kb.op("pe", lambda e: e.matmul(g_[:, :], wg_[:, k, fc * 128:(fc + 1) * 128], hT[:, k, :], start=(k == 0), stop=(k == 7)),
                                      reads=[t_wg, t_hT], writes=[t_g])
                            for k in range(8):
                                kb.op("pe", lambda e: e.matmul(u_[:, :], wu_[:, k, fc * 128:(fc + 1) * 128], hT[:, k, :], start=(k == 0), stop=(k == 7)),
                                      reads=[t_wu, t_hT], writes=[t_u])
                            sg_, t_sg = sgr.next()
                            kb.op("act", lambda e: e.activation(out=sg_[:], in_=g_[:, :], func=AF.Silu), reads=[t_g], writes=[t_sg])
                            kb.op("dve", lambda e: e.tensor_tensor(out=at[:, fc, :], in0=u_[:, :], in1=sg_[:], op=ALU.mult), reads=[t_u, t_sg], writes=[t_at])
                        while pending:
                            pending.pop(0)()
                        pending.append(lambda at=at, t_at=t_at, wd_=wd_, t_wd=t_wd, acc=acc, t_acc=t_acc, first=(fg == 0), last=(fg == 6), i=i:
                                       emit_down(at, t_at, wd_, t_wd, acc, t_acc, first, last, i))
                while pending:
                    pending.pop(0)()
            with kb.phase():
                gg, _, t_gg, _ = self.load_mod_tiles(l, 5, None, "norm_ffn_post", plus_one=False)
                R = {"tmp": Ring(kb, 2, [128, D], F32), "stat2": Ring(kb, 4, [128, 8], F32), "junk2": Ring(kb, 1, [128, 512], BF16),
                     "xn": Ring(kb, 2, [128, D], F32)}
                icr = Ring(kb, 4, [128, 1], I32)
                xr = Ring(kb, 4, [128, D], F32); ylr = Ring(kb, 3, [128, D], F32); yhr = Ring(kb, 3, [128, D], F32); mxr = Ring(kb, 4, [128, D], F32)
                R["stat2"] = Ring(kb, 8, [128, 8], F32); R["tmp"] = Ring(kb, 3, [128, D], F32); R["xn"] = Ring(kb, 3, [128, D], F32)
                C = [dict() for _ in range(NT)]

                def c_load(j):
                    r0 = lat_tok0(j)
                    xt, t_x = xr.next()
                    kb.dma("sp", [(xt[:], xsrc[r0:r0 + 128, :])], writes=[t_x])
                    yl, t_yl = ylr.next(); yh, t_yh = yhr.next()
                    for (yt, ty, ix) in ((yl, t_yl, ilo), (yh, t_yh, ihi)):
                        kb.dma_custom("pool", lambda g: g.indirect_dma_start(out=yt[:, :], out_offset=None, in_=self.ysorted[:, :],
                                                                             in_offset=bass.IndirectOffsetOnAxis(ap=ix[:, j:j + 1], axis=0),
                                                                             bounds_check=None),
                                      reads=[t_rt], writes=[ty])
                    C[j].update(xt=xt, t_x=t_x, yl=yl, t_yl=t_yl, yh=yh, t_yh=t_yh)

                def c_mix(j):
                    c = C[j]
                    mx, t_mx = mxr.next()
                    kb.op("dve", lambda e: e.tensor_scalar(out=mx[:], in0=c["yl"][:], scalar1=glo[:, j:j + 1], scalar2=None, op0=ALU.mult), reads=[c["t_yl"], t_rt], writes=[t_mx])
                    kb.op("dve", lambda e: e.scalar_tensor_tensor(out=mx[:], in0=c["yh"][:], scalar=ghi[:, j:j + 1], op0=ALU.mult, in1=mx[:], op1=ALU.add),
                          reads=[c["t_yh"], t_rt, t_mx], writes=[t_mx])
                    c.update(hs=[mx[:, 0:512], mx[:, 512:1024]], ths=[t_mx, t_mx])

                def c_p1(j):
                    C[j]["st"], C[j]["t_st"] = self.pn1(C[j]["hs"], C[j]["ths"], R)

                def c_p2(j):
                    c = C[j]
                    xn, t_xn = self.pn2(c["hs"], c["ths"], c["st"], c["t_st"], c["xt"], c["t_x"], gg, t_gg, j // 16, R)
                    kb.dma("sp", [(xdst[j * 128:(j + 1) * 128, :], xn[:])], reads=[t_xn])
                self.run_pipeline(NT, [c_load, c_mix, c_p1, c_p2])
def core_inputs(inputs, core, consts):
    b0 = core * NB
    m = {}
    xs = []
    for b in range(b0, b0 + NB):
        xs.append(inputs["ctx"][b]); xs.append(inputs["x"][b])
    m["xin"] = np.ascontiguousarray(np.concatenate(xs, axis=0), dtype=np.float32)
    cv = np.stack([inputs["c"][b0], inputs["c"][b0 + 1], inputs["c_ctx"]], axis=0)
    m["cT"] = np.ascontiguousarray(cv.reshape(3, 8, 128).transpose(2, 1, 0), dtype=np.float32)
    dup = lambda a: np.concatenate([a, a], axis=0)
    L = inputs["s5_a_re"].shape[0]
    par = np.zeros((L, 128, 3, 32), np.float32); sb = np.zeros((L, 128, 32, 2, 16), np.float32); sc = np.zeros((L, 128, 32, 2, 16), np.float32)
    for l in range(L):
        par[l, :, 0, :] = dup(inputs["s5_a_re"][l].reshape(32, 64).T)
        par[l, :, 1, :] = dup(inputs["s5_a_im"][l].reshape(32, 64).T)
        par[l, :, 2, :] = inputs["s5_log_dt"][l].reshape(1, 32)
        sb[l, :, :, 0, :] = dup(inputs["s5_b_re"][l].reshape(32, 64, 16).transpose(1, 0, 2))
        sb[l, :, :, 1, :] = dup(inputs["s5_b_im"][l].reshape(32, 64, 16).transpose(1, 0, 2))
        sc[l, :, :, 0, :] = dup(inputs["s5_c_re"][l].reshape(32, 16, 64).transpose(2, 0, 1))
        sc[l, :, :, 1, :] = dup(inputs["s5_c_im"][l].reshape(32, 16, 64).transpose(2, 0, 1))
    m["s5_par"], m["s5_b"], m["s5_c"] = par, sb, sc
    m["s5_dd"] = np.ascontiguousarray(inputs["s5_d"].reshape(L, 2, 128).transpose(0, 2, 1))
    return m


_CACHE = {}


def build_program():
    P = Prog()
    P.declare(); P.consts_sb()
    xin = P.I["xin"]
    P.phase_adaln(0)
    P.prep = P.moe_prep_gen(None)
    P.phase_win(0, xin, prep_win=True)
    P.phase_attn(0, True, prep_every=2); P.phase_fnet(0, True); P.phase_s5(0)
    P.phase_wout(0, xin, P.xA, True, prep_wout=True)
    P.phase_ffn(0, P.xA, P.xB, False, False)
    P.phase_adaln(1)
    P.phase_win(1, P.xB, prep_win=True)
    P.phase_attn(1, False, prep_every=2); P.phase_fnet(1, False); P.phase_s5(1)
    P.phase_wout(1, P.xB, P.xA, False)
    P.phase_moe_prep()
    P.phase_moe_sparse(1, P.xA, P.out)
    P.kb.barrier()
    return P


def kernel(**inputs):
    inputs = {k: np.asarray(v) for k, v in inputs.items()}
    if "P" not in _CACHE:
        _CACHE["P"] = build_program()
    P = _CACHE["P"]
    n_cores = 8
    shared = {}
    for k in P.I:
        if k in P.consts:
            shared[k] = P.consts[k]
        elif k in inputs:
            shared[k] = np.ascontiguousarray(inputs[k], dtype=np.float32)
    in_maps = []
    for core in range(n_cores):
        m = core_inputs(inputs, core, P.consts)
        for k, v in shared.items():
            if k not in m:
                m[k] = v
        in_maps.append(m)
    res = run_bass_kernel_spmd(P.kb.nc, in_maps, core_ids=list(range(n_cores)))
    outs = [np.asarray(r["out"], dtype=np.float32).reshape(NB, SEQ, D) for r in res.results]
    return np.concatenate(outs, axis=0)
```

```python
import contextlib, math
import numpy as np
import ml_dtypes
import concourse.bass as bass
import concourse.mybir as mybir
from concourse.bass_utils import run_bass_kernel_spmd

F32 = mybir.dt.float32
BF16 = mybir.dt.bfloat16
I32 = mybir.dt.int32
AF = mybir.ActivationFunctionType
ALU = mybir.AluOpType
AX = mybir.AxisListType
NPBF = ml_dtypes.bfloat16

SEM_LIMIT = 30000
EPS = 1e-6
D = 1024
NB = 2
CTX = 256
SEQ = 2048
TPB = CTX + SEQ
NTOK = NB * TPB
NTILE = NTOK // 128
TILES_PB = TPB // 128
DEPTH = 2
F_DENSE = 2816
F_EXPERT = 3584
N_EXP = 8
MOE_TS = 512
MOE_TILES = (2 * NB * SEQ + N_EXP * (MOE_TS - 1)) // MOE_TS + 1
MOE_SLOTS = MOE_TILES * MOE_TS


class Tok:
    __slots__ = ("name", "w", "r")

    def __init__(self, name=""):
        self.name = name
        self.w = None
        self.r = {}


class Eng:
    def __init__(self, kb, name, eng):
        self.kb, self.name, self.eng = kb, name, eng
        self.sem = None
        self.cnt = 0
        self.waited = {}

    def new_sem(self):
        self.sem = self.kb.es.enter_context(self.kb.nc.semaphore(f"s_{self.name}_{self.kb.nsem}"))
        self.kb.nsem += 1
        self.cnt = 0


class KB:
    def __init__(self):
        self.nc = bass.Bass("TRN2", target_bir_lowering=False)
        self.es = contextlib.ExitStack()
        self.nsem = 0
        nc = self.nc
        self.E = {}
        for name, eng in (("pe", nc.tensor), ("act", nc.scalar), ("dve", nc.vector),
                          ("pool", nc.gpsimd), ("sp", nc.sync)):
            e = Eng(self, name, eng)
            e.new_sem()
            self.E[name] = e
        self.dma_sems, self.dma_vals = [], []
        for i in range(64):
            self.dma_sems.append(self.es.enter_context(nc.semaphore(f"s_dma{i}")))
            self.dma_vals.append(0)
        self.dma_cursor = {"sp": 0, "pool": 0}
        self.nalloc = 0
        self.ninstr = 0
        self.stack = [self.es]
        self.dram_t = {}

    def sb(self, shape, dtype, name=None):
        self.nalloc += 1
        return self.stack[-1].enter_context(self.nc.sbuf_tensor(name or f"sb{self.nalloc}", list(shape), dtype))

    def ps(self, shape, dtype, name=None):
        self.nalloc += 1
        esz = 4 if dtype == F32 else 2
        n = int(np.prod(shape[1:]))
        assert n * esz <= 2048, shape
        t = self.stack[-1].enter_context(self.nc.psum_tensor(name or f"ps{self.nalloc}", [128, 2048 // esz], dtype))
        ap = t[0:shape[0], 0:n]
        if len(shape) > 2:
            names = [f"d{i}" for i in range(len(shape) - 1)]
            pat = "p (" + " ".join(names) + ") -> p " + " ".join(names)
            ap = ap.rearrange(pat, **{nm: int(v) for nm, v in zip(names[:-1], shape[1:-1])})
        return ap

    def dram(self, name, shape, dtype, kind="Internal"):
        t = self.nc.dram_tensor(name, list(shape), dtype, kind=kind)
        self.dram_t[name] = t
        return t.ap()

    @contextlib.contextmanager
    def phase(self):
        st = contextlib.ExitStack()
        self.stack.append(st)
        try:
            yield
        finally:
            self.barrier()
            self.stack.pop()
            st.close()

    def barrier(self):
        evs = [(e.sem, e.cnt) for e in self.E.values() if e.cnt > 0]
        evs += [(s, v) for s, v in zip(self.dma_sems, self.dma_vals) if v > 0]
        for e in self.E.values():
            for ev in evs:
                if ev[0] is e.sem:
                    continue
                self._wait(e, ev)

    def _wait(self, e, ev):
        if ev is None:
            return
        if isinstance(ev, list):
            for e_ in ev:
                self._wait(e, e_)
            return
        sem, val = ev
        k = id(sem)
        if e.waited.get(k, 0) >= val:
            return
        if e.name == "pe" and sem is e.sem:
            return
        e.eng.wait_ge(sem, val)
        e.waited[k] = val

    def _deps(self, e, reads, writes):
        for t in reads:
            self._wait(e, t.w)
        for t in writes:
            self._wait(e, t.w)
            for ev in t.r.values():
                self._wait(e, ev)

    def _commit(self, ev, reads, writes):
        for t in reads:
            for e_ in (ev if isinstance(ev, list) else [ev]):
                t.r[id(e_[0])] = e_
        for t in writes:
            t.w = ev
            t.r = {}

    def op(self, en, fn, reads=(), writes=()):
        e = self.E[en]
        if e.cnt >= SEM_LIMIT:
            e.new_sem()
        self._deps(e, reads, writes)
        ins = fn(e.eng)
        e.cnt += 1
        ins.then_inc(e.sem, 1)
        ev = (e.sem, e.cnt)
        self._commit(ev, reads, writes)
        self.ninstr += 1
        return ev

    def _next_dma_sem(self, qn):
        half = len(self.dma_sems) // 2
        c = self.dma_cursor[qn]
        self.dma_cursor[qn] = (c + 1) % half
        return c + (half if qn == "pool" else 0)

    def dma(self, qn, pairs, reads=(), writes=(), **kw):
        e = self.E[qn]
        self._deps(e, reads, writes)
        if qn == "pool" and len(pairs) > 1:
            evs = []
            for (o, i) in pairs:
                j = self._next_dma_sem(qn)
                sem = self.dma_sems[j]
                if self.dma_vals[j] > 0:
                    self._wait(e, (sem, self.dma_vals[j]))
                if self.dma_vals[j] > SEM_LIMIT:
                    raise RuntimeError("dma sem overflow")
                e.eng.dma_start(out=o, in_=i, **kw).then_inc(sem, 16)
                self.dma_vals[j] += 16
                self.ninstr += 1
                evs.append((sem, self.dma_vals[j]))
            self._commit(evs, reads, writes)
            return evs
        j = self._next_dma_sem(qn)
        sem = self.dma_sems[j]
        if self.dma_vals[j] > 0:
            self._wait(e, (sem, self.dma_vals[j]))
        if self.dma_vals[j] > SEM_LIMIT:
            raise RuntimeError("dma sem overflow")
        for (o, i) in pairs:
            e.eng.dma_start(out=o, in_=i, **kw).then_inc(sem, 16)
            self.dma_vals[j] += 16
            self.ninstr += 1
        ev = (sem, self.dma_vals[j])
        self._commit(ev, reads, writes)
        return ev

    def dma_custom(self, qn, fn, reads=(), writes=()):
        e = self.E[qn]
        self._deps(e, reads, writes)
        j = self._next_dma_sem(qn)
        sem = self.dma_sems[j]
        if self.dma_vals[j] > 0:
            self._wait(e, (sem, self.dma_vals[j]))
        if self.dma_vals[j] > SEM_LIMIT:
            raise RuntimeError("dma sem overflow")
        fn(e.eng).then_inc(sem, 16)
        self.dma_vals[j] += 16
        self.ninstr += 1
        ev = (sem, self.dma_vals[j])
        self._commit(ev, reads, writes)
        return ev

    def finish(self, toks):
        e = self.E["sp"]
        for t in toks:
            self._wait(e, t.w)
            for ev in t.r.values():
                self._wait(e, ev)


class Ring:
    def __init__(self, kb, n, shape, dtype, kind="sb", name="ring"):
        self.items = []
        for i in range(n):
            t = kb.sb(shape, dtype) if kind == "sb" else kb.ps(shape, dtype)
            self.items.append((t, Tok(f"{name}{i}")))
        self.i = 0

    def next(self):
        it = self.items[self.i]
        self.i = (self.i + 1) % len(self.items)
        return it

def host_consts():
    c = {}
    c["ident_bf"] = np.eye(128, dtype=np.float32).astype(NPBF)
    c["ident_f"] = np.eye(128, dtype=np.float32)
    n_freq = 16
    inv = 10000.0 ** (-np.arange(n_freq, dtype=np.float64) / n_freq)
    t = np.arange(SEQ)
    rows = (t // 64).astype(np.float64)
    cols = (t % 64).astype(np.float64)
    ang = np.stack([rows[:, None] * inv, cols[:, None] * inv], axis=1)
    ang = (np.stack([rows[:, None].astype(np.float32) * inv.astype(np.float32),
                     cols[:, None].astype(np.float32) * inv.astype(np.float32)], axis=1)).astype(np.float64)
    cos = np.cos(ang); sin = np.sin(ang)
    cos2 = np.stack([cos, cos], axis=2)
    sinS = np.stack([-sin, sin], axis=2)
    c["rope_cos"] = np.ascontiguousarray(cos2.reshape(16, 128, 64).transpose(1, 0, 2)).astype(np.float32)
    c["rope_sin"] = np.ascontiguousarray(sinS.reshape(16, 128, 64).transpose(1, 0, 2)).astype(np.float32)
    def dftm(L):
        t = np.arange(L)
        ph = (np.outer(t, t) % L).astype(np.float64) * (2 * np.pi / L)
        sc = 1.0 / math.sqrt(L * 64)
        return (np.cos(ph) * sc).astype(NPBF), (np.sin(ph) * sc).astype(NPBF)
    c["dft_c"], c["dft_s"] = dftm(SEQ)
    c["dftc_c"], c["dftc_s"] = dftm(CTX)
    t = np.arange(64)
    ph = np.outer(t, t) * (2 * np.pi / 64)
    cb = np.zeros((128, 2, 128), np.float32)
    for g in range(2):
        cb[g * 64:(g + 1) * 64, 0, g * 64:(g + 1) * 64] = np.cos(ph)
        cb[g * 64:(g + 1) * 64, 1, g * 64:(g + 1) * 64] = -np.sin(ph)
    c["cblk"] = cb
    k = np.arange(128)
    ws = np.zeros((128, 8, 240), np.float32)
    for r in range(8):
        for kk in range(128):
            if kk // 16 == r:
                ws[kk, r, 112 + kk % 16] = 1.0
    c["wsel"] = ws.astype(NPBF)
    sblk = (k // 16)[:, None]; tblk = (k // 16)[None, :]
    c["s5_mask"] = np.ascontiguousarray(np.stack([(tblk >= sblk), (tblk <= sblk)], axis=1).astype(np.float32))
    tri = np.zeros((128, 2, 128), np.float32)
    tri[:, 0, :] = (k[:, None] < k[None, :])
    tri[:, 1, :] = 1.0
    c["moe_tri"] = tri.astype(NPBF)
    c["moe_iota"] = np.ascontiguousarray((np.arange(7)[None, :] * 128 + k[:, None]).astype(np.float32))
    c["moe_thr"] = np.ascontiguousarray(np.broadcast_to((np.arange(MOE_TILES) * MOE_TS)[None, :], (128, MOE_TILES)).astype(np.float32))
    return c


class Prog:
    def __init__(self, debug=()):
        self.kb = KB()
        self.debug = set(debug)
        self.I = {}
        self.consts = host_consts()
        self.prep = None
        self.prep_rings_cur = [None]
        self.prep_inflight = []

    def inp(self, name, shape, dtype):
        ap = self.kb.dram(name, shape, dtype, kind="ExternalInput")
        self.I[name] = ap
        return ap

    def scratch(self, name, shape, dtype):
        kind = "ExternalOutput" if name in self.debug else "Internal"
        return self.kb.dram(name, shape, dtype, kind=kind)

    def declare(self):
        inp = self.inp
        inp("xin", [NTOK, D], F32)
        inp("cT", [128, 8, 3], F32)
        inp("ada_w", [DEPTH, D, 6 * D], F32)
        inp("ada_b", [DEPTH, 6 * D], F32)
        for n in ("norm_mix_pre", "norm_mix_post", "norm_ffn_pre", "norm_ffn_post"):
            inp(n, [DEPTH, D], F32)
        inp("w_in", [DEPTH, D, 2048], F32)
        inp("w_out", [DEPTH, D, D], F32)
        for n in ("diff_lq1", "diff_lk1", "diff_lq2", "diff_lk2"):
            inp(n, [DEPTH, 64], F32)
        inp("diff_subln", [DEPTH, 128], F32)
        inp("fnet_w", [DEPTH, 256, 256], F32)
        inp("s5_par", [DEPTH, 128, 3, 32], F32)
        inp("s5_b", [DEPTH, 128, 32, 2, 16], F32)
        inp("s5_c", [DEPTH, 128, 32, 2, 16], F32)
        inp("s5_dd", [DEPTH, 128, 2], F32)
        inp("s5_w_glu", [DEPTH, 256, 256], F32)
        inp("ffn_w_gate", [1, D, F_DENSE], F32); inp("ffn_w_up", [1, D, F_DENSE], F32); inp("ffn_w_down", [1, F_DENSE, D], F32)
        inp("moe_router", [1, D, N_EXP], F32)
        inp("moe_w_gate", [1, N_EXP, D, F_EXPERT], F32); inp("moe_w_up", [1, N_EXP, D, F_EXPERT], F32); inp("moe_w_down", [1, N_EXP, F_EXPERT, D], F32)
        for k, v in self.consts.items():
            inp(k, list(v.shape), BF16 if v.dtype == NPBF else F32)
        sc = self.scratch
        self.modD = [sc(f"modD{l}", [3, 6 * D], F32) for l in range(DEPTH)]
        self.qT = sc("qT", [128, 4, NTOK], BF16)
        self.kT = sc("kT", [128, 4, NTOK], BF16)
        self.vD = sc("vD", [128, 4, NTILE, 130], BF16)
        self.fD = sc("fD", [128, NTILE, 256], BF16)
        self.uT = sc("uT", [128, 2, NTOK], BF16)
        self.catT = sc("catT", [128, 8, NTOK], BF16)
        self.xA = sc("xA", [NTOK, D], F32)
        self.xB = sc("xB", [NTOK, D], F32)
        self.h2T = sc("h2T", [128, 8, NTOK], BF16)
        self.out = self.kb.dram("out", [NB * SEQ, D], F32, kind="ExternalOutput")
        self.moe_declare()

    def phase_adaln(self, l):
        kb, I = self.kb, self.I
        with kb.phase():
            cT = kb.sb([128, 8, 3], F32); sT = kb.sb([128, 8, 3], F32)
            bias = kb.sb([3, 6 * D], F32); mod = kb.sb([3, 6 * D], F32)
            t_c, t_s, t_b, t_m = Tok(), Tok(), Tok(), Tok()
            kb.dma("sp", [(cT[:], I["cT"][:, :, :])], writes=[t_c])
            kb.dma("sp", [(bias[:], I["ada_b"][l].partition_broadcast(3))], writes=[t_b])
            kb.op("act", lambda e: e.activation(out=sT[:], in_=cT[:], func=AF.Silu), reads=[t_c], writes=[t_s])
            wr = Ring(kb, 2, [128, 8, 512], F32, name="adaw")
            pr = Ring(kb, 2, [128, 512], F32, kind="ps", name="adap")
            wv = I["ada_w"][l].rearrange("(k p) n -> p k n", p=128)
            for nt in range(12):
                w, tw = wr.next()
                kb.dma("sp", [(w[:], wv[:, :, nt * 512:(nt + 1) * 512])], writes=[tw])
                p, tp = pr.next()
                for k in range(8):
                    kb.op("pe", lambda e: e.matmul(p[0:3, :], sT[:, k, :], w[:, k, :], start=(k == 0), stop=(k == 7)),
                          reads=[t_s, tw], writes=[tp])
                kb.op("dve", lambda e: e.tensor_tensor(out=mod[0:3, nt * 512:(nt + 1) * 512], in0=p[0:3, :],
                                                       in1=bias[0:3, nt * 512:(nt + 1) * 512], op=ALU.add),
                      reads=[tp, t_b], writes=[t_m])
            kb.dma("sp", [(self.modD[l][:, :], mod[0:3, :])], reads=[t_m])
            if l == 0:
                z = kb.sb([128, 8192], BF16); t_z = Tok()
                kb.op("pool", lambda e: e.memset(z[:], 0.0), writes=[t_z])
                hsv = self.hsorted.rearrange("(a p r) d -> a p (r d)", p=128, r=8)
                for a in range(MOE_SLOTS // 1024):
                    kb.dma("sp", [(hsv[a], z[:])], reads=[t_z])

    def load_mod_tiles(self, l, off_sc, off_sh, gain_name, plus_one=True):
        kb, I = self.kb, self.I
        gsc = kb.sb([128, 3, D], F32); sh = kb.sb([128, 3, D], F32); gn = kb.sb([128, D], F32)
        t_g, t_s, t_n = Tok(), Tok(), Tok()
        kb.dma("sp", [(gn[:], I[gain_name][l].partition_broadcast(128))], writes=[t_n])
        kb.dma("sp", [(gsc[:, j, :], self.modD[l][j, off_sc * D:(off_sc + 1) * D].partition_broadcast(128)) for j in range(3)],
               writes=[t_g])
        if off_sh is not None:
            kb.dma("sp", [(sh[:, j, :], self.modD[l][j, off_sh * D:(off_sh + 1) * D].partition_broadcast(128)) for j in range(3)],
                   writes=[t_s])
        for j in range(3):
            kb.op("dve", lambda e: e.scalar_tensor_tensor(out=gsc[:, j, :], in0=gsc[:, j, :], scalar=(1.0 if plus_one else 0.0), op0=ALU.add,
                                                          in1=gn[:], op1=ALU.mult),
                  reads=[t_g, t_n], writes=[t_g])
        return gsc, sh, t_g, t_s

    def norm_mod_tile(self, xt, t_x, gsc, sh, t_g, t_s, ms, R):
        kb = self.kb
        junk, t_j = R["junk"].next()
        st, t_st = R["stat"].next()
        kb.op("act", lambda e: e.activation(out=junk[:], in_=xt[:], func=AF.Square, accum_out=st[:, 0:1]),
              reads=[t_x], writes=[t_j, t_st])
        kb.op("act", lambda e: e.activation(out=st[:, 1:2], in_=st[:, 0:1], func=AF.Sqrt, scale=1.0 / D, bias=self.eps_t[:, 0:1]),
              reads=[t_st], writes=[t_st])
        kb.op("dve", lambda e: e.reciprocal(out=st[:, 2:3], in_=st[:, 1:2]), reads=[t_st], writes=[t_st])
        tmp, t_t = R["tmp"].next()
        kb.op("dve", lambda e: e.scalar_tensor_tensor(out=tmp[:], in0=xt[:], scalar=st[:, 2:3], op0=ALU.mult,
                                                      in1=gsc[:, ms, :], op1=ALU.mult),
              reads=[t_x, t_st, t_g], writes=[t_t])
        hb, t_h = R["hb"].next()
        kb.op("pool", lambda e: e.tensor_tensor(out=hb[:], in0=tmp[:], in1=sh[:, ms, :], op=ALU.add),
              reads=[t_t, t_s], writes=[t_h])
        return hb, t_h

    def consts_sb(self):
        kb, I = self.kb, self.I
        self.ident_bf = kb.sb([128, 128], BF16); self.t_ident = Tok()
        kb.dma("sp", [(self.ident_bf[:], I["ident_bf"][:, :])], writes=[self.t_ident])
        self.ident_f = kb.sb([128, 128], F32)
        kb.dma("sp", [(self.ident_f[:], I["ident_f"][:, :])], writes=[self.t_ident])
        self.eps_t = kb.sb([128, 1], F32)
        kb.op("pool", lambda e: e.memset(self.eps_t[:], EPS), writes=[self.t_ident])

    def phase_win(self, l, xsrc, prep_win=False):
        kb, I = self.kb, self.I
        with kb.phase():
            gsc, sh, t_g, t_s = self.load_mod_tiles(l, 1, 0, "norm_mix_pre")
            wb = kb.sb([128, 8, 2048], BF16); t_w = Tok()
            wv = I["w_in"][l].rearrange("(k p) n -> p k n", p=128)
            kb.dma("pool", [(wb[:, :, c * 512:(c + 1) * 512], wv[:, :, c * 512:(c + 1) * 512]) for c in range(4)], writes=[t_w])
            rc = kb.sb([128, 16, 64], F32); rs = kb.sb([128, 16, 64], F32); t_r = Tok()
            kb.dma("sp", [(rc[:], I["rope_cos"][:, :, :]), (rs[:], I["rope_sin"][:, :, :])], writes=[t_r])
            R = {"junk": Ring(kb, 1, [128, D], BF16), "stat": Ring(kb, 8, [128, 4], F32), "tmp": Ring(kb, 2, [128, D], F32),
                 "hb": Ring(kb, 3, [128, D], BF16)}
            xr = Ring(kb, 4, [128, D], F32)
            pT = Ring(kb, 1, [128, 8, 128], BF16, kind="ps")
            hTr = Ring(kb, 3, [128, 8, 128], BF16)
            pq = Ring(kb, 2, [128, 512], F32, kind="ps"); pk = Ring(kb, 2, [128, 512], F32, kind="ps")
            pv = Ring(kb, 1, [128, 512], F32, kind="ps"); pfu = Ring(kb, 1, [128, 512], F32, kind="ps")
            ptq = Ring(kb, 1, [128, 2, 4, 128], BF16, kind="ps")
            ropet = Ring(kb, 2, [128, 512], F32); ropem = Ring(kb, 2, [128, 256], F32)
            qbr = Ring(kb, 2, [128, 512], BF16); kbr = Ring(kb, 2, [128, 512], BF16)
            qTg = Ring(kb, 2, [128, 4, 512], BF16); kTg = Ring(kb, 2, [128, 4, 512], BF16)
            uTg = Ring(kb, 2, [128, 2, 512], BF16)
            vtr = Ring(kb, 2, [128, 4, 130], BF16); fbr = Ring(kb, 2, [128, 256], BF16)
            for vt, tv in vtr.items:
                kb.op("pool", lambda e: e.memset(vt[:, :, 128:130], 1.0), writes=[tv])
            C = [dict() for _ in range(NTILE)]
            G = {}

            def s0(ti):
                xt, t_x = xr.next()
                kb.dma("sp", [(xt[:], xsrc[ti * 128:(ti + 1) * 128, :])], writes=[t_x])
                st, t_st = self.nm1(xt, t_x, R)
                C[ti].update(xt=xt, t_x=t_x, st=st, t_st=t_st)

            def s1(ti):
                c = C[ti]
                b, j = divmod(ti, TILES_PB)
                ms = 2 if j < 2 else b
                c["hb"], c["t_h"] = self.nm2(c["xt"], c["t_x"], c["st"], c["t_st"], gsc, sh, t_g, t_s, ms, R)

            def s2(ti):
                c = C[ti]
                p, t_p = pT.next()
                for k in range(8):
                    kb.op("pe", lambda e: e.transpose(out=p[:, k, :], in_=c["hb"][:, k * 128:(k + 1) * 128], identity=self.ident_bf[:]),
                          reads=[c["t_h"], self.t_ident], writes=[t_p])
                hT, t_hT = hTr.next()
                kb.op("act", lambda e: e.copy(out=hT[:], in_=p[:]), reads=[t_p], writes=[t_hT])
                c.update(hT=hT, t_hT=t_hT)

            def s_mm(ti):
                c = C[ti]
                hT, t_hT = c["hT"], c["t_hT"]
                outs = []
                for ring, c0, n in ((pq, 0, 512), (pk, 512, 512), (pv, 1024, 512), (pfu, 1536, 256)):
                    pp, t_pp = ring.next()
                    for k in range(8):
                        kb.op("pe", lambda e: e.matmul(pp[:, 0:n], hT[:, k, :], wb[:, k, c0:c0 + n], start=(k == 0), stop=(k == 7)),
                              reads=[t_hT, t_w], writes=[t_pp])
                    outs.append((pp, t_pp))
                ppf, t_pf = outs[3]
                for ct in range(2):
                    for k in range(8):
                        kb.op("pe", lambda e: e.matmul(ppf[:, 256 + ct * 128:256 + (ct + 1) * 128], wb[:, k, 1792 + ct * 128:1792 + (ct + 1) * 128],
                                                       hT[:, k, :], start=(k == 0), stop=(k == 7)),
                              reads=[t_hT, t_w], writes=[t_pf])
                c["outs"] = outs

            def s_post(ti):
                c = C[ti]
                b, j = divmod(ti, TILES_PB)
                is_ctx = j < 2
                if j == 0 or (j >= 2 and (j - 2) % 4 == 0):
                    G["gq"] = qTg.next(); G["gk"] = kTg.next(); G["gu"] = uTg.next()
                    G["gstart"] = ti; G["gi"] = 0
                    G["gn"] = 2 if j == 0 else 4
                (gq, t_gq), (gk, t_gk), (gu, t_gu) = G["gq"], G["gk"], G["gu"]
                gi = G["gi"]
                (ppq, t_pq), (ppk, t_pk), (ppv, t_pv), (ppf, t_pf) = c["outs"]
                pt, t_pt = ptq.next()
                for which, (pp, t_pp), bring in ((0, (ppq, t_pq), qbr), (1, (ppk, t_pk), kbr)):
                    xb, t_xb = bring.next()
                    if is_ctx:
                        kb.op("dve", lambda e: e.tensor_copy(out=xb[:], in_=pp[:]), reads=[t_pp], writes=[t_xb])
                    else:
                        lt = j - 2
                        t1, t_t1 = ropet.next()
                        kb.op("dve", lambda e: e.tensor_tensor(out=t1[:].rearrange("p (a c) -> p a c", a=8), in0=pp[:].rearrange("p (a c) -> p a c", a=8),
                                                               in1=rc[:, lt:lt + 1, :].broadcast_to([128, 8, 64]), op=ALU.mult),
                              reads=[t_pp, t_r], writes=[t_t1])
                        x5 = pp[:].rearrange("p (a b j f) -> p a b j f", a=8, b=2, j=2)
                        t5 = t1[:].rearrange("p (a b j f) -> p a b j f", a=8, b=2, j=2)
                        o5 = xb[:].rearrange("p (a b j f) -> p a b j f", a=8, b=2, j=2)
                        s4 = rs[:, lt, :].rearrange("p (b j f) -> p b j f", b=2, j=2)
                        for jj in range(2):
                            m, t_m = ropem.next()
                            m4 = m[:].rearrange("p (a b f) -> p a b f", a=8, b=2)
                            kb.op("dve", lambda e: e.tensor_tensor(out=m4, in0=x5[:, :, :, 1 - jj, :],
                                                                   in1=s4[:, :, jj, :].unsqueeze(1).broadcast_to([128, 8, 2, 16]), op=ALU.mult),
                                  reads=[t_pp, t_r], writes=[t_m])
                            kb.op("dve", lambda e: e.tensor_tensor(out=o5[:, :, :, jj, :], in0=t5[:, :, :, jj, :], in1=m4, op=ALU.add),
                                  reads=[t_t1, t_m], writes=[t_xb])
                    for h in range(4):
                        kb.op("pe", lambda e: e.transpose(out=pt[:, which, h, :], in_=xb[:, h * 128:(h + 1) * 128], identity=self.ident_bf[:]),
                              reads=[t_xb, self.t_ident], writes=[t_pt])
                kb.op("act", lambda e: e.copy(out=gq[:, :, gi * 128:(gi + 1) * 128], in_=pt[:, 0, :, :]), reads=[t_pt], writes=[t_gq])
                kb.op("act", lambda e: e.copy(out=gk[:, :, gi * 128:(gi + 1) * 128], in_=pt[:, 1, :, :]), reads=[t_pt], writes=[t_gk])
                vt, t_v = vtr.next()
                kb.op("act", lambda e: e.copy(out=vt[:, :, 0:128], in_=ppv[:].rearrange("p (h d) -> p h d", h=4)), reads=[t_pv], writes=[t_v])
                kb.dma("sp", [(self.vD[:, :, ti, :], vt[:])], reads=[t_v])
                fb, t_f = fbr.next()
                kb.op("dve", lambda e: e.tensor_copy(out=fb[:], in_=ppf[:, 0:256]), reads=[t_pf], writes=[t_f])
                kb.dma("sp", [(self.fD[:, ti, :], fb[:])], reads=[t_f])
                kb.op("act", lambda e: e.copy(out=gu[:, :, gi * 128:(gi + 1) * 128], in_=ppf[:, 256:512].rearrange("p (c t) -> p c t", c=2)),
                      reads=[t_pf], writes=[t_gu])
                G["gi"] = gi + 1
                if G["gi"] == G["gn"]:
                    c0 = G["gstart"] * 128; w = G["gn"] * 128
                    kb.dma("sp", [(self.qT[:, :, c0:c0 + w], gq[:, :, 0:w])], reads=[t_gq])
                    kb.dma("sp", [(self.kT[:, :, c0:c0 + w], gk[:, :, 0:w])], reads=[t_gk])
                    kb.dma("sp", [(self.uT[:, :, c0:c0 + w], gu[:, :, 0:w])], reads=[t_gu])
                C[ti].clear()

            self.prep_begin()
            for it in range(NTILE + 4):
                if prep_win and it < NTILE:
                    self.prep_tick(1)
                for st_, off in ((s0, 0), (s1, 1), (s2, 2), (s_post, 4), (s_mm, 3)):
                    ti = it - off
                    if 0 <= ti < NTILE:
                        st_(ti)
            self.prep_flush()

    def phase_attn(self, l, need_ctx, prep_every=0):
        kb, I = self.kb, self.I
        lam_init = 0.8 - 0.6 * math.exp(-0.3 * l)
        with kb.phase():
            lq = kb.sb([128, 4, 64], F32); t_lq = Tok()
            kb.dma("sp", [(lq[:, i, :], I[n][l].partition_broadcast(128)) for i, n in
                          enumerate(("diff_lq1", "diff_lk1", "diff_lq2", "diff_lk2"))], writes=[t_lq])
            lt = kb.sb([128, 2, 64], F32); ls = kb.sb([128, 8], F32); t_ls = Tok()
            kb.op("dve", lambda e: e.tensor_tensor(out=lt[:, 0, :], in0=lq[:, 0, :], in1=lq[:, 1, :], op=ALU.mult), reads=[t_lq], writes=[t_ls])
            kb.op("dve", lambda e: e.tensor_tensor(out=lt[:, 1, :], in0=lq[:, 2, :], in1=lq[:, 3, :], op=ALU.mult), reads=[t_lq, t_ls], writes=[t_ls])
            kb.op("dve", lambda e: e.tensor_reduce(out=ls[:, 0:2], in_=lt[:], op=ALU.add, axis=AX.X), reads=[t_ls], writes=[t_ls])
            kb.op("act", lambda e: e.activation(out=ls[:, 2:4], in_=ls[:, 0:2], func=AF.Exp), reads=[t_ls], writes=[t_ls])
            kb.op("dve", lambda e: e.tensor_tensor(out=ls[:, 4:5], in0=ls[:, 3:4], in1=ls[:, 2:3], op=ALU.subtract), reads=[t_ls], writes=[t_ls])
            kb.op("dve", lambda e: e.tensor_scalar(out=ls[:, 5:6], in0=ls[:, 4:5], scalar1=-lam_init, scalar2=None, op0=ALU.add), reads=[t_ls], writes=[t_ls])
            nlam = ls[:, 5:6]
            sg = kb.sb([128, 128], F32); t_sg = Tok()
            kb.dma("sp", [(sg[:], I["diff_subln"][l].partition_broadcast(128))], writes=[t_sg])
            kb.op("dve", lambda e: e.tensor_scalar(out=sg[:], in0=sg[:], scalar1=1.0 - lam_init, scalar2=None, op0=ALU.mult), reads=[t_sg], writes=[t_sg])
            kr = Ring(kb, 2, [128, TPB], BF16); qr = Ring(kb, 2, [128, 2, TPB], BF16); vr = Ring(kb, 2, [128, TILES_PB, 130], BF16)
            psr = Ring(kb, 3, [128, 2, 256], F32, kind="ps")
            pacc = [Ring(kb, 2, [128, 2, 130], F32, kind="ps") for _ in range(2)]
            pto = Ring(kb, 1, [128, 2, 128], BF16, kind="ps")
            ptr = Ring(kb, 3, [128, 2, 256], BF16)
            o1r = Ring(kb, 4, [128, 128], F32); o2r = Ring(kb, 4, [128, 128], F32); junkr = Ring(kb, 1, [128, 128], BF16)
            str_ = Ring(kb, 6, [128, 8], F32); abr = Ring(kb, 2, [128, 128], BF16); aTr = Ring(kb, 2, [128, 256], BF16)
            for qz, t_qz in qr.items:
                kb.op("pool", lambda e: e.memset(qz[:], 0.0), writes=[t_qz])
            pending = []
            self.prep_begin()
            qt_count = 0
            for b in range(NB):
                t0 = b * TPB
                for h in range(4):
                    kT, t_k = kr.next(); qT, t_q = qr.next(); vv, t_v = vr.next()
                    kb.dma("sp", [(kT[:], self.kT[:, h, t0:t0 + TPB])], writes=[t_k])
                    kb.dma("sp", [(qT[m * 64:(m + 1) * 64, m, :], self.qT[m * 64:(m + 1) * 64, h, t0:t0 + TPB]) for m in range(2)], writes=[t_q])
                    kb.dma("sp", [(vv[:], self.vD[:, h, b * TILES_PB:(b + 1) * TILES_PB, :])], writes=[t_v])
                    qts = [(CTX + i * 256, list(range(TILES_PB))) for i in range(8)]
                    if need_ctx:
                        qts.append((0, [0, 1]))
                    for (q0, kts) in qts:
                        qt_count += 1
                        if prep_every and qt_count % prep_every == 0:
                            self.prep_tick(1)
                        a1, t_a1 = pacc[0].next(); a2, t_a2 = pacc[1].next()
                        accs = ((a1, t_a1), (a2, t_a2))

                        def emit_st(kt):
                            ps, t_ps = psr.next()
                            for m in range(2):
                                kb.op("pe", lambda e: e.matmul(ps[:, m, :], kT[:, kt * 128:(kt + 1) * 128],
                                                               qT[:, m, q0:q0 + 256], start=(m == 0), stop=True, skip_group_check=True),
                                      reads=[t_k, t_q], writes=[t_ps])
                            return ps, t_ps
                        ahead = [emit_st(kts[0])]
                        if len(kts) > 1:
                            ahead.append(emit_st(kts[1]))
                        for ki, kt in enumerate(kts):
                            ps, t_ps = ahead.pop(0)
                            if ki + 2 < len(kts):
                                ahead.append(emit_st(kts[ki + 2]))
                            pt, t_pt = ptr.next()
                            kb.op("act", lambda e: e.activation(out=pt[:], in_=ps[:], func=AF.Exp, scale=0.125), reads=[t_ps], writes=[t_pt])
                            for m in range(2):
                                for s in range(2):
                                    kb.op("pe", lambda e: e.matmul(accs[m][0][:, s, 0:129], pt[:, m, s * 128:(s + 1) * 128], vv[:, kt, 0:129],
                                                                   start=(ki == 0 and s == 0), stop=(ki == len(kts) - 1), skip_group_check=True),
                                          reads=[t_pt, t_v], writes=[accs[m][1]])
                            while pending and pending[0][0] <= ki:
                                pending.pop(0)[1]()
                        while pending:
                            pending.pop(0)[1]()
                        sts, o2s = [], []
                        for s in range(2):
                            st, t_st = str_.next()
                            kb.op("dve", lambda e: e.reciprocal(out=st[:, 0:1], in_=a1[:, s, 128:129]), reads=[t_a1], writes=[t_st])
                            kb.op("dve", lambda e: e.reciprocal(out=st[:, 1:2], in_=a2[:, s, 128:129]), reads=[t_a2, t_st], writes=[t_st])
                            kb.op("dve", lambda e: e.tensor_tensor(out=st[:, 2:3], in0=st[:, 1:2], in1=nlam, op=ALU.mult), reads=[t_st, t_ls], writes=[t_st])
                            o1, t_o1 = o1r.next(); o2, t_o2 = o2r.next()
                            kb.op("dve", lambda e: e.tensor_scalar(out=o1[:], in0=a1[:, s, 0:128], scalar1=st[:, 0:1], scalar2=None, op0=ALU.mult),
                                  reads=[t_a1, t_st], writes=[t_o1])
                            kb.op("dve", lambda e: e.scalar_tensor_tensor(out=o2[:], in0=a2[:, s, 0:128], scalar=st[:, 2:3], op0=ALU.mult,
                                                                          in1=o1[:], op1=ALU.add), reads=[t_a2, t_st, t_o1], writes=[t_o2])
                            kb.op("dve", lambda e: e.tensor_tensor(out=o1[:], in0=o2[:], in1=o2[:], op=ALU.mult), reads=[t_o2, t_o1], writes=[t_o1])
                            kb.op("dve", lambda e: e.tensor_reduce(out=st[:, 3:4], in_=o1[:], op=ALU.add, axis=AX.X), reads=[t_o1, t_st], writes=[t_st])
                            sts.append((st, t_st)); o2s.append((o2, t_o2))

                        def n2(sts=sts):
                            for st, t_st in sts:
                                kb.op("act", lambda e: e.activation(out=st[:, 4:5], in_=st[:, 3:4], func=AF.Ln, scale=1.0 / 128, bias=self.eps_t[:, 0:1]),
                                      reads=[t_st], writes=[t_st])
                                kb.op("act", lambda e: e.activation(out=st[:, 5:6], in_=st[:, 4:5], func=AF.Exp, scale=-0.5),
                                      reads=[t_st], writes=[t_st])

                        def n3(sts=sts, o2s=o2s, h=h, c0=t0 + q0):
                            po, t_po = pto.next()
                            for s in range(2):
                                st, t_st = sts[s]; o2, t_o2 = o2s[s]
                                ab, t_ab = abr.next()
                                kb.op("dve", lambda e: e.scalar_tensor_tensor(out=ab[:], in0=o2[:], scalar=st[:, 5:6], op0=ALU.mult, in1=sg[:], op1=ALU.mult),
                                      reads=[t_o2, t_st, t_sg], writes=[t_ab])
                                kb.op("pe", lambda e: e.transpose(out=po[:, s, :], in_=ab[:], identity=self.ident_bf[:]), reads=[t_ab, self.t_ident], writes=[t_po])
                            aT, t_aT = aTr.next()
                            kb.op("dve", lambda e: e.tensor_copy(out=aT[:], in_=po[:].rearrange("p s t -> p (s t)")), reads=[t_po], writes=[t_aT])
                            kb.dma("sp", [(self.catT[:, h, c0:c0 + 256], aT[:])], reads=[t_aT])
                        pending.append((7, n2)); pending.append((10, n3))
            while pending:
                pending.pop(0)[1]()
            self.prep_flush()

    def phase_fnet(self, l, need_ctx):
        kb, I = self.kb, self.I
        with kb.phase():
            fw = kb.sb([128, 2, 256], F32); cb = kb.sb([128, 2, 128], F32); t_fw = Tok()
            kb.dma("sp", [(fw[:], I["fnet_w"][l].rearrange("(c p) m -> p c m", p=128)), (cb[:], I["cblk"][:, :, :])], writes=[t_fw])
            W = kb.sb([128, 2, 2, 256], BF16); t_W = Tok()
            pw = Ring(kb, 2, [128, 512], F32, kind="ps")
            for ab in range(2):
                for ct in range(2):
                    p, t_p = pw.next()
                    kb.op("pe", lambda e: e.matmul(p[:, 0:256], cb[:, ab, :], fw[:, ct, :], start=True, stop=True), reads=[t_fw], writes=[t_p])
                    kb.op("dve", lambda e: e.tensor_copy(out=W[:, ab, ct, :], in_=p[:, 0:256]), reads=[t_p], writes=[t_W])
            fa = kb.sb([128, NTILE, 256], BF16); t_fa = Tok()
            kb.dma("sp", [(fa[:], self.fD[:, :, :])], writes=[t_fa])
            dr = [Ring(kb, 2, [128, 16, 512], BF16) for _ in range(2)]
            absb = Ring(kb, 2, [128, 2, 2, 512], BF16); fo = Ring(kb, 2, [128, 512], BF16)
            pf = Ring(kb, 2, [128, 512], F32, kind="ps")

            def dft(b, tiles, mats, n, tok0):
                ab_t, t_ab = absb.next()
                for ab in range(2):
                    m, t_m = mats[ab]
                    for ct in range(2):
                        p, t_p = pw.next()
                        for i, tt in enumerate(tiles):
                            kb.op("pe", lambda e: e.matmul(p[:, 0:n], fa[:, tt, ct * 128:(ct + 1) * 128], m[:, i, 0:n], start=(i == 0), stop=(i == len(tiles) - 1)),
                                  reads=[t_fa, t_m], writes=[t_p])
                        eng = "act" if ct == 0 else "dve"
                        if eng == "act":
                            kb.op("act", lambda e: e.copy(out=ab_t[:, ab, ct, 0:n], in_=p[:, 0:n]), reads=[t_p], writes=[t_ab])
                        else:
                            kb.op("dve", lambda e: e.tensor_copy(out=ab_t[:, ab, ct, 0:n], in_=p[:, 0:n]), reads=[t_p], writes=[t_ab])
                for mt in range(2):
                    p, t_p = pf.next()
                    i = 0
                    for ab in range(2):
                        for ct in range(2):
                            kb.op("pe", lambda e: e.matmul(p[:, 0:n], W[:, ab, ct, mt * 128:(mt + 1) * 128], ab_t[:, ab, ct, 0:n], start=(i == 0), stop=(i == 3)),
                                  reads=[t_W, t_ab], writes=[t_p])
                            i += 1
                    f, t_f = fo.next()
                    kb.op("act", lambda e: e.copy(out=f[:, 0:n], in_=p[:, 0:n]), reads=[t_p], writes=[t_f])
                    kb.dma("sp", [(self.catT[:, 4 + mt, tok0:tok0 + n], f[:, 0:n])], reads=[t_f])

            mode = getattr(self, "fn_mode", "WMC")
            for pt in range(4 if "M" in mode else 0):
                mats = []
                for ab, nm in enumerate(("dft_c", "dft_s")):
                    m, t_m = dr[ab].next()
                    src = I[nm].rearrange("(t p) n -> p t n", p=128)
                    kb.dma("sp", [(m[:, q * 4:(q + 1) * 4, :], src[:, q * 4:(q + 1) * 4, pt * 512:(pt + 1) * 512]) for q in range(4)], writes=[t_m])
                    mats.append((m, t_m))
                for b in range(NB):
                    dft(b, [b * TILES_PB + 2 + i for i in range(16)], mats, 512, b * TPB + CTX + pt * 512)
            if need_ctx and "C" in mode:
                mats = []
                for ab, nm in enumerate(("dftc_c", "dftc_s")):
                    m, t_m = dr[ab].next()
                    kb.dma("sp", [(m[:, 0:2, 0:256], I[nm].rearrange("(t p) n -> p t n", p=128))], writes=[t_m])
                    mats.append((m, t_m))
                for b in range(NB):
                    dft(b, [b * TILES_PB + i for i in range(2)], mats, 256, b * TPB)

    def cmul(self, o_r, o_i, a_r, a_i, b_r, b_i, t1, t2, T, eng="dve"):
        kb = self.kb
        tt = lambda o, x, y, op: kb.op(eng, lambda e: e.tensor_tensor(out=o, in0=x, in1=y, op=op), reads=T, writes=T)
        tt(t1, a_r, b_r, ALU.mult); tt(t2, a_i, b_i, ALU.mult); tt(o_r, t1, t2, ALU.subtract)
        tt(t1, a_r, b_i, ALU.mult); tt(t2, a_i, b_r, ALU.mult); tt(o_i, t1, t2, ALU.add)

    def phase_s5(self, l):
        kb, I = self.kb, self.I
        TWO_PI = 2.0 * math.pi
        MAGIC = 12582912.0
        with kb.phase():
            A = kb.sb([128, 32, 128], BF16); BsRI = kb.sb([128, 32, 2, 128], BF16); CqRI = kb.sb([128, 32, 2, 128], BF16)
            Wsel = kb.sb([128, 8, 240], BF16); V = None; Hb = None
            PL = kb.sb([128, 2, 10, 16], F32)
            nPLi = kb.sb([128, 10, 16], F32)
            dd = kb.sb([128, 2], F32); wg = kb.sb([128, 2, 256], BF16)
            t_A, t_Bs, t_Cq, t_W, t_V, t_H, t_PL, t_misc = (Tok() for _ in range(8))
            kb.dma("sp", [(Wsel[:], I["wsel"][:, :, :])], writes=[t_W])
            kb.dma("sp", [(dd[:], I["s5_dd"][l])], writes=[t_misc])
            kb.dma("pool", [(wg[:], I["s5_w_glu"][l].rearrange("(c p) m -> p c m", p=128))], writes=[t_misc])
            with kb.phase():
                T = [Tok()]
                par = kb.sb([128, 3, 32], F32)
                bc = kb.sb([128, 2, 32, 2, 16], F32)
                msk = kb.sb([128, 2, 128], F32)
                kb.dma("sp", [(par[:], I["s5_par"][l]), (bc[:, 0], I["s5_b"][l]), (bc[:, 1], I["s5_c"][l]), (msk[:], I["s5_mask"][:, :, :])], writes=T)
                w = kb.sb([128, 24, 32], F32)
                W_ = lambda i: w[:, i, :]
                ts = lambda o, x, s1, o0, s2=None, o1=None: kb.op("dve", lambda e: e.tensor_scalar(out=o, in0=x, scalar1=s1, scalar2=s2, op0=o0, **({"op1": o1} if o1 else {})), reads=T, writes=T)
                tt = lambda o, x, y, op: kb.op("dve", lambda e: e.tensor_tensor(out=o, in0=x, in1=y, op=op), reads=T, writes=T)
                act = lambda o, x, f, **kw: kb.op("act", lambda e: e.activation(out=o, in_=x, func=f, **kw), reads=T, writes=T)
                are, aim, ldt = par[:, 0, :], par[:, 1, :], par[:, 2, :]
                dt, mag, ang, lr, li = W_(0), W_(1), W_(2), W_(3), W_(4)
                act(dt, ldt, AF.Exp)
                tt(mag, are, dt, ALU.mult); act(mag, mag, AF.Exp)
                tt(ang, aim, dt, ALU.mult)

                def sin_of(o, x, shift):
                    a, r = W_(5), W_(6)
                    ts(a, x, shift, ALU.add)
                    ts(r, a, 1.0 / TWO_PI, ALU.mult)
                    ts(r, r, MAGIC, ALU.add)
                    ts(r, r, MAGIC, ALU.subtract)
                    kb.op("dve", lambda e: e.scalar_tensor_tensor(out=a, in0=r, scalar=-TWO_PI, op0=ALU.mult, in1=a, op1=ALU.add), reads=T, writes=T)
                    act(o, a, AF.Sin)
                sin_of(li, ang, 0.0); sin_of(lr, ang, math.pi / 2)
                tt(lr, lr, mag, ALU.mult); tt(li, li, mag, ALU.mult)
                nr, den, cr, ci, t1, t2 = W_(7), W_(8), W_(9), W_(10), W_(11), W_(12)
                ts(nr, lr, -1.0, ALU.add)
                tt(t1, are, are, ALU.mult); tt(t2, aim, aim, ALU.mult); tt(den, t1, t2, ALU.add)
                kb.op("dve", lambda e: e.reciprocal(out=den, in_=den), reads=T, writes=T)
                tt(t1, nr, are, ALU.mult); tt(t2, li, aim, ALU.mult); tt(cr, t1, t2, ALU.add); tt(cr, cr, den, ALU.mult)
                tt(t1, li, are, ALU.mult); tt(t2, nr, aim, ALU.mult); tt(ci, t1, t2, ALU.subtract); tt(ci, ci, den, ALU.mult)
                ilr, ili, m2 = W_(13), W_(14), W_(15)
                tt(t1, lr, lr, ALU.mult); tt(t2, li, li, ALU.mult); tt(m2, t1, t2, ALU.add)
                kb.op("dve", lambda e: e.reciprocal(out=m2, in_=m2), reads=T, writes=T)
                tt(ilr, lr, m2, ALU.mult); tt(ili, li, m2, ALU.mult); ts(ili, ili, -1.0, ALU.mult)
                bb = kb.sb([128, 2, 32, 16], F32); tb = kb.sb([128, 2, 32, 16], F32)
                bcast = lambda v: v.unsqueeze(2).broadcast_to([128, 32, 16])
                self.cmul(bb[:, 0], bb[:, 1], bcast(cr), bcast(ci), bc[:, 0, :, 0, :], bc[:, 0, :, 1, :], tb[:, 0], tb[:, 1], T)
                mu = kb.sb([128, 2, 2, 3, 32], F32)
                cp = lambda o, x: kb.op("dve", lambda e: e.tensor_copy(out=o, in_=x), reads=T, writes=T)
                for ri, (fw_, bw_) in enumerate(((ilr, lr), (ili, li))):
                    cp(mu[:, 0, ri, 0, 0:16], fw_[:, 0:16]); cp(mu[:, 0, ri, 0, 16:32], bw_[:, 16:32])
                    cp(mu[:, 1, ri, 0, 0:16], bw_[:, 0:16]); cp(mu[:, 1, ri, 0, 16:32], fw_[:, 16:32])
                for tb_i in range(2):
                    for pw in range(2):
                        self.cmul(mu[:, tb_i, 0, pw + 1], mu[:, tb_i, 1, pw + 1], mu[:, tb_i, 0, pw], mu[:, tb_i, 1, pw],
                                  mu[:, tb_i, 0, pw], mu[:, tb_i, 1, pw], t1, t2, T)
                ch = kb.sb([128, 2, 2, 32, 8], F32)
                for tb_i in range(2):
                    kb.op("dve", lambda e: e.memset(ch[:, tb_i, 0, :, 0:1], 1.0), reads=T, writes=T)
                    kb.op("dve", lambda e: e.memset(ch[:, tb_i, 1, :, 0:1], 0.0), reads=T, writes=T)
                    n = 1
                    tmpc = kb.sb([128, 2, 32, 4], F32)
                    for pw in range(3):
                        mb = lambda ri: mu[:, tb_i, ri, pw, :].unsqueeze(2).broadcast_to([128, 32, n])
                        self.cmul(ch[:, tb_i, 0, :, n:2 * n], ch[:, tb_i, 1, :, n:2 * n], ch[:, tb_i, 0, :, 0:n], ch[:, tb_i, 1, :, 0:n],
                                  mb(0), mb(1), tmpc[:, 0, :, 0:n], tmpc[:, 1, :, 0:n], T)
                        n *= 2
                l8 = kb.sb([128, 2, 32], F32); l7 = kb.sb([128, 2, 32], F32); l2 = kb.sb([128, 2, 2, 32], F32)
                self.cmul(l2[:, 0, 0], l2[:, 0, 1], lr, li, lr, li, t1, t2, T)
                self.cmul(l2[:, 1, 0], l2[:, 1, 1], l2[:, 0, 0], l2[:, 0, 1], l2[:, 0, 0], l2[:, 0, 1], t1, t2, T)
                self.cmul(l8[:, 0], l8[:, 1], l2[:, 1, 0], l2[:, 1, 1], l2[:, 1, 0], l2[:, 1, 1], t1, t2, T)
                self.cmul(l7[:, 0], l7[:, 1], l8[:, 0], l8[:, 1], ilr, ili, t1, t2, T)
                sf = kb.sb([128, 2, 2, 32], F32)
                cp(sf[:, 0, 0, 0:16], l7[:, 0, 0:16]); cp(sf[:, 0, 1, 0:16], l7[:, 1, 0:16])
                kb.op("dve", lambda e: e.memset(sf[:, 0, 0, 16:32], 1.0), reads=T, writes=T)
                kb.op("dve", lambda e: e.memset(sf[:, 0, 1, 16:32], 0.0), reads=T, writes=T)
                cp(sf[:, 1, 0, 0:16], lr[:, 0:16]); cp(sf[:, 1, 1, 0:16], li[:, 0:16])
                cp(sf[:, 1, 0, 16:32], l8[:, 0, 16:32]); cp(sf[:, 1, 1, 16:32], l8[:, 1, 16:32])
                ch2 = kb.sb([128, 2, 2, 32, 8], F32)
                tmp8 = kb.sb([128, 2, 32, 8], F32)
                for tb_i in range(2):
                    sb_ = lambda ri: sf[:, tb_i, ri, :].unsqueeze(2).broadcast_to([128, 32, 8])
                    self.cmul(ch2[:, tb_i, 0], ch2[:, tb_i, 1], ch[:, tb_i, 0], ch[:, tb_i, 1], sb_(0), sb_(1), tmp8[:, 0], tmp8[:, 1], T)
                full = kb.sb([128, 2, 32, 8, 16], F32); ftmp = kb.sb([128, 2, 32, 8, 16], F32)
                st = kb.sb([128, 32, 128], BF16)
                pA = Ring(kb, 2, [128, 4, 128], F32, kind="ps"); pTt = Ring(kb, 2, [128, 4, 128], BF16, kind="ps")
                KBst = kb.sb([128, 32, 128], BF16); QCst = kb.sb([128, 32, 128], BF16)

                def build(chain, tb_i, src_r, src_i, dst, neg_im):
                    cb = lambda ri: chain[:, tb_i, ri].unsqueeze(3).broadcast_to([128, 32, 8, 16])
                    sbq = lambda v: v.unsqueeze(2).broadcast_to([128, 32, 8, 16])
                    self.cmul(full[:, 0], full[:, 1], cb(0), cb(1), sbq(src_r), sbq(src_i), ftmp[:, 0], ftmp[:, 1], T)
                    cp(dst[0:64], full[0:64, 0].rearrange("p a s c -> p a (s c)"))
                    if neg_im:
                        ts(dst[64:128], full[64:128, 1].rearrange("p a s c -> p a (s c)"), -1.0, ALU.mult)
                    else:
                        cp(dst[64:128], full[64:128, 1].rearrange("p a s c -> p a (s c)"))
                build(ch, 0, bb[:, 0], bb[:, 1], KBst, False)
                build(ch, 1, bc[:, 1, :, 0, :], bc[:, 1, :, 1, :], QCst, True)
                for q4 in range(8):
                    p, t_p = pA.next()
                    for i in range(4):
                        dg = q4 * 4 + i
                        kb.op("pe", lambda e: e.matmul(p[:, i, :], KBst[:, dg, :], QCst[:, dg, :], start=(i == 0), stop=True, skip_group_check=True), reads=T, writes=[t_p])
                    d = 0 if q4 < 4 else 1
                    kb.op("dve", lambda e: e.tensor_tensor(out=A[:, q4 * 4:(q4 + 1) * 4, :], in0=p[:],
                                                           in1=msk[:, d:d + 1, :].broadcast_to([128, 4, 128]), op=ALU.mult),
                          reads=[t_p] + T, writes=[t_A])
                kb.op("pool", lambda e: e.memset(BsRI[:], 0.0), writes=[t_Bs])
                kb.op("pool", lambda e: e.memset(CqRI[:], 0.0), writes=[t_Cq])
                build(ch2, 0, bb[:, 0], bb[:, 1], st, False)
                for q4 in range(8):
                    p, t_p = pTt.next()
                    for i in range(4):
                        dg = q4 * 4 + i
                        kb.op("pe", lambda e: e.transpose(out=p[:, i, :], in_=st[:, dg, :], identity=self.ident_bf[:]), reads=T + [self.t_ident], writes=[t_p])
                    for i in range(4):
                        dg = q4 * 4 + i
                        g2 = dg % 2
                        kb.op("act", lambda e: e.copy(out=BsRI[:, dg, :, g2 * 64:(g2 + 1) * 64], in_=p[:, i, :].rearrange("p (r q) -> p r q", r=2)),
                              reads=[t_p], writes=[t_Bs])
                build(ch2, 1, bc[:, 1, :, 0, :], bc[:, 1, :, 1, :], st, True)
                for g2 in range(2):
                    rows = slice(g2 * 64, (g2 + 1) * 64)
                    sv = st[rows].rearrange("p (a g) x -> p a g x", g=2)
                    dv = CqRI[rows].rearrange("p (a g) r x -> p a g r x", g=2)
                    fr = full[rows, 0].rearrange("p (a g) s c -> p a g (s c)", g=2)
                    fi = full[rows, 1].rearrange("p (a g) s c -> p a g (s c)", g=2)
                    kb.op("dve", lambda e: e.tensor_copy(out=dv[:, :, g2, 0, :], in_=fr[:, :, g2, :]), reads=T, writes=[t_Cq])
                    kb.op("dve", lambda e: e.tensor_scalar(out=dv[:, :, g2, 1, :], in0=fi[:, :, g2, :], scalar1=-1.0, scalar2=None, op0=ALU.mult), reads=T, writes=[t_Cq])
                for ri in range(2):
                    for g2 in range(2):
                        rows = slice(g2 * 64, (g2 + 1) * 64)
                        kb.op("dve", lambda e: e.tensor_copy(out=PL[rows, ri, 0, :], in_=l8[rows, ri, :].rearrange("p (a g) -> p a g", g=2)[:, :, g2]),
                              reads=T, writes=[t_PL])
                pt1 = kb.sb([128, 16], F32); pt2 = kb.sb([128, 16], F32)
                for i in range(9):
                    self.cmul(PL[:, 0, i + 1], PL[:, 1, i + 1], PL[:, 0, i], PL[:, 1, i], PL[:, 0, i], PL[:, 1, i], pt1[:], pt2[:], [t_PL])
                kb.op("dve", lambda e: e.tensor_scalar(out=nPLi[:], in0=PL[:, 1], scalar1=-1.0, scalar2=None, op0=ALU.mult), reads=[t_PL], writes=[t_PL])
            self._s5_main(l, A, BsRI, CqRI, Wsel, V, Hb, PL, nPLi, dd, wg, (t_A, t_Bs, t_Cq, t_W, t_V, t_H, t_PL, t_misc))

    def _s5_main(self, l, A, BsRI, CqRI, Wsel, V, Hb, PL, nPLi, dd, wg, toks):
        kb, I = self.kb, self.I
        t_A, t_Bs, t_Cq, t_W, t_V, t_H, t_PL, t_misc = toks
        NCH = 288
        with kb.phase():
            V = kb.sb([128, 16, NB, 320], BF16)
            Hb = kb.sb([128, 2, 2, 8, NB, 288], BF16)
            with kb.phase():
                with kb.phase():
                    U = kb.sb([128, 2, NTOK], BF16); t_U = Tok()
                    kb.dma("sp", [(U[:, ct, :], self.uT[:, ct, :]) for ct in range(2)], writes=[t_U])
                    pv = Ring(kb, 2, [128, NCH], F32, kind="ps")
                    i = 0
                    for g in range(16):
                        gt, gl = divmod(g, 8)
                        for b in range(NB):
                            p, t_p = pv.next()
                            ub = U[:, gt, b * TPB:(b + 1) * TPB].rearrange("p (k s) -> p k s", s=8)
                            for s in range(8):
                                kb.op("pe", lambda e: e.matmul(p[:, :], Wsel[:, gl, 112 - 16 * s:240 - 16 * s], ub[:, :, s], start=(s == 0), stop=(s == 7)),
                                      reads=[t_U, t_W], writes=[t_p])
                            if i % 2 == 0:
                                kb.op("act", lambda e: e.copy(out=V[:, g, b, 0:NCH], in_=p[:, :]), reads=[t_p], writes=[t_V])
                                kb.op("act", lambda e: e.copy(out=V[:, g, b, NCH:320], in_=p[:, 0:32]), reads=[t_p], writes=[t_V])
                            else:
                                kb.op("dve", lambda e: e.tensor_copy(out=V[:, g, b, 0:NCH], in_=p[:, :]), reads=[t_p], writes=[t_V])
                                kb.op("dve", lambda e: e.tensor_copy(out=V[:, g, b, NCH:320], in_=p[:, 0:32]), reads=[t_p], writes=[t_V])
                            i += 1
                X = kb.sb([128, 2, 2, 8, NB, NCH], F32)
                sctmp = None
                t_X = [Tok(), Tok()]
                t_XG = [[[Tok(), Tok()] for _ in range(8)] for _ in range(2)]
                pS = Ring(kb, 4, [128, NCH], F32, kind="ps")
                for d in range(2):
                    k0 = 0 if d == 0 else 32
                    for gp in range(8):
                        for b in range(NB):
                            for ri in range(2):
                                p, t_p = pS.next()
                                for g2 in range(2):
                                    g = 2 * gp + g2
                                    kb.op("pe", lambda e: e.matmul(p[:, :], BsRI[:, d * 16 + g, ri, :], V[:, g, b, k0:k0 + NCH], start=(g2 == 0), stop=(g2 == 1)),
                                          reads=[t_Bs, t_V], writes=[t_p])
                                if ri == 0:
                                    kb.op("act", lambda e: e.copy(out=X[:, 0, ri, gp, b, :], in_=p[:, :]), reads=[t_p], writes=[t_XG[0][gp][ri]])
                                else:
                                    kb.op("dve", lambda e: e.tensor_copy(out=X[:, 0, ri, gp, b, :], in_=p[:, :]), reads=[t_p], writes=[t_XG[0][gp][ri]])
                    cur = 0
                    for si in range(9):
                        sh = 1 << si
                        nxt = 1 - cur
                        n = NCH - sh
                        if d == 0:
                            dst, src, keep = slice(sh, NCH), slice(0, n), slice(0, sh)
                        else:
                            dst, src, keep = slice(0, n), slice(sh, NCH), slice(n, NCH)
                        for ri in range(2):
                            kb.op("pool", lambda e: e.tensor_copy(out=X[:, nxt, ri, :, :, keep], in_=X[:, cur, ri, :, :, keep]), reads=[t_XG[cur][g_][ri] for g_ in range(8)], writes=[t_XG[nxt][g_][ri] for g_ in range(8)])
                        for opi in range(4):
                            for gp in range(8):
                                c = d * 8 + gp
                                Pr, Pi, nPi = PL[:, 0, si, c:c + 1], PL[:, 1, si, c:c + 1], nPLi[:, si, c:c + 1]
                                xr, xi = X[:, cur, 0, gp], X[:, cur, 1, gp]
                                yr, yi = X[:, nxt, 0, gp], X[:, nxt, 1, gp]
                                o_, a_, sc_, b_ = ((yr, xr, Pr, xr), (yi, xi, Pr, xi), (yr, xi, nPi, yr), (yi, xr, Pi, yi))[opi]
                                kb.op("dve", lambda e: e.scalar_tensor_tensor(out=o_[:, :, dst], in0=a_[:, :, src], scalar=sc_, op0=ALU.mult, in1=b_[:, :, dst], op1=ALU.add),
                                      reads=[t_XG[cur][gp][0], t_XG[cur][gp][1], t_PL], writes=[t_XG[nxt][gp][opi % 2]])
                        cur = nxt
                    for ri in range(2):
                        kb.op("act", lambda e: e.copy(out=Hb[:, d, ri], in_=X[:, cur, ri]), reads=[t_XG[cur][g_][ri] for g_ in range(8)], writes=[t_H])
            with kb.phase():
                U = kb.sb([128, 2, NTOK], BF16); t_U = Tok()
                kb.dma("sp", [(U[:, ct, :], self.uT[:, ct, :]) for ct in range(2)], writes=[t_U])
                Yc = kb.sb([128, 16, NB, NCH], BF16); t_Y = Tok()
                G = kb.sb([128, 2, NTOK], BF16); t_G = Tok()
                py = Ring(kb, 2, [128, NCH], F32, kind="ps")
                i = 0
                for g in range(16):
                    gp, g2 = divmod(g, 2)
                    for b in range(NB):
                        p, t_p = py.next()
                        mm = lambda o, lh, rh, first=False: kb.op("pe", lambda e: e.matmul(o, lh, rh, start=first, stop=True, skip_group_check=True),
                                                                  reads=[t_A, t_Cq, t_V, t_H], writes=[t_p])
                        mm(p[:, 0:NCH], A[:, g, :], V[:, g, b, 0:NCH], True)
                        for ri in range(2):
                            mm(p[:, 1:NCH], CqRI[:, g, ri, :], Hb[:, 0, ri, gp, b, 0:NCH - 1])
                        mm(p[:, 32:NCH], A[:, 16 + g, :], V[:, g, b, 32:NCH])
                        mm(p[:, 0:32], A[:, 16 + g, :], V[:, g, b, NCH:320])
                        for ri in range(2):
                            mm(p[:, 32:NCH], CqRI[:, 16 + g, ri, :], Hb[:, 1, ri, gp, b, 1:257])
                            mm(p[:, 0:31], CqRI[:, 16 + g, ri, :], Hb[:, 1, ri, gp, b, 257:NCH])
                        if i % 2 == 0:
                            kb.op("act", lambda e: e.copy(out=Yc[:, g, b, :], in_=p[:, :]), reads=[t_p], writes=[t_Y])
                        else:
                            kb.op("dve", lambda e: e.tensor_copy(out=Yc[:, g, b, :], in_=p[:, :]), reads=[t_p], writes=[t_Y])
                        i += 1
                pu = Ring(kb, 2, [128, 64, 8], F32, kind="ps")
                yyr = Ring(kb, 2, [128, 512], F32)
                for gt in range(2):
                    for b in range(NB):
                        for seg in range(5):
                            nk = 64 if seg < 4 else 32
                            p, t_p = pu.next()
                            first = True
                            for t in range(8):
                                for gl in range(8):
                                    kb.op("pe", lambda e: e.matmul(p[:, 0:nk, t], Wsel[:, t, 112 - 16 * gl:240 - 16 * gl], Yc[:, gt * 8 + gl, b, seg * 64:seg * 64 + nk],
                                                                   start=first, stop=True, skip_group_check=True), reads=[t_W, t_Y], writes=[t_p])
                                    first = False
                            tok0 = b * TPB + seg * 512
                            nt = nk * 8
                            yy, t_yy = yyr.next()
                            kb.op("dve", lambda e: e.scalar_tensor_tensor(out=yy[:, 0:nt], in0=U[:, gt, tok0:tok0 + nt], scalar=dd[:, gt:gt + 1], op0=ALU.mult,
                                                                          in1=p[:, 0:nk, :].rearrange("p k t -> p (k t)"), op1=ALU.add),
                                  reads=[t_U, t_misc, t_p], writes=[t_yy])
                            kb.op("act", lambda e: e.activation(out=G[:, gt, tok0:tok0 + nt], in_=yy[:, 0:nt], func=AF.Gelu_apprx_tanh), reads=[t_yy], writes=[t_G])
                pz = Ring(kb, 2, [128, 512], F32, kind="ps")
                sgr = Ring(kb, 2, [128, 512], BF16); sor = Ring(kb, 2, [128, 512], BF16)
                for tt_ in range(NTOK // 512):
                    for mt in range(2):
                        p, t_p = pz.next()
                        for gt in range(2):
                            kb.op("pe", lambda e: e.matmul(p[:, :], wg[:, gt, mt * 128:(mt + 1) * 128], G[:, gt, tt_ * 512:(tt_ + 1) * 512], start=(gt == 0), stop=(gt == 1)),
                                  reads=[t_misc, t_G], writes=[t_p])
                        sg_, t_sg = sgr.next()
                        kb.op("act", lambda e: e.activation(out=sg_[:], in_=p[:, :], func=AF.Sigmoid), reads=[t_p], writes=[t_sg])
                        so, t_so = sor.next()
                        kb.op("dve", lambda e: e.tensor_tensor(out=so[:], in0=G[:, mt, tt_ * 512:(tt_ + 1) * 512], in1=sg_[:], op=ALU.mult), reads=[t_G, t_sg], writes=[t_so])
                        kb.dma("sp", [(self.catT[:, 6 + mt, tt_ * 512:(tt_ + 1) * 512], so[:])], reads=[t_so])

    def groups(self, with_ctx):
        gs = []
        for b in range(NB):
            if with_ctx:
                gs.append((b * TPB, CTX, 2))
            for i in range(4):
                gs.append((b * TPB + CTX + i * 512, 512, b))
        return gs

    def post_norm_tile(self, halves, t_halves, xt, t_x, gg, t_gg, ms, R):
        kb = self.kb
        st, t_st = R["stat2"].next()
        for nh in range(2):
            jk, t_jk = R["junk2"].next()
            kb.op("act", lambda e: e.activation(out=jk[:], in_=halves[nh], func=AF.Square, accum_out=st[:, nh:nh + 1]),
                  reads=[t_halves[nh], t_st], writes=[t_jk, t_st])
        kb.op("dve", lambda e: e.tensor_tensor(out=st[:, 2:3], in0=st[:, 0:1], in1=st[:, 1:2], op=ALU.add), reads=[t_st], writes=[t_st])
        kb.op("act", lambda e: e.activation(out=st[:, 3:4], in_=st[:, 2:3], func=AF.Sqrt, scale=1.0 / D, bias=self.eps_t[:, 0:1]), reads=[t_st], writes=[t_st])
        kb.op("dve", lambda e: e.reciprocal(out=st[:, 4:5], in_=st[:, 3:4]), reads=[t_st], writes=[t_st])
        tmp, t_t = R["tmp"].next()
        for nh in range(2):
            kb.op("dve", lambda e: e.scalar_tensor_tensor(out=tmp[:, nh * 512:(nh + 1) * 512], in0=halves[nh], scalar=st[:, 4:5], op0=ALU.mult,
                                                          in1=gg[:, ms, nh * 512:(nh + 1) * 512], op1=ALU.mult),
                  reads=[t_halves[nh], t_st, t_gg], writes=[t_t])
        xn, t_xn = R["xn"].next()
        kb.op("pool", lambda e: e.tensor_tensor(out=xn[:], in0=tmp[:], in1=xt[:], op=ALU.add), reads=[t_t, t_x], writes=[t_xn])
        return xn, t_xn

    def pn1(self, halves, t_halves, R):
        kb = self.kb
        st, t_st = R["stat2"].next()
        for nh in range(2):
            jk, t_jk = R["junk2"].next()
            kb.op("act", lambda e: e.activation(out=jk[:], in_=halves[nh], func=AF.Square, accum_out=st[:, nh:nh + 1]),
                  reads=[t_halves[nh], t_st], writes=[t_jk, t_st])
        kb.op("dve", lambda e: e.tensor_tensor(out=st[:, 2:3], in0=st[:, 0:1], in1=st[:, 1:2], op=ALU.add), reads=[t_st], writes=[t_st])
        kb.op("act", lambda e: e.activation(out=st[:, 3:4], in_=st[:, 2:3], func=AF.Sqrt, scale=1.0 / D, bias=self.eps_t[:, 0:1]), reads=[t_st], writes=[t_st])
        kb.op("dve", lambda e: e.reciprocal(out=st[:, 4:5], in_=st[:, 3:4]), reads=[t_st], writes=[t_st])
        return st, t_st

    def pn2(self, halves, t_halves, st, t_st, xt, t_x, gg, t_gg, ms, R):
        kb = self.kb
        tmp, t_t = R["tmp"].next()
        for nh in range(2):
            kb.op("dve", lambda e: e.scalar_tensor_tensor(out=tmp[:, nh * 512:(nh + 1) * 512], in0=halves[nh], scalar=st[:, 4:5], op0=ALU.mult,
                                                          in1=gg[:, ms, nh * 512:(nh + 1) * 512], op1=ALU.mult),
                  reads=[t_halves[nh], t_st, t_gg], writes=[t_t])
        xn, t_xn = R["xn"].next()
        kb.op("pool", lambda e: e.tensor_tensor(out=xn[:], in0=tmp[:], in1=xt[:], op=ALU.add), reads=[t_t, t_x], writes=[t_xn])
        return xn, t_xn

    def nm1(self, xt, t_x, R):
        kb = self.kb
        junk, t_j = R["junk"].next()
        st, t_st = R["stat"].next()
        kb.op("act", lambda e: e.activation(out=junk[:], in_=xt[:], func=AF.Square, accum_out=st[:, 0:1]), reads=[t_x], writes=[t_j, t_st])
        kb.op("act", lambda e: e.activation(out=st[:, 1:2], in_=st[:, 0:1], func=AF.Sqrt, scale=1.0 / D, bias=self.eps_t[:, 0:1]), reads=[t_st], writes=[t_st])
        kb.op("dve", lambda e: e.reciprocal(out=st[:, 2:3], in_=st[:, 1:2]), reads=[t_st], writes=[t_st])
        return st, t_st

    def nm2(self, xt, t_x, st, t_st, gsc, sh, t_g, t_s, ms, R):
        kb = self.kb
        tmp, t_t = R["tmp"].next()
        kb.op("dve", lambda e: e.scalar_tensor_tensor(out=tmp[:], in0=xt[:], scalar=st[:, 2:3], op0=ALU.mult, in1=gsc[:, ms, :], op1=ALU.mult),
              reads=[t_x, t_st, t_g], writes=[t_t])
        hb, t_h = R["hb"].next()
        kb.op("pool", lambda e: e.tensor_tensor(out=hb[:], in0=tmp[:], in1=sh[:, ms, :], op=ALU.add), reads=[t_t, t_s], writes=[t_h])
        return hb, t_h

    @staticmethod
    def run_pipeline(n, stages):
        for it in range(n + len(stages) - 1):
            for s_, f in enumerate(stages):
                j = it - s_
                if 0 <= j < n:
                    f(j)

    def phase_wout(self, l, xsrc, xdst, need_ctx, prep_wout=False):
        kb, I = self.kb, self.I
        with kb.phase():
            gg, _, t_gg, _ = self.load_mod_tiles(l, 2, None, "norm_mix_post", plus_one=False)
            gsc2, sh2, t_g2, t_s2 = self.load_mod_tiles(l, 4, 3, "norm_ffn_pre")
            wo = kb.sb([128, 8, D], BF16); t_wo = Tok()
            wv = I["w_out"][l].rearrange("(k p) n -> p k n", p=128)
            kb.dma("pool", [(wo[:, :, c * 512:(c + 1) * 512], wv[:, :, c * 512:(c + 1) * 512]) for c in range(2)], writes=[t_wo])
            R = {"junk": Ring(kb, 1, [128, D], BF16), "stat": Ring(kb, 8, [128, 4], F32), "tmp": Ring(kb, 3, [128, D], F32),
                 "hb": Ring(kb, 3, [128, D], BF16), "stat2": Ring(kb, 8, [128, 8], F32), "junk2": Ring(kb, 1, [128, 512], BF16),
                 "xn": Ring(kb, 4, [128, D], F32)}
            xr = Ring(kb, 4, [128, D], F32)
            cgr = Ring(kb, 2, [128, 8, 512], BF16); hgr = Ring(kb, 2, [128, 8, 512], BF16)
            pm = [Ring(kb, 3, [128, 512], F32, kind="ps") for _ in range(2)]
            pT = Ring(kb, 2, [128, 8, 128], BF16, kind="ps")
            items = []
            for (tok0, w, ms) in self.groups(need_ctx):
                for j in range(w // 128):
                    items.append((tok0, w, ms, j))
            C = [dict() for _ in items]

            self.prep_begin()

            def s_mm(i):
                tok0, w, ms, j = items[i]
                if prep_wout:
                    self.prep_tick(1)
                if j == 0:
                    cg, t_cg = cgr.next()
                    kb.dma("sp", [(cg[:, :, 0:w], self.catT[:, :, tok0:tok0 + w])], writes=[t_cg])
                    self._cg = (cg, t_cg)
                    self._hg = hgr.next()
                cg, t_cg = self._cg
                C[i]["hg"] = self._hg
                r0 = tok0 + j * 128
                xt, t_x = xr.next()
                kb.dma("sp", [(xt[:], xsrc[r0:r0 + 128, :])], writes=[t_x])
                hs, ths = [], []
                for nh in range(2):
                    p, t_p = pm[nh].next()
                    for k in range(8):
                        kb.op("pe", lambda e: e.matmul(p[:, :], cg[:, k, j * 128:(j + 1) * 128], wo[:, k, nh * 512:(nh + 1) * 512], start=(k == 0), stop=(k == 7)),
                              reads=[t_cg, t_wo], writes=[t_p])
                    hs.append(p[:, :]); ths.append(t_p)
                C[i].update(hs=hs, ths=ths, xt=xt, t_x=t_x)

            def s_p1(i):
                c = C[i]
                c["st"], c["t_st"] = self.pn1(c["hs"], c["ths"], R)

            def s_p2(i):
                c = C[i]
                tok0, w, ms, j = items[i]
                r0 = tok0 + j * 128
                c["xn"], c["t_xn"] = self.pn2(c["hs"], c["ths"], c["st"], c["t_st"], c["xt"], c["t_x"], gg, t_gg, ms, R)
                kb.dma("sp", [(xdst[r0:r0 + 128, :], c["xn"][:])], reads=[c["t_xn"]])

            def s_n1(i):
                c = C[i]
                c["st2"], c["t_st2"] = self.nm1(c["xn"], c["t_xn"], R)

            def s_n2(i):
                c = C[i]
                tok0, w, ms, j = items[i]
                r0 = tok0 + j * 128
                c["hb"], c["t_h"] = self.nm2(c["xn"], c["t_xn"], c["st2"], c["t_st2"], gsc2, sh2, t_g2, t_s2, ms, R)
                if l == 1:
                    kb.dma("sp", [(self.h2tm[r0:r0 + 128, :], c["hb"][:])], reads=[c["t_h"]])

            def s_t(i):
                c = C[i]
                tok0, w, ms, j = items[i]
                hg, t_hg = c["hg"]
                p, t_p = pT.next()
                for k in range(8):
                    kb.op("pe", lambda e: e.transpose(out=p[:, k, :], in_=c["hb"][:, k * 128:(k + 1) * 128], identity=self.ident_bf[:]),
                          reads=[c["t_h"], self.t_ident], writes=[t_p])
                kb.op("act", lambda e: e.copy(out=hg[:, :, j * 128:(j + 1) * 128], in_=p[:]), reads=[t_p], writes=[t_hg])
                if j == w // 128 - 1:
                    kb.dma("sp", [(self.h2T[:, :, tok0:tok0 + w], hg[:, :, 0:w])], reads=[t_hg])
                C[i].clear()
            self.run_pipeline(len(items), [s_mm, s_p1, s_p2, s_n1, s_n2, s_t])
            self.prep_flush()

    def phase_ffn(self, l, xsrc, xdst, moe, final):
        kb, I = self.kb, self.I
        FG = 512
        NFC = FG // 128
        for b in range(NB):
            tok0 = b * TPB + (CTX if moe else 0)
            TG = SEQ if moe else TPB
            ntile = TG // 128
            with kb.phase():
                hT = kb.sb([128, 8, TG], BF16); t_hT = Tok()
                kb.dma("sp", [(hT[:, k, :], self.h2T[:, k, tok0:tok0 + TG]) for k in range(8)], writes=[t_hT])
                acc = kb.sb([128, ntile, D], F32); t_acc = [Tok() for _ in range(ntile)]
                gates = None
                if moe:
                    gates = kb.sb([128, ntile, 8], F32); t_gt = Tok()
                    with kb.phase():
                        rb = kb.sb([128, 8, 8], BF16); t_rb = Tok()
                        kb.dma("pool", [(rb[:], I["moe_router"][0].rearrange("(k p) e -> p k e", p=128))], writes=[t_rb])
                        pl = Ring(kb, 2, [128, 8], F32, kind="ps")
                        wk = Ring(kb, 2, [128, 6, 8], F32); sm = Ring(kb, 2, [128, 8], F32)
                        for j in range(ntile):
                            p, t_p = pl.next()
                            for k in range(8):
                                kb.op("pe", lambda e: e.matmul(p[:, :], hT[:, k, j * 128:(j + 1) * 128], rb[:, k, :], start=(k == 0), stop=(k == 7)),
                                      reads=[t_hT, t_rb], writes=[t_p])
                            w_, t_w = wk.next(); s_, t_s = sm.next()
                            T = [t_w, t_s]
                            kb.op("dve", lambda e: e.tensor_reduce(out=s_[:, 0:1], in_=p[:, :], op=ALU.max, axis=AX.X), reads=[t_p], writes=T)
                            kb.op("dve", lambda e: e.tensor_scalar(out=s_[:, 1:2], in0=s_[:, 0:1], scalar1=-1.0, scalar2=None, op0=ALU.mult), reads=T, writes=T)
                            kb.op("act", lambda e: e.activation(out=w_[:, 0, :], in_=p[:, :], func=AF.Exp, bias=s_[:, 1:2]), reads=[t_p] + T, writes=T)
                            kb.op("dve", lambda e: e.tensor_scalar(out=w_[:, 1, :], in0=w_[:, 0, :], scalar1=1.0, scalar2=None, op0=ALU.is_lt), reads=T, writes=T)
                            kb.op("dve", lambda e: e.tensor_tensor(out=w_[:, 2, :], in0=w_[:, 0, :], in1=w_[:, 1, :], op=ALU.mult), reads=T, writes=T)
                            kb.op("dve", lambda e: e.tensor_reduce(out=s_[:, 2:3], in_=w_[:, 2, :], op=ALU.max, axis=AX.X), reads=T, writes=T)
                            kb.op("dve", lambda e: e.tensor_scalar(out=s_[:, 3:4], in0=s_[:, 2:3], scalar1=1.0, scalar2=None, op0=ALU.add), reads=T, writes=T)
                            kb.op("dve", lambda e: e.reciprocal(out=s_[:, 4:5], in_=s_[:, 3:4]), reads=T, writes=T)
                            kb.op("dve", lambda e: e.tensor_scalar(out=w_[:, 3, :], in0=w_[:, 0, :], scalar1=s_[:, 2:3], scalar2=None, op0=ALU.is_ge), reads=T, writes=T)
                            kb.op("dve", lambda e: e.tensor_tensor(out=w_[:, 4, :], in0=w_[:, 0, :], in1=w_[:, 3, :], op=ALU.mult), reads=T, writes=T)
                            kb.op("dve", lambda e: e.tensor_scalar(out=gates[:, j, :], in0=w_[:, 4, :], scalar1=s_[:, 4:5], scalar2=None, op0=ALU.mult), reads=T, writes=[t_gt])
                with kb.phase():
                    wgr = Ring(kb, 2, [128, 8, FG], BF16); wur = Ring(kb, 2, [128, 8, FG], BF16); wdr = Ring(kb, 2, [128, NFC, D], BF16)
                    actr = Ring(kb, 2, [128, NFC, TG], BF16)
                    sgr = Ring(kb, 3, [128, 512], BF16); gtm = Ring(kb, 2, [128, 512], F32) if moe else None
                    evr = Ring(kb, 2, [128, 512], F32)
                    pG = Ring(kb, 2, [128, 512], F32, kind="ps"); pU = Ring(kb, 2, [128, 512], F32, kind="ps")
                    pD = Ring(kb, 3, [128, 512], F32, kind="ps"); pB = Ring(kb, 1, [128, 4, 128], F32, kind="ps")
                    gbr = Ring(kb, 2, [128, TG], BF16) if moe else None
                    gxr = Ring(kb, 2, [128, 128], BF16) if moe else None
                    experts = range(N_EXP) if moe else [0]
                    F = F_EXPERT if moe else F_DENSE
                    state = {"first": True, "nbank": 0}
                    pending = []

                    def emit_down(at, t_at, wd_, t_wd, nfc, tiles, first):
                        for j in tiles:
                            for nh in range(2):
                                p, t_p = pD.next()
                                for fc in range(nfc):
                                    kb.op("pe", lambda e: e.matmul(p[:, :], at[:, fc, j * 128:(j + 1) * 128], wd_[:, fc, nh * 512:(nh + 1) * 512],
                                                                   start=(fc == 0), stop=(fc == nfc - 1)), reads=[t_at, t_wd], writes=[t_p])
                                dst = acc[:, j, nh * 512:(nh + 1) * 512]
                                if first:
                                    kb.op("act", lambda e: e.copy(out=dst, in_=p[:, :]), reads=[t_p], writes=[t_acc[j]])
                                else:
                                    kb.op("dve", lambda e: e.tensor_tensor(out=dst, in0=p[:, :], in1=dst, op=ALU.add), reads=[t_p, t_acc[j]], writes=[t_acc[j]])
                                state["nbank"] += 1

                    for ex in experts:
                        if moe:
                            Wg, Wu, Wd = I["moe_w_gate"][0, ex], I["moe_w_up"][0, ex], I["moe_w_down"][0, ex]
                            gb, t_gb = gbr.next()
                            for q4 in range(ntile // 4):
                                p, t_p = pB.next()
                                for i in range(4):
                                    j = q4 * 4 + i
                                    gx, t_gx = gxr.next()
                                    kb.op("dve", lambda e: e.tensor_copy(out=gx[:], in_=gates[:, j, ex:ex + 1].broadcast_to([128, 128])), reads=[t_gt], writes=[t_gx])
                                    kb.op("pe", lambda e: e.matmul(p[:, i, :], gx[:], self.ident_bf[:], start=(i == 0), stop=True, skip_group_check=True), reads=[t_gx, self.t_ident], writes=[t_p])
                                kb.op("act", lambda e: e.copy(out=gb[:, q4 * 512:(q4 + 1) * 512], in_=p[:].rearrange("p a b -> p (a b)")), reads=[t_p], writes=[t_gb])
                        else:
                            Wg, Wu, Wd = I["ffn_w_gate"][0], I["ffn_w_up"][0], I["ffn_w_down"][0]
                        wgv = Wg.rearrange("(k p) n -> p k n", p=128); wuv = Wu.rearrange("(k p) n -> p k n", p=128)
                        wdv = Wd.rearrange("(c p) n -> p c n", p=128)
                        f0 = 0
                        while f0 < F:
                            fw = min(FG, F - f0)
                            nfc = fw // 128
                            wg_, t_wg = wgr.next(); wu_, t_wu = wur.next(); wd_, t_wd = wdr.next()
                            kb.dma("pool", [(wg_[:, 0:4, 0:fw], wgv[:, 0:4, f0:f0 + fw]), (wg_[:, 4:8, 0:fw], wgv[:, 4:8, f0:f0 + fw])], writes=[t_wg])
                            kb.dma("pool", [(wu_[:, 0:4, 0:fw], wuv[:, 0:4, f0:f0 + fw]), (wu_[:, 4:8, 0:fw], wuv[:, 4:8, f0:f0 + fw])], writes=[t_wu])
                            kb.dma("pool", [(wd_[:, 0:nfc, :], wdv[:, f0 // 128:f0 // 128 + nfc, :])], writes=[t_wd])
                            at, t_at = actr.next()
                            c0 = 0
                            while c0 < TG:
                                n = min(512, TG - c0)
                                for fc in range(nfc):
                                    g_, t_g = pG.next(); u_, t_u = pU.next()
                                    for k in range(8):
                                        kb.op("pe", lambda e: e.matmul(g_[:, 0:n], wg_[:, k, fc * 128:(fc + 1) * 128], hT[:, k, c0:c0 + n], start=(k == 0), stop=(k == 7)),
                                              reads=[t_wg, t_hT], writes=[t_g])
                                    for k in range(8):
                                        kb.op("pe", lambda e: e.matmul(u_[:, 0:n], wu_[:, k, fc * 128:(fc + 1) * 128], hT[:, k, c0:c0 + n], start=(k == 0), stop=(k == 7)),
                                              reads=[t_wu, t_hT], writes=[t_u])
                                    sg_, t_sg = sgr.next()
                                    kb.op("act", lambda e: e.activation(out=sg_[:, 0:n], in_=g_[:, 0:n], func=AF.Silu), reads=[t_g], writes=[t_sg])
                                    if moe:
                                        tm, t_tm = gtm.next()
                                        kb.op("dve", lambda e: e.tensor_tensor(out=tm[:, 0:n], in0=u_[:, 0:n], in1=gb[:, c0:c0 + n], op=ALU.mult), reads=[t_u, t_gb], writes=[t_tm])
                                        kb.op("dve", lambda e: e.tensor_tensor(out=at[:, fc, c0:c0 + n], in0=tm[:, 0:n], in1=sg_[:, 0:n], op=ALU.mult), reads=[t_tm, t_sg], writes=[t_at])
                                    else:
                                        kb.op("dve", lambda e: e.tensor_tensor(out=at[:, fc, c0:c0 + n], in0=u_[:, 0:n], in1=sg_[:, 0:n], op=ALU.mult), reads=[t_u, t_sg], writes=[t_at])
                                while pending:
                                    pending.pop(0)()
                                tiles = list(range(c0 // 128, (c0 + n) // 128))
                                pending.append(lambda at=at, t_at=t_at, wd_=wd_, t_wd=t_wd, nfc=nfc, tiles=tiles, first=state["first"]:
                                               emit_down(at, t_at, wd_, t_wd, nfc, tiles, first))
                                c0 += n
                            state["first"] = False
                            f0 += fw
                    while pending:
                        pending.pop(0)()
                with kb.phase():
                    gg, _, t_gg, _ = self.load_mod_tiles(l, 5, None, "norm_ffn_post", plus_one=False)
                    R = {"tmp": Ring(kb, 3, [128, D], F32), "stat2": Ring(kb, 8, [128, 8], F32), "junk2": Ring(kb, 1, [128, 512], BF16),
                         "xn": Ring(kb, 3, [128, D], F32)}
                    xr = Ring(kb, 4, [128, D], F32)
                    C = [dict() for _ in range(ntile)]

                    def f_load(j):
                        xt, t_x = xr.next()
                        kb.dma("sp", [(xt[:], xsrc[tok0 + j * 128:tok0 + (j + 1) * 128, :])], writes=[t_x])
                        C[j].update(xt=xt, t_x=t_x, hs=[acc[:, j, 0:512], acc[:, j, 512:1024]], ths=[t_acc[j], t_acc[j]])

                    def f_p1(j):
                        C[j]["st"], C[j]["t_st"] = self.pn1(C[j]["hs"], C[j]["ths"], R)

                    def f_p2(j):
                        c = C[j]
                        is_ctx = (not moe) and j < 2
                        ms = 2 if is_ctx else b
                        xn, t_xn = self.pn2(c["hs"], c["ths"], c["st"], c["t_st"], c["xt"], c["t_x"], gg, t_gg, ms, R)
                        if final:
                            o0 = b * SEQ + j * 128
                            kb.dma("sp", [(xdst[o0:o0 + 128, :], xn[:])], reads=[t_xn])
                        else:
                            r0 = tok0 + j * 128
                            kb.dma("sp", [(xdst[r0:r0 + 128, :], xn[:])], reads=[t_xn])
                    self.run_pipeline(ntile, [f_load, f_p1, f_p2])

    def moe_declare(self):
        sc = self.scratch
        self.h2tm = sc("h2tm", [NTOK, D], BF16)
        nrow = N_EXP * 7 * 128
        self.Wg_s = sc("Wg_s", [nrow, 8 * 512], BF16); self.Wu_s = sc("Wu_s", [nrow, 8 * 512], BF16)
        self.Wd_s = sc("Wd_s", [nrow, 4 * D], BF16)
        self.hsorted = sc("hsorted", [MOE_SLOTS, D], BF16)
        self.ysorted = sc("ysorted", [MOE_SLOTS, D], F32)

    def moe_prep_gen(self, rings):
        kb, I = self.kb, self.I
        inflight = self.prep_inflight
        for ex in range(N_EXP):
            wgv = I["moe_w_gate"][0, ex].rearrange("(k p) n -> p k n", p=128)
            wuv = I["moe_w_up"][0, ex].rearrange("(k p) n -> p k n", p=128)
            wdv = I["moe_w_down"][0, ex].rearrange("(c p) n -> p c n", p=128)
            for fg in range(7):
                r0 = (ex * 7 + fg) * 128
                for kind, src, dst in ((0, wgv, self.Wg_s), (0, wuv, self.Wu_s), (1, wdv, self.Wd_s)):
                    t, tk = self.prep_rings_cur[0][kind].next()
                    if kind == 0:
                        kb.dma("pool", [(t[:, 0:4, :], src[:, 0:4, fg * 512:(fg + 1) * 512]), (t[:, 4:8, :], src[:, 4:8, fg * 512:(fg + 1) * 512])], writes=[tk])
                        flat = t[:].rearrange("p k n -> p (k n)")
                    else:
                        kb.dma("pool", [(t[:], src[:, fg * 4:(fg + 1) * 4, :])], writes=[tk])
                        flat = t[:].rearrange("p c n -> p (c n)")
                    inflight.append((dst[r0:r0 + 128, :], flat, tk))
                    if len(inflight) > 2:
                        d_, f_, k_ = inflight.pop(0)
                        kb.dma("pool", [(d_, f_)], reads=[k_])
                    yield
        self.prep_flush()
        yield

    def prep_begin(self):
        if self.prep is not None:
            self.prep_rings_cur[0] = self.prep_rings()

    def prep_tick(self, k=1):
        for _ in range(k):
            if self.prep is not None and next(self.prep, "done") == "done":
                self.prep = None

    def prep_flush(self):
        while self.prep_inflight:
            d_, f_, k_ = self.prep_inflight.pop(0)
            self.kb.dma("pool", [(d_, f_)], reads=[k_])

    def prep_rings(self):
        kb = self.kb
        return [Ring(kb, 3, [128, 8, 512], BF16), Ring(kb, 2, [128, 4, D], BF16)]

    def phase_moe_prep(self):
        kb = self.kb
        if self.prep is None:
            return
        with kb.phase():
            rings = self.prep_rings()
            self.prep_rings_cur[0] = rings
            for _ in self.prep:
                pass
            self.prep_flush()
            self.prep = None

    def phase_moe_sparse(self, l, xsrc, xdst):
        kb, I = self.kb, self.I
        NT = NB * SEQ // 128
        BIG = 1.0e6
        MAGIC = 12582912.0
        lat_tok0 = lambda j: (j // 16) * TPB + CTX + (j % 16) * 128
        with kb.phase():
            glo = kb.sb([128, NT], F32); ghi = kb.sb([128, NT], F32)
            ilo = kb.sb([128, NT], I32); ihi = kb.sb([128, NT], I32)
            widx = kb.sb([128, MOE_TILES, 7], I32)
            t_rt = Tok()
            with kb.phase():
                rb = kb.sb([128, 8, 8], BF16); t_rb = Tok()
                kb.dma("pool", [(rb[:], I["moe_router"][0].rearrange("(k p) e -> p k e", p=128))], writes=[t_rb])
                tri = kb.sb([128, 2, 128], BF16); io7 = kb.sb([128, 7], F32); thr = kb.sb([128, MOE_TILES], F32)
                kb.dma("sp", [(tri[:], I["moe_tri"][:, :, :]), (io7[:], I["moe_iota"][:, :]), (thr[:], I["moe_thr"][:, :])], writes=[t_rb])
                hT = kb.sb([128, 8, NB * SEQ], BF16); t_hT = Tok()
                kb.dma("sp", [(hT[:, k, b * SEQ:(b + 1) * SEQ], self.h2T[:, k, b * TPB + CTX:(b + 1) * TPB]) for k in range(8) for b in range(NB)], writes=[t_hT])
                gates = kb.sb([128, NT, 8], F32); maskf = kb.sb([128, NT, 8], F32); maskb = kb.sb([128, NT, 8], BF16)
                rank = kb.sb([128, NT, 8], F32)
                t_g, t_m, t_rk = Tok(), Tok(), Tok()
                pl = Ring(kb, 2, [128, 8], F32, kind="ps")
                L = kb.sb([128, NT, 8], F32); ex = kb.sb([128, NT, 8], F32); w2 = kb.sb([128, NT, 8], F32)
                sm = kb.sb([128, 4, NT], F32)
                t_L = Tok()
                for j in range(NT):
                    p, t_p = pl.next()
                    for k in range(8):
                        kb.op("pe", lambda e: e.matmul(p[:, :], hT[:, k, j * 128:(j + 1) * 128], rb[:, k, :], start=(k == 0), stop=(k == 7)),
                              reads=[t_hT, t_rb], writes=[t_p])
                    kb.op("act", lambda e: e.copy(out=L[:, j, :], in_=p[:, :]), reads=[t_p], writes=[t_L])
                TL = [t_L]
                bc = lambda v: v.unsqueeze(2).broadcast_to([128, NT, 8])
                vop = lambda fn, wr=None: kb.op("dve", fn, reads=TL + [t_m, t_g], writes=(wr or TL))
                vop(lambda e: e.tensor_reduce(out=sm[:, 0, :], in_=L[:], op=ALU.max, axis=AX.X))
                vop(lambda e: e.tensor_tensor(out=w2[:], in0=L[:], in1=bc(sm[:, 0, :]), op=ALU.subtract))
                kb.op("act", lambda e: e.activation(out=ex[:], in_=w2[:], func=AF.Exp), reads=TL, writes=TL)
                vop(lambda e: e.tensor_scalar(out=w2[:], in0=ex[:], scalar1=1.0, scalar2=None, op0=ALU.is_lt))
                vop(lambda e: e.tensor_tensor(out=w2[:], in0=w2[:], in1=ex[:], op=ALU.mult))
                vop(lambda e: e.tensor_reduce(out=sm[:, 1, :], in_=w2[:], op=ALU.max, axis=AX.X))
                vop(lambda e: e.tensor_scalar(out=sm[:, 2, :], in0=sm[:, 1, :], scalar1=1.0, scalar2=None, op0=ALU.add))
                vop(lambda e: e.reciprocal(out=sm[:, 3, :], in_=sm[:, 2, :]))
                vop(lambda e: e.tensor_tensor(out=maskf[:], in0=ex[:], in1=bc(sm[:, 1, :]), op=ALU.is_ge), wr=TL + [t_m])
                vop(lambda e: e.tensor_copy(out=maskb[:], in_=maskf[:]), wr=TL + [t_m])
                vop(lambda e: e.tensor_tensor(out=w2[:], in0=ex[:], in1=maskf[:], op=ALU.mult))
                vop(lambda e: e.tensor_tensor(out=gates[:], in0=w2[:], in1=bc(sm[:, 3, :]), op=ALU.mult), wr=TL + [t_g])
                prk = Ring(kb, 2, [128, 8], F32, kind="ps")
                for j in range(NT + 1):
                    p, t_p = prk.next()
                    n = 0
                    for i in range(min(j, NT)):
                        kb.op("pe", lambda e: e.matmul(p[:, :], tri[:, 1, :], maskb[:, i, :], start=(n == 0), stop=(j == NT and i == NT - 1)),
                              reads=[t_m, t_rb], writes=[t_p])
                        n += 1
                    if j < NT:
                        kb.op("pe", lambda e: e.matmul(p[:, :], tri[:, 0, :], maskb[:, j, :], start=(n == 0), stop=True), reads=[t_m, t_rb], writes=[t_p])
                        kb.op("act", lambda e: e.copy(out=rank[:, j, :], in_=p[:, :]), reads=[t_p], writes=[t_rk])
                    else:
                        tot = kb.sb([128, 8], F32)
                        kb.op("act", lambda e: e.copy(out=tot[:], in_=p[:, :]), reads=[t_p], writes=[t_rk])
                T = [t_rk]
                ts = lambda o, x, s1, o0: kb.op("dve", lambda e: e.tensor_scalar(out=o, in0=x, scalar1=s1, scalar2=None, op0=o0), reads=T + [t_m, t_g, t_rb], writes=T)
                tt = lambda o, x, y, op: kb.op("dve", lambda e: e.tensor_tensor(out=o, in0=x, in1=y, op=op), reads=T + [t_m, t_g, t_rb], writes=T)
                red = lambda o, x, op: kb.op("dve", lambda e: e.tensor_reduce(out=o, in_=x, op=op, axis=AX.X), reads=T, writes=T)
                pad = kb.sb([128, 8], F32); incl = kb.sb([128, 8], F32); base = kb.sb([128, 8], F32)
                ts(pad[:], tot[:], 511.0, ALU.add); ts(pad[:], pad[:], 1.0 / 512, ALU.mult)
                ts(pad[:], pad[:], -0.5 + 1.0 / 1024, ALU.add); ts(pad[:], pad[:], MAGIC, ALU.add); ts(pad[:], pad[:], MAGIC, ALU.subtract)
                ts(pad[:], pad[:], 512.0, ALU.mult)
                kb.op("dve", lambda e: e.tensor_copy(out=incl[:, 0:1], in_=pad[:, 0:1]), reads=T, writes=T)
                for e_ in range(1, 8):
                    tt(incl[:, e_:e_ + 1], incl[:, e_ - 1:e_], pad[:, e_:e_ + 1], ALU.add)
                tt(base[:], incl[:], pad[:], ALU.subtract)
                slot = kb.sb([128, NT, 8], F32); v1 = kb.sb([128, NT, 8], F32); v2 = kb.sb([128, NT, 8], F32)
                slo = kb.sb([128, NT], F32); shi = kb.sb([128, NT], F32)
                tt(slot[:], rank[:], base[:].unsqueeze(1).broadcast_to([128, NT, 8]), ALU.add)
                tt(v2[:], slot[:], maskf[:], ALU.mult)
                ts(v1[:], maskf[:], -BIG, ALU.mult); ts(v1[:], v1[:], BIG, ALU.add); tt(v1[:], v1[:], v2[:], ALU.add)
                red(slo[:], v1[:], ALU.min)
                tt(v1[:], v2[:], maskf[:], ALU.add); ts(v1[:], v1[:], -1.0, ALU.add)
                red(shi[:], v1[:], ALU.max)
                for sl_, g_ in ((slo, glo), (shi, ghi)):
                    tt(v1[:], slot[:], sl_[:].unsqueeze(2).broadcast_to([128, NT, 8]), ALU.is_equal)
                    tt(v1[:], v1[:], gates[:], ALU.mult)
                    kb.op("dve", lambda e: e.tensor_reduce(out=g_[:], in_=v1[:], op=ALU.add, axis=AX.X), reads=T, writes=T + [t_rt])
                kb.op("dve", lambda e: e.tensor_copy(out=ilo[:], in_=slo[:]), reads=T, writes=[t_rt])
                kb.op("dve", lambda e: e.tensor_copy(out=ihi[:], in_=shi[:]), reads=T, writes=[t_rt])
                cmp_ = kb.sb([128, MOE_TILES, 8], F32); cnt = kb.sb([128, MOE_TILES], F32); wf = kb.sb([128, MOE_TILES, 7], F32)
                tt(cmp_[:], incl[:].unsqueeze(1).broadcast_to([128, MOE_TILES, 8]), thr[:].unsqueeze(2).broadcast_to([128, MOE_TILES, 8]), ALU.is_le)
                red(cnt[:], cmp_[:], ALU.add)
                ts(cnt[:], cnt[:], 7.0, ALU.min); ts(cnt[:], cnt[:], 896.0, ALU.mult)
                tt(wf[:], cnt[:].unsqueeze(2).broadcast_to([128, MOE_TILES, 7]), io7[:].unsqueeze(1).broadcast_to([128, MOE_TILES, 7]), ALU.add)
                kb.op("dve", lambda e: e.tensor_copy(out=widx[:], in_=wf[:]), reads=T, writes=[t_rt])
            with kb.phase():
                t_fill = Tok()
                hr = Ring(kb, 3, [128, D], BF16); icr = Ring(kb, 4, [128, 1], I32)
                for j in range(NT):
                    hb, t_h = hr.next()
                    r0 = lat_tok0(j)
                    kb.dma("sp", [(hb[:], self.h2tm[r0:r0 + 128, :])], writes=[t_h])
                    for ix in (ilo, ihi):
                        kb.dma_custom("pool", lambda g: g.indirect_dma_start(out=self.hsorted[:, :], out_offset=bass.IndirectOffsetOnAxis(ap=ix[:, j:j + 1], axis=0),
                                                                             in_=hb[:, :], in_offset=None, bounds_check=None),
                                      reads=[t_h, t_rt, t_fill])
            with kb.phase():
                hsr = Ring(kb, 4, [128, D], BF16); hTr = Ring(kb, 2, [128, 8, MOE_TS], BF16)
                wgr = Ring(kb, 3, [128, 8, 512], BF16); wur = Ring(kb, 3, [128, 8, 512], BF16); wdr = Ring(kb, 3, [128, 4, D], BF16)
                atr = Ring(kb, 2, [128, 4, MOE_TS], BF16); sgr = Ring(kb, 3, [128, 512], BF16)
                accr = Ring(kb, 2, [128, 4, D], F32); icr = Ring(kb, 8, [128, 1], I32)
                pT = Ring(kb, 1, [128, 8, 128], BF16, kind="ps")
                pG = Ring(kb, 2, [128, 512], F32, kind="ps"); pU = Ring(kb, 2, [128, 512], F32, kind="ps"); pD = Ring(kb, 3, [128, 512], F32, kind="ps")
                pending = []

                def emit_down(at, t_at, wd_, t_wd, acc, t_acc, first, last, i):
                    for sub in range(4):
                        for nh in range(2):
                            p, t_p = pD.next()
                            for fc in range(4):
                                kb.op("pe", lambda e: e.matmul(p[:, :], at[:, fc, sub * 128:(sub + 1) * 128], wd_[:, fc, nh * 512:(nh + 1) * 512], start=(fc == 0), stop=(fc == 3)),
                                      reads=[t_at, t_wd], writes=[t_p])
                            dst = acc[:, sub, nh * 512:(nh + 1) * 512]
                            if first:
                                kb.op("act", lambda e: e.copy(out=dst, in_=p[:, :]), reads=[t_p], writes=[t_acc])
                            else:
                                kb.op("dve", lambda e: e.tensor_tensor(out=dst, in0=p[:, :], in1=dst, op=ALU.add), reads=[t_p, t_acc], writes=[t_acc])
                    if last:
                        kb.dma("sp", [(self.ysorted[i * MOE_TS + sub * 128:i * MOE_TS + (sub + 1) * 128, :], acc[:, sub, :]) for sub in range(4)], reads=[t_acc])

                def build_hT(i):
                    hT, t_hT = hTr.next()
                    for sub in range(4):
                        hs, t_hs = hsr.next()
                        r0 = i * MOE_TS + sub * 128
                        kb.dma("sp", [(hs[:], self.hsorted[r0:r0 + 128, :])], writes=[t_hs])
                        p, t_p = pT.next()
                        for k in range(8):
                            kb.op("pe", lambda e: e.transpose(out=p[:, k, :], in_=hs[:, k * 128:(k + 1) * 128], identity=self.ident_bf[:]),
                                  reads=[t_hs, self.t_ident], writes=[t_p])
                        kb.op("act", lambda e: e.copy(out=hT[:, :, sub * 128:(sub + 1) * 128], in_=p[:]), reads=[t_p], writes=[t_hT])
                    return hT, t_hT
                nxt_hT = build_hT(0)
                for i in range(MOE_TILES):
                    hT, t_hT = nxt_hT
                    acc, t_acc = accr.next()
                    for fg in range(7):
                        if fg == 5 and i + 1 < MOE_TILES:
                            nxt_hT = build_hT(i + 1)
                        wg_, t_wg = wgr.next(); wu_, t_wu = wur.next(); wd_, t_wd = wdr.next()
                        for (wt, tw, src) in ((wg_, t_wg, self.Wg_s), (wu_, t_wu, self.Wu_s), (wd_, t_wd, self.Wd_s)):
                            flat = wt[:].rearrange("p a n -> p (a n)")
                            kb.dma_custom("pool", lambda g: g.indirect_dma_start(out=flat, out_offset=None, in_=src[:, :],
                                                                                 in_offset=bass.IndirectOffsetOnAxis(ap=widx[:, i, fg:fg + 1], axis=0),
                                                                                 bounds_check=None),
                                          reads=[t_rt], writes=[tw])
                        at, t_at = atr.next()
                        for fc in range(4):
                            g_, t_g = pG.next(); u_, t_u = pU.next()
                            for k in range(8):
                                kb.op("pe", lambda e: e.matmul(g_[:, :], wg_[:, k, fc * 128:(fc + 1) * 128], hT[:, k, :], start=(k == 0), stop=(k == 7)),
                                      reads=[t_wg, t_hT], writes=[t_g])
                            for k in range(8):
                                kb.op("pe", lambda e: e.matmul(u_[:, :], wu_[:, k, fc * 128:(fc + 1) * 128], hT[:, k, :], start=(k == 0), stop=(k == 7)),
                                      reads=[t_wu, t_hT], writes=[t_u])
                            sg_, t_sg = sgr.next()
                            kb.op("act", lambda e: e.activation(out=sg_[:], in_=g_[:, :], func=AF.Silu), reads=[t_g], writes=[t_sg])
                            kb.op("dve", lambda e: e.tensor_tensor(out=at[:, fc, :], in0=u_[:, :], in1=sg_[:], op=ALU.mult), reads=[t_u, t_sg], writes=[t_at])
                        while pending:
                            pending.pop(0)()
                        pending.append(lambda at=at, t_at=t_at, wd_=wd_, t_wd=t_wd, acc=acc, t_acc=t_acc, first=(fg == 0), last=(fg == 6), i=i:
                                       emit_down(at, t_at, wd_, t_wd, acc, t_acc, first, last, i))
                while pending:
                    pending.pop(0)()
            with kb.phase():
                gg, _, t_gg, _ = self.load_mod_tiles(l, 5, None, "norm_ffn_post", plus_one=False)
                R = {"tmp": Ring(kb, 2, [128, D], F32), "stat2": Ring(kb, 4, [128, 8], F32), "junk2": Ring(kb, 1, [128, 512], BF16),
                     "xn": Ring(kb, 2, [128, D], F32)}
                icr = Ring(kb, 4, [128, 1], I32)
                xr = Ring(kb, 4, [128, D], F32); ylr = Ring(kb, 3, [128, D], F32); yhr = Ring(kb, 3, [128, D], F32); mxr = Ring(kb, 4, [128, D], F32)
                R["stat2"] = Ring(kb, 8, [128, 8], F32); R["tmp"] = Ring(kb, 3, [128, D], F32); R["xn"] = Ring(kb, 3, [128, D], F32)
                C = [dict() for _ in range(NT)]

                def c_load(j):
                    r0 = lat_tok0(j)
                    xt, t_x = xr.next()
                    kb.dma("sp", [(xt[:], xsrc[r0:r0 + 128, :])], writes=[t_x])
                    yl, t_yl = ylr.next(); yh, t_yh = yhr.next()
                    for (yt, ty, ix) in ((yl, t_yl, ilo), (yh, t_yh, ihi)):
                        kb.dma_custom("pool", lambda g: g.indirect_dma_start(out=yt[:, :], out_offset=None, in_=self.ysorted[:, :],
                                                                             in_offset=bass.IndirectOffsetOnAxis(ap=ix[:, j:j + 1], axis=0),
                                                                             bounds_check=None),
                                      reads=[t_rt], writes=[ty])
                    C[j].update(xt=xt, t_x=t_x, yl=yl, t_yl=t_yl, yh=yh, t_yh=t_yh)

                def c_mix(j):
                    c = C[j]
                    mx, t_mx = mxr.next()
                    kb.op("dve", lambda e: e.tensor_scalar(out=mx[:], in0=c["yl"][:], scalar1=glo[:, j:j + 1], scalar2=None, op0=ALU.mult), reads=[c["t_yl"], t_rt], writes=[t_mx])
                    kb.op("dve", lambda e: e.scalar_tensor_tensor(out=mx[:], in0=c["yh"][:], scalar=ghi[:, j:j + 1], op0=ALU.mult, in1=mx[:], op1=ALU.add),
                          reads=[c["t_yh"], t_rt, t_mx], writes=[t_mx])
                    c.update(hs=[mx[:, 0:512], mx[:, 512:1024]], ths=[t_mx, t_mx])

                def c_p1(j):
                    C[j]["st"], C[j]["t_st"] = self.pn1(C[j]["hs"], C[j]["ths"], R)

                def c_p2(j):
                    c = C[j]
                    xn, t_xn = self.pn2(c["hs"], c["ths"], c["st"], c["t_st"], c["xt"], c["t_x"], gg, t_gg, j // 16, R)
                    kb.dma("sp", [(xdst[j * 128:(j + 1) * 128, :], xn[:])], reads=[t_xn])
                self.run_pipeline(NT, [c_load, c_mix, c_p1, c_p2])
def core_inputs(inputs, core, consts):
    b0 = core * NB
    m = {}
    xs = []
    for b in range(b0, b0 + NB):
        xs.append(inputs["ctx"][b]); xs.append(inputs["x"][b])
    m["xin"] = np.ascontiguousarray(np.concatenate(xs, axis=0), dtype=np.float32)
    cv = np.stack([inputs["c"][b0], inputs["c"][b0 + 1], inputs["c_ctx"]], axis=0)
    m["cT"] = np.ascontiguousarray(cv.reshape(3, 8, 128).transpose(2, 1, 0), dtype=np.float32)
    dup = lambda a: np.concatenate([a, a], axis=0)
    L = inputs["s5_a_re"].shape[0]
    par = np.zeros((L, 128, 3, 32), np.float32); sb = np.zeros((L, 128, 32, 2, 16), np.float32); sc = np.zeros((L, 128, 32, 2, 16), np.float32)
    for l in range(L):
        par[l, :, 0, :] = dup(inputs["s5_a_re"][l].reshape(32, 64).T)
        par[l, :, 1, :] = dup(inputs["s5_a_im"][l].reshape(32, 64).T)
        par[l, :, 2, :] = inputs["s5_log_dt"][l].reshape(1, 32)
        sb[l, :, :, 0, :] = dup(inputs["s5_b_re"][l].reshape(32, 64, 16).transpose(1, 0, 2))
        sb[l, :, :, 1, :] = dup(inputs["s5_b_im"][l].reshape(32, 64, 16).transpose(1, 0, 2))
        sc[l, :, :, 0, :] = dup(inputs["s5_c_re"][l].reshape(32, 16, 64).transpose(2, 0, 1))
        sc[l, :, :, 1, :] = dup(inputs["s5_c_im"][l].reshape(32, 16, 64).transpose(2, 0, 1))
    m["s5_par"], m["s5_b"], m["s5_c"] = par, sb, sc
    m["s5_dd"] = np.ascontiguousarray(inputs["s5_d"].reshape(L, 2, 128).transpose(0, 2, 1))
    return m


_CACHE = {}


def build_program():
    P = Prog()
    P.declare(); P.consts_sb()
    xin = P.I["xin"]
    P.phase_adaln(0)
    P.prep = P.moe_prep_gen(None)
    P.phase_win(0, xin, prep_win=True)
    P.phase_attn(0, True, prep_every=2); P.phase_fnet(0, True); P.phase_s5(0)
    P.phase_wout(0, xin, P.xA, True, prep_wout=True)
    P.phase_ffn(0, P.xA, P.xB, False, False)
    P.phase_adaln(1)
    P.phase_win(1, P.xB, prep_win=True)
    P.phase_attn(1, False, prep_every=2); P.phase_fnet(1, False); P.phase_s5(1)
    P.phase_wout(1, P.xB, P.xA, False)
    P.phase_moe_prep()
    P.phase_moe_sparse(1, P.xA, P.out)
    P.kb.barrier()
    return P


def kernel(**inputs):
    inputs = {k: np.asarray(v) for k, v in inputs.items()}
    if "P" not in _CACHE:
        _CACHE["P"] = build_program()
    P = _CACHE["P"]
    n_cores = 8
    shared = {}
    for k in P.I:
        if k in P.consts:
            shared[k] = P.consts[k]
        elif k in inputs:
            shared[k] = np.ascontiguousarray(inputs[k], dtype=np.float32)
    in_maps = []
    for core in range(n_cores):
        m = core_inputs(inputs, core, P.consts)
        for k, v in shared.items():
            if k not in m:
                m[k] = v
        in_maps.append(m)
    res = run_bass_kernel_spmd(P.kb.nc, in_maps, core_ids=list(range(n_cores)))
    outs = [np.asarray(r["out"], dtype=np.float32).reshape(NB, SEQ, D) for r in res.results]
    return np.concatenate(outs, axis=0)
```
